# Optimizing a Trainium2 kernel written in Bass

```python
import jax
import jax.numpy as jnp
from jax import lax
import numpy as np

D_MODEL = 1024
BATCH = 8
SEQ = 2048
DEPTH = 1

GRID_W = 64
CTX_LEN = 256
EPS = 1e-6

GLA_HEADS = 4
GLA_DK = 64
GLA_DV = 128
GLA_RANK = 16
GLA_TAU = 16.0
GLA_CHUNK = 64
GLA_QK_W = GLA_HEADS * GLA_DK
GLA_V_W = GLA_HEADS * GLA_DV

MLA_HEADS = 8
MLA_Q_RANK = 256
MLA_KV_RANK = 128
MLA_NOPE = 64
MLA_ROPE = 32
MLA_V = 64
MLA_QK = MLA_NOPE + MLA_ROPE
MLA_SCALE = MLA_QK ** -0.5
ROPE_BASE = 10000.0
Q_BLOCK = 128

N_EXPERTS = 32
TOP_K = 4
D_FF = 1024
SWIGLU_LIMIT = 7.0
SWIGLU_ALPHA = 1.702

IN_SPLITS = (GLA_QK_W, GLA_QK_W, GLA_V_W, GLA_V_W, GLA_RANK, GLA_RANK,
             MLA_Q_RANK, MLA_KV_RANK, MLA_ROPE, D_MODEL, D_MODEL)
D_IN = sum(IN_SPLITS)

kernel_name = "hybrid_gla_mla_moe_diffusion_block"


def _rmsnorm(x, g):
    xf = x.astype(jnp.float32)
    y = xf * lax.rsqrt(jnp.mean(xf * xf, axis=-1, keepdims=True) + EPS)
    return (y * g.astype(jnp.float32)).astype(x.dtype)


def _modulate(h, shift, scale):
    return h * (1.0 + scale) + shift


def _heads(t, n_heads):
    b, t_len, _ = t.shape
    return t.reshape(b, t_len, n_heads, -1).transpose(0, 2, 1, 3)


def _merge_heads(t):
    b, h, t_len, d = t.shape
    return t.transpose(0, 2, 1, 3).reshape(b, t_len, h * d)


def _flip(t):
    return t[:, :, ::-1]


def _axial_rope_angles(rows):
    r, col = jnp.meshgrid(jnp.arange(rows, dtype=jnp.float32),
                          jnp.arange(GRID_W, dtype=jnp.float32), indexing="ij")
    half = MLA_ROPE // 2
    inv_freq = ROPE_BASE ** (-jnp.arange(0, half, 2, dtype=jnp.float32) / half)
    return r.reshape(-1)[:, None] * inv_freq, col.reshape(-1)[:, None] * inv_freq


def _rotate(v, ang):
    v1, v2 = jnp.split(v, 2, axis=-1)
    cos = jnp.cos(ang).astype(v.dtype)
    sin = jnp.sin(ang).astype(v.dtype)
    return jnp.concatenate([v1 * cos - v2 * sin, v2 * cos + v1 * sin], axis=-1)


def _axial_rope(v, ang_row, ang_col):
    v_row, v_col = jnp.split(v, 2, axis=-1)
    return jnp.concatenate([_rotate(v_row, ang_row), _rotate(v_col, ang_col)], axis=-1)


def _gla_chunked(q, k, v, log_a, s0):
    b, h, t_len, dk = q.shape
    dv = v.shape[-1]
    n = t_len // GLA_CHUNK

    def rs(t):
        return t.reshape(b, h, n, GLA_CHUNK, t.shape[-1]).astype(jnp.float32)

    qc, kc, vc, la = rs(q), rs(k), rs(v), rs(log_a)
    cum = jnp.cumsum(la, axis=3)
    cum_last = cum[:, :, :, -1:, :]
    q_dec = qc * jnp.exp(cum)
    k_inv = kc * jnp.exp(-cum)
    k_to_end = kc * jnp.exp(cum_last - cum)
    mask = jnp.tril(jnp.ones((GLA_CHUNK, GLA_CHUNK), dtype=bool))
    att = jnp.where(mask, jnp.einsum("bhncd,bhnsd->bhncs", q_dec, k_inv), 0.0)
    o_intra = jnp.einsum("bhncs,bhnse->bhnce", att, vc)
    ds = jnp.einsum("bhncd,bhnce->bhnde", k_to_end, vc)
    decay = jnp.exp(cum_last[:, :, :, 0, :])

    def step(s, inp):
        d, dsn = inp
        return d[..., None] * s + dsn, s

    s_final, s_in = lax.scan(step, s0, (jnp.moveaxis(decay, 2, 0), jnp.moveaxis(ds, 2, 0)))
    s_in = jnp.moveaxis(s_in, 0, 2)
    o_inter = jnp.einsum("bhncd,bhnde->bhnce", q_dec, s_in)
    o = (o_intra + o_inter).reshape(b, h, t_len, dv)
    return o.astype(v.dtype), s_final


def _block_attention(q, k, v):
    b, h, t_len, dq = q.shape
    nb = t_len // Q_BLOCK
    qb = q.reshape(b, h, nb, Q_BLOCK, dq).transpose(2, 0, 1, 3, 4)

    def one_block(qi):
        s = jnp.einsum("bhqd,bhkd->bhqk", qi, k).astype(jnp.float32) * MLA_SCALE
        p = jax.nn.softmax(s, axis=-1)
        return jnp.einsum("bhqk,bhkd->bhqd", p.astype(v.dtype), v)

    o = lax.map(one_block, qb)
    return o.transpose(1, 2, 0, 3, 4).reshape(b, h, t_len, v.shape[-1])


def _mixer_features(z, p, ang):
    offsets = np.cumsum(IN_SPLITS)[:-1].tolist()
    (zq, zk, zv, zg, za_f, za_b, zdq, zdkv, zkr, zm_gla, zm_mla) = jnp.split(z, offsets, axis=-1)
    b, t_len, _ = z.shape
    gq = _heads(zq, GLA_HEADS) * (GLA_DK ** -0.5)
    gk = _heads(zk, GLA_HEADS)
    gv = _heads(zv, GLA_HEADS)
    la_f = _heads(jax.nn.log_sigmoid((za_f @ p["gla_w_a2_f"] + p["gla_b_a_f"]).astype(jnp.float32)) / GLA_TAU, GLA_HEADS)
    la_b = _heads(jax.nn.log_sigmoid((za_b @ p["gla_w_a2_b"] + p["gla_b_a_b"]).astype(jnp.float32)) / GLA_TAU, GLA_HEADS)
    mq = _heads(_rmsnorm(zdq, p["mla_g_q"]) @ p["mla_w_uq"], MLA_HEADS)
    q_nope, q_rope = mq[..., :MLA_NOPE], mq[..., MLA_NOPE:]
    ckv = _rmsnorm(zdkv, p["mla_g_kv"])
    k_nope = _heads(ckv @ p["mla_w_uk"], MLA_HEADS)
    mv = _heads(ckv @ p["mla_w_uv"], MLA_HEADS)
    k_rope = zkr[:, None]
    if ang is not None:
        q_rope = _axial_rope(q_rope, *ang)
        k_rope = _axial_rope(k_rope, *ang)
    mq = jnp.concatenate([q_nope, q_rope], axis=-1)
    mk = jnp.concatenate([k_nope, jnp.broadcast_to(k_rope, (b, MLA_HEADS, t_len, MLA_ROPE))], axis=-1)
    return {"gq": gq, "gk": gk, "gv": gv, "la_f": la_f, "la_b": la_b, "zg": zg,
            "mq": mq, "mk": mk, "mv": mv, "zm_gla": zm_gla, "zm_mla": zm_mla}


def _mixer_output(o_gla, o_mla, f, p):
    og = _merge_heads(_rmsnorm(o_gla, p["gla_g_norm"])) * jax.nn.silu(f["zg"])
    br_gla = og @ p["w_br_gla"]
    br_mla = _merge_heads(o_mla) @ p["w_br_mla"]
    merged = jax.nn.sigmoid(f["zm_gla"]) * br_gla + jax.nn.sigmoid(f["zm_mla"]) * br_mla
    return merged @ p["w_out"]


def _moe(h, p):
    b, t_len, d = h.shape
    tok = h.reshape(-1, d)
    logits = (tok @ p["router_w"] + p["router_b"]).astype(jnp.float32)
    top_v, top_i = lax.top_k(logits, TOP_K)
    wts = jax.nn.softmax(top_v, axis=-1)
    combine = jnp.sum(jax.nn.one_hot(top_i, N_EXPERTS, dtype=jnp.float32) * wts[..., None], axis=1)
    combine = combine.astype(tok.dtype)
    out = jnp.zeros_like(tok)
    for e in range(N_EXPERTS):
        gate = jnp.minimum(tok @ p["w_gate"][e] + p["b_gate"][e], SWIGLU_LIMIT)
        up = jnp.clip(tok @ p["w_up"][e] + p["b_up"][e], -SWIGLU_LIMIT, SWIGLU_LIMIT)
        act = (up + 1.0) * gate * jax.nn.sigmoid(SWIGLU_ALPHA * gate)
        out = out + combine[:, e:e + 1] * (act @ p["w_down"][e] + p["b_down"][e])
    return out.reshape(b, t_len, d)


def _layer(x, ctx, mod_x, mod_c, p, ang, ctx_out):
    sh_a, sc_a, gt_a, sh_f, sc_f, gt_f = mod_x
    csh_a, csc_a, cgt_a, csh_f, csc_f, cgt_f = mod_c
    hx = _modulate(_rmsnorm(x, p["g_pre_mix"]), sh_a, sc_a)
    hc = _modulate(_rmsnorm(ctx, p["g_pre_mix"]), csh_a, csc_a)
    fx = _mixer_features(hx @ p["w_in"], p, ang)
    fc = _mixer_features(hc @ p["w_in"], p, None)
    s0 = jnp.zeros((x.shape[0], GLA_HEADS, GLA_DK, GLA_DV), jnp.float32)
    oc_fwd, s_ctx_fwd = _gla_chunked(fc["gq"], fc["gk"], fc["gv"], fc["la_f"], s0)
    ox_fwd, _ = _gla_chunked(fx["gq"], fx["gk"], fx["gv"], fx["la_f"], s_ctx_fwd)
    oc_bwd, s_ctx_bwd = _gla_chunked(_flip(fc["gq"]), _flip(fc["gk"]), _flip(fc["gv"]), _flip(fc["la_b"]), s0)
    ox_bwd, _ = _gla_chunked(_flip(fx["gq"]), _flip(fx["gk"]), _flip(fx["gv"]), _flip(fx["la_b"]), s_ctx_bwd)
    ox_gla = ox_fwd + _flip(ox_bwd)
    k_all = jnp.concatenate([fx["mk"], fc["mk"]], axis=2)
    v_all = jnp.concatenate([fx["mv"], fc["mv"]], axis=2)
    ox_mla = _block_attention(fx["mq"], k_all, v_all)
    x = x + gt_a * _rmsnorm(_mixer_output(ox_gla, ox_mla, fx, p), p["g_post_mix"])
    hx = _modulate(_rmsnorm(x, p["g_pre_ffn"]), sh_f, sc_f)
    x = x + gt_f * _rmsnorm(_moe(hx, p), p["g_post_ffn"])
    if ctx_out:
        oc_gla = oc_fwd + _flip(oc_bwd)
        oc_mla = _block_attention(fc["mq"], fc["mk"], fc["mv"])
        ctx = ctx + cgt_a * _rmsnorm(_mixer_output(oc_gla, oc_mla, fc, p), p["g_post_mix"])
        hc = _modulate(_rmsnorm(ctx, p["g_pre_ffn"]), csh_f, csc_f)
        ctx = ctx + cgt_f * _rmsnorm(_moe(hc, p), p["g_post_ffn"])
    return x, ctx


def setup_inputs(seed: int = 0) -> dict:
    key = jax.random.key(seed)
    ks = jax.random.split(key, 32)
    f32 = jnp.float32
    L = DEPTH

    def nrm(k, shape, scale=1.0):
        return jax.random.normal(k, shape, f32) * scale

    def gain(k, n):
        return 1.0 + nrm(k, (L, n), 0.1)

    return {
        "x": nrm(ks[0], (BATCH, SEQ, D_MODEL)),
        "c": nrm(ks[1], (BATCH, D_MODEL)),
        "ctx": nrm(ks[2], (BATCH, CTX_LEN, D_MODEL)),
        "c_ctx": nrm(ks[3], (D_MODEL,)),
        "w_mod": nrm(ks[4], (L, D_MODEL, 6 * D_MODEL), 0.5 * D_MODEL ** -0.5),
        "b_mod": nrm(ks[5], (L, 6 * D_MODEL), 0.02),
        "g_pre_mix": gain(ks[6], D_MODEL),
        "g_post_mix": gain(ks[7], D_MODEL),
        "g_pre_ffn": gain(ks[8], D_MODEL),
        "g_post_ffn": gain(ks[9], D_MODEL),
        "w_in": nrm(ks[10], (L, D_MODEL, D_IN), D_MODEL ** -0.5),
        "gla_w_a2_f": nrm(ks[11], (L, GLA_RANK, GLA_QK_W), GLA_RANK ** -0.5),
        "gla_b_a_f": nrm(ks[12], (L, GLA_QK_W), 0.02),
        "gla_w_a2_b": nrm(ks[13], (L, GLA_RANK, GLA_QK_W), GLA_RANK ** -0.5),
        "gla_b_a_b": nrm(ks[14], (L, GLA_QK_W), 0.02),
        "gla_g_norm": gain(ks[15], GLA_DV),
        "mla_g_q": gain(ks[16], MLA_Q_RANK),
        "mla_w_uq": nrm(ks[17], (L, MLA_Q_RANK, MLA_HEADS * MLA_QK), MLA_Q_RANK ** -0.5),
        "mla_g_kv": gain(ks[18], MLA_KV_RANK),
        "mla_w_uk": nrm(ks[19], (L, MLA_KV_RANK, MLA_HEADS * MLA_NOPE), MLA_KV_RANK ** -0.5),
        "mla_w_uv": nrm(ks[20], (L, MLA_KV_RANK, MLA_HEADS * MLA_V), MLA_KV_RANK ** -0.5),
        "w_br_gla": nrm(ks[21], (L, GLA_V_W, D_MODEL), GLA_V_W ** -0.5),
        "w_br_mla": nrm(ks[22], (L, MLA_HEADS * MLA_V, D_MODEL), (MLA_HEADS * MLA_V) ** -0.5),
        "w_out": nrm(ks[23], (L, D_MODEL, D_MODEL), D_MODEL ** -0.5),
        "router_w": nrm(ks[24], (L, D_MODEL, N_EXPERTS), D_MODEL ** -0.5),
        "router_b": nrm(ks[25], (L, N_EXPERTS), 0.01),
        "w_gate": nrm(ks[26], (L, N_EXPERTS, D_MODEL, D_FF), D_MODEL ** -0.5),
        "b_gate": nrm(ks[27], (L, N_EXPERTS, D_FF), 0.02),
        "w_up": nrm(ks[28], (L, N_EXPERTS, D_MODEL, D_FF), D_MODEL ** -0.5),
        "b_up": nrm(ks[29], (L, N_EXPERTS, D_FF), 0.02),
        "w_down": nrm(ks[30], (L, N_EXPERTS, D_FF, D_MODEL), D_FF ** -0.5),
        "b_down": nrm(ks[31], (L, N_EXPERTS, D_MODEL), 0.02),
    }


def reference(x, c, ctx, c_ctx, w_mod, b_mod, g_pre_mix, g_post_mix, g_pre_ffn, g_post_ffn,
              w_in, gla_w_a2_f, gla_b_a_f, gla_w_a2_b, gla_b_a_b, gla_g_norm,
              mla_g_q, mla_w_uq, mla_g_kv, mla_w_uk, mla_w_uv,
              w_br_gla, w_br_mla, w_out, router_w, router_b,
              w_gate, b_gate, w_up, b_up, w_down, b_down):
    ROWS = x.shape[1] // GRID_W
    ang = _axial_rope_angles(ROWS)
    for l in range(DEPTH):
        p = {
            "g_pre_mix": g_pre_mix[l], "g_post_mix": g_post_mix[l],
            "g_pre_ffn": g_pre_ffn[l], "g_post_ffn": g_post_ffn[l],
            "w_in": w_in[l],
            "gla_w_a2_f": gla_w_a2_f[l], "gla_b_a_f": gla_b_a_f[l],
            "gla_w_a2_b": gla_w_a2_b[l], "gla_b_a_b": gla_b_a_b[l],
            "gla_g_norm": gla_g_norm[l],
            "mla_g_q": mla_g_q[l], "mla_w_uq": mla_w_uq[l],
            "mla_g_kv": mla_g_kv[l], "mla_w_uk": mla_w_uk[l], "mla_w_uv": mla_w_uv[l],
            "w_br_gla": w_br_gla[l], "w_br_mla": w_br_mla[l], "w_out": w_out[l],
            "router_w": router_w[l], "router_b": router_b[l],
            "w_gate": w_gate[l], "b_gate": b_gate[l], "w_up": w_up[l], "b_up": b_up[l],
            "w_down": w_down[l], "b_down": b_down[l],
        }
        mod_x = jnp.split((jax.nn.silu(c) @ w_mod[l] + b_mod[l])[:, None, :], 6, axis=-1)
        mod_c = jnp.split(jax.nn.silu(c_ctx) @ w_mod[l] + b_mod[l], 6, axis=-1)
        x, ctx = _layer(x, ctx, mod_x, mod_c, p, ang, l < DEPTH - 1)
    return x
```

```python
import numpy as np
from contextlib import ExitStack
import concourse.bass as bass
import concourse.mybir as mybir
from concourse.bass_utils import run_bass_kernel_spmd

F32 = mybir.dt.float32
BF16 = mybir.dt.bfloat16
I32 = mybir.dt.int32
AF = mybir.ActivationFunctionType
ALU = mybir.AluOpType
AX = mybir.AxisListType
ENGS = ("pe", "act", "dve", "pool", "sp")

T = 2048
TC = 256
NT = 18
NTOK = 2304
D = 1024
KC = 8
D_IN = 4032
EPS = 1e-6
NE = 32
MLA_SCALE = 96 ** -0.5
SG = 256
NSTEP = 8
NSLOT = T * 4 + NE * SG
MOE_MODE = "sparse"


class Buf:
    __slots__ = ("name", "writers", "readers", "dma_sem", "dma_cnt", "ap", "inherit", "off", "words")

    def __init__(self, name, ap=None):
        self.name = name
        self.ap = ap
        self.writers = []
        self.readers = []
        self.inherit = []
        self.off = None
        self.dma_sem = None
        self.dma_cnt = 0


class Op:
    __slots__ = ("eng", "fn", "idx", "deps", "marked", "dma_buf", "dma_val", "mark_no", "grp")

    def __init__(self, eng, fn):
        self.eng = eng
        self.fn = fn
        self.grp = None
        self.deps = []
        self.marked = False
        self.dma_buf = None
        self.dma_val = 0
        self.mark_no = 0


class Sched:
    def __init__(self, nc, arena_words):
        self.nc = nc
        self.ops = {e: [] for e in ENGS}
        self.bufs = []
        self.arena = nc.alloc_sbuf_tensor("arena", [128, arena_words], F32)
        self.free = [(0, arena_words)]
        self.dead = []
        self.nbuf = 0
        self.cur_grp = None
        self.ngrp = 0

    def alloc(self, name, free_shape, dt=F32):
        n = 1
        for s in free_shape:
            n *= s
        words = (n * (2 if dt == BF16 else 4) + 3) // 4
        words = (words + 7) // 8 * 8
        off = None
        for i, (o, sz) in enumerate(self.free):
            if sz >= words:
                off = o
                if sz == words:
                    self.free.pop(i)
                else:
                    self.free[i] = (o + words, sz - words)
                break
        if off is None:
            raise RuntimeError("arena full allocating %s (%d words); free=%s" % (name, words, self.free))
        v = self.arena[:, off:off + words]
        if dt == BF16:
            v = v.bitcast(BF16)
        v = v[:, 0:n]
        if len(free_shape) == 2:
            v = v.rearrange("p (a b) -> p a b", b=free_shape[1])
        elif len(free_shape) == 3:
            v = v.rearrange("p (a b c) -> p a b c", b=free_shape[1], c=free_shape[2])
        self.nbuf += 1
        b = Buf("%s_%d" % (name, self.nbuf), v)
        b.off, b.words = off, words
        for (o, w, toks) in self.dead:
            if o < off + words and off < o + w:
                b.inherit.extend(toks)
        self.bufs.append(b)
        return b

    def release(self, *bufs):
        for b in bufs:
            toks = [("op", t[1], "raw") if t[0] == "op" else t for t in (b.writers + b.readers + b.inherit)]
            self.dead.append((b.off, b.words, toks))
            self.free.append((b.off, b.words))
        self.free.sort()
        m = []
        for o, w in self.free:
            if m and m[-1][0] + m[-1][1] == o:
                m[-1] = (m[-1][0], m[-1][1] + w)
            else:
                m.append((o, w))
        self.free = m

    def psum(self, name):
        t = self.nc.alloc_psum_tensor(name, [128, 512], F32)
        b = Buf(name, t[:, :])
        self.bufs.append(b)
        return b

    def virt(self, name):
        b = Buf(name, None)
        self.bufs.append(b)
        return b

    def _add(self, eng, fn, reads, writes, dma_buf=None):
        op = Op(eng, fn)
        op.idx = len(self.ops[eng])
        op.grp = self.cur_grp
        is_dma = dma_buf is not None
        deps = []
        for b in reads:
            deps.extend(b.writers)
            deps.extend(b.inherit)
            if b.off is None:
                deps.extend([("op", t[1], "raw") for t in b.readers if t[0] == "op" and t[1].eng != eng])
        for b in writes:
            deps.extend(b.inherit)
            deps.extend(b.readers)
            if is_dma and not b.readers and b.writers and all(w[0] == "dma" for w in b.writers):
                pass
            else:
                deps.extend(b.writers)
        fl = []
        for d in deps:
            if d[0] == "op":
                o = d[1]
                if o.eng == eng and not is_dma:
                    if eng == "pe" or eng == "sp":
                        continue
                    if d[2] != "raw":
                        continue
            fl.append(d)
        op.deps = fl
        if is_dma:
            dma_buf.dma_cnt += 16
            op.dma_buf = dma_buf
            op.dma_val = dma_buf.dma_cnt
            tok_w = ("dma", dma_buf, op.dma_val)
            tok_r = tok_w
        else:
            tok_w = ("op", op, "raw")
            tok_r = ("op", op, "war")
        for b in reads:
            b.readers.append(tok_r)
            if len(b.readers) > 64:
                last = {}
                keep = []
                for t in b.readers:
                    if t[0] == "op":
                        last[t[1].eng] = t
                    else:
                        keep.append(t)
                b.readers = keep[-32:] + list(last.values())
        for b in writes:
            if is_dma and not b.readers and b.writers and all(w[0] == "dma" for w in b.writers):
                b.writers.append(tok_w)
            else:
                b.writers = [tok_w]
                b.readers = []
        self.ops[eng].append(op)
        return op

    def op(self, eng, fn, reads=(), writes=()):
        return self._add(eng, fn, list(reads), list(writes))

    def dma(self, eng, out_ap, in_ap, reads=(), writes=(), sem_buf=None, **kw):
        if sem_buf is None:
            sem_buf = (list(writes) + list(reads))[0]

        def fn(e, out_ap=out_ap, in_ap=in_ap, kw=kw):
            return e.dma_start(out=out_ap, in_=in_ap, **kw)
        return self._add(eng, fn, list(reads), list(writes), dma_buf=sem_buf)

    def begin_group(self, thr):
        self.ngrp += 1
        self.cur_grp = (self.ngrp, thr)

    def end_group(self):
        self.cur_grp = None

    def regload(self, src_buf, ap):
        for e in ENGS:
            self._add(e, ("regload", ap), [src_buf], [])

    def dma_fn(self, eng, fn, reads=(), writes=(), sem_buf=None):
        return self._add(eng, fn, list(reads), list(writes), dma_buf=sem_buf)

    def emit(self, final_bufs=()):
        nc = self.nc
        for e in ENGS:
            for op in self.ops[e]:
                for d in op.deps:
                    if d[0] == "op":
                        d[1].marked = True
        for e in ENGS:
            n = 0
            for op in self.ops[e]:
                if op.marked:
                    n += 1
                    op.mark_no = n
        with ExitStack() as st:
            esem = {e: st.enter_context(nc.semaphore("s_" + e)) for e in ENGS}
            for b in self.bufs:
                if b.dma_cnt > 0:
                    b.dma_sem = st.enter_context(nc.semaphore("d_" + b.name))
            self.bc_reg = st.enter_context(nc.gpsimd.register("rbc"))
            engobj = {"pe": nc.tensor, "act": nc.scalar, "dve": nc.vector, "pool": nc.gpsimd, "sp": nc.sync}
            self.cregs = {e: st.enter_context(engobj[e].register("creg_" + e)) for e in ENGS}
            print("semaphores used:", 5 + sum(1 for b in self.bufs if b.dma_cnt > 0))
            block = st.enter_context(nc.Block())
            handles = {"pe": block.tensor, "act": block.scalar, "dve": block.vector,
                       "pool": block.gpsimd, "sp": block.sync}
            stats = {}
            for e in ENGS:
                ops = self.ops[e]

                def body(eng, ops=ops, e=e):
                    waited = {}
                    nw = [0]
                    if e == "pool":
                        eng.reg_mov(self.bc_reg, NSLOT - 1)
                    creg = self.cregs[e]

                    def emit_op(op, wt):
                        need = {}
                        for d in op.deps:
                            if d[0] == "op":
                                key = ("e", d[1].eng)
                                val = d[1].mark_no
                            else:
                                key = ("d", d[1].name)
                                val = d[2]
                            if val > need.get(key, (0, None))[0]:
                                need[key] = (val, d)
                        for key, (val, d) in need.items():
                            if wt.get(key, 0) >= val:
                                continue
                            wt[key] = val
                            sem = esem[d[1].eng] if d[0] == "op" else d[1].dma_sem
                            eng.wait_ge(sem, val)
                            nw[0] += 1
                        if isinstance(op.fn, tuple):
                            eng.reg_load(creg, op.fn[1])
                            return
                        ins = op.fn(eng)
                        if op.dma_buf is not None:
                            ins.then_inc(op.dma_buf.dma_sem, 16)
                        elif op.marked:
                            ins.then_inc(esem[e], 1)
                    def comp_of(gops):
                        comp = []
                        marked = [o for o in gops if o.marked and o.dma_buf is None]
                        if marked:
                            comp.append((esem[e], marked[0].mark_no - 1, len(marked)))
                        dm = {}
                        for o in gops:
                            if o.dma_buf is not None:
                                if o.dma_buf.name not in dm:
                                    dm[o.dma_buf.name] = [o.dma_buf.dma_sem, o.dma_val - 16, 0]
                                dm[o.dma_buf.name][2] += 16
                        comp.extend(tuple(v) for v in dm.values())
                        return comp

                    def chain(runs, k, wt):
                        rest = [o for r_ in runs[k:] for o in r_]
                        thr = runs[k][0].grp[1]
                        with eng.If_lt(creg, thr + 1):
                            for sem, before, delta in comp_of(rest):
                                if before > 0:
                                    eng.wait_ge(sem, before)
                                eng.sem_inc(sem, delta)
                        with eng.Else():
                            for o in runs[k]:
                                emit_op(o, wt)
                            if k + 1 < len(runs):
                                chain(runs, k + 1, wt)
                    i = 0
                    n = len(ops)
                    while i < n:
                        op = ops[i]
                        if op.grp is None:
                            emit_op(op, waited)
                            i += 1
                            continue
                        runs = []
                        j = i
                        while j < n and ops[j].grp is not None and (not runs or ops[j].grp[1] > runs[-1][0].grp[1] or ops[j].grp is runs[-1][0].grp):
                            if runs and ops[j].grp is runs[-1][0].grp:
                                runs[-1].append(ops[j])
                            else:
                                runs.append([ops[j]])
                            j += 1
                        chain(runs, 0, dict(waited))
                        i = j
                    if e == "sp":
                        for b in final_bufs:
                            if b.dma_cnt:
                                eng.wait_ge(b.dma_sem, b.dma_cnt)
                    stats[e] = (len(ops), nw[0])
                handles[e](body)
            self.stats = stats


def _consts():
    c = {}
    idx = np.arange(128)
    same = (idx[:, None] // 64) == (idx[None, :] // 64)
    s = idx[:, None]
    t = idx[None, :]
    tri = np.zeros((128, 4, 128), np.float32)
    tri[:, 0] = same & (s <= t)
    tri[:, 1] = same & (s >= t)
    tri[:, 2] = same & (s > t)
    tri[:, 3] = same & (s < t)
    c["tri"] = tri
    mask = np.zeros((128, 2, 4, 128), np.float32)
    mask[:, 0] = (same & (t >= s))[:, None, :]
    mask[:, 1] = (same & (t <= s))[:, None, :]
    c["mask"] = mask.reshape(128, 2, 512)
    c["ident"] = np.eye(128, dtype=np.float32)
    half = 16
    inv_freq = (10000.0 ** (-np.arange(0, half, 2, dtype=np.float32) / half)).astype(np.float32)
    tok = np.arange(T)
    ang_row = (tok // 64).astype(np.float32)[:, None] * inv_freq
    ang_col = (tok % 64).astype(np.float32)[:, None] * inv_freq
    cosr, sinr = np.cos(ang_row), np.sin(ang_row)
    cosc, sinc = np.cos(ang_col), np.sin(ang_col)
    cos32 = np.concatenate([cosr, cosr, cosc, cosc], axis=1)
    sin32 = np.concatenate([-sinr, sinr, -sinc, sinc], axis=1)
    ropet = np.zeros((128, 2, NTOK), np.float32)
    ropet[64:96, 0, :TC] = 1.0
    ropet[64:96, 0, TC:] = cos32.T
    ropet[64:96, 1, TC:] = sin32.T
    c["ropet"] = ropet
    e96 = np.zeros((128, 128), np.float32)
    e96[:96, 96] = 1.0
    c["e96"] = e96
    sel = np.zeros((NE, NE, 128), np.float32)
    for e in range(NE):
        sel[e, e, :] = 1.0
    c["sel"] = sel.reshape(NE, NE * 128)
    rt = np.zeros((128, 176), np.float32)
    rt[:, 0:128] = (s < t)
    ke = np.arange(NE)
    rt[0:NE, 128:160] = (ke[:, None] < ke[None, :])
    rt[:, 160:176] = np.arange(16, dtype=np.float32)[None, :] * 128 + idx[:, None]
    c["rt"] = rt
    return c


def _kc_layout(v):
    return np.ascontiguousarray(v.reshape(-1, 128).T)


class K:
    def __init__(self, stage=99, dbg=False):
        self.stage = stage
        nc = bass.Bass("TRN2", target_bir_lowering=False)
        self.nc = nc
        self.S = Sched(nc, 51200 + 1000)
        S = self.S
        dt = nc.dram_tensor
        self.din = {}

        def inp(name, shape):
            self.din[name] = dt(name, list(shape), F32, kind="ExternalInput").ap()
            return self.din[name]
        self.x = inp("x", [T, D])
        self.ctx = inp("ctx", [TC, D])
        self.cc = inp("cc", [128, 16])
        self.w_mod = inp("w_mod", [D, 6 * D])
        self.bmodT = inp("bmodT", [128, 48])
        self.bmod_row = inp("bmod_row", [1, 6 * D])
        self.gT = inp("gT", [128, 32])
        self.gpost_row = inp("gpost_row", [1, 2 * D])
        self.w_in = inp("w_in", [D, D_IN])
        self.wa2 = inp("wa2", [32, 512])
        self.ba_row = inp("ba_row", [1, 512])
        self.gnb = inp("gnb", [128, 512])
        self.gq = inp("gq", [128, 2])
        self.gkv = inp("gkv", [128, 1])
        self.w_uq = inp("w_uq", [256, 768])
        self.w_uk = inp("w_uk", [128, 512])
        self.w_uv = inp("w_uv", [128, 512])
        self.w_br_gla = inp("w_br_gla", [512, D])
        self.w_br_mla = inp("w_br_mla", [512, D])
        self.w_out = inp("w_out", [D, D])
        self.router_w = inp("router_w", [D, NE])
        self.router_b = inp("router_b", [1, NE])
        self.w_gate = inp("w_gate", [NE, D, D])
        self.w_up = inp("w_up", [NE, D, D])
        self.w_down = inp("w_down", [NE, D, D])
        self.bgT = inp("bgT", [128, NE * 8])
        self.buT = inp("buT", [128, NE * 8])
        self.b_down = inp("b_down", [NE, D])
        self.c_tri = inp("c_tri", [128, 512])
        self.c_mask = inp("c_mask", [128, 1024])
        self.c_ident = inp("c_ident", [128, 128])
        self.c_ropet = inp("c_ropet", [128, 2 * NTOK])
        self.c_e96 = inp("c_e96", [128, 128])
        self.c_sel = inp("c_sel", [NE, NE * 128])
        self.c_rt = inp("c_rt", [128, 176])
        self.xs_d = dt("xs_d", [NSLOT, D], BF16, kind="Internal").ap()
        self.ys_d = dt("ys_d", [NSLOT, D], F32, kind="Internal").ap()
        self.out = dt("out", [T, D], F32, kind="ExternalOutput").ap()
        self.x1d = dt("x1d", [T, D], F32, kind="Internal").ap()
        self.dbg = dt("dbg", [128, 8192], F32, kind="ExternalOutput").ap() if dbg else None
        self.P = [S.psum("ps%d" % i) for i in range(8)]
        self.wq_i = 0
        self.build()

    def MM(self, out, lhsT, rhs, start=True, stop=True, r=(), w=()):
        self.S.op("pe", lambda e: e.matmul(out, lhsT, rhs, start=start, stop=stop, skip_group_check=True), r, w)

    def TR(self, out, in_, ident, r=(), w=()):
        self.S.op("pe", lambda e: e.transpose(out, in_, ident), r, w)

    def ACT(self, out, in_, func, r=(), w=(), **kw):
        self.S.op("act", lambda e: e.activation(out, in_, func, **kw), r, w)

    def TT(self, eng, out, a, b, op, r=(), w=()):
        self.S.op(eng, lambda e: e.tensor_tensor(out, a, b, op=op), r, w)

    def TS(self, eng, out, a, s1, s2, op0, op1=None, r=(), w=()):
        if op1 is None:
            self.S.op(eng, lambda e: e.tensor_scalar(out, a, s1, None, op0=op0), r, w)
        else:
            self.S.op(eng, lambda e: e.tensor_scalar(out, a, s1, s2, op0=op0, op1=op1), r, w)

    def STT(self, out, a, s, b, op0, op1, r=(), w=()):
        self.S.op("dve", lambda e: e.scalar_tensor_tensor(out, a, s, b, op0=op0, op1=op1), r, w)

    def CP(self, eng, out, in_, r=(), w=()):
        if eng == "act":
            self.S.op("act", lambda e: e.copy(out, in_), r, w)
        else:
            self.S.op(eng, lambda e: e.tensor_copy(out, in_), r, w)

    def MS(self, eng, ap, val, w=()):
        self.S.op(eng, lambda e: e.memset(ap, val), (), w)

    def DMA(self, q, out, in_, r=(), w=(), sem=None, **kw):
        self.S.dma(q, out, in_, reads=r, writes=w, sem_buf=sem, **kw)

    def rstd_from_ss(self, ss_ap, n, buf, tmp_ap):
        self.ACT(tmp_ap, ss_ap, AF.Ln, r=[buf], w=[buf], scale=1.0 / n, bias=self.eps_ap)
        self.ACT(ss_ap, tmp_ap, AF.Exp, r=[buf], w=[buf], scale=-0.5)

    def load_w(self, dst_ap, dst_buf, src_ap, rows, cols, q="pool"):
        if rows > 128:
            self.DMA("pool", dst_ap, src_ap.rearrange("(k p) n -> p k n", p=128), w=[dst_buf])
        else:
            self.DMA("pool", dst_ap[0:rows, 0:cols], src_ap, w=[dst_buf])

    def dump(self, col, buf, ap, ncols, parts=128):
        if self.dbg is None:
            return
        S = self.S
        tmp = S.alloc("dbgtmp", [ncols], F32)
        self.CP("dve", tmp.ap[0:parts, :], ap, r=[buf], w=[tmp])
        self.DMA("pool", self.dbg[0:parts, col:col + ncols], tmp.ap[0:parts, :], r=[tmp], sem=tmp)
        self.dbg_bufs.append(tmp)

    def build(self):
        S = self.S
        P = self.P
        self.dbg_bufs = []
        cst = S.alloc("cst", [2048], F32)
        tri = cst.ap[:, 0:512].rearrange("p (a b) -> p a b", b=128)
        identf = cst.ap[:, 512:640]
        e96f = cst.ap[:, 640:768]
        self.eps_ap = cst.ap[:, 768:769]
        onesf = cst.ap[:, 896:1024]
        self.DMA("sp", cst.ap[:, 0:512], self.c_tri, w=[cst])
        self.DMA("sp", identf, self.c_ident, w=[cst])
        self.DMA("sp", e96f, self.c_e96, w=[cst])
        self.MS("pool", cst.ap[:, 768:769], EPS, w=[cst])
        self.MS("pool", onesf, 1.0, w=[cst])
        self.rowmask = cst.ap[:, 1024:1026]
        self.MS("pool", cst.ap[:, 1024:1026], 0.0, w=[cst])
        self.MS("pool", cst.ap[0:64, 1024:1025], 1.0, w=[cst])
        self.MS("pool", cst.ap[64:128, 1025:1026], 1.0, w=[cst])
        cstb = S.alloc("cstb", [2048], BF16)
        identb = cstb.ap[:, 0:128]
        onesb = cstb.ap[:, 128:256]
        e96b = cstb.ap[:, 256:384]
        maskb = cstb.ap[:, 1024:2048].rearrange("p (a b) -> p a b", b=512)
        self.CP("pool", identb, identf, r=[cst], w=[cstb])
        self.CP("pool", onesb, onesf, r=[cst], w=[cstb])
        self.CP("pool", e96b, e96f, r=[cst], w=[cstb])
        self.wstage = [S.alloc("wst", [8, 256], F32) for _ in range(3)]
        mstage = self.wstage[0]
        self.DMA("sp", mstage.ap[:, 0:4, :], self.c_mask.rearrange("p (a b) -> p a b", b=256), w=[mstage])
        self.CP("pool", cstb.ap[:, 1024:2048].rearrange("p (a b) -> p a b", b=256), mstage.ap[:, 0:4, :], r=[mstage], w=[cstb])
        stat = S.alloc("stat", [64], F32)

        modb = S.alloc("mod", [48, 2], F32)
        vec = S.alloc("vec", [16 + 48 + 32 + 32 + 16 + 16 + 8 + 8], F32)
        cc = vec.ap[:, 0:16].rearrange("p (a b) -> p a b", b=2)
        bmodT = vec.ap[:, 16:64]
        gT = vec.ap[:, 64:96].rearrange("p (a b) -> p a b", b=8)
        A1 = vec.ap[:, 96:112].rearrange("p (a b) -> p a b", b=2)
        B1v = None
        A2 = vec.ap[:, 112:120]
        gqv = vec.ap[:, 128:130]
        gkvv = vec.ap[:, 130:131]
        self.DMA("sp", vec.ap[:, 0:16], self.cc, w=[vec])
        self.DMA("sp", bmodT, self.bmodT, w=[vec])
        self.DMA("sp", vec.ap[:, 64:96], self.gT, w=[vec])
        self.DMA("sp", gqv, self.gq, w=[vec])
        self.DMA("sp", gkvv, self.gkv, w=[vec])
        self.ACT(cc, cc, AF.Silu, r=[vec], w=[vec])
        rows = S.alloc("rows", [4096], F32)
        self.DMA("sp", rows.ap[0:1, 2048:4096], self.gpost_row, w=[rows])
        browst = S.alloc("browst", [2048], F32)
        self.DMA("sp", browst.ap[0:1, 0:1024], self.bmod_row[:, 2048:3072], w=[browst])
        self.DMA("sp", browst.ap[0:1, 1024:2048], self.bmod_row[:, 5120:6144], w=[browst])
        wm_view = self.w_mod.rearrange("(k p) n -> p k n", p=128)
        ccb = S.alloc("ccb", [8, 2], BF16)
        self.CP("dve", ccb.ap[:, :, :], cc, r=[vec], w=[ccb])
        wmb = [S.alloc("wmb", [KC, 512], BF16) for _ in range(3)]
        for j in range(12):
            st = wmb[j % 3]
            self.DMA("pool", st.ap[:, :, :], wm_view[:, :, j * 512:(j + 1) * 512], w=[st])
            for m in range(4):
                col = (j * 4 + m) * 2
                for kc in range(KC):
                    self.MM(P[0].ap[:, col:col + 2], st.ap[:, kc, m * 128:(m + 1) * 128], ccb.ap[:, kc, :],
                            start=(kc == 0), stop=(kc == KC - 1), r=[st, ccb], w=[P[0]])
            if j in (4, 5, 10, 11):
                ro = {4: 0, 5: 512, 10: 1024, 11: 1536}[j]
                for kc in range(KC):
                    self.MM(P[1].ap[0:1, :], ccb.ap[:, kc, 0:1], st.ap[:, kc, :], start=(kc == 0), stop=(kc == KC - 1),
                            r=[st, ccb], w=[P[1]])
                self.TT("dve", rows.ap[0:1, ro:ro + 512], P[1].ap[0:1, :], browst.ap[0:1, ro:ro + 512], ALU.add,
                        r=[P[1], browst], w=[rows])
        S.release(ccb, *wmb)
        S.release(browst)
        self.TT("dve", modb.ap[:, :, :], P[0].ap[:, 0:96].rearrange("p (a b) -> p a b", b=2),
                bmodT.unsqueeze(2).to_broadcast([128, 48, 2]), ALU.add, r=[P[0], vec], w=[modb])
        self.STT(A1, modb.ap[:, 8:16, :], 1.0, gT[:, 0, :].unsqueeze(2).to_broadcast([128, 8, 2]), ALU.add, ALU.mult,
                 r=[modb, vec], w=[vec])
        self.STT(A2, modb.ap[:, 32:40, 0], 1.0, gT[:, 2, :], ALU.add, ALU.mult, r=[modb, vec], w=[vec])
        self.TT("dve", rows.ap[0:1, 0:2048], rows.ap[0:1, 0:2048], rows.ap[0:1, 2048:4096], ALU.mult, r=[rows], w=[rows])
        Gb = S.alloc("Gb", [2, 1024], F32)
        for g in range(2):
            for hh in range(2):
                pb = P[2 + hh]
                self.MM(pb.ap[:, :], onesf[0:1, :], rows.ap[0:1, g * 1024 + hh * 512: g * 1024 + hh * 512 + 512],
                        r=[cst, rows], w=[pb])
                self.CP("act", Gb.ap[:, g, hh * 512:(hh + 1) * 512], pb.ap[:, :], r=[pb], w=[Gb])
        S.release(rows, *self.wstage)
        if self.stage == 0:
            self.dump(0, modb, modb.ap[:, :, :].rearrange("p a b -> p (a b)"), 96)
            self.dump(96, vec, vec.ap[:, 96:120], 24)
            self.dump(128, Gb, Gb.ap[:, :, :].rearrange("p a b -> p (a b)"), 2048)
            return self.finish()

        hT = S.alloc("hT", [KC, NTOK], BF16)
        xts = [S.alloc("xt", [D], F32) for _ in range(3)]
        xns = [S.alloc("xn", [D], BF16) for _ in range(2)]
        junk = S.alloc("junk", [D], BF16)
        for i in range(NT):
            xt = xts[i % 3]
            xn = xns[i % 2]
            src = self.ctx[i * 128:(i + 1) * 128, :] if i < 2 else self.x[(i - 2) * 128:(i - 1) * 128, :]
            v = 1 if i < 2 else 0
            self.DMA("sp", xt.ap[:, :], src, w=[xt])
            ss = stat.ap[:, (i % 4) * 2:(i % 4) * 2 + 1]
            tmp = stat.ap[:, (i % 4) * 2 + 1:(i % 4) * 2 + 2]
            self.ACT(junk.ap[:, :], xt.ap[:, :], AF.Square, r=[xt], w=[junk, stat], accum_out=ss)
            self.rstd_from_ss(ss, D, stat, tmp)
            self.TS("dve", xn.ap[:, :], xt.ap[:, :], ss, None, ALU.mult, r=[xt, stat], w=[xn])
            pt = P[i % 2]
            ptb = pt.ap.bitcast(BF16)
            for kc in range(KC):
                self.TR(ptb[:, kc * 128:(kc + 1) * 128], xn.ap[:, kc * 128:(kc + 1) * 128], identb, r=[xn, cstb], w=[pt])
            for kc in range(KC):
                o = hT.ap[:, kc, i * 128:(i + 1) * 128]
                if kc % 2 == 0:
                    self.TS("dve", o, ptb[:, kc * 128:(kc + 1) * 128], A1[:, kc, v:v + 1], modb.ap[:, kc, v:v + 1],
                            ALU.mult, ALU.add, r=[pt, vec, modb], w=[hT])
                else:
                    self.ACT(o, ptb[:, kc * 128:(kc + 1) * 128], AF.Identity, r=[pt, vec, modb], w=[hT],
                             scale=A1[:, kc, v:v + 1], bias=modb.ap[:, kc, v:v + 1])
        S.release(*xts, *xns, junk)
        if self.stage == 1:
            for kc in range(2):
                self.dump(kc * 2304, hT, hT.ap[:, kc, :], 2304)
            return self.finish()
        self.cst, self.cstb, self.stat, self.vec, self.modb, self.Gb, self.hT, self.junk = cst, cstb, stat, vec, modb, Gb, hT, junk
        self.tri, self.identf, self.identb, self.onesf, self.onesb, self.e96b, self.maskb = tri, identf, identb, onesf, onesb, e96b, maskb
        self.A2, self.gqv, self.gkvv = A2, gqv, gkvv
        self.phase_gla()
        if self.stage == 25:
            return self.finish()
        if self.stage == 3:
            for hc in range(4):
                self.dump(hc * 2048, self.ogT, self.ogT.ap[:, hc, :], 2048)
            return self.finish()
        self.phase_mla()
        if self.stage == 4:
            for h in range(4):
                self.dump(h * 2048, self.omlaT, self.omlaT.ap[0:64, h, :], 2048, parts=64)
            return self.finish()
        self.phase_merge()
        if self.stage == 5:
            return self.finish()
        if MOE_MODE == "dense":
            self.phase_moe()
        else:
            self.phase_moe_sparse()
        self.finish()

    def phase_mla(self):
        S, P = self.S, self.P
        hT, cstb, cst, stat = self.hT, self.cstb, self.cst, self.stat
        identb, onesf, onesb, identf = self.identb, self.onesf, self.onesb, self.identf
        wB = S.alloc("wB", [KC, 416], BF16)
        self.load_w(wB.ap[:, :, :], wB, self.w_in[:, 1568:1984], 1024, 416)
        wkrs = S.alloc("wkrs", [KC, 32], BF16)
        for a, b_ in ((0, 8), (8, 0), (16, 24), (24, 16)):
            self.CP("pool", wkrs.ap[:, :, a:a + 8], wB.ap[:, :, 384 + b_:384 + b_ + 8], r=[wB], w=[wkrs])
        wuq = S.alloc("wuq", [2, 768], BF16)
        self.load_w(wuq.ap[:, :, :], wuq, self.w_uq, 256, 768)
        wuqs = S.alloc("wuqs", [2, 8, 32], BF16)
        for h in range(8):
            for a, b_ in ((0, 8), (8, 0), (16, 24), (24, 16)):
                self.CP("pool", wuqs.ap[:, :, h, a:a + 8], wuq.ap[:, :, h * 96 + 64 + b_:h * 96 + 64 + b_ + 8], r=[wuq], w=[wuqs])
        wukv = S.alloc("wukv", [1024], BF16)
        self.load_w(wukv.ap[:, 0:512], wukv, self.w_uk, 128, 512)
        self.load_w(wukv.ap[:, 512:1024], wukv, self.w_uv, 128, 512)
        zdqnT = S.alloc("zdqnT", [2, T], BF16)
        ckvT = S.alloc("ckvT", [NTOK], BF16)
        krT = S.alloc("krT", [NTOK], BF16)
        zn = [S.alloc("zn", [384], BF16) for _ in range(2)]
        junk = S.alloc("junk2", [384], F32)
        for i in range(NT):
            pa = P[i % 2]
            pt = P[2 + i % 2]
            ptb = pt.ap.bitcast(BF16)
            z = zn[i % 2]
            sc = (i % 4) * 4
            if i >= 2:
                for kc in range(KC):
                    self.MM(pa.ap[:, 0:256], hT.ap[:, kc, i * 128:(i + 1) * 128], wB.ap[:, kc, 0:256],
                            start=(kc == 0), stop=(kc == KC - 1), r=[wB, hT], w=[pa])
                self.ACT(junk.ap[:, 0:256], pa.ap[:, 0:256], AF.Square, r=[pa], w=[junk, stat], accum_out=stat.ap[:, sc:sc + 1])
                self.rstd_from_ss(stat.ap[:, sc:sc + 1], 256, stat, stat.ap[:, sc + 1:sc + 2])
                self.TS("dve", z.ap[:, 0:256], pa.ap[:, 0:256], stat.ap[:, sc:sc + 1], None, ALU.mult, r=[pa, stat], w=[z])
            for kc in range(KC):
                self.MM(pa.ap[:, 256:384], hT.ap[:, kc, i * 128:(i + 1) * 128], wB.ap[:, kc, 256:384],
                        start=(kc == 0), stop=(kc == KC - 1), r=[wB, hT], w=[pa])
            self.ACT(junk.ap[:, 256:384], pa.ap[:, 256:384], AF.Square, r=[pa], w=[junk, stat], accum_out=stat.ap[:, sc + 2:sc + 3])
            self.rstd_from_ss(stat.ap[:, sc + 2:sc + 3], 128, stat, stat.ap[:, sc + 3:sc + 4])
            self.TS("dve", z.ap[:, 256:384], pa.ap[:, 256:384], stat.ap[:, sc + 2:sc + 3], None, ALU.mult, r=[pa, stat], w=[z])
            if i >= 2:
                for c in range(2):
                    self.TR(ptb[:, c * 128:(c + 1) * 128], z.ap[:, c * 128:(c + 1) * 128], identb, r=[z, cstb], w=[pt])
                    self.TS("dve", zdqnT.ap[:, c, (i - 2) * 128:(i - 1) * 128], ptb[:, c * 128:(c + 1) * 128], self.gqv[:, c:c + 1], None,
                            ALU.mult, r=[pt, self.vec], w=[zdqnT])
            self.TR(ptb[:, 256:384], z.ap[:, 256:384], identb, r=[z, cstb], w=[pt])
            self.TS("dve", ckvT.ap[:, i * 128:(i + 1) * 128], ptb[:, 256:384], self.gkvv[:, 0:1], None, ALU.mult,
                    r=[pt, self.vec], w=[ckvT])
        TB_ALL = [(0, 256), (256, 512), (768, 512), (1280, 512), (1792, 512)]
        rp = [S.alloc("rp", [2, 512], F32) for _ in range(2)]
        rt = S.alloc("rt", [2, 512], F32)
        ropv = self.c_ropet.rearrange("p (a b) -> p a b", b=NTOK)
        for bi, (c0, nn) in enumerate(TB_ALL):
            r_ = rp[bi % 2]
            self.DMA("sp", r_.ap[64:96, :, 0:nn], ropv[64:96, :, c0:c0 + nn], w=[r_])
            for kc in range(KC):
                self.MM(P[4].ap[64:96, 0:nn], wB.ap[:, kc, 384:416], hT.ap[:, kc, c0:c0 + nn], start=(kc == 0), stop=(kc == KC - 1),
                        r=[wB, hT], w=[P[4]])
            for kc in range(KC):
                self.MM(P[5].ap[64:96, 0:nn], wkrs.ap[:, kc, :], hT.ap[:, kc, c0:c0 + nn], start=(kc == 0), stop=(kc == KC - 1),
                        r=[wkrs, hT], w=[P[5]])
            self.TT("dve", rt.ap[64:96, 0, 0:nn], P[4].ap[64:96, 0:nn], r_.ap[64:96, 0, 0:nn], ALU.mult, r=[P[4], r_], w=[rt])
            self.TT("dve", rt.ap[64:96, 1, 0:nn], P[5].ap[64:96, 0:nn], r_.ap[64:96, 1, 0:nn], ALU.mult, r=[P[5], r_], w=[rt])
            self.TT("pool", krT.ap[64:96, c0:c0 + nn], rt.ap[64:96, 0, 0:nn], rt.ap[64:96, 1, 0:nn], ALU.add, r=[rt], w=[krT])
        S.release(wB, wkrs, junk, *zn)
        v = S.alloc("v", [NT, 8, 128], BF16)
        self.MS("pool", v.ap[:, :, 0:4, 64:128], 0.0, w=[v])
        self.MS("pool", v.ap[:, :, 0:4, 64:65], 1.0, w=[v])
        self.MS("pool", v.ap[:, :, 4:8, 0:64], 0.0, w=[v])
        self.MS("pool", v.ap[:, :, 4:8, 0:1], 1.0, w=[v])
        for i in range(NT):
            pb = P[i % 2]
            self.MM(pb.ap[:, :], ckvT.ap[:, i * 128:(i + 1) * 128], wukv.ap[:, 512:1024], r=[ckvT, wukv], w=[pb])
            self.CP("act", v.ap[:, i, 0:4, 0:64], pb.ap[:, 0:256].rearrange("p (a b) -> p a b", b=64), r=[pb], w=[v])
            self.CP("dve", v.ap[:, i, 4:8, 64:128], pb.ap[:, 256:512].rearrange("p (a b) -> p a b", b=64), r=[pb], w=[v])
        omlaT = S.alloc("omlaT", [4, T], BF16)
        self.omlaT = omlaT
        kThs = [S.alloc("kTh", [NTOK], BF16) for _ in range(2)]
        qThs = [S.alloc("qTh", [T], BF16) for _ in range(2)]
        pT = [S.alloc("pT", [512], BF16) for _ in range(3)]
        rden = S.alloc("rden", [512], F32)
        bcs = S.alloc("bcs", [512], F32)
        sqb = S.alloc("sqb", [NTOK], BF16)
        mxs = [S.alloc("mx", [8], F32) for _ in range(2)]
        nb = [0]

        def build(h):
            kTh, qTh = kThs[h % 2], qThs[h % 2]
            for (c0, nn) in TB_ALL:
                pb = P[nb[0] % 2]
                nb[0] += 1
                self.MM(pb.ap[0:64, 0:nn], wukv.ap[:, h * 64:(h + 1) * 64], ckvT.ap[:, c0:c0 + nn], r=[wukv, ckvT], w=[pb])
                self.CP("dve", kTh.ap[0:64, c0:c0 + nn], pb.ap[0:64, 0:nn], r=[pb], w=[kTh])
            self.CP("pool", kTh.ap[64:96, :], krT.ap[64:96, :], r=[krT], w=[kTh])
            for qb in range(4):
                c0 = TC + qb * 512
                r_ = rp[qb % 2]
                self.DMA("sp", r_.ap[64:96, :, :], ropv[64:96, :, c0:c0 + 512], w=[r_])
                pq, pr = P[2], P[3]
                for c in range(2):
                    self.MM(pq.ap[0:96, :], wuq.ap[:, c, h * 96:(h + 1) * 96], zdqnT.ap[:, c, qb * 512:(qb + 1) * 512],
                            start=(c == 0), stop=(c == 1), r=[wuq, zdqnT], w=[pq])
                for c in range(2):
                    self.MM(pr.ap[64:96, :], wuqs.ap[:, c, h, :], zdqnT.ap[:, c, qb * 512:(qb + 1) * 512],
                            start=(c == 0), stop=(c == 1), r=[wuqs, zdqnT], w=[pr])
                self.CP("dve", qTh.ap[0:64, qb * 512:(qb + 1) * 512], pq.ap[0:64, :], r=[pq], w=[qTh])
                self.TT("dve", rt.ap[64:96, 0, :], pq.ap[64:96, :], r_.ap[64:96, 0, :], ALU.mult, r=[pq, r_], w=[rt])
                self.TT("dve", rt.ap[64:96, 1, :], pr.ap[64:96, :], r_.ap[64:96, 1, :], ALU.mult, r=[pr, r_], w=[rt])
                self.TT("pool", qTh.ap[64:96, qb * 512:(qb + 1) * 512], rt.ap[64:96, 0, :], rt.ap[64:96, 1, :], ALU.add, r=[rt], w=[qTh])
            sq = sqb
            mx = mxs[h % 2]
            pn = P[3]
            for which, src_, ntile in ((0, qTh, 16), (1, kTh, NT)):
                self.TT("pool", sq.ap[0:96, 0:ntile * 128], src_.ap[0:96, 0:ntile * 128], src_.ap[0:96, 0:ntile * 128], ALU.mult,
                        r=[src_], w=[sq])
                for t_ in range(ntile):
                    self.MM(pn.ap[:, which * 32 + t_:which * 32 + t_ + 1], sq.ap[0:96, t_ * 128:(t_ + 1) * 128], onesb[0:96, 0:1],
                            r=[sq, cstb], w=[pn])
                self.S.op("dve", lambda e, o=mx.ap[:, which:which + 1], a=pn.ap[:, which * 32:which * 32 + ntile]: e.reduce_max(o, a, axis=AX.X),
                          [pn], [mx])
            for which in range(2):
                self.TR(pn.ap[0:1, 64 + which * 128:64 + (which + 1) * 128], mx.ap[:, which:which + 1], identf, r=[mx, cst], w=[pn])
                self.S.op("dve", lambda e, o=mx.ap[0:1, 2 + which:3 + which], a=pn.ap[0:1, 64 + which * 128:64 + (which + 1) * 128]:
                          e.reduce_max(o, a, axis=AX.X), [pn], [mx])
            self.TT("dve", mx.ap[0:1, 4:5], mx.ap[0:1, 2:3], mx.ap[0:1, 3:4], ALU.mult, r=[mx], w=[mx])
            self.ACT(mx.ap[0:1, 5:6], mx.ap[0:1, 4:5], AF.Ln, r=[mx], w=[mx], bias=self.eps_ap[0:1, :])
            self.ACT(mx.ap[0:1, 6:7], mx.ap[0:1, 5:6], AF.Exp, r=[mx], w=[mx], scale=0.5)
            self.MM(pn.ap[:, 400:401], onesf[0:1, :], mx.ap[0:1, 6:7], r=[cst, mx], w=[pn])
            self.TS("dve", mx.ap[:, 7:8], pn.ap[:, 400:401], -1.02 * MLA_SCALE, None, ALU.mult, r=[pn], w=[mx])

        def tail(h, qb):
            po = P[6 + qb % 2]
            dr, lo = (64, 0) if h < 4 else (0, 64)
            self.CP("act", rden.ap[dr:dr + 1, :], po.ap[dr:dr + 1, :], r=[po], w=[rden])
            pbc = P[2]
            self.MM(pbc.ap[lo:lo + 64, :], onesf[dr:dr + 1, 0:64], rden.ap[dr:dr + 1, :], r=[cst, rden], w=[pbc])
            self.S.op("dve", lambda e, o=bcs.ap[lo:lo + 64, :], a=pbc.ap[lo:lo + 64, :]: e.reciprocal(o, a), [pbc], [bcs])
            self.TT("dve", omlaT.ap[lo:lo + 64, h % 4, qb * 512:(qb + 1) * 512], po.ap[lo:lo + 64, :], bcs.ap[lo:lo + 64, :], ALU.mult,
                    r=[po, bcs], w=[omlaT])
        build(0)
        pend = None
        for h in range(8):
            kTh, qTh = kThs[h % 2], qThs[h % 2]
            if h + 1 < 8:
                build(h + 1)
            for qb in range(4):
                po = P[6 + qb % 2]

                def qk(kt, qb=qb, kTh=kTh, qTh=qTh):
                    self.MM(P[4 + kt % 2].ap[:, :], kTh.ap[0:96, kt * 128:(kt + 1) * 128], qTh.ap[0:96, qb * 512:(qb + 1) * 512],
                            r=[kTh, qTh], w=[P[4 + kt % 2]])
                qk(0)
                for kt in range(NT):
                    psb = P[4 + kt % 2]
                    p_ = pT[kt % 3]
                    if kt + 1 < NT:
                        qk(kt + 1)
                    self.ACT(p_.ap[:, :], psb.ap[:, :], AF.Exp, r=[psb, mxs[h % 2]], w=[p_], scale=MLA_SCALE, bias=mxs[h % 2].ap[:, 7:8])
                    self.MM(po.ap[:, :], v.ap[:, kt, h, :], p_.ap[:, :], start=(kt == 0), stop=(kt == NT - 1), r=[v, p_], w=[po])
                    if kt == 2 and pend is not None:
                        tail(*pend)
                        pend = None
                pend = (h, qb)
        tail(*pend)
        kTh, qTh = kThs[0], qThs[0]
        S.release(kThs[1], qThs[1], sqb, *mxs)
        S.release(wuq, wuqs, wukv, zdqnT, ckvT, krT, v, kTh, qTh, rden, bcs, rt, *pT, *rp)

    def phase_gla(self):
        S, P = self.S, self.P
        hT, cstb, cst, stat = self.hT, self.cstb, self.cst, self.stat
        TB_ALL = [(0, 256), (256, 512), (768, 512), (1280, 512), (1792, 512)]
        TB_X = TB_ALL[1:]
        wA = S.alloc("wA", [KC, 1056], BF16)
        self.load_w(wA.ap[:, :, 0:1024], wA, self.w_in[:, 0:1024], 1024, 1024)
        self.load_w(wA.ap[:, :, 1024:1056], wA, self.w_in[:, 1536:1568], 1024, 32)
        wzg = S.alloc("wzg", [KC, 512], BF16)
        self.load_w(wzg.ap[:, :, :], wzg, self.w_in[:, 1024:1536], 1024, 512)
        wa2b = S.alloc("wa2b", [1024], BF16)
        self.load_w(wa2b.ap[:, 0:512], wa2b, self.wa2, 32, 512)
        self.load_w(wa2b.ap[:, 512:1024], wa2b, self.ba_row, 1, 512)
        gnb = S.alloc("gnb", [512], F32)
        self.DMA("sp", gnb.ap[:, :], self.gnb, w=[gnb])
        self.xsz = S.virt("xsz")
        zt = S.alloc("zt", [D], BF16)
        self.zt = zt
        self.MS("pool", zt.ap[:, :], 0.0, w=[zt])
        for c in range(NSLOT // 128):
            self.DMA("sp", self.xs_d[c * 128:(c + 1) * 128, :], zt.ap[:, :], r=[zt], w=[self.xsz], sem=zt)
        gqT = S.alloc("gqT", [2, T], BF16)
        gkT = S.alloc("gkT", [2, NTOK], BF16)
        gk = S.alloc("gk", [NT, 256], BF16)
        gv = S.alloc("gv", [NT, 512], BF16)
        zaT = S.alloc("zaT", [NTOK], BF16)
        n = 0
        for (c0, nn) in TB_ALL:
            for m in range(4):
                if m < 2 and c0 < TC:
                    continue
                pb = P[n % 2]
                n += 1
                for kc in range(KC):
                    self.MM(pb.ap[:, 0:nn], wA.ap[:, kc, m * 128:(m + 1) * 128], hT.ap[:, kc, c0:c0 + nn],
                            start=(kc == 0), stop=(kc == KC - 1), r=[wA, hT], w=[pb])
                if m < 2:
                    self.CP("act", gqT.ap[:, m, c0 - TC:c0 - TC + nn], pb.ap[:, 0:nn], r=[pb], w=[gqT])
                else:
                    self.CP("dve", gkT.ap[:, m - 2, c0:c0 + nn], pb.ap[:, 0:nn], r=[pb], w=[gkT])
            pb = P[n % 2]
            n += 1
            for kc in range(KC):
                self.MM(pb.ap[0:32, 0:nn], wA.ap[:, kc, 1024:1056], hT.ap[:, kc, c0:c0 + nn],
                        start=(kc == 0), stop=(kc == KC - 1), r=[wA, hT], w=[pb])
            self.CP("act", zaT.ap[0:32, c0:c0 + nn], pb.ap[0:32, 0:nn], r=[pb], w=[zaT])
        for i in range(NT):
            pa, pv = P[2 + i % 2], P[4 + i % 2]
            for kc in range(KC):
                self.MM(pa.ap[:, 0:256], hT.ap[:, kc, i * 128:(i + 1) * 128], wA.ap[:, kc, 256:512],
                        start=(kc == 0), stop=(kc == KC - 1), r=[wA, hT], w=[pa])
            for kc in range(KC):
                self.MM(pv.ap[:, :], hT.ap[:, kc, i * 128:(i + 1) * 128], wA.ap[:, kc, 512:1024],
                        start=(kc == 0), stop=(kc == KC - 1), r=[wA, hT], w=[pv])
            self.CP("act", gk.ap[:, i, :], pa.ap[:, 0:256], r=[pa], w=[gk])
            self.CP("dve", gv.ap[:, i, :], pv.ap[:, :], r=[pv], w=[gv])
        S.release(wA)
        if self.stage == 2:
            for m in range(2):
                self.dump(m * 2048, gqT, gqT.ap[:, m, :], 2048)
            self.dump(4096, gv, gv.ap[:, 5, :], 512)
            self.dump(4608, gk, gk.ap[:, 5, :], 256)
            self.dump(4864, zaT, zaT.ap[:, 0:2304], 2304)
            self.ogT = gqT
            return

        ogT = S.alloc("ogT", [4, T], BF16)
        self.ogT = ogT
        hist = S.alloc("hist", [32, 256], BF16)
        Sst = [S.alloc("Sst", [256], F32) for _ in range(2)]
        Sbr = [S.alloc("Sbr", [256], BF16) for _ in range(2)]
        R = 2
        la_b = [S.alloc("la", [256], F32) for _ in range(R)]
        lt_b = [S.alloc("lt", [256], F32) for _ in range(R)]
        ET_b = [S.alloc("ET", [256], F32) for _ in range(R)]
        EI_b = [S.alloc("EI", [256], F32) for _ in range(R)]
        KS_b = [S.alloc("KS", [256], F32) for _ in range(R)]
        qd_b = [S.alloc("qd", [2, 2, 128], BF16) for _ in range(2)]
        qt_b = [S.alloc("qt", [2, 128], F32) for _ in range(2)]
        ki_b = [S.alloc("ki", [2, 128], BF16) for _ in range(R)]
        kte_b = [S.alloc("kte", [256], BF16) for _ in range(R)]
        att_b = [S.alloc("att", [512], BF16) for _ in range(R)]
        ot_b = S.alloc("ot", [512], F32)
        sq_b = S.alloc("sq", [512], F32)
        sz_b = S.alloc("sz", [512], F32)
        og_b = S.alloc("og", [512], BF16)
        self.cnt = 0
        tri, maskb, onesb, identb = self.tri, self.maskb, self.onesb, self.identb
        for d in range(2):
            self.MS("pool", Sst[d].ap[:, :], 0.0, w=[Sst[d]])
        self.MS("pool", Sbr[0].ap[:, :], 0.0, w=[Sbr[0]])

        def prep(i, d, want_q):
            c = self.cnt
            self.cnt += 1
            r = c % R
            pl, pc = P[0 + c % 2], P[2 + c % 2]
            la, lt, ET, EI, KS = la_b[r], lt_b[r], ET_b[r], EI_b[r], KS_b[r]
            self.MM(pl.ap[:, 0:256], zaT.ap[0:32, i * 128:(i + 1) * 128], wa2b.ap[0:32, d * 256:(d + 1) * 256],
                    start=True, stop=False, r=[zaT, wa2b], w=[pl])
            self.MM(pl.ap[:, 0:256], onesb[0:1, :], wa2b.ap[0:1, 512 + d * 256:512 + (d + 1) * 256],
                    start=False, stop=True, r=[cstb, wa2b], w=[pl])
            self.ACT(lt.ap[:, :], pl.ap[:, 0:256], AF.Abs, r=[pl], w=[lt])
            self.ACT(lt.ap[:, :], lt.ap[:, :], AF.Exp, r=[lt], w=[lt], scale=-1.0)
            self.ACT(lt.ap[:, :], lt.ap[:, :], AF.Ln, r=[lt], w=[lt], bias=self.onesf[:, 0:1])
            self.TS("dve", la.ap[:, :], pl.ap[:, 0:256], 0.0, 1.0 / 16, ALU.min, ALU.mult, r=[pl], w=[la])
            self.STT(la.ap[:, :], lt.ap[:, :], -1.0 / 16, la.ap[:, :], ALU.mult, ALU.add, r=[lt, la], w=[la])
            for j in range(2):
                self.MM(pc.ap[:, j * 128:(j + 1) * 128], la.ap[:, j * 128:(j + 1) * 128], tri[:, d, :], r=[la, cst], w=[pc])
            self.MM(pc.ap[:, 256:512], tri[:, 2 + d, :], la.ap[:, :], r=[la, cst], w=[pc])
            self.ACT(ET.ap[:, :], pc.ap[:, 0:256], AF.Exp, r=[pc], w=[ET])
            self.ACT(KS.ap[:, :], pc.ap[:, 256:512], AF.Exp, r=[pc], w=[KS])
            kte = kte_b[r]
            self.TT("pool", kte.ap[:, :], gk.ap[:, i, :], KS.ap[:, :], ALU.mult, r=[gk, KS], w=[kte])
            out = {"pl": pl, "pc": pc, "ET": ET, "kte": kte}
            if want_q:
                xc = (i - 2) * 128
                self.ACT(EI.ap[:, :], pc.ap[:, 0:256], AF.Exp, r=[pc], w=[EI], scale=-1.0)
                qd = qd_b[c % 2]
                ki = ki_b[r]
                qt = qt_b[c % 2]
                self.STT(qt.ap[:, :, :], gqT.ap[:, :, xc:xc + 128], 0.125, ET.ap[:, :].rearrange("p (a b) -> p a b", b=128),
                         ALU.mult, ALU.mult, r=[gqT, ET], w=[qt])
                for p in range(2):
                    self.TS("pool" if p else "dve", qd.ap[:, p, :, :], qt.ap[:, :, :], self.rowmask[:, p:p + 1], None, ALU.mult,
                            r=[qt, cst], w=[qd])
                self.TT("pool", ki.ap[:, :, :], gkT.ap[:, :, i * 128:(i + 1) * 128],
                        EI.ap[:, :].rearrange("p (a b) -> p a b", b=128), ALU.mult, r=[gkT, EI], w=[ki])
                out["qd"], out["ki"] = qd, ki
            return out

        def ds_update(i, d, b, cs):
            pl, ET, kte = b["pl"], b["ET"], b["kte"]
            for h in range(4):
                self.MM(pl.ap[(h % 2) * 64:(h % 2) * 64 + 64, 256 + (h // 2) * 128:256 + (h // 2) * 128 + 128],
                        kte.ap[cs:cs + 64, h * 64:(h + 1) * 64], gv.ap[cs:cs + 64, i, h * 128:(h + 1) * 128],
                        r=[kte, gv], w=[pl])
            tl = cs + 63 if d == 0 else cs
            for j in range(2):
                self.STT(Sst[d].ap[:, j * 128:(j + 1) * 128], Sst[d].ap[:, j * 128:(j + 1) * 128],
                         ET.ap[:, j * 128 + tl:j * 128 + tl + 1], pl.ap[:, 256 + j * 128:256 + (j + 1) * 128],
                         ALU.mult, ALU.add, r=[Sst[d], ET, pl], w=[Sst[d]])

        order_b = [1, 0] + list(range(17, 1, -1))
        for i in order_b:
            b = prep(i, 1, False)
            for cs in (64, 0):
                if i >= 2:
                    ch = (i - 2) * 2 + (1 if cs == 64 else 0)
                    self.CP("act", hist.ap[:, ch, :], Sst[1].ap[:, :], r=[Sst[1]], w=[hist])
                ds_update(i, 1, b, cs)

        if self.stage == 25:
            S.release(ot_b, sq_b, sz_b)
            self.dump(0, hist, hist.ap[:, 31, :], 256)
            self.dump(256, hist, hist.ap[:, 0, :], 256)
            return
        live = 0
        for i in range(NT):
            bf = prep(i, 0, i >= 2)
            if i < 2:
                for cs in (0, 64):
                    ds_update(i, 0, bf, cs)
                if i == 1:
                    self.CP("act", Sbr[0].ap[:, :], Sst[0].ap[:, :], r=[Sst[0]], w=[Sbr[0]])
                continue
            bb = prep(i, 1, True)
            xi = i - 2
            xc = xi * 128
            pos = (P[6], P[7])
            pss = (P[4], P[5])
            first = [True, True]
            acol = lambda h: ((h % 2) * 2 + h // 2) * 128
            for d, b in ((0, bf), (1, bb)):
                att = att_b[d]
                qd, ki = b["qd"], b["ki"]
                for h in range(4):
                    hp = (h % 2) * 64
                    self.MM(pss[h % 2].ap[:, (h // 2) * 128:(h // 2) * 128 + 128], ki.ap[:, h // 2, :],
                            qd.ap[:, h % 2, h // 2, :], r=[ki, qd], w=[pss[h % 2]])
                for p in range(2):
                    self.TT("dve", att.ap[:, p * 256:(p + 1) * 256], pss[p].ap[:, 0:256], maskb[:, d, 0:256], ALU.mult,
                            r=[pss[p], cstb], w=[att])
                for h in range(4):
                    self.MM(pos[h % 2].ap[:, (h // 2) * 128:(h // 2) * 128 + 128], att.ap[:, acol(h):acol(h) + 128],
                            gv.ap[:, i, h * 128:(h + 1) * 128], start=first[h % 2], stop=False, r=[att, gv], w=[pos[h % 2]])
                    first[h % 2] = False
            qd = bb["qd"]
            for cs in (64, 0):
                ch = xi * 2 + (1 if cs == 64 else 0)
                for h in range(4):
                    hp = (h % 2) * 64
                    self.MM(pos[h % 2].ap[cs:cs + 64, (h // 2) * 128:(h // 2) * 128 + 128], qd.ap[:, h % 2, h // 2, cs:cs + 64],
                            hist.ap[:, ch, (h // 2) * 128:(h // 2) * 128 + 128], start=False, stop=False,
                            r=[qd, hist], w=[pos[h % 2]])
            qd = bf["qd"]
            for cs in (0, 64):
                sb = Sbr[live % 2]
                for h in range(4):
                    hp = (h % 2) * 64
                    self.MM(pos[h % 2].ap[cs:cs + 64, (h // 2) * 128:(h // 2) * 128 + 128], qd.ap[:, h % 2, h // 2, cs:cs + 64],
                            sb.ap[:, (h // 2) * 128:(h // 2) * 128 + 128], start=False, stop=(cs == 64 and h >= 2),
                            r=[qd, sb], w=[pos[h % 2]])
                ds_update(i, 0, bf, cs)
                live += 1
                self.CP("act", Sbr[live % 2].ap[:, :], Sst[0].ap[:, :], r=[Sst[0]], w=[Sbr[live % 2]])
            pz = bf["pc"]
            for kc in range(KC):
                self.MM(pz.ap[:, :], hT.ap[:, kc, i * 128:(i + 1) * 128], wzg.ap[:, kc, :], start=(kc == 0), stop=(kc == KC - 1),
                        r=[hT, wzg], w=[pz])
            self.ACT(sz_b.ap[:, :], pz.ap[:, :], AF.Silu, r=[pz], w=[sz_b])
            for h in range(4):
                self.CP("act" if h % 2 == 0 else "dve", ot_b.ap[:, h * 128:(h + 1) * 128],
                        pos[h % 2].ap[:, (h // 2) * 128:(h // 2) * 128 + 128], r=[pos[h % 2]], w=[ot_b])
            self.TT("pool", sq_b.ap[:, :], ot_b.ap[:, :], ot_b.ap[:, :], ALU.mult, r=[ot_b], w=[sq_b])
            st4 = stat.ap[:, 16:20]
            st4b = stat.ap[:, 20:24]
            self.S.op("dve", lambda e, o=st4, a=sq_b.ap[:, :].rearrange("p (a b) -> p a b", b=128): e.reduce_sum(o, a, axis=AX.X),
                      [sq_b], [stat])
            self.ACT(st4b, st4, AF.Ln, r=[stat], w=[stat], scale=1.0 / 128, bias=self.eps_ap)
            self.ACT(st4, st4b, AF.Exp, r=[stat], w=[stat], scale=-0.5)
            self.TT("dve", sq_b.ap[:, :].rearrange("p (a b) -> p a b", b=128), ot_b.ap[:, :].rearrange("p (a b) -> p a b", b=128),
                    st4.unsqueeze(2).to_broadcast([128, 4, 128]), ALU.mult, r=[ot_b, stat], w=[sq_b])
            self.TT("pool", sq_b.ap[:, :], sq_b.ap[:, :], gnb.ap[:, :], ALU.mult, r=[sq_b, gnb], w=[sq_b])
            self.TT("dve", og_b.ap[:, :], sq_b.ap[:, :], sz_b.ap[:, :], ALU.mult, r=[sq_b, sz_b], w=[og_b])
            pt = P[4]
            ptb = pt.ap.bitcast(BF16)
            for h in range(4):
                self.TR(ptb[:, h * 128:(h + 1) * 128], og_b.ap[:, h * 128:(h + 1) * 128], identb, r=[og_b, cstb], w=[pt])
            self.CP("act", ogT.ap[:, :, xc:xc + 128], ptb[:, 0:512].rearrange("p (a b) -> p a b", b=128), r=[pt], w=[ogT])
        S.release(gqT, gkT, gk, gv, zaT, hist, wzg, wa2b, gnb, ot_b, sq_b, sz_b, og_b,
                  *Sst, *Sbr, *la_b, *lt_b, *ET_b, *EI_b, *KS_b, *qd_b, *qt_b, *ki_b, *kte_b, *att_b)

    def phase_merge(self):
        S, P = self.S, self.P
        hT, ogT, omlaT, stat, cst, cstb = self.hT, self.ogT, self.omlaT, self.stat, self.cst, self.cstb
        mT = S.alloc("mT", [KC, T], BF16)
        wbg = S.alloc("wbg", [4, D], BF16)
        self.load_w(wbg.ap[:, :, :], wbg, self.w_br_gla, 512, D)
        wbm = S.alloc("wbm", [4, D], BF16)
        src = self.w_br_mla.rearrange("(k p) n -> p k n", p=64)
        self.DMA("pool", wbm.ap[0:64, :, :], src[:, 0:4, :], w=[wbm])
        self.DMA("pool", wbm.ap[64:128, :, :], src[:, 4:8, :], w=[wbm])
        wz = [S.alloc("wz", [KC, 256], BF16) for _ in range(2)]
        sg = [S.alloc("sg", [512], F32) for _ in range(2)]
        sm = [S.alloc("sm", [512], F32) for _ in range(2)]
        n = 0
        for m in range(KC):
            w_ = wz[m % 2]
            self.load_w(w_.ap[:, :, 0:128], w_, self.w_in[:, 1984 + m * 128:1984 + (m + 1) * 128], 1024, 128)
            self.load_w(w_.ap[:, :, 128:256], w_, self.w_in[:, 3008 + m * 128:3008 + (m + 1) * 128], 1024, 128)
            for tb in range(4):
                c0, xc0 = TC + tb * 512, tb * 512
                o = (n % 2) * 4
                sg_, sm_ = sg[n % 2], sm[n % 2]
                n += 1
                for kc in range(KC):
                    self.MM(P[o].ap[:, :], w_.ap[:, kc, 0:128], hT.ap[:, kc, c0:c0 + 512], start=(kc == 0), stop=(kc == KC - 1),
                            r=[w_, hT], w=[P[o]])
                for kc in range(KC):
                    self.MM(P[o + 1].ap[:, :], w_.ap[:, kc, 128:256], hT.ap[:, kc, c0:c0 + 512], start=(kc == 0), stop=(kc == KC - 1),
                            r=[w_, hT], w=[P[o + 1]])
                for hc in range(4):
                    self.MM(P[o + 2].ap[:, :], wbg.ap[:, hc, m * 128:(m + 1) * 128], ogT.ap[:, hc, xc0:xc0 + 512],
                            start=(hc == 0), stop=(hc == 3), r=[wbg, ogT], w=[P[o + 2]])
                for j in range(4):
                    self.MM(P[o + 3].ap[:, :], wbm.ap[:, j, m * 128:(m + 1) * 128], omlaT.ap[:, j, xc0:xc0 + 512],
                            start=(j == 0), stop=(j == 3), r=[wbm, omlaT], w=[P[o + 3]])
                self.ACT(sg_.ap[:, :], P[o].ap[:, :], AF.Sigmoid, r=[P[o]], w=[sg_])
                self.ACT(sm_.ap[:, :], P[o + 1].ap[:, :], AF.Sigmoid, r=[P[o + 1]], w=[sm_])
                self.TT("dve", sg_.ap[:, :], sg_.ap[:, :], P[o + 2].ap[:, :], ALU.mult, r=[sg_, P[o + 2]], w=[sg_])
                self.TT("dve", sm_.ap[:, :], sm_.ap[:, :], P[o + 3].ap[:, :], ALU.mult, r=[sm_, P[o + 3]], w=[sm_])
                self.TT("pool", mT.ap[:, m, xc0:xc0 + 512], sg_.ap[:, :], sm_.ap[:, :], ALU.add, r=[sg_, sm_], w=[mT])
        S.release(hT, ogT, omlaT, wbg, wbm, *wz, *sg, *sm)
        wo = S.alloc("wo", [KC, D], BF16)
        self.load_w(wo.ap[:, :, :], wo, self.w_out, D, D)
        rw = S.alloc("rw", [KC * NE + NE], F32)
        rwv = rw.ap[:, 0:KC * NE].rearrange("p (a b) -> p a b", b=NE)
        self.DMA("sp", rwv, self.router_w.rearrange("(k p) n -> p k n", p=128), w=[rw])
        self.DMA("sp", rw.ap[0:1, KC * NE:KC * NE + NE], self.router_b, w=[rw])
        comb = S.alloc("comb", [16, NE], F32)
        self.comb = comb
        rt_ = S.alloc("rt", [176 + 144], F32)
        self.DMA("sp", rt_.ap[:, 0:176], self.c_rt, w=[rt_])
        tsub = S.alloc("tsub", [128], BF16)
        self.CP("pool", tsub.ap[:, :], rt_.ap[:, 0:128], r=[rt_], w=[tsub])
        Umat = rt_.ap[:, 128:160]
        IO = rt_.ap[:, 160:176]
        cb = rt_.ap[:, 176:208]
        posf4 = rt_.ap[:, 208:212]
        oh = rt_.ap[:, 224:256]
        oh2 = rt_.ap[:, 256:288]
        posf = rt_.ap[:, 288:320]
        self.MS("pool", rt_.ap[:, 176:320], 0.0, w=[rt_])
        wk = S.alloc("wk", [16, 4], F32)
        posi = S.alloc("posi", [16, 4], F32)
        posi_v = posi.ap.bitcast(I32)
        self.wk, self.posi, self.posi_v = wk, posi, posi_v
        maskb16 = S.alloc("maskb16", [NE], BF16)
        rank_all = S.alloc("rank", [16, NE], F32)
        lgall = S.alloc("lgall", [16, 40], F32)
        xn2tm = S.alloc("xn2tm", [16, D], BF16)
        xsz, xsd = self.xsz, S.virt("xsd")
        self.xsd, self.ysd = xsd, S.virt("ysd")
        xt = [S.alloc("xt2", [D], F32) for _ in range(2)]
        tt = [S.alloc("tt2", [D], F32) for _ in range(2)]
        hx2f = S.alloc("hx2f", [KC, 128], F32)
        lg = S.alloc("lg", [128], F32)
        junk = S.alloc("junk3", [D], BF16)
        Gb, A2, modb, identf, onesf = self.Gb, self.A2, self.modb, self.identf, self.onesf
        for xi in range(16):
            pa, pb = P[(xi % 2) * 2], P[(xi % 2) * 2 + 1]
            x_, t_ = xt[xi % 2], tt[xi % 2]
            self.DMA("sp", x_.ap[:, :], self.x[xi * 128:(xi + 1) * 128, :], w=[x_])
            for hf, pp in enumerate((pa, pb)):
                for m in range(KC):
                    self.MM(pp.ap[:, :], mT.ap[:, m, xi * 128:(xi + 1) * 128], wo.ap[:, m, hf * 512:(hf + 1) * 512],
                            start=(m == 0), stop=(m == KC - 1), r=[mT, wo], w=[pp])
            sc = 24 + (xi % 2) * 8
            for hf, pp in enumerate((pa, pb)):
                self.ACT(junk.ap[:, 0:512], pp.ap[:, :], AF.Square, r=[pp], w=[junk, stat], accum_out=stat.ap[:, sc + hf:sc + hf + 1])
            self.TT("dve", stat.ap[:, sc:sc + 1], stat.ap[:, sc:sc + 1], stat.ap[:, sc + 1:sc + 2], ALU.add, r=[stat], w=[stat])
            self.rstd_from_ss(stat.ap[:, sc:sc + 1], D, stat, stat.ap[:, sc + 2:sc + 3])
            for hf, pp in enumerate((pa, pb)):
                self.STT(t_.ap[:, hf * 512:(hf + 1) * 512], pp.ap[:, :], stat.ap[:, sc:sc + 1], Gb.ap[:, 0, hf * 512:(hf + 1) * 512],
                         ALU.mult, ALU.mult, r=[pp, stat, Gb], w=[t_])
            self.TT("pool", x_.ap[:, :], t_.ap[:, :], x_.ap[:, :], ALU.add, r=[t_, x_], w=[x_])
            self.DMA("pool", self.x1d[xi * 128:(xi + 1) * 128, :], x_.ap[:, :], r=[x_], sem=x_)
            self.ACT(junk.ap[:, :], x_.ap[:, :], AF.Square, r=[x_], w=[junk, stat], accum_out=stat.ap[:, sc + 3:sc + 4])
            self.rstd_from_ss(stat.ap[:, sc + 3:sc + 4], D, stat, stat.ap[:, sc + 4:sc + 5])
            self.TS("dve", t_.ap[:, :], x_.ap[:, :], stat.ap[:, sc + 3:sc + 4], None, ALU.mult, r=[x_, stat], w=[t_])
            for kc in range(KC):
                pt = P[4 + kc // 4]
                self.TR(pt.ap[:, (kc % 4) * 128:(kc % 4 + 1) * 128], t_.ap[:, kc * 128:(kc + 1) * 128], identf, r=[t_, cst], w=[pt])
            for kc in range(KC):
                pt = P[4 + kc // 4]
                src_ = pt.ap[:, (kc % 4) * 128:(kc % 4 + 1) * 128]
                if kc % 2 == 0:
                    self.TS("dve", hx2f.ap[:, kc, :], src_, A2[:, kc:kc + 1], modb.ap[:, 24 + kc, 0:1], ALU.mult, ALU.add,
                            r=[pt, self.vec, modb], w=[hx2f])
                else:
                    self.ACT(hx2f.ap[:, kc, :], src_, AF.Identity, r=[pt, self.vec, modb], w=[hx2f],
                             scale=A2[:, kc:kc + 1], bias=modb.ap[:, 24 + kc, 0:1])
            pr = P[6 + xi % 2]
            for kc in range(KC):
                self.MM(pr.ap[:, 0:NE], hx2f.ap[:, kc, :], rwv[:, kc, :], start=(kc == 0), stop=False, r=[hx2f, rw], w=[pr])
            self.MM(pr.ap[:, 0:NE], onesf[0:1, :], rw.ap[0:1, KC * NE:KC * NE + NE], start=False, stop=True, r=[cst, rw], w=[pr])
            self.CP("act", lg.ap[:, 0:NE], pr.ap[:, 0:NE], r=[pr], w=[lg])
            self.S.op("dve", lambda e, o=lg.ap[:, 32:40], a=lg.ap[:, 0:NE]: e.max(out=o, in_=a), [lg], [lg])
            self.TS("dve", lg.ap[:, 64:96], lg.ap[:, 0:NE], lg.ap[:, 35:36], None, ALU.is_ge, r=[lg], w=[lg])
            self.TS("dve", lg.ap[:, 40:41], lg.ap[:, 32:33], -1.0, None, ALU.mult, r=[lg], w=[lg])
            self.ACT(lg.ap[:, 96:128], lg.ap[:, 0:NE], AF.Exp, r=[lg], w=[lg], bias=lg.ap[:, 40:41])
            self.TT("dve", lg.ap[:, 96:128], lg.ap[:, 96:128], lg.ap[:, 64:96], ALU.mult, r=[lg], w=[lg])
            self.S.op("dve", lambda e, o=lg.ap[:, 41:42], a=lg.ap[:, 96:128]: e.reduce_sum(o, a, axis=AX.X), [lg], [lg])
            self.S.op("dve", lambda e, o=lg.ap[:, 42:43], a=lg.ap[:, 41:42]: e.reciprocal(o, a), [lg], [lg])
            self.TS("dve", comb.ap[:, xi, :], lg.ap[:, 96:128], lg.ap[:, 42:43], None, ALU.mult, r=[lg], w=[comb])
            self.CP("pool", maskb16.ap[:, :], lg.ap[:, 64:96], r=[lg], w=[maskb16])
            self.MM(pr.ap[:, 64:96], tsub.ap[:, :], maskb16.ap[:, :], r=[tsub, maskb16], w=[pr])
            self.MM(pr.ap[:, 128:160], self.onesb, maskb16.ap[:, :], r=[self.cstb, maskb16], w=[pr])
            self.TT("dve", rank_all.ap[:, xi, :], pr.ap[:, 64:96], cb, ALU.add, r=[pr, rt_], w=[rank_all])
            self.TT("dve", cb, cb, pr.ap[:, 128:160], ALU.add, r=[pr, rt_], w=[rt_])
            self.CP("dve", lgall.ap[:, xi, :], lg.ap[:, 0:40], r=[lg], w=[lgall])
            self.CP("pool", xn2tm.ap[:, xi, :], t_.ap[:, :], r=[t_], w=[xn2tm])
        meta = S.alloc("meta", [32 + 32 + 128 + 512 + 512], F32)
        self.meta = meta
        nt = meta.ap[:, 0:32]
        nt_i = meta.ap[:, 32:64].bitcast(I32)
        ntT = meta.ap[:, 64:192]
        sidxf = meta.ap[:, 192:704].rearrange("p (a b) -> p a b", b=16)
        sidx_i = meta.ap[:, 704:1216].bitcast(I32).rearrange("p (a b) -> p a b", b=16)
        self.nt_i, self.sidx_i = nt_i, sidx_i
        self.MS("pool", nt, 0.0, w=[meta])
        for m in range(NSTEP):
            self.STT(nt, cb, float(SG * m), nt, ALU.is_gt, ALU.add, r=[rt_, meta], w=[meta])
        self.CP("dve", nt_i, nt, r=[meta], w=[meta])
        pq = P[4]
        self.TR(pq.ap[0:NE, 0:128], nt, identf, r=[meta, cst], w=[pq])
        self.CP("act", ntT[0:NE, :], pq.ap[0:NE, 0:128], r=[pq], w=[meta])
        self.MM(pq.ap[:, 128:160], ntT[0:NE, :], Umat[0:NE, :], r=[meta, rt_], w=[pq])
        base = rt_.ap[:, 176:208]
        self.TS("dve", base, pq.ap[:, 128:160], float(SG), None, ALU.mult, r=[pq, rt_], w=[rt_])
        for e in range(NE):
            self.TS("dve", sidxf[:, e, :], IO, base[:, e:e + 1], None, ALU.add, r=[rt_, meta], w=[meta])
        self.CP("dve", sidx_i, sidxf, r=[meta], w=[meta])
        S.release(mT, wo, rw, hx2f, lg, junk, self.zt, tsub, maskb16, *tt)
        oh4 = S.alloc("oh4", [16, 4, NE], F32)
        pr4 = S.alloc("pr4", [16, 4, NE], F32)
        pf4 = S.alloc("pf4", [16, 4], F32)
        shp = [128, 16, 4, NE]
        self.TT("dve", rank_all.ap[:, :, :], rank_all.ap[:, :, :], base.unsqueeze(1).to_broadcast([128, 16, NE]), ALU.add,
                r=[rank_all, rt_], w=[rank_all])
        self.TT("dve", oh4.ap[:, :, :, :], lgall.ap[:, :, 0:NE].unsqueeze(2).to_broadcast(shp),
                lgall.ap[:, :, 32:36].unsqueeze(3).to_broadcast(shp), ALU.is_equal, r=[lgall], w=[oh4])
        self.TT("dve", pr4.ap[:, :, :, :], oh4.ap[:, :, :, :], rank_all.ap[:, :, :].unsqueeze(2).to_broadcast(shp), ALU.mult,
                r=[oh4, rank_all], w=[pr4])
        self.S.op("dve", lambda e, o=pf4.ap[:, :, :], a=pr4.ap[:, :, :, :]: e.reduce_sum(o, a, axis=AX.X), [pr4], [pf4])
        self.CP("dve", posi_v[:, :, :], pf4.ap[:, :, :], r=[pf4], w=[posi])
        self.TT("pool", pr4.ap[:, :, :, :], oh4.ap[:, :, :, :], comb.ap[:, :, :].unsqueeze(2).to_broadcast(shp), ALU.mult,
                r=[oh4, comb, pf4], w=[pr4])
        self.S.op("dve", lambda e, o=wk.ap[:, :, :], a=pr4.ap[:, :, :, :]: e.reduce_sum(o, a, axis=AX.X), [pr4], [wk])
        for xi in range(16):
            for k in range(4):
                self.S.dma_fn("pool", lambda e, o=self.xs_d[:, :], off=posi_v[:, xi, k:k + 1], i_=xn2tm.ap[:, xi, :]:
                              e.indirect_dma_start(out=o, out_offset=bass.IndirectOffsetOnAxis(ap=off, axis=0), in_=i_, in_offset=None,
                                                   bounds_check=self.S.bc_reg, oob_is_err=False),
                              reads=[xsz, xn2tm, posi], writes=[xsd], sem_buf=xn2tm)
        S.release(oh4, pr4, pf4, rank_all, lgall)
        self.x1_bufs = xt
        self.rt_ = rt_
        self.xn2tm = xn2tm

    def phase_moe(self):
        S, P = self.S, self.P
        hx2T, comb, stat, cst = self.hx2T, self.comb, self.stat, self.cst
        identf, Gb = self.identf, self.Gb
        xt = self.x1_bufs
        combT = S.alloc("combT", [T], F32)
        for xi in range(16):
            pb = P[xi % 2]
            self.TR(pb.ap[0:NE, 0:128], comb.ap[:, xi, :], identf, r=[comb, cst], w=[pb])
            self.CP("act", combT.ap[0:NE, xi * 128:(xi + 1) * 128], pb.ap[0:NE, 0:128], r=[pb], w=[combT])
        S.release(comb)
        bias = S.alloc("ebias", [512 + D], F32)
        self.DMA("sp", bias.ap[:, 0:256], self.bgT, w=[bias])
        self.DMA("sp", bias.ap[:, 256:512], self.buT, w=[bias])
        self.DMA("sp", bias.ap[0:NE, 512:512 + D], self.b_down, w=[bias])
        self.TS("dve", bias.ap[:, 256:512], bias.ap[:, 256:512], 1.0, None, ALU.add, r=[bias], w=[bias])
        wg = S.alloc("wg", [KC, D], BF16)
        wu = S.alloc("wu", [KC, D], BF16)
        wd = S.alloc("wd", [KC, D], BF16)
        act = S.alloc("act", [KC, 512], BF16)
        oacc = S.alloc("oacc", [KC, 1024], F32)
        sel = [S.alloc("sel", [128], F32) for _ in range(2)]
        cwb = S.alloc("cw", [512], F32)
        gb_ = [S.alloc("g", [512], F32) for _ in range(2)]
        sb_ = [S.alloc("s", [512], F32) for _ in range(1)]
        ub_ = [S.alloc("u", [512], F32) for _ in range(2)]
        junk = S.alloc("junk4", [512], BF16)
        selv = self.c_sel
        out_bufs = []
        for half in range(2):
            t0 = half * 1024
            for dc in range(KC):
                for tb in range(2):
                    pb = P[4 + (dc * 2 + tb) % 2]
                    self.MM(pb.ap[:, :], bias.ap[0:NE, 512 + dc * 128:512 + (dc + 1) * 128], combT.ap[0:NE, t0 + tb * 512:t0 + (tb + 1) * 512],
                            r=[bias, combT], w=[pb])
                    self.CP("act", oacc.ap[:, dc, tb * 512:(tb + 1) * 512], pb.ap[:, :], r=[pb], w=[oacc])
            for e in range(NE):
                self.load_w(wg.ap[:, :, :], wg, self.w_gate[e], D, D)
                self.load_w(wu.ap[:, :, :], wu, self.w_up[e], D, D)
                self.load_w(wd.ap[:, :, :], wd, self.w_down[e], D, D)
                se = sel[e % 2]
                self.DMA("sp", se.ap[0:NE, :], selv[:, e * 128:(e + 1) * 128], w=[se])
                for tb in range(2):
                    tk = t0 + tb * 512
                    self.MM(P[6].ap[:, :], se.ap[0:NE, :], combT.ap[0:NE, tk:tk + 512], r=[se, combT], w=[P[6]])
                    self.CP("act", cwb.ap[:, :], P[6].ap[:, :], r=[P[6]], w=[cwb])
                    for f in range(KC):
                        pg, pu = P[f % 2], P[2 + f % 2]
                        g_, s_, u_ = gb_[f % 2], sb_[0], ub_[f % 2]
                        for kc in range(KC):
                            self.MM(pg.ap[:, :], wg.ap[:, kc, f * 128:(f + 1) * 128], hx2T.ap[:, kc, tk:tk + 512],
                                    start=(kc == 0), stop=(kc == KC - 1), r=[wg, hx2T], w=[pg])
                        for kc in range(KC):
                            self.MM(pu.ap[:, :], wu.ap[:, kc, f * 128:(f + 1) * 128], hx2T.ap[:, kc, tk:tk + 512],
                                    start=(kc == 0), stop=(kc == KC - 1), r=[wu, hx2T], w=[pu])
                        bcol = e * 8 + f
                        self.TS("dve", g_.ap[:, :], pg.ap[:, :], bias.ap[:, bcol:bcol + 1], 7.0, ALU.add, ALU.min, r=[pg, bias], w=[g_])
                        self.ACT(s_.ap[:, :], g_.ap[:, :], AF.Sigmoid, r=[g_], w=[s_], scale=1.702)
                        self.TS("dve", u_.ap[:, :], pu.ap[:, :], bias.ap[:, 256 + bcol:256 + bcol + 1], -6.0, ALU.add, ALU.max,
                                r=[pu, bias], w=[u_])
                        self.STT(u_.ap[:, :], u_.ap[:, :], 8.0, cwb.ap[:, :], ALU.min, ALU.mult, r=[u_, cwb], w=[u_])
                        self.TT("pool", g_.ap[:, :], g_.ap[:, :], s_.ap[:, :], ALU.mult, r=[g_, s_], w=[g_])
                        self.TT("pool", act.ap[:, f, :], g_.ap[:, :], u_.ap[:, :], ALU.mult, r=[g_, u_], w=[act])
                    for dc in range(KC):
                        pd = P[4 + dc % 2]
                        for f in range(KC):
                            self.MM(pd.ap[:, :], wd.ap[:, f, dc * 128:(dc + 1) * 128], act.ap[:, f, :],
                                    start=(f == 0), stop=(f == KC - 1), r=[wd, act], w=[pd])
                        self.TT("dve", oacc.ap[:, dc, tb * 512:(tb + 1) * 512], oacc.ap[:, dc, tb * 512:(tb + 1) * 512], pd.ap[:, :],
                                ALU.add, r=[oacc, pd], w=[oacc])
            for j in range(8):
                xi = half * 8 + j
                x_ = xt[xi % 2]
                self.DMA("sp", x_.ap[:, :], self.x1d[xi * 128:(xi + 1) * 128, :], w=[x_])
                pa, pb = P[(j % 2) * 2], P[(j % 2) * 2 + 1]
                for dc in range(KC):
                    pp = pa if dc < 4 else pb
                    self.TR(pp.ap[:, (dc % 4) * 128:(dc % 4 + 1) * 128], oacc.ap[:, dc, j * 128:(j + 1) * 128], identf,
                            r=[oacc, cst], w=[pp])
                sc = 40 + (j % 2) * 4
                for hf, pp in enumerate((pa, pb)):
                    self.ACT(junk.ap[:, :], pp.ap[:, :], AF.Square, r=[pp], w=[junk, stat], accum_out=stat.ap[:, sc + hf:sc + hf + 1])
                self.TT("dve", stat.ap[:, sc:sc + 1], stat.ap[:, sc:sc + 1], stat.ap[:, sc + 1:sc + 2], ALU.add, r=[stat], w=[stat])
                self.rstd_from_ss(stat.ap[:, sc:sc + 1], D, stat, stat.ap[:, sc + 2:sc + 3])
                for hf, pp in enumerate((pa, pb)):
                    self.STT(cwb.ap[:, :], pp.ap[:, :], stat.ap[:, sc:sc + 1], Gb.ap[:, 1, hf * 512:(hf + 1) * 512], ALU.mult, ALU.mult,
                             r=[pp, stat, Gb], w=[cwb])
                    self.TT("pool", x_.ap[:, hf * 512:(hf + 1) * 512], x_.ap[:, hf * 512:(hf + 1) * 512], cwb.ap[:, :], ALU.add,
                            r=[x_, cwb], w=[x_])
                self.DMA("pool", self.out[xi * 128:(xi + 1) * 128, :], x_.ap[:, :], r=[x_], sem=x_)
        self.final_bufs = list(xt)

    def phase_moe_sparse(self):
        S, P = self.S, self.P
        stat, cst, cstb, Gb = self.stat, self.cst, self.cstb, self.Gb
        identb, onesf, A2, modb = self.identb, self.onesf, self.A2, self.modb
        xsd, ysd, wk, posi_v, posi = self.xsd, self.ysd, self.wk, self.posi_v, self.posi
        meta, nt_i, sidx_i = self.meta, self.nt_i, self.sidx_i
        S.release(self.comb, self.xn2tm)
        bias = S.alloc("ebias", [512], F32)
        self.DMA("sp", bias.ap[:, 0:256], self.bgT, w=[bias])
        self.DMA("sp", bias.ap[:, 256:512], self.buT, w=[bias])
        self.TS("dve", bias.ap[:, 256:512], bias.ap[:, 256:512], 1.0, None, ALU.add, r=[bias], w=[bias])
        W = [[S.alloc("w%d" % k, [KC, D], BF16) for k in range(3)] for _ in range(2)]
        xg0 = [S.alloc("xg0", [2, D], BF16) for _ in range(2)]
        xgn = [S.alloc("xgn", [2, D], BF16) for _ in range(2)]
        xg = xg0 + xgn
        XT = [S.alloc("XT", [KC, SG], BF16) for _ in range(2)]
        acts = [S.alloc("act", [SG], BF16) for _ in range(KC)]
        gb_ = [S.alloc("g", [SG], F32) for _ in range(2)]
        sb_ = [S.alloc("s", [SG], F32) for _ in range(2)]
        ub_ = [S.alloc("u", [SG], F32) for _ in range(2)]
        ys = [S.alloc("ys", [D], F32) for _ in range(2)]
        bdr = [S.alloc("bdr", [D], F32) for _ in range(1)]
        bdrb = [S.alloc("bdrb", [D], BF16) for _ in range(2)]
        srcs = (self.w_gate, self.w_up, self.w_down)

        def prep(x_, xt_):
            for bk in range(2):
                pt = P[4 + bk]
                ptb = pt.ap.bitcast(BF16)
                for k4 in range(4):
                    kc = bk * 4 + k4
                    for h in range(2):
                        self.TR(ptb[:, k4 * SG + h * 128:k4 * SG + (h + 1) * 128], x_.ap[:, h, kc * 128:(kc + 1) * 128], identb,
                                r=[x_, cstb], w=[pt])
                for k4 in range(4):
                    kc = bk * 4 + k4
                    if k4 % 2 == 0:
                        self.TS("dve", xt_.ap[:, kc, :], ptb[:, k4 * SG:(k4 + 1) * SG], A2[:, kc:kc + 1], modb.ap[:, 24 + kc, 0:1],
                                ALU.mult, ALU.add, r=[pt, self.vec, modb], w=[xt_])
                    else:
                        self.ACT(xt_.ap[:, kc, :], ptb[:, k4 * SG:(k4 + 1) * SG], AF.Identity, r=[pt, self.vec, modb], w=[xt_],
                                 scale=A2[:, kc:kc + 1], bias=modb.ap[:, 24 + kc, 0:1])

        def loadw(e):
            for k in range(3):
                self.load_w(W[e % 2][k].ap[:, :, :], W[e % 2][k], srcs[k][e], D, D)
        def gather(buf, e, j):
            for h in range(2):
                self.S.dma_fn("pool", lambda en, o=buf.ap[:, h, :], off=sidx_i[:, e, 2 * j + h:2 * j + h + 1], i_=self.xs_d[:, :]:
                              en.indirect_dma_start(out=o, out_offset=None, in_=i_, in_offset=bass.IndirectOffsetOnAxis(ap=off, axis=0),
                                                    bounds_check=self.S.bc_reg, oob_is_err=False),
                              reads=[xsd, meta], writes=[buf], sem_buf=buf)
        gather(xg0[0], 0, 0)
        loadw(0)
        nst = 0
        ngs = 0
        for e in range(NE):
            gather(xgn[1], e, 1)
            if e + 1 < NE:
                gather(xg0[(e + 1) % 2], e + 1, 0)
                loadw(e + 1)
            wg, wu, wd = W[e % 2]
            br = bdr[0]
            self.DMA("sp", br.ap[0:1, :], self.b_down[e:e + 1, :], w=[br])
            brb = bdrb[e % 2]
            self.CP("act", brb.ap[0:1, :], br.ap[0:1, :], r=[br], w=[brb])
            prep(xg0[e % 2], XT[ngs % 2])
            S.regload(meta, nt_i[0:1, e:e + 1])
            for j in range(NSTEP):
                S.begin_group(j)
                xt_ = XT[ngs % 2]
                ngs += 1
                if j + 2 < NSTEP:
                    gather(xgn[j % 2], e, j + 2)
                for f in range(KC):
                    pg, pu = P[f % 2], P[2 + f % 2]
                    g_, s_, u_ = gb_[f % 2], sb_[f % 2], ub_[f % 2]
                    for kc in range(KC):
                        self.MM(pg.ap[:, 0:SG], wg.ap[:, kc, f * 128:(f + 1) * 128], xt_.ap[:, kc, :],
                                start=(kc == 0), stop=(kc == KC - 1), r=[wg, xt_], w=[pg])
                    for kc in range(KC):
                        self.MM(pu.ap[:, 0:SG], wu.ap[:, kc, f * 128:(f + 1) * 128], xt_.ap[:, kc, :],
                                start=(kc == 0), stop=(kc == KC - 1), r=[wu, xt_], w=[pu])
                    bcol = e * 8 + f
                    self.TS("dve", g_.ap[:, :], pg.ap[:, 0:SG], bias.ap[:, bcol:bcol + 1], 7.0, ALU.add, ALU.min, r=[pg, bias], w=[g_])
                    self.ACT(s_.ap[:, :], g_.ap[:, :], AF.Sigmoid, r=[g_], w=[s_], scale=1.702)
                    self.TS("dve", u_.ap[:, :], pu.ap[:, 0:SG], bias.ap[:, 256 + bcol:256 + bcol + 1], -6.0, ALU.add, ALU.max,
                            r=[pu, bias], w=[u_])
                    self.TT("pool", g_.ap[:, :], g_.ap[:, :], s_.ap[:, :], ALU.mult, r=[g_, s_], w=[g_])
                    self.STT(acts[f].ap[:, :], u_.ap[:, :], 8.0, g_.ap[:, :], ALU.min, ALU.mult, r=[u_, g_], w=[acts[f]])
                if j + 1 < NSTEP:
                    prep(xgn[(j + 1) % 2], XT[ngs % 2])
                for h in range(2):
                    y_ = ys[nst % 2]
                    nst += 1
                    for hf in range(2):
                        pd = P[6 + hf]
                        for f in range(KC):
                            self.MM(pd.ap[:, :], acts[f].ap[:, h * 128:(h + 1) * 128], wd.ap[:, f, hf * 512:(hf + 1) * 512],
                                    start=(f == 0), stop=False, r=[acts[f], wd], w=[pd])
                        self.MM(pd.ap[:, :], self.onesb[0:1, :], brb.ap[0:1, hf * 512:(hf + 1) * 512], start=False, stop=True,
                                r=[cstb, brb], w=[pd])
                        self.CP("act" if hf == 0 else "dve", y_.ap[:, hf * 512:(hf + 1) * 512], pd.ap[:, :], r=[pd], w=[y_])
                    self.S.dma_fn("pool", lambda en, o=self.ys_d[:, :], off=sidx_i[:, e, 2 * j + h:2 * j + h + 1], i_=y_.ap[:, :]:
                                  en.indirect_dma_start(out=o, out_offset=bass.IndirectOffsetOnAxis(ap=off, axis=0), in_=i_, in_offset=None,
                                                        bounds_check=self.S.bc_reg, oob_is_err=False),
                                  reads=[y_, meta], writes=[ysd], sem_buf=y_)
                S.end_group()
        S.release(*W[0], *W[1], *xg, *XT, *acts, *gb_, *sb_, *ub_, *bdr, *bdrb, bias)
        yk = [S.alloc("yk", [D], F32) for _ in range(8)]
        acc = [S.alloc("acc", [D], F32) for _ in range(2)]
        junk = S.alloc("junk5", [D], BF16)
        xt = self.x1_bufs
        outb = []
        for xi in range(16):
            x_ = xt[xi % 2]
            a_ = acc[xi % 2]
            self.DMA("sp", x_.ap[:, :], self.x1d[xi * 128:(xi + 1) * 128, :], w=[x_])
            for k in range(4):
                y_ = yk[(xi % 2) * 4 + k]
                self.S.dma_fn("pool", lambda e, o=y_.ap[:, :], off=posi_v[:, xi, k:k + 1], i_=self.ys_d[:, :]:
                              e.indirect_dma_start(out=o, out_offset=None, in_=i_, in_offset=bass.IndirectOffsetOnAxis(ap=off, axis=0),
                                                   bounds_check=self.S.bc_reg, oob_is_err=False),
                              reads=[ysd, posi], writes=[y_], sem_buf=y_)
                if k == 0:
                    self.TS("dve", a_.ap[:, :], y_.ap[:, :], wk.ap[:, xi, 0:1], None, ALU.mult, r=[y_, wk], w=[a_])
                else:
                    self.STT(a_.ap[:, :], y_.ap[:, :], wk.ap[:, xi, k:k + 1], a_.ap[:, :], ALU.mult, ALU.add, r=[y_, wk, a_], w=[a_])
            sc = 40 + (xi % 2) * 4
            self.ACT(junk.ap[:, :], a_.ap[:, :], AF.Square, r=[a_], w=[junk, stat], accum_out=stat.ap[:, sc:sc + 1])
            self.rstd_from_ss(stat.ap[:, sc:sc + 1], D, stat, stat.ap[:, sc + 2:sc + 3])
            if self.dbg is not None and xi in (0, 9):
                self.dump((0 if xi == 0 else 1) * 1024, a_, a_.ap[:, :], 1024)
                self.dump(2048 + (0 if xi == 0 else 1) * 8, wk, wk.ap[:, xi, :], 4)
                self.dump(2048 + 16 + (0 if xi == 0 else 1) * 8, posi, posi.ap[:, xi, :], 4)
            self.STT(a_.ap[:, :], a_.ap[:, :], stat.ap[:, sc:sc + 1], Gb.ap[:, 1, :], ALU.mult, ALU.mult, r=[a_, stat, Gb], w=[a_])
            self.TT("pool", x_.ap[:, :], x_.ap[:, :], a_.ap[:, :], ALU.add, r=[x_, a_], w=[x_])
            self.DMA("sp", self.out[xi * 128:(xi + 1) * 128, :], x_.ap[:, :], r=[x_], sem=x_)
        self.final_bufs = list(xt)

    def finish(self):
        S = self.S
        if self.stage < 90:
            z = S.alloc("zout", [128], F32)
            self.MS("pool", z.ap[:, :], 0.0, w=[z])
            self.DMA("pool", self.out[0:128, 0:128], z.ap[:, :], r=[z], sem=z)
            self.dbg_bufs.append(z)
        S.emit(final_bufs=self.dbg_bufs + getattr(self, "final_bufs", []))


def _host_inputs(inputs, b):
    f = lambda a: np.ascontiguousarray(np.asarray(a, dtype=np.float32))
    cst = _consts()
    m = {}
    m["x"] = f(inputs["x"][b])
    m["ctx"] = f(inputs["ctx"][b])
    cc = np.stack([_kc_layout(f(inputs["c"][b])), _kc_layout(f(inputs["c_ctx"]))], axis=-1)
    m["cc"] = f(cc.reshape(128, 16))
    m["w_mod"] = f(inputs["w_mod"][0])
    m["bmodT"] = _kc_layout(f(inputs["b_mod"][0]))
    m["bmod_row"] = f(inputs["b_mod"][0]).reshape(1, -1)
    gT = np.stack([_kc_layout(f(inputs[k][0])) for k in ("g_pre_mix", "g_post_mix", "g_pre_ffn", "g_post_ffn")], axis=1)
    m["gT"] = f(gT.reshape(128, 32))
    m["gpost_row"] = f(np.concatenate([f(inputs["g_post_mix"][0]), f(inputs["g_post_ffn"][0])]).reshape(1, -1))
    m["w_in"] = f(inputs["w_in"][0])
    wa2 = np.zeros((32, 512), np.float32)
    wa2[0:16, 0:256] = inputs["gla_w_a2_f"][0]
    wa2[16:32, 256:512] = inputs["gla_w_a2_b"][0]
    m["wa2"] = wa2
    m["ba_row"] = f(np.concatenate([f(inputs["gla_b_a_f"][0]), f(inputs["gla_b_a_b"][0])]).reshape(1, 512))
    m["gnb"] = f(np.tile(f(inputs["gla_g_norm"][0])[None, :], (128, 4)))
    m["gq"] = _kc_layout(f(inputs["mla_g_q"][0]))
    m["gkv"] = _kc_layout(f(inputs["mla_g_kv"][0]))
    m["w_uq"] = f(inputs["mla_w_uq"][0])
    m["w_uk"] = f(inputs["mla_w_uk"][0])
    m["w_uv"] = f(inputs["mla_w_uv"][0])
    m["w_br_gla"] = f(inputs["w_br_gla"][0])
    m["w_br_mla"] = f(inputs["w_br_mla"][0])
    m["w_out"] = f(inputs["w_out"][0])
    m["router_w"] = f(inputs["router_w"][0])
    m["router_b"] = f(inputs["router_b"][0]).reshape(1, NE)
    m["w_gate"] = f(inputs["w_gate"][0])
    m["w_up"] = f(inputs["w_up"][0])
    m["w_down"] = f(inputs["w_down"][0])
    bg = f(inputs["b_gate"][0]).reshape(NE, 8, 128).transpose(2, 0, 1)
    bu = f(inputs["b_up"][0]).reshape(NE, 8, 128).transpose(2, 0, 1)
    m["bgT"] = f(bg.reshape(128, NE * 8))
    m["buT"] = f(bu.reshape(128, NE * 8))
    m["b_down"] = f(inputs["b_down"][0])
    m["c_tri"] = f(cst["tri"].reshape(128, 512))
    m["c_mask"] = f(cst["mask"].reshape(128, 1024))
    m["c_ident"] = cst["ident"]
    m["c_ropet"] = f(cst["ropet"].reshape(128, 2 * NTOK))
    m["c_e96"] = cst["e96"]
    m["c_sel"] = cst["sel"]
    m["c_rt"] = cst["rt"]
    return m


_SHARED = ("w_mod", "bmodT", "bmod_row", "gT", "gpost_row", "w_in", "wa2", "ba_row", "gnb", "gq", "gkv", "w_uq", "w_uk",
           "w_uv", "w_br_gla", "w_br_mla", "w_out", "router_w", "router_b", "w_gate", "w_up", "w_down", "bgT", "buT",
           "b_down", "c_tri", "c_mask", "c_ident", "c_ropet", "c_e96", "c_sel", "c_rt")


def kernel(**inputs):
    k = K(stage=99)
    m0 = _host_inputs(inputs, 0)
    in_maps = [m0]
    for b in range(1, 8):
        mb = dict(m0)
        mb["x"] = np.ascontiguousarray(np.asarray(inputs["x"][b], dtype=np.float32))
        mb["ctx"] = np.ascontiguousarray(np.asarray(inputs["ctx"][b], dtype=np.float32))
        cc = np.stack([_kc_layout(np.asarray(inputs["c"][b], np.float32)),
                       _kc_layout(np.asarray(inputs["c_ctx"], np.float32))], axis=-1)
        mb["cc"] = np.ascontiguousarray(cc.reshape(128, 16))
        in_maps.append(mb)
    res = run_bass_kernel_spmd(k.nc, in_maps, core_ids=list(range(8)))
    return np.stack([np.asarray(r["out"], dtype=np.float32) for r in res.results], axis=0)
```

```python
import numpy as np
from contextlib import ExitStack
import concourse.bass as bass
import concourse.mybir as mybir
from concourse.bass_utils import run_bass_kernel_spmd

F32 = mybir.dt.float32
BF16 = mybir.dt.bfloat16
I32 = mybir.dt.int32
AF = mybir.ActivationFunctionType
ALU = mybir.AluOpType
AX = mybir.AxisListType
ENGS = ("pe", "act", "dve", "pool", "sp")

T = 2048
TC = 256
NT = 18
NTOK = 2304
D = 1024
KC = 8
D_IN = 4032
EPS = 1e-6
NE = 32
MLA_SCALE = 96 ** -0.5
SG = 256
NSTEP = 8
NSLOT = T * 4 + NE * SG
MOE_MODE = "sparse"


class Buf:
    __slots__ = ("name", "writers", "readers", "dma_sem", "dma_cnt", "ap", "inherit", "off", "words")

    def __init__(self, name, ap=None):
        self.name = name
        self.ap = ap
        self.writers = []
        self.readers = []
        self.inherit = []
        self.off = None
        self.dma_sem = None
        self.dma_cnt = 0


class Op:
    __slots__ = ("eng", "fn", "idx", "deps", "marked", "dma_buf", "dma_val", "mark_no", "grp")

    def __init__(self, eng, fn):
        self.eng = eng
        self.fn = fn
        self.grp = None
        self.deps = []
        self.marked = False
        self.dma_buf = None
        self.dma_val = 0
        self.mark_no = 0


class Sched:
    def __init__(self, nc, arena_words):
        self.nc = nc
        self.ops = {e: [] for e in ENGS}
        self.bufs = []
        self.arena = nc.alloc_sbuf_tensor("arena", [128, arena_words], F32)
        self.free = [(0, arena_words)]
        self.dead = []
        self.nbuf = 0
        self.cur_grp = None
        self.ngrp = 0

    def alloc(self, name, free_shape, dt=F32):
        n = 1
        for s in free_shape:
            n *= s
        words = (n * (2 if dt == BF16 else 4) + 3) // 4
        words = (words + 7) // 8 * 8
        off = None
        for i, (o, sz) in enumerate(self.free):
            if sz >= words:
                off = o
                if sz == words:
                    self.free.pop(i)
                else:
                    self.free[i] = (o + words, sz - words)
                break
        if off is None:
            raise RuntimeError("arena full allocating %s (%d words); free=%s" % (name, words, self.free))
        v = self.arena[:, off:off + words]
        if dt == BF16:
            v = v.bitcast(BF16)
        v = v[:, 0:n]
        if len(free_shape) == 2:
            v = v.rearrange("p (a b) -> p a b", b=free_shape[1])
        elif len(free_shape) == 3:
            v = v.rearrange("p (a b c) -> p a b c", b=free_shape[1], c=free_shape[2])
        self.nbuf += 1
        b = Buf("%s_%d" % (name, self.nbuf), v)
        b.off, b.words = off, words
        for (o, w, toks) in self.dead:
            if o < off + words and off < o + w:
                b.inherit.extend(toks)
        self.bufs.append(b)
        return b

    def release(self, *bufs):
        for b in bufs:
            toks = [("op", t[1], "raw") if t[0] == "op" else t for t in (b.writers + b.readers + b.inherit)]
            self.dead.append((b.off, b.words, toks))
            self.free.append((b.off, b.words))
        self.free.sort()
        m = []
        for o, w in self.free:
            if m and m[-1][0] + m[-1][1] == o:
                m[-1] = (m[-1][0], m[-1][1] + w)
            else:
                m.append((o, w))
        self.free = m

    def psum(self, name):
        t = self.nc.alloc_psum_tensor(name, [128, 512], F32)
        b = Buf(name, t[:, :])
        self.bufs.append(b)
        return b

    def virt(self, name):
        b = Buf(name, None)
        self.bufs.append(b)
        return b

    def _add(self, eng, fn, reads, writes, dma_buf=None):
        op = Op(eng, fn)
        op.idx = len(self.ops[eng])
        op.grp = self.cur_grp
        is_dma = dma_buf is not None
        deps = []
        for b in reads:
            deps.extend(b.writers)
            deps.extend(b.inherit)
            if b.off is None:
                deps.extend([("op", t[1], "raw") for t in b.readers if t[0] == "op" and t[1].eng != eng])
        for b in writes:
            deps.extend(b.inherit)
            deps.extend(b.readers)
            if is_dma and not b.readers and b.writers and all(w[0] == "dma" for w in b.writers):
                pass
            else:
                deps.extend(b.writers)
        fl = []
        for d in deps:
            if d[0] == "op":
                o = d[1]
                if o.eng == eng and not is_dma:
                    if eng == "pe" or eng == "sp":
                        continue
                    if d[2] != "raw":
                        continue
            fl.append(d)
        op.deps = fl
        if is_dma:
            dma_buf.dma_cnt += 16
            op.dma_buf = dma_buf
            op.dma_val = dma_buf.dma_cnt
            tok_w = ("dma", dma_buf, op.dma_val)
            tok_r = tok_w
        else:
            tok_w = ("op", op, "raw")
            tok_r = ("op", op, "war")
        for b in reads:
            b.readers.append(tok_r)
            if len(b.readers) > 64:
                last = {}
                keep = []
                for t in b.readers:
                    if t[0] == "op":
                        last[t[1].eng] = t
                    else:
                        keep.append(t)
                b.readers = keep[-32:] + list(last.values())
        for b in writes:
            if is_dma and not b.readers and b.writers and all(w[0] == "dma" for w in b.writers):
                b.writers.append(tok_w)
            else:
                b.writers = [tok_w]
                b.readers = []
        self.ops[eng].append(op)
        return op

    def op(self, eng, fn, reads=(), writes=()):
        return self._add(eng, fn, list(reads), list(writes))

    def dma(self, eng, out_ap, in_ap, reads=(), writes=(), sem_buf=None, **kw):
        if sem_buf is None:
            sem_buf = (list(writes) + list(reads))[0]

        def fn(e, out_ap=out_ap, in_ap=in_ap, kw=kw):
            return e.dma_start(out=out_ap, in_=in_ap, **kw)
        return self._add(eng, fn, list(reads), list(writes), dma_buf=sem_buf)

    def begin_group(self, thr):
        self.ngrp += 1
        self.cur_grp = (self.ngrp, thr)

    def end_group(self):
        self.cur_grp = None

    def regload(self, src_buf, ap):
        for e in ENGS:
            self._add(e, ("regload", ap), [src_buf], [])

    def dma_fn(self, eng, fn, reads=(), writes=(), sem_buf=None):
        return self._add(eng, fn, list(reads), list(writes), dma_buf=sem_buf)

    def emit(self, final_bufs=()):
        nc = self.nc
        for e in ENGS:
            for op in self.ops[e]:
                for d in op.deps:
                    if d[0] == "op":
                        d[1].marked = True
        for e in ENGS:
            n = 0
            for op in self.ops[e]:
                if op.marked:
                    n += 1
                    op.mark_no = n
        with ExitStack() as st:
            esem = {e: st.enter_context(nc.semaphore("s_" + e)) for e in ENGS}
            for b in self.bufs:
                if b.dma_cnt > 0:
                    b.dma_sem = st.enter_context(nc.semaphore("d_" + b.name))
            self.bc_reg = st.enter_context(nc.gpsimd.register("rbc"))
            engobj = {"pe": nc.tensor, "act": nc.scalar, "dve": nc.vector, "pool": nc.gpsimd, "sp": nc.sync}
            self.cregs = {e: st.enter_context(engobj[e].register("creg_" + e)) for e in ENGS}
            print("semaphores used:", 5 + sum(1 for b in self.bufs if b.dma_cnt > 0))
            block = st.enter_context(nc.Block())
            handles = {"pe": block.tensor, "act": block.scalar, "dve": block.vector,
                       "pool": block.gpsimd, "sp": block.sync}
            stats = {}
            for e in ENGS:
                ops = self.ops[e]

                def body(eng, ops=ops, e=e):
                    waited = {}
                    nw = [0]
                    if e == "pool":
                        eng.reg_mov(self.bc_reg, NSLOT - 1)
                    creg = self.cregs[e]

                    def emit_op(op, wt):
                        need = {}
                        for d in op.deps:
                            if d[0] == "op":
                                key = ("e", d[1].eng)
                                val = d[1].mark_no
                            else:
                                key = ("d", d[1].name)
                                val = d[2]
                            if val > need.get(key, (0, None))[0]:
                                need[key] = (val, d)
                        for key, (val, d) in need.items():
                            if wt.get(key, 0) >= val:
                                continue
                            wt[key] = val
                            sem = esem[d[1].eng] if d[0] == "op" else d[1].dma_sem
                            eng.wait_ge(sem, val)
                            nw[0] += 1
                        if isinstance(op.fn, tuple):
                            eng.reg_load(creg, op.fn[1])
                            return
                        ins = op.fn(eng)
                        if op.dma_buf is not None:
                            ins.then_inc(op.dma_buf.dma_sem, 16)
                        elif op.marked:
                            ins.then_inc(esem[e], 1)
                    def comp_of(gops):
                        comp = []
                        marked = [o for o in gops if o.marked and o.dma_buf is None]
                        if marked:
                            comp.append((esem[e], marked[0].mark_no - 1, len(marked)))
                        dm = {}
                        for o in gops:
                            if o.dma_buf is not None:
                                if o.dma_buf.name not in dm:
                                    dm[o.dma_buf.name] = [o.dma_buf.dma_sem, o.dma_val - 16, 0]
                                dm[o.dma_buf.name][2] += 16
                        comp.extend(tuple(v) for v in dm.values())
                        return comp

                    def chain(runs, k, wt):
                        rest = [o for r_ in runs[k:] for o in r_]
                        thr = runs[k][0].grp[1]
                        with eng.If_lt(creg, thr + 1):
                            for sem, before, delta in comp_of(rest):
                                if before > 0:
                                    eng.wait_ge(sem, before)
                                eng.sem_inc(sem, delta)
                        with eng.Else():
                            for o in runs[k]:
                                emit_op(o, wt)
                            if k + 1 < len(runs):
                                chain(runs, k + 1, wt)
                    i = 0
                    n = len(ops)
                    while i < n:
                        op = ops[i]
                        if op.grp is None:
                            emit_op(op, waited)
                            i += 1
                            continue
                        runs = []
                        j = i
                        while j < n and ops[j].grp is not None and (not runs or ops[j].grp[1] > runs[-1][0].grp[1] or ops[j].grp is runs[-1][0].grp):
                            if runs and ops[j].grp is runs[-1][0].grp:
                                runs[-1].append(ops[j])
                            else:
                                runs.append([ops[j]])
                            j += 1
                        chain(runs, 0, dict(waited))
                        i = j
                    if e == "sp":
                        for b in final_bufs:
                            if b.dma_cnt:
                                eng.wait_ge(b.dma_sem, b.dma_cnt)
                    stats[e] = (len(ops), nw[0])
                handles[e](body)
            self.stats = stats


def _consts():
    c = {}
    idx = np.arange(128)
    same = (idx[:, None] // 64) == (idx[None, :] // 64)
    s = idx[:, None]
    t = idx[None, :]
    tri = np.zeros((128, 4, 128), np.float32)
    tri[:, 0] = same & (s <= t)
    tri[:, 1] = same & (s >= t)
    tri[:, 2] = same & (s > t)
    tri[:, 3] = same & (s < t)
    c["tri"] = tri
    mask = np.zeros((128, 2, 4, 128), np.float32)
    mask[:, 0] = (same & (t >= s))[:, None, :]
    mask[:, 1] = (same & (t <= s))[:, None, :]
    c["mask"] = mask.reshape(128, 2, 512)
    c["ident"] = np.eye(128, dtype=np.float32)
    half = 16
    inv_freq = (10000.0 ** (-np.arange(0, half, 2, dtype=np.float32) / half)).astype(np.float32)
    tok = np.arange(T)
    ang_row = (tok // 64).astype(np.float32)[:, None] * inv_freq
    ang_col = (tok % 64).astype(np.float32)[:, None] * inv_freq
    cosr, sinr = np.cos(ang_row), np.sin(ang_row)
    cosc, sinc = np.cos(ang_col), np.sin(ang_col)
    cos32 = np.concatenate([cosr, cosr, cosc, cosc], axis=1)
    sin32 = np.concatenate([-sinr, sinr, -sinc, sinc], axis=1)
    ropet = np.zeros((128, 2, NTOK), np.float32)
    ropet[64:96, 0, :TC] = 1.0
    ropet[64:96, 0, TC:] = cos32.T
    ropet[64:96, 1, TC:] = sin32.T
    c["ropet"] = ropet
    e96 = np.zeros((128, 128), np.float32)
    e96[:96, 96] = 1.0
    c["e96"] = e96
    sel = np.zeros((NE, NE, 128), np.float32)
    for e in range(NE):
        sel[e, e, :] = 1.0
    c["sel"] = sel.reshape(NE, NE * 128)
    rt = np.zeros((128, 176), np.float32)
    rt[:, 0:128] = (s < t)
    ke = np.arange(NE)
    rt[0:NE, 128:160] = (ke[:, None] < ke[None, :])
    rt[:, 160:176] = np.arange(16, dtype=np.float32)[None, :] * 128 + idx[:, None]
    c["rt"] = rt
    return c


def _kc_layout(v):
    return np.ascontiguousarray(v.reshape(-1, 128).T)


class K:
    def __init__(self, stage=99, dbg=False):
        self.stage = stage
        nc = bass.Bass("TRN2", target_bir_lowering=False)
        self.nc = nc
        self.S = Sched(nc, 51200 + 1000)
        S = self.S
        dt = nc.dram_tensor
        self.din = {}

        def inp(name, shape):
            self.din[name] = dt(name, list(shape), F32, kind="ExternalInput").ap()
            return self.din[name]
        self.x = inp("x", [T, D])
        self.ctx = inp("ctx", [TC, D])
        self.cc = inp("cc", [128, 16])
        self.w_mod = inp("w_mod", [D, 6 * D])
        self.bmodT = inp("bmodT", [128, 48])
        self.bmod_row = inp("bmod_row", [1, 6 * D])
        self.gT = inp("gT", [128, 32])
        self.gpost_row = inp("gpost_row", [1, 2 * D])
        self.w_in = inp("w_in", [D, D_IN])
        self.wa2 = inp("wa2", [32, 512])
        self.ba_row = inp("ba_row", [1, 512])
        self.gnb = inp("gnb", [128, 512])
        self.gq = inp("gq", [128, 2])
        self.gkv = inp("gkv", [128, 1])
        self.w_uq = inp("w_uq", [256, 768])
        self.w_uk = inp("w_uk", [128, 512])
        self.w_uv = inp("w_uv", [128, 512])
        self.w_br_gla = inp("w_br_gla", [512, D])
        self.w_br_mla = inp("w_br_mla", [512, D])
        self.w_out = inp("w_out", [D, D])
        self.router_w = inp("router_w", [D, NE])
        self.router_b = inp("router_b", [1, NE])
        self.w_gate = inp("w_gate", [NE, D, D])
        self.w_up = inp("w_up", [NE, D, D])
        self.w_down = inp("w_down", [NE, D, D])
        self.bgT = inp("bgT", [128, NE * 8])
        self.buT = inp("buT", [128, NE * 8])
        self.b_down = inp("b_down", [NE, D])
        self.c_tri = inp("c_tri", [128, 512])
        self.c_mask = inp("c_mask", [128, 1024])
        self.c_ident = inp("c_ident", [128, 128])
        self.c_ropet = inp("c_ropet", [128, 2 * NTOK])
        self.c_e96 = inp("c_e96", [128, 128])
        self.c_sel = inp("c_sel", [NE, NE * 128])
        self.c_rt = inp("c_rt", [128, 176])
        self.xs_d = dt("xs_d", [NSLOT, D], BF16, kind="Internal").ap()
        self.ys_d = dt("ys_d", [NSLOT, D], F32, kind="Internal").ap()
        self.out = dt("out", [T, D], F32, kind="ExternalOutput").ap()
        self.x1d = dt("x1d", [T, D], F32, kind="Internal").ap()
        self.dbg = dt("dbg", [128, 8192], F32, kind="ExternalOutput").ap() if dbg else None
        self.P = [S.psum("ps%d" % i) for i in range(8)]
        self.wq_i = 0
        self.build()

    def MM(self, out, lhsT, rhs, start=True, stop=True, r=(), w=()):
        self.S.op("pe", lambda e: e.matmul(out, lhsT, rhs, start=start, stop=stop, skip_group_check=True), r, w)

    def TR(self, out, in_, ident, r=(), w=()):
        self.S.op("pe", lambda e: e.transpose(out, in_, ident), r, w)

    def ACT(self, out, in_, func, r=(), w=(), **kw):
        self.S.op("act", lambda e: e.activation(out, in_, func, **kw), r, w)

    def TT(self, eng, out, a, b, op, r=(), w=()):
        self.S.op(eng, lambda e: e.tensor_tensor(out, a, b, op=op), r, w)

    def TS(self, eng, out, a, s1, s2, op0, op1=None, r=(), w=()):
        if op1 is None:
            self.S.op(eng, lambda e: e.tensor_scalar(out, a, s1, None, op0=op0), r, w)
        else:
            self.S.op(eng, lambda e: e.tensor_scalar(out, a, s1, s2, op0=op0, op1=op1), r, w)

    def STT(self, out, a, s, b, op0, op1, r=(), w=()):
        self.S.op("dve", lambda e: e.scalar_tensor_tensor(out, a, s, b, op0=op0, op1=op1), r, w)

    def CP(self, eng, out, in_, r=(), w=()):
        if eng == "act":
            self.S.op("act", lambda e: e.copy(out, in_), r, w)
        else:
            self.S.op(eng, lambda e: e.tensor_copy(out, in_), r, w)

    def MS(self, eng, ap, val, w=()):
        self.S.op(eng, lambda e: e.memset(ap, val), (), w)

    def DMA(self, q, out, in_, r=(), w=(), sem=None, **kw):
        self.S.dma(q, out, in_, reads=r, writes=w, sem_buf=sem, **kw)

    def rstd_from_ss(self, ss_ap, n, buf, tmp_ap):
        self.ACT(tmp_ap, ss_ap, AF.Ln, r=[buf], w=[buf], scale=1.0 / n, bias=self.eps_ap)
        self.ACT(ss_ap, tmp_ap, AF.Exp, r=[buf], w=[buf], scale=-0.5)

    def load_w(self, dst_ap, dst_buf, src_ap, rows, cols, q="pool"):
        if rows > 128:
            self.DMA("pool", dst_ap, src_ap.rearrange("(k p) n -> p k n", p=128), w=[dst_buf])
        else:
            self.DMA("pool", dst_ap[0:rows, 0:cols], src_ap, w=[dst_buf])

    def dump(self, col, buf, ap, ncols, parts=128):
        if self.dbg is None:
            return
        S = self.S
        tmp = S.alloc("dbgtmp", [ncols], F32)
        self.CP("dve", tmp.ap[0:parts, :], ap, r=[buf], w=[tmp])
        self.DMA("pool", self.dbg[0:parts, col:col + ncols], tmp.ap[0:parts, :], r=[tmp], sem=tmp)
        self.dbg_bufs.append(tmp)

    def build(self):
        S = self.S
        P = self.P
        self.dbg_bufs = []
        cst = S.alloc("cst", [2048], F32)
        tri = cst.ap[:, 0:512].rearrange("p (a b) -> p a b", b=128)
        identf = cst.ap[:, 512:640]
        e96f = cst.ap[:, 640:768]
        self.eps_ap = cst.ap[:, 768:769]
        onesf = cst.ap[:, 896:1024]
        self.DMA("sp", cst.ap[:, 0:512], self.c_tri, w=[cst])
        self.DMA("sp", identf, self.c_ident, w=[cst])
        self.DMA("sp", e96f, self.c_e96, w=[cst])
        self.MS("pool", cst.ap[:, 768:769], EPS, w=[cst])
        self.MS("pool", onesf, 1.0, w=[cst])
        self.rowmask = cst.ap[:, 1024:1026]
        self.MS("pool", cst.ap[:, 1024:1026], 0.0, w=[cst])
        self.MS("pool", cst.ap[0:64, 1024:1025], 1.0, w=[cst])
        self.MS("pool", cst.ap[64:128, 1025:1026], 1.0, w=[cst])
        cstb = S.alloc("cstb", [2048], BF16)
        identb = cstb.ap[:, 0:128]
        onesb = cstb.ap[:, 128:256]
        e96b = cstb.ap[:, 256:384]
        maskb = cstb.ap[:, 1024:2048].rearrange("p (a b) -> p a b", b=512)
        self.CP("pool", identb, identf, r=[cst], w=[cstb])
        self.CP("pool", onesb, onesf, r=[cst], w=[cstb])
        self.CP("pool", e96b, e96f, r=[cst], w=[cstb])
        self.wstage = [S.alloc("wst", [8, 256], F32) for _ in range(3)]
        mstage = self.wstage[0]
        self.DMA("sp", mstage.ap[:, 0:4, :], self.c_mask.rearrange("p (a b) -> p a b", b=256), w=[mstage])
        self.CP("pool", cstb.ap[:, 1024:2048].rearrange("p (a b) -> p a b", b=256), mstage.ap[:, 0:4, :], r=[mstage], w=[cstb])
        stat = S.alloc("stat", [64], F32)

        modb = S.alloc("mod", [48, 2], F32)
        vec = S.alloc("vec", [16 + 48 + 32 + 32 + 16 + 16 + 8 + 8], F32)
        cc = vec.ap[:, 0:16].rearrange("p (a b) -> p a b", b=2)
        bmodT = vec.ap[:, 16:64]
        gT = vec.ap[:, 64:96].rearrange("p (a b) -> p a b", b=8)
        A1 = vec.ap[:, 96:112].rearrange("p (a b) -> p a b", b=2)
        B1v = None
        A2 = vec.ap[:, 112:120]
        gqv = vec.ap[:, 128:130]
        gkvv = vec.ap[:, 130:131]
        self.DMA("sp", vec.ap[:, 0:16], self.cc, w=[vec])
        self.DMA("sp", bmodT, self.bmodT, w=[vec])
        self.DMA("sp", vec.ap[:, 64:96], self.gT, w=[vec])
        self.DMA("sp", gqv, self.gq, w=[vec])
        self.DMA("sp", gkvv, self.gkv, w=[vec])
        self.ACT(cc, cc, AF.Silu, r=[vec], w=[vec])
        rows = S.alloc("rows", [4096], F32)
        self.DMA("sp", rows.ap[0:1, 2048:4096], self.gpost_row, w=[rows])
        browst = S.alloc("browst", [2048], F32)
        self.DMA("sp", browst.ap[0:1, 0:1024], self.bmod_row[:, 2048:3072], w=[browst])
        self.DMA("sp", browst.ap[0:1, 1024:2048], self.bmod_row[:, 5120:6144], w=[browst])
        wm_view = self.w_mod.rearrange("(k p) n -> p k n", p=128)
        ccb = S.alloc("ccb", [8, 2], BF16)
        self.CP("dve", ccb.ap[:, :, :], cc, r=[vec], w=[ccb])
        wmb = [S.alloc("wmb", [KC, 512], BF16) for _ in range(3)]
        for j in range(12):
            st = wmb[j % 3]
            self.DMA("pool", st.ap[:, :, :], wm_view[:, :, j * 512:(j + 1) * 512], w=[st])
            for m in range(4):
                col = (j * 4 + m) * 2
                for kc in range(KC):
                    self.MM(P[0].ap[:, col:col + 2], st.ap[:, kc, m * 128:(m + 1) * 128], ccb.ap[:, kc, :],
                            start=(kc == 0), stop=(kc == KC - 1), r=[st, ccb], w=[P[0]])
            if j in (4, 5, 10, 11):
                ro = {4: 0, 5: 512, 10: 1024, 11: 1536}[j]
                for kc in range(KC):
                    self.MM(P[1].ap[0:1, :], ccb.ap[:, kc, 0:1], st.ap[:, kc, :], start=(kc == 0), stop=(kc == KC - 1),
                            r=[st, ccb], w=[P[1]])
                self.TT("dve", rows.ap[0:1, ro:ro + 512], P[1].ap[0:1, :], browst.ap[0:1, ro:ro + 512], ALU.add,
                        r=[P[1], browst], w=[rows])
        S.release(ccb, *wmb)
        S.release(browst)
        self.TT("dve", modb.ap[:, :, :], P[0].ap[:, 0:96].rearrange("p (a b) -> p a b", b=2),
                bmodT.unsqueeze(2).to_broadcast([128, 48, 2]), ALU.add, r=[P[0], vec], w=[modb])
        self.STT(A1, modb.ap[:, 8:16, :], 1.0, gT[:, 0, :].unsqueeze(2).to_broadcast([128, 8, 2]), ALU.add, ALU.mult,
                 r=[modb, vec], w=[vec])
        self.STT(A2, modb.ap[:, 32:40, 0], 1.0, gT[:, 2, :], ALU.add, ALU.mult, r=[modb, vec], w=[vec])
        self.TT("dve", rows.ap[0:1, 0:2048], rows.ap[0:1, 0:2048], rows.ap[0:1, 2048:4096], ALU.mult, r=[rows], w=[rows])
        Gb = S.alloc("Gb", [2, 1024], F32)
        for g in range(2):
            for hh in range(2):
                pb = P[2 + hh]
                self.MM(pb.ap[:, :], onesf[0:1, :], rows.ap[0:1, g * 1024 + hh * 512: g * 1024 + hh * 512 + 512],
                        r=[cst, rows], w=[pb])
                self.CP("act", Gb.ap[:, g, hh * 512:(hh + 1) * 512], pb.ap[:, :], r=[pb], w=[Gb])
        S.release(rows, *self.wstage)
        if self.stage == 0:
            self.dump(0, modb, modb.ap[:, :, :].rearrange("p a b -> p (a b)"), 96)
            self.dump(96, vec, vec.ap[:, 96:120], 24)
            self.dump(128, Gb, Gb.ap[:, :, :].rearrange("p a b -> p (a b)"), 2048)
            return self.finish()

        hT = S.alloc("hT", [KC, NTOK], BF16)
        xts = [S.alloc("xt", [D], F32) for _ in range(3)]
        xns = [S.alloc("xn", [D], BF16) for _ in range(2)]
        junk = S.alloc("junk", [D], BF16)
        for i in range(NT):
            xt = xts[i % 3]
            xn = xns[i % 2]
            src = self.ctx[i * 128:(i + 1) * 128, :] if i < 2 else self.x[(i - 2) * 128:(i - 1) * 128, :]
            v = 1 if i < 2 else 0
            self.DMA("sp", xt.ap[:, :], src, w=[xt])
            ss = stat.ap[:, (i % 4) * 2:(i % 4) * 2 + 1]
            tmp = stat.ap[:, (i % 4) * 2 + 1:(i % 4) * 2 + 2]
            self.ACT(junk.ap[:, :], xt.ap[:, :], AF.Square, r=[xt], w=[junk, stat], accum_out=ss)
            self.rstd_from_ss(ss, D, stat, tmp)
            self.TS("dve", xn.ap[:, :], xt.ap[:, :], ss, None, ALU.mult, r=[xt, stat], w=[xn])
            pt = P[i % 2]
            ptb = pt.ap.bitcast(BF16)
            for kc in range(KC):
                self.TR(ptb[:, kc * 128:(kc + 1) * 128], xn.ap[:, kc * 128:(kc + 1) * 128], identb, r=[xn, cstb], w=[pt])
            for kc in range(KC):
                o = hT.ap[:, kc, i * 128:(i + 1) * 128]
                if kc % 2 == 0:
                    self.TS("dve", o, ptb[:, kc * 128:(kc + 1) * 128], A1[:, kc, v:v + 1], modb.ap[:, kc, v:v + 1],
                            ALU.mult, ALU.add, r=[pt, vec, modb], w=[hT])
                else:
                    self.ACT(o, ptb[:, kc * 128:(kc + 1) * 128], AF.Identity, r=[pt, vec, modb], w=[hT],
                             scale=A1[:, kc, v:v + 1], bias=modb.ap[:, kc, v:v + 1])
        S.release(*xts, *xns, junk)
        if self.stage == 1:
            for kc in range(2):
                self.dump(kc * 2304, hT, hT.ap[:, kc, :], 2304)
            return self.finish()
        self.cst, self.cstb, self.stat, self.vec, self.modb, self.Gb, self.hT, self.junk = cst, cstb, stat, vec, modb, Gb, hT, junk
        self.tri, self.identf, self.identb, self.onesf, self.onesb, self.e96b, self.maskb = tri, identf, identb, onesf, onesb, e96b, maskb
        self.A2, self.gqv, self.gkvv = A2, gqv, gkvv
        self.phase_gla()
        if self.stage == 25:
            return self.finish()
        if self.stage == 3:
            for hc in range(4):
                self.dump(hc * 2048, self.ogT, self.ogT.ap[:, hc, :], 2048)
            return self.finish()
        self.phase_mla()
        if self.stage == 4:
            for h in range(4):
                self.dump(h * 2048, self.omlaT, self.omlaT.ap[0:64, h, :], 2048, parts=64)
            return self.finish()
        self.phase_merge()
        if self.stage == 5:
            return self.finish()
        if MOE_MODE == "dense":
            self.phase_moe()
        else:
            self.phase_moe_sparse()
        self.finish()

    def phase_mla(self):
        S, P = self.S, self.P
        hT, cstb, cst, stat = self.hT, self.cstb, self.cst, self.stat
        identb, onesf, onesb, identf = self.identb, self.onesf, self.onesb, self.identf
        wB = S.alloc("wB", [KC, 416], BF16)
        self.load_w(wB.ap[:, :, :], wB, self.w_in[:, 1568:1984], 1024, 416)
        wkrs = S.alloc("wkrs", [KC, 32], BF16)
        for a, b_ in ((0, 8), (8, 0), (16, 24), (24, 16)):
            self.CP("pool", wkrs.ap[:, :, a:a + 8], wB.ap[:, :, 384 + b_:384 + b_ + 8], r=[wB], w=[wkrs])
        wuq = S.alloc("wuq", [2, 768], BF16)
        self.load_w(wuq.ap[:, :, :], wuq, self.w_uq, 256, 768)
        wuqs = S.alloc("wuqs", [2, 8, 32], BF16)
        for h in range(8):
            for a, b_ in ((0, 8), (8, 0), (16, 24), (24, 16)):
                self.CP("pool", wuqs.ap[:, :, h, a:a + 8], wuq.ap[:, :, h * 96 + 64 + b_:h * 96 + 64 + b_ + 8], r=[wuq], w=[wuqs])
        wukv = S.alloc("wukv", [1024], BF16)
        self.load_w(wukv.ap[:, 0:512], wukv, self.w_uk, 128, 512)
        self.load_w(wukv.ap[:, 512:1024], wukv, self.w_uv, 128, 512)
        zdqnT = S.alloc("zdqnT", [2, T], BF16)
        ckvT = S.alloc("ckvT", [NTOK], BF16)
        krT = S.alloc("krT", [NTOK], BF16)
        zn = [S.alloc("zn", [384], BF16) for _ in range(2)]
        junk = S.alloc("junk2", [384], F32)
        for i in range(NT):
            pa = P[i % 2]
            pt = P[2 + i % 2]
            ptb = pt.ap.bitcast(BF16)
            z = zn[i % 2]
            sc = (i % 4) * 4
            if i >= 2:
                for kc in range(KC):
                    self.MM(pa.ap[:, 0:256], hT.ap[:, kc, i * 128:(i + 1) * 128], wB.ap[:, kc, 0:256],
                            start=(kc == 0), stop=(kc == KC - 1), r=[wB, hT], w=[pa])
                self.ACT(junk.ap[:, 0:256], pa.ap[:, 0:256], AF.Square, r=[pa], w=[junk, stat], accum_out=stat.ap[:, sc:sc + 1])
                self.rstd_from_ss(stat.ap[:, sc:sc + 1], 256, stat, stat.ap[:, sc + 1:sc + 2])
                self.TS("dve", z.ap[:, 0:256], pa.ap[:, 0:256], stat.ap[:, sc:sc + 1], None, ALU.mult, r=[pa, stat], w=[z])
            for kc in range(KC):
                self.MM(pa.ap[:, 256:384], hT.ap[:, kc, i * 128:(i + 1) * 128], wB.ap[:, kc, 256:384],
                        start=(kc == 0), stop=(kc == KC - 1), r=[wB, hT], w=[pa])
            self.ACT(junk.ap[:, 256:384], pa.ap[:, 256:384], AF.Square, r=[pa], w=[junk, stat], accum_out=stat.ap[:, sc + 2:sc + 3])
            self.rstd_from_ss(stat.ap[:, sc + 2:sc + 3], 128, stat, stat.ap[:, sc + 3:sc + 4])
            self.TS("dve", z.ap[:, 256:384], pa.ap[:, 256:384], stat.ap[:, sc + 2:sc + 3], None, ALU.mult, r=[pa, stat], w=[z])
            if i >= 2:
                for c in range(2):
                    self.TR(ptb[:, c * 128:(c + 1) * 128], z.ap[:, c * 128:(c + 1) * 128], identb, r=[z, cstb], w=[pt])
                    self.TS("dve", zdqnT.ap[:, c, (i - 2) * 128:(i - 1) * 128], ptb[:, c * 128:(c + 1) * 128], self.gqv[:, c:c + 1], None,
                            ALU.mult, r=[pt, self.vec], w=[zdqnT])
            self.TR(ptb[:, 256:384], z.ap[:, 256:384], identb, r=[z, cstb], w=[pt])
            self.TS("dve", ckvT.ap[:, i * 128:(i + 1) * 128], ptb[:, 256:384], self.gkvv[:, 0:1], None, ALU.mult,
                    r=[pt, self.vec], w=[ckvT])
        TB_ALL = [(0, 256), (256, 512), (768, 512), (1280, 512), (1792, 512)]
        rp = [S.alloc("rp", [2, 512], F32) for _ in range(2)]
        rt = S.alloc("rt", [2, 512], F32)
        ropv = self.c_ropet.rearrange("p (a b) -> p a b", b=NTOK)
        for bi, (c0, nn) in enumerate(TB_ALL):
            r_ = rp[bi % 2]
            self.DMA("sp", r_.ap[64:96, :, 0:nn], ropv[64:96, :, c0:c0 + nn], w=[r_])
            for kc in range(KC):
                self.MM(P[4].ap[64:96, 0:nn], wB.ap[:, kc, 384:416], hT.ap[:, kc, c0:c0 + nn], start=(kc == 0), stop=(kc == KC - 1),
                        r=[wB, hT], w=[P[4]])
            for kc in range(KC):
                self.MM(P[5].ap[64:96, 0:nn], wkrs.ap[:, kc, :], hT.ap[:, kc, c0:c0 + nn], start=(kc == 0), stop=(kc == KC - 1),
                        r=[wkrs, hT], w=[P[5]])
            self.TT("dve", rt.ap[64:96, 0, 0:nn], P[4].ap[64:96, 0:nn], r_.ap[64:96, 0, 0:nn], ALU.mult, r=[P[4], r_], w=[rt])
            self.TT("dve", rt.ap[64:96, 1, 0:nn], P[5].ap[64:96, 0:nn], r_.ap[64:96, 1, 0:nn], ALU.mult, r=[P[5], r_], w=[rt])
            self.TT("pool", krT.ap[64:96, c0:c0 + nn], rt.ap[64:96, 0, 0:nn], rt.ap[64:96, 1, 0:nn], ALU.add, r=[rt], w=[krT])
        S.release(wB, wkrs, junk, *zn)
        v = S.alloc("v", [NT, 8, 128], BF16)
        self.MS("pool", v.ap[:, :, 0:4, 64:128], 0.0, w=[v])
        self.MS("pool", v.ap[:, :, 0:4, 64:65], 1.0, w=[v])
        self.MS("pool", v.ap[:, :, 4:8, 0:64], 0.0, w=[v])
        self.MS("pool", v.ap[:, :, 4:8, 0:1], 1.0, w=[v])
        for i in range(NT):
            pb = P[i % 2]
            self.MM(pb.ap[:, :], ckvT.ap[:, i * 128:(i + 1) * 128], wukv.ap[:, 512:1024], r=[ckvT, wukv], w=[pb])
            self.CP("act", v.ap[:, i, 0:4, 0:64], pb.ap[:, 0:256].rearrange("p (a b) -> p a b", b=64), r=[pb], w=[v])
            self.CP("dve", v.ap[:, i, 4:8, 64:128], pb.ap[:, 256:512].rearrange("p (a b) -> p a b", b=64), r=[pb], w=[v])
        omlaT = S.alloc("omlaT", [4, T], BF16)
        self.omlaT = omlaT
        kThs = [S.alloc("kTh", [NTOK], BF16) for _ in range(2)]
        qThs = [S.alloc("qTh", [T], BF16) for _ in range(2)]
        pT = [S.alloc("pT", [512], BF16) for _ in range(3)]
        rden = S.alloc("rden", [512], F32)
        bcs = S.alloc("bcs", [512], F32)
        sqb = S.alloc("sqb", [NTOK], BF16)
        mxs = [S.alloc("mx", [8], F32) for _ in range(2)]
        nb = [0]

        def build(h):
            kTh, qTh = kThs[h % 2], qThs[h % 2]
            for (c0, nn) in TB_ALL:
                pb = P[nb[0] % 2]
                nb[0] += 1
                self.MM(pb.ap[0:64, 0:nn], wukv.ap[:, h * 64:(h + 1) * 64], ckvT.ap[:, c0:c0 + nn], r=[wukv, ckvT], w=[pb])
                self.CP("dve", kTh.ap[0:64, c0:c0 + nn], pb.ap[0:64, 0:nn], r=[pb], w=[kTh])
            self.CP("pool", kTh.ap[64:96, :], krT.ap[64:96, :], r=[krT], w=[kTh])
            for qb in range(4):
                c0 = TC + qb * 512
                r_ = rp[qb % 2]
                self.DMA("sp", r_.ap[64:96, :, :], ropv[64:96, :, c0:c0 + 512], w=[r_])
                pq, pr = P[2], P[3]
                for c in range(2):
                    self.MM(pq.ap[0:96, :], wuq.ap[:, c, h * 96:(h + 1) * 96], zdqnT.ap[:, c, qb * 512:(qb + 1) * 512],
                            start=(c == 0), stop=(c == 1), r=[wuq, zdqnT], w=[pq])
                for c in range(2):
                    self.MM(pr.ap[64:96, :], wuqs.ap[:, c, h, :], zdqnT.ap[:, c, qb * 512:(qb + 1) * 512],
                            start=(c == 0), stop=(c == 1), r=[wuqs, zdqnT], w=[pr])
                self.CP("dve", qTh.ap[0:64, qb * 512:(qb + 1) * 512], pq.ap[0:64, :], r=[pq], w=[qTh])
                self.TT("dve", rt.ap[64:96, 0, :], pq.ap[64:96, :], r_.ap[64:96, 0, :], ALU.mult, r=[pq, r_], w=[rt])
                self.TT("dve", rt.ap[64:96, 1, :], pr.ap[64:96, :], r_.ap[64:96, 1, :], ALU.mult, r=[pr, r_], w=[rt])
                self.TT("pool", qTh.ap[64:96, qb * 512:(qb + 1) * 512], rt.ap[64:96, 0, :], rt.ap[64:96, 1, :], ALU.add, r=[rt], w=[qTh])

        def build_stab(h):
            kTh, qTh = kThs[h % 2], qThs[h % 2]
            sq = sqb
            mx = mxs[h % 2]
            pn = P[3]
            for which, src_, ntile in ((0, qTh, 16), (1, kTh, NT)):
                self.TT("pool", sq.ap[0:96, 0:ntile * 128], src_.ap[0:96, 0:ntile * 128], src_.ap[0:96, 0:ntile * 128], ALU.mult,
                        r=[src_], w=[sq])
                for t_ in range(ntile):
                    self.MM(pn.ap[:, which * 32 + t_:which * 32 + t_ + 1], sq.ap[0:96, t_ * 128:(t_ + 1) * 128], onesb[0:96, 0:1],
                            r=[sq, cstb], w=[pn])
                self.S.op("dve", lambda e, o=mx.ap[:, which:which + 1], a=pn.ap[:, which * 32:which * 32 + ntile]: e.reduce_max(o, a, axis=AX.X),
                          [pn], [mx])
            for which in range(2):
                self.TR(pn.ap[0:1, 64 + which * 128:64 + (which + 1) * 128], mx.ap[:, which:which + 1], identf, r=[mx, cst], w=[pn])
                self.S.op("dve", lambda e, o=mx.ap[0:1, 2 + which:3 + which], a=pn.ap[0:1, 64 + which * 128:64 + (which + 1) * 128]:
                          e.reduce_max(o, a, axis=AX.X), [pn], [mx])
            self.TT("dve", mx.ap[0:1, 4:5], mx.ap[0:1, 2:3], mx.ap[0:1, 3:4], ALU.mult, r=[mx], w=[mx])
            self.ACT(mx.ap[0:1, 5:6], mx.ap[0:1, 4:5], AF.Ln, r=[mx], w=[mx], bias=self.eps_ap[0:1, :])
            self.ACT(mx.ap[0:1, 6:7], mx.ap[0:1, 5:6], AF.Exp, r=[mx], w=[mx], scale=0.5)
            self.MM(pn.ap[:, 400:401], onesf[0:1, :], mx.ap[0:1, 6:7], r=[cst, mx], w=[pn])
            self.TS("dve", mx.ap[:, 7:8], pn.ap[:, 400:401], -1.02 * MLA_SCALE, None, ALU.mult, r=[pn], w=[mx])

        def tail(h, qb):
            po = P[6 + qb % 2]
            dr, lo = (64, 0) if h < 4 else (0, 64)
            self.CP("act", rden.ap[dr:dr + 1, :], po.ap[dr:dr + 1, :], r=[po], w=[rden])
            pbc = P[2]
            self.MM(pbc.ap[lo:lo + 64, :], onesf[dr:dr + 1, 0:64], rden.ap[dr:dr + 1, :], r=[cst, rden], w=[pbc])
            self.S.op("dve", lambda e, o=bcs.ap[lo:lo + 64, :], a=pbc.ap[lo:lo + 64, :]: e.reciprocal(o, a), [pbc], [bcs])
            self.TT("dve", omlaT.ap[lo:lo + 64, h % 4, qb * 512:(qb + 1) * 512], po.ap[lo:lo + 64, :], bcs.ap[lo:lo + 64, :], ALU.mult,
                    r=[po, bcs], w=[omlaT])
        build(0)
        build_stab(0)
        pend = None
        for h in range(8):
            kTh, qTh = kThs[h % 2], qThs[h % 2]
            if h + 1 < 8:
                build(h + 1)
            for qb in range(4):
                po = P[6 + qb % 2]

                def qk(kt, qb=qb, kTh=kTh, qTh=qTh):
                    self.MM(P[4 + kt % 2].ap[:, :], kTh.ap[0:96, kt * 128:(kt + 1) * 128], qTh.ap[0:96, qb * 512:(qb + 1) * 512],
                            r=[kTh, qTh], w=[P[4 + kt % 2]])
                qk(0)
                for kt in range(NT):
                    psb = P[4 + kt % 2]
                    p_ = pT[kt % 3]
                    if kt + 1 < NT:
                        qk(kt + 1)
                    self.ACT(p_.ap[:, :], psb.ap[:, :], AF.Exp, r=[psb, mxs[h % 2]], w=[p_], scale=MLA_SCALE, bias=mxs[h % 2].ap[:, 7:8])
                    self.MM(po.ap[:, :], v.ap[:, kt, h, :], p_.ap[:, :], start=(kt == 0), stop=(kt == NT - 1), r=[v, p_], w=[po])
                    if kt == 2 and pend is not None:
                        tail(*pend)
                        pend = None
                    if kt == 9 and qb == 2 and h + 1 < 8:
                        build_stab(h + 1)
                pend = (h, qb)
        tail(*pend)
        kTh, qTh = kThs[0], qThs[0]
        S.release(kThs[1], qThs[1], sqb, *mxs)
        S.release(wuq, wuqs, wukv, zdqnT, ckvT, krT, v, kTh, qTh, rden, bcs, rt, *pT, *rp)

    def phase_gla(self):
        S, P = self.S, self.P
        hT, cstb, cst, stat = self.hT, self.cstb, self.cst, self.stat
        TB_ALL = [(0, 256), (256, 512), (768, 512), (1280, 512), (1792, 512)]
        TB_X = TB_ALL[1:]
        wA = S.alloc("wA", [KC, 1056], BF16)
        self.load_w(wA.ap[:, :, 0:1024], wA, self.w_in[:, 0:1024], 1024, 1024)
        self.load_w(wA.ap[:, :, 1024:1056], wA, self.w_in[:, 1536:1568], 1024, 32)
        wzg = S.alloc("wzg", [KC, 512], BF16)
        self.load_w(wzg.ap[:, :, :], wzg, self.w_in[:, 1024:1536], 1024, 512)
        wa2b = S.alloc("wa2b", [1024], BF16)
        self.load_w(wa2b.ap[:, 0:512], wa2b, self.wa2, 32, 512)
        self.load_w(wa2b.ap[:, 512:1024], wa2b, self.ba_row, 1, 512)
        gnb = S.alloc("gnb", [512], F32)
        self.DMA("sp", gnb.ap[:, :], self.gnb, w=[gnb])
        self.xsz = S.virt("xsz")
        zt = S.alloc("zt", [D], BF16)
        self.zt = zt
        self.MS("pool", zt.ap[:, :], 0.0, w=[zt])
        for c in range(NSLOT // 128):
            self.DMA("sp", self.xs_d[c * 128:(c + 1) * 128, :], zt.ap[:, :], r=[zt], w=[self.xsz], sem=zt)
        gqT = S.alloc("gqT", [2, T], BF16)
        gkT = S.alloc("gkT", [2, NTOK], BF16)
        gk = S.alloc("gk", [NT, 256], BF16)
        gv = S.alloc("gv", [NT, 512], BF16)
        zaT = S.alloc("zaT", [NTOK], BF16)
        n = 0
        for (c0, nn) in TB_ALL:
            for m in range(4):
                if m < 2 and c0 < TC:
                    continue
                pb = P[n % 2]
                n += 1
                for kc in range(KC):
                    self.MM(pb.ap[:, 0:nn], wA.ap[:, kc, m * 128:(m + 1) * 128], hT.ap[:, kc, c0:c0 + nn],
                            start=(kc == 0), stop=(kc == KC - 1), r=[wA, hT], w=[pb])
                if m < 2:
                    self.CP("act", gqT.ap[:, m, c0 - TC:c0 - TC + nn], pb.ap[:, 0:nn], r=[pb], w=[gqT])
                else:
                    self.CP("dve", gkT.ap[:, m - 2, c0:c0 + nn], pb.ap[:, 0:nn], r=[pb], w=[gkT])
            pb = P[n % 2]
            n += 1
            for kc in range(KC):
                self.MM(pb.ap[0:32, 0:nn], wA.ap[:, kc, 1024:1056], hT.ap[:, kc, c0:c0 + nn],
                        start=(kc == 0), stop=(kc == KC - 1), r=[wA, hT], w=[pb])
            self.CP("act", zaT.ap[0:32, c0:c0 + nn], pb.ap[0:32, 0:nn], r=[pb], w=[zaT])
        for i in range(NT):
            pa, pv = P[2 + i % 2], P[4 + i % 2]
            for kc in range(KC):
                self.MM(pa.ap[:, 0:256], hT.ap[:, kc, i * 128:(i + 1) * 128], wA.ap[:, kc, 256:512],
                        start=(kc == 0), stop=(kc == KC - 1), r=[wA, hT], w=[pa])
            for kc in range(KC):
                self.MM(pv.ap[:, :], hT.ap[:, kc, i * 128:(i + 1) * 128], wA.ap[:, kc, 512:1024],
                        start=(kc == 0), stop=(kc == KC - 1), r=[wA, hT], w=[pv])
            self.CP("act", gk.ap[:, i, :], pa.ap[:, 0:256], r=[pa], w=[gk])
            self.CP("dve", gv.ap[:, i, :], pv.ap[:, :], r=[pv], w=[gv])
        S.release(wA)
        if self.stage == 2:
            for m in range(2):
                self.dump(m * 2048, gqT, gqT.ap[:, m, :], 2048)
            self.dump(4096, gv, gv.ap[:, 5, :], 512)
            self.dump(4608, gk, gk.ap[:, 5, :], 256)
            self.dump(4864, zaT, zaT.ap[:, 0:2304], 2304)
            self.ogT = gqT
            return

        ogT = S.alloc("ogT", [4, T], BF16)
        self.ogT = ogT
        hist = S.alloc("hist", [32, 256], BF16)
        Sst = [S.alloc("Sst", [256], F32) for _ in range(2)]
        Sbr = [S.alloc("Sbr", [256], BF16) for _ in range(2)]
        R = 2
        la_b = [S.alloc("la", [256], F32) for _ in range(R)]
        lt_b = [S.alloc("lt", [256], F32) for _ in range(R)]
        ET_b = [S.alloc("ET", [256], F32) for _ in range(R)]
        EI_b = [S.alloc("EI", [256], F32) for _ in range(R)]
        KS_b = [S.alloc("KS", [256], F32) for _ in range(R)]
        qd_b = [S.alloc("qd", [2, 2, 128], BF16) for _ in range(2)]
        qt_b = [S.alloc("qt", [2, 128], F32) for _ in range(2)]
        ki_b = [S.alloc("ki", [2, 128], BF16) for _ in range(R)]
        kte_b = [S.alloc("kte", [256], BF16) for _ in range(R)]
        att_b = [S.alloc("att", [512], BF16) for _ in range(R)]
        ot_b = S.alloc("ot", [512], F32)
        sq_b = S.alloc("sq", [512], F32)
        sz_b = S.alloc("sz", [512], F32)
        og_b = S.alloc("og", [512], BF16)
        self.cnt = 0
        tri, maskb, onesb, identb = self.tri, self.maskb, self.onesb, self.identb
        for d in range(2):
            self.MS("pool", Sst[d].ap[:, :], 0.0, w=[Sst[d]])
        self.MS("pool", Sbr[0].ap[:, :], 0.0, w=[Sbr[0]])

        def prep(i, d, want_q):
            c = self.cnt
            self.cnt += 1
            r = c % R
            pl, pc = P[0 + c % 2], P[2 + c % 2]
            la, lt, ET, EI, KS = la_b[r], lt_b[r], ET_b[r], EI_b[r], KS_b[r]
            self.MM(pl.ap[:, 0:256], zaT.ap[0:32, i * 128:(i + 1) * 128], wa2b.ap[0:32, d * 256:(d + 1) * 256],
                    start=True, stop=False, r=[zaT, wa2b], w=[pl])
            self.MM(pl.ap[:, 0:256], onesb[0:1, :], wa2b.ap[0:1, 512 + d * 256:512 + (d + 1) * 256],
                    start=False, stop=True, r=[cstb, wa2b], w=[pl])
            self.ACT(lt.ap[:, :], pl.ap[:, 0:256], AF.Abs, r=[pl], w=[lt])
            self.ACT(lt.ap[:, :], lt.ap[:, :], AF.Exp, r=[lt], w=[lt], scale=-1.0)
            self.ACT(lt.ap[:, :], lt.ap[:, :], AF.Ln, r=[lt], w=[lt], bias=self.onesf[:, 0:1])
            self.TS("dve", la.ap[:, :], pl.ap[:, 0:256], 0.0, 1.0 / 16, ALU.min, ALU.mult, r=[pl], w=[la])
            self.STT(la.ap[:, :], lt.ap[:, :], -1.0 / 16, la.ap[:, :], ALU.mult, ALU.add, r=[lt, la], w=[la])
            for j in range(2):
                self.MM(pc.ap[:, j * 128:(j + 1) * 128], la.ap[:, j * 128:(j + 1) * 128], tri[:, d, :], r=[la, cst], w=[pc])
            self.MM(pc.ap[:, 256:512], tri[:, 2 + d, :], la.ap[:, :], r=[la, cst], w=[pc])
            self.ACT(ET.ap[:, :], pc.ap[:, 0:256], AF.Exp, r=[pc], w=[ET])
            self.ACT(KS.ap[:, :], pc.ap[:, 256:512], AF.Exp, r=[pc], w=[KS])
            kte = kte_b[r]
            self.TT("pool", kte.ap[:, :], gk.ap[:, i, :], KS.ap[:, :], ALU.mult, r=[gk, KS], w=[kte])
            out = {"pl": pl, "pc": pc, "ET": ET, "kte": kte}
            if want_q:
                xc = (i - 2) * 128
                self.ACT(EI.ap[:, :], pc.ap[:, 0:256], AF.Exp, r=[pc], w=[EI], scale=-1.0)
                qd = qd_b[c % 2]
                ki = ki_b[r]
                qt = qt_b[c % 2]
                self.STT(qt.ap[:, :, :], gqT.ap[:, :, xc:xc + 128], 0.125, ET.ap[:, :].rearrange("p (a b) -> p a b", b=128),
                         ALU.mult, ALU.mult, r=[gqT, ET], w=[qt])
                for p in range(2):
                    self.TS("pool" if p else "dve", qd.ap[:, p, :, :], qt.ap[:, :, :], self.rowmask[:, p:p + 1], None, ALU.mult,
                            r=[qt, cst], w=[qd])
                self.TT("pool", ki.ap[:, :, :], gkT.ap[:, :, i * 128:(i + 1) * 128],
                        EI.ap[:, :].rearrange("p (a b) -> p a b", b=128), ALU.mult, r=[gkT, EI], w=[ki])
                out["qd"], out["ki"] = qd, ki
            return out

        def ds_update(i, d, b, cs):
            pl, ET, kte = b["pl"], b["ET"], b["kte"]
            for h in range(4):
                self.MM(pl.ap[(h % 2) * 64:(h % 2) * 64 + 64, 256 + (h // 2) * 128:256 + (h // 2) * 128 + 128],
                        kte.ap[cs:cs + 64, h * 64:(h + 1) * 64], gv.ap[cs:cs + 64, i, h * 128:(h + 1) * 128],
                        r=[kte, gv], w=[pl])
            tl = cs + 63 if d == 0 else cs
            for j in range(2):
                self.STT(Sst[d].ap[:, j * 128:(j + 1) * 128], Sst[d].ap[:, j * 128:(j + 1) * 128],
                         ET.ap[:, j * 128 + tl:j * 128 + tl + 1], pl.ap[:, 256 + j * 128:256 + (j + 1) * 128],
                         ALU.mult, ALU.add, r=[Sst[d], ET, pl], w=[Sst[d]])

        order_b = [1, 0] + list(range(17, 1, -1))
        for i in order_b:
            b = prep(i, 1, False)
            for cs in (64, 0):
                if i >= 2:
                    ch = (i - 2) * 2 + (1 if cs == 64 else 0)
                    self.CP("act", hist.ap[:, ch, :], Sst[1].ap[:, :], r=[Sst[1]], w=[hist])
                ds_update(i, 1, b, cs)

        if self.stage == 25:
            S.release(ot_b, sq_b, sz_b)
            self.dump(0, hist, hist.ap[:, 31, :], 256)
            self.dump(256, hist, hist.ap[:, 0, :], 256)
            return
        live = 0
        for i in range(NT):
            bf = prep(i, 0, i >= 2)
            if i < 2:
                for cs in (0, 64):
                    ds_update(i, 0, bf, cs)
                if i == 1:
                    self.CP("act", Sbr[0].ap[:, :], Sst[0].ap[:, :], r=[Sst[0]], w=[Sbr[0]])
                continue
            bb = prep(i, 1, True)
            xi = i - 2
            xc = xi * 128
            pos = (P[6], P[7])
            pss = (P[4], P[5])
            first = [True, True]
            acol = lambda h: ((h % 2) * 2 + h // 2) * 128
            for d, b in ((0, bf), (1, bb)):
                att = att_b[d]
                qd, ki = b["qd"], b["ki"]
                for h in range(4):
                    hp = (h % 2) * 64
                    self.MM(pss[h % 2].ap[:, (h // 2) * 128:(h // 2) * 128 + 128], ki.ap[:, h // 2, :],
                            qd.ap[:, h % 2, h // 2, :], r=[ki, qd], w=[pss[h % 2]])
                for p in range(2):
                    self.TT("dve", att.ap[:, p * 256:(p + 1) * 256], pss[p].ap[:, 0:256], maskb[:, d, 0:256], ALU.mult,
                            r=[pss[p], cstb], w=[att])
                for h in range(4):
                    self.MM(pos[h % 2].ap[:, (h // 2) * 128:(h // 2) * 128 + 128], att.ap[:, acol(h):acol(h) + 128],
                            gv.ap[:, i, h * 128:(h + 1) * 128], start=first[h % 2], stop=False, r=[att, gv], w=[pos[h % 2]])
                    first[h % 2] = False
            qd = bb["qd"]
            for cs in (64, 0):
                ch = xi * 2 + (1 if cs == 64 else 0)
                for h in range(4):
                    hp = (h % 2) * 64
                    self.MM(pos[h % 2].ap[cs:cs + 64, (h // 2) * 128:(h // 2) * 128 + 128], qd.ap[:, h % 2, h // 2, cs:cs + 64],
                            hist.ap[:, ch, (h // 2) * 128:(h // 2) * 128 + 128], start=False, stop=False,
                            r=[qd, hist], w=[pos[h % 2]])
            qd = bf["qd"]
            for cs in (0, 64):
                sb = Sbr[live % 2]
                for h in range(4):
                    hp = (h % 2) * 64
                    self.MM(pos[h % 2].ap[cs:cs + 64, (h // 2) * 128:(h // 2) * 128 + 128], qd.ap[:, h % 2, h // 2, cs:cs + 64],
                            sb.ap[:, (h // 2) * 128:(h // 2) * 128 + 128], start=False, stop=(cs == 64 and h >= 2),
                            r=[qd, sb], w=[pos[h % 2]])
                ds_update(i, 0, bf, cs)
                live += 1
                self.CP("act", Sbr[live % 2].ap[:, :], Sst[0].ap[:, :], r=[Sst[0]], w=[Sbr[live % 2]])
            pz = bf["pc"]
            for kc in range(KC):
                self.MM(pz.ap[:, :], hT.ap[:, kc, i * 128:(i + 1) * 128], wzg.ap[:, kc, :], start=(kc == 0), stop=(kc == KC - 1),
                        r=[hT, wzg], w=[pz])
            self.ACT(sz_b.ap[:, :], pz.ap[:, :], AF.Silu, r=[pz], w=[sz_b])
            for h in range(4):
                self.CP("act" if h % 2 == 0 else "dve", ot_b.ap[:, h * 128:(h + 1) * 128],
                        pos[h % 2].ap[:, (h // 2) * 128:(h // 2) * 128 + 128], r=[pos[h % 2]], w=[ot_b])
            self.TT("pool", sq_b.ap[:, :], ot_b.ap[:, :], ot_b.ap[:, :], ALU.mult, r=[ot_b], w=[sq_b])
            st4 = stat.ap[:, 16:20]
            st4b = stat.ap[:, 20:24]
            self.S.op("dve", lambda e, o=st4, a=sq_b.ap[:, :].rearrange("p (a b) -> p a b", b=128): e.reduce_sum(o, a, axis=AX.X),
                      [sq_b], [stat])
            self.ACT(st4b, st4, AF.Ln, r=[stat], w=[stat], scale=1.0 / 128, bias=self.eps_ap)
            self.ACT(st4, st4b, AF.Exp, r=[stat], w=[stat], scale=-0.5)
            self.TT("dve", sq_b.ap[:, :].rearrange("p (a b) -> p a b", b=128), ot_b.ap[:, :].rearrange("p (a b) -> p a b", b=128),
                    st4.unsqueeze(2).to_broadcast([128, 4, 128]), ALU.mult, r=[ot_b, stat], w=[sq_b])
            self.TT("pool", sq_b.ap[:, :], sq_b.ap[:, :], gnb.ap[:, :], ALU.mult, r=[sq_b, gnb], w=[sq_b])
            self.TT("dve", og_b.ap[:, :], sq_b.ap[:, :], sz_b.ap[:, :], ALU.mult, r=[sq_b, sz_b], w=[og_b])
            pt = P[4]
            ptb = pt.ap.bitcast(BF16)
            for h in range(4):
                self.TR(ptb[:, h * 128:(h + 1) * 128], og_b.ap[:, h * 128:(h + 1) * 128], identb, r=[og_b, cstb], w=[pt])
            self.CP("act", ogT.ap[:, :, xc:xc + 128], ptb[:, 0:512].rearrange("p (a b) -> p a b", b=128), r=[pt], w=[ogT])
        S.release(gqT, gkT, gk, gv, zaT, hist, wzg, wa2b, gnb, ot_b, sq_b, sz_b, og_b,
                  *Sst, *Sbr, *la_b, *lt_b, *ET_b, *EI_b, *KS_b, *qd_b, *qt_b, *ki_b, *kte_b, *att_b)

    def phase_merge(self):
        S, P = self.S, self.P
        hT, ogT, omlaT, stat, cst, cstb = self.hT, self.ogT, self.omlaT, self.stat, self.cst, self.cstb
        mT = S.alloc("mT", [KC, T], BF16)
        wbg = S.alloc("wbg", [4, D], BF16)
        self.load_w(wbg.ap[:, :, :], wbg, self.w_br_gla, 512, D)
        wbm = S.alloc("wbm", [4, D], BF16)
        src = self.w_br_mla.rearrange("(k p) n -> p k n", p=64)
        self.DMA("pool", wbm.ap[0:64, :, :], src[:, 0:4, :], w=[wbm])
        self.DMA("pool", wbm.ap[64:128, :, :], src[:, 4:8, :], w=[wbm])
        wz = [S.alloc("wz", [KC, 256], BF16) for _ in range(2)]
        sg = [S.alloc("sg", [512], F32) for _ in range(2)]
        sm = [S.alloc("sm", [512], F32) for _ in range(2)]
        n = 0
        for m in range(KC):
            w_ = wz[m % 2]
            self.load_w(w_.ap[:, :, 0:128], w_, self.w_in[:, 1984 + m * 128:1984 + (m + 1) * 128], 1024, 128)
            self.load_w(w_.ap[:, :, 128:256], w_, self.w_in[:, 3008 + m * 128:3008 + (m + 1) * 128], 1024, 128)
            for tb in range(4):
                c0, xc0 = TC + tb * 512, tb * 512
                o = (n % 2) * 4
                sg_, sm_ = sg[n % 2], sm[n % 2]
                n += 1
                for kc in range(KC):
                    self.MM(P[o].ap[:, :], w_.ap[:, kc, 0:128], hT.ap[:, kc, c0:c0 + 512], start=(kc == 0), stop=(kc == KC - 1),
                            r=[w_, hT], w=[P[o]])
                for kc in range(KC):
                    self.MM(P[o + 1].ap[:, :], w_.ap[:, kc, 128:256], hT.ap[:, kc, c0:c0 + 512], start=(kc == 0), stop=(kc == KC - 1),
                            r=[w_, hT], w=[P[o + 1]])
                for hc in range(4):
                    self.MM(P[o + 2].ap[:, :], wbg.ap[:, hc, m * 128:(m + 1) * 128], ogT.ap[:, hc, xc0:xc0 + 512],
                            start=(hc == 0), stop=(hc == 3), r=[wbg, ogT], w=[P[o + 2]])
                for j in range(4):
                    self.MM(P[o + 3].ap[:, :], wbm.ap[:, j, m * 128:(m + 1) * 128], omlaT.ap[:, j, xc0:xc0 + 512],
                            start=(j == 0), stop=(j == 3), r=[wbm, omlaT], w=[P[o + 3]])
                self.ACT(sg_.ap[:, :], P[o].ap[:, :], AF.Sigmoid, r=[P[o]], w=[sg_])
                self.ACT(sm_.ap[:, :], P[o + 1].ap[:, :], AF.Sigmoid, r=[P[o + 1]], w=[sm_])
                self.TT("dve", sg_.ap[:, :], sg_.ap[:, :], P[o + 2].ap[:, :], ALU.mult, r=[sg_, P[o + 2]], w=[sg_])
                self.TT("dve", sm_.ap[:, :], sm_.ap[:, :], P[o + 3].ap[:, :], ALU.mult, r=[sm_, P[o + 3]], w=[sm_])
                self.TT("pool", mT.ap[:, m, xc0:xc0 + 512], sg_.ap[:, :], sm_.ap[:, :], ALU.add, r=[sg_, sm_], w=[mT])
        S.release(hT, ogT, omlaT, wbg, wbm, *wz, *sg, *sm)
        wo = S.alloc("wo", [KC, D], BF16)
        self.load_w(wo.ap[:, :, :], wo, self.w_out, D, D)
        rw = S.alloc("rw", [KC * NE + NE], F32)
        rwv = rw.ap[:, 0:KC * NE].rearrange("p (a b) -> p a b", b=NE)
        self.DMA("sp", rwv, self.router_w.rearrange("(k p) n -> p k n", p=128), w=[rw])
        self.DMA("sp", rw.ap[0:1, KC * NE:KC * NE + NE], self.router_b, w=[rw])
        comb = S.alloc("comb", [16, NE], F32)
        self.comb = comb
        rt_ = S.alloc("rt", [176 + 144], F32)
        self.DMA("sp", rt_.ap[:, 0:176], self.c_rt, w=[rt_])
        tsub = S.alloc("tsub", [128], BF16)
        self.CP("pool", tsub.ap[:, :], rt_.ap[:, 0:128], r=[rt_], w=[tsub])
        Umat = rt_.ap[:, 128:160]
        IO = rt_.ap[:, 160:176]
        cb = rt_.ap[:, 176:208]
        posf4 = rt_.ap[:, 208:212]
        oh = rt_.ap[:, 224:256]
        oh2 = rt_.ap[:, 256:288]
        posf = rt_.ap[:, 288:320]
        self.MS("pool", rt_.ap[:, 176:320], 0.0, w=[rt_])
        wk = S.alloc("wk", [16, 4], F32)
        posi = S.alloc("posi", [16, 4], F32)
        posi_v = posi.ap.bitcast(I32)
        self.wk, self.posi, self.posi_v = wk, posi, posi_v
        maskb16 = S.alloc("maskb16", [NE], BF16)
        rank_all = S.alloc("rank", [16, NE], F32)
        lgall = S.alloc("lgall", [16, 40], F32)
        xn2tm = S.alloc("xn2tm", [16, D], BF16)
        xsz, xsd = self.xsz, S.virt("xsd")
        self.xsd, self.ysd = xsd, S.virt("ysd")
        xt = [S.alloc("xt2", [D], F32) for _ in range(2)]
        tt = [S.alloc("tt2", [D], F32) for _ in range(2)]
        hx2f = S.alloc("hx2f", [KC, 128], F32)
        lg = S.alloc("lg", [128], F32)
        junk = S.alloc("junk3", [D], BF16)
        Gb, A2, modb, identf, onesf = self.Gb, self.A2, self.modb, self.identf, self.onesf
        for xi in range(16):
            pa, pb = P[(xi % 2) * 2], P[(xi % 2) * 2 + 1]
            x_, t_ = xt[xi % 2], tt[xi % 2]
            self.DMA("sp", x_.ap[:, :], self.x[xi * 128:(xi + 1) * 128, :], w=[x_])
            for hf, pp in enumerate((pa, pb)):
                for m in range(KC):
                    self.MM(pp.ap[:, :], mT.ap[:, m, xi * 128:(xi + 1) * 128], wo.ap[:, m, hf * 512:(hf + 1) * 512],
                            start=(m == 0), stop=(m == KC - 1), r=[mT, wo], w=[pp])
            sc = 24 + (xi % 2) * 8
            for hf, pp in enumerate((pa, pb)):
                self.ACT(junk.ap[:, 0:512], pp.ap[:, :], AF.Square, r=[pp], w=[junk, stat], accum_out=stat.ap[:, sc + hf:sc + hf + 1])
            self.TT("dve", stat.ap[:, sc:sc + 1], stat.ap[:, sc:sc + 1], stat.ap[:, sc + 1:sc + 2], ALU.add, r=[stat], w=[stat])
            self.rstd_from_ss(stat.ap[:, sc:sc + 1], D, stat, stat.ap[:, sc + 2:sc + 3])
            for hf, pp in enumerate((pa, pb)):
                self.STT(t_.ap[:, hf * 512:(hf + 1) * 512], pp.ap[:, :], stat.ap[:, sc:sc + 1], Gb.ap[:, 0, hf * 512:(hf + 1) * 512],
                         ALU.mult, ALU.mult, r=[pp, stat, Gb], w=[t_])
            self.TT("pool", x_.ap[:, :], t_.ap[:, :], x_.ap[:, :], ALU.add, r=[t_, x_], w=[x_])
            self.DMA("pool", self.x1d[xi * 128:(xi + 1) * 128, :], x_.ap[:, :], r=[x_], sem=x_)
            self.ACT(junk.ap[:, :], x_.ap[:, :], AF.Square, r=[x_], w=[junk, stat], accum_out=stat.ap[:, sc + 3:sc + 4])
            self.rstd_from_ss(stat.ap[:, sc + 3:sc + 4], D, stat, stat.ap[:, sc + 4:sc + 5])
            self.TS("dve", t_.ap[:, :], x_.ap[:, :], stat.ap[:, sc + 3:sc + 4], None, ALU.mult, r=[x_, stat], w=[t_])
            for kc in range(KC):
                pt = P[4 + kc // 4]
                self.TR(pt.ap[:, (kc % 4) * 128:(kc % 4 + 1) * 128], t_.ap[:, kc * 128:(kc + 1) * 128], identf, r=[t_, cst], w=[pt])
            for kc in range(KC):
                pt = P[4 + kc // 4]
                src_ = pt.ap[:, (kc % 4) * 128:(kc % 4 + 1) * 128]
                if kc % 2 == 0:
                    self.TS("dve", hx2f.ap[:, kc, :], src_, A2[:, kc:kc + 1], modb.ap[:, 24 + kc, 0:1], ALU.mult, ALU.add,
                            r=[pt, self.vec, modb], w=[hx2f])
                else:
                    self.ACT(hx2f.ap[:, kc, :], src_, AF.Identity, r=[pt, self.vec, modb], w=[hx2f],
                             scale=A2[:, kc:kc + 1], bias=modb.ap[:, 24 + kc, 0:1])
            pr = P[6 + xi % 2]
            for kc in range(KC):
                self.MM(pr.ap[:, 0:NE], hx2f.ap[:, kc, :], rwv[:, kc, :], start=(kc == 0), stop=False, r=[hx2f, rw], w=[pr])
            self.MM(pr.ap[:, 0:NE], onesf[0:1, :], rw.ap[0:1, KC * NE:KC * NE + NE], start=False, stop=True, r=[cst, rw], w=[pr])
            self.CP("act", lg.ap[:, 0:NE], pr.ap[:, 0:NE], r=[pr], w=[lg])
            self.S.op("dve", lambda e, o=lg.ap[:, 32:40], a=lg.ap[:, 0:NE]: e.max(out=o, in_=a), [lg], [lg])
            self.TS("dve", lg.ap[:, 64:96], lg.ap[:, 0:NE], lg.ap[:, 35:36], None, ALU.is_ge, r=[lg], w=[lg])
            self.TS("dve", lg.ap[:, 40:41], lg.ap[:, 32:33], -1.0, None, ALU.mult, r=[lg], w=[lg])
            self.ACT(lg.ap[:, 96:128], lg.ap[:, 0:NE], AF.Exp, r=[lg], w=[lg], bias=lg.ap[:, 40:41])
            self.TT("dve", lg.ap[:, 96:128], lg.ap[:, 96:128], lg.ap[:, 64:96], ALU.mult, r=[lg], w=[lg])
            self.S.op("dve", lambda e, o=lg.ap[:, 41:42], a=lg.ap[:, 96:128]: e.reduce_sum(o, a, axis=AX.X), [lg], [lg])
            self.S.op("dve", lambda e, o=lg.ap[:, 42:43], a=lg.ap[:, 41:42]: e.reciprocal(o, a), [lg], [lg])
            self.TS("dve", comb.ap[:, xi, :], lg.ap[:, 96:128], lg.ap[:, 42:43], None, ALU.mult, r=[lg], w=[comb])
            self.CP("pool", maskb16.ap[:, :], lg.ap[:, 64:96], r=[lg], w=[maskb16])
            self.MM(pr.ap[:, 64:96], tsub.ap[:, :], maskb16.ap[:, :], r=[tsub, maskb16], w=[pr])
            self.MM(pr.ap[:, 128:160], self.onesb, maskb16.ap[:, :], r=[self.cstb, maskb16], w=[pr])
            self.TT("dve", rank_all.ap[:, xi, :], pr.ap[:, 64:96], cb, ALU.add, r=[pr, rt_], w=[rank_all])
            self.TT("dve", cb, cb, pr.ap[:, 128:160], ALU.add, r=[pr, rt_], w=[rt_])
            self.CP("dve", lgall.ap[:, xi, :], lg.ap[:, 0:40], r=[lg], w=[lgall])
            self.CP("pool", xn2tm.ap[:, xi, :], t_.ap[:, :], r=[t_], w=[xn2tm])
        meta = S.alloc("meta", [32 + 32 + 128 + 512 + 512], F32)
        self.meta = meta
        nt = meta.ap[:, 0:32]
        nt_i = meta.ap[:, 32:64].bitcast(I32)
        ntT = meta.ap[:, 64:192]
        sidxf = meta.ap[:, 192:704].rearrange("p (a b) -> p a b", b=16)
        sidx_i = meta.ap[:, 704:1216].bitcast(I32).rearrange("p (a b) -> p a b", b=16)
        self.nt_i, self.sidx_i = nt_i, sidx_i
        self.MS("pool", nt, 0.0, w=[meta])
        for m in range(NSTEP):
            self.STT(nt, cb, float(SG * m), nt, ALU.is_gt, ALU.add, r=[rt_, meta], w=[meta])
        self.CP("dve", nt_i, nt, r=[meta], w=[meta])
        pq = P[4]
        self.TR(pq.ap[0:NE, 0:128], nt, identf, r=[meta, cst], w=[pq])
        self.CP("act", ntT[0:NE, :], pq.ap[0:NE, 0:128], r=[pq], w=[meta])
        self.MM(pq.ap[:, 128:160], ntT[0:NE, :], Umat[0:NE, :], r=[meta, rt_], w=[pq])
        base = rt_.ap[:, 176:208]
        self.TS("dve", base, pq.ap[:, 128:160], float(SG), None, ALU.mult, r=[pq, rt_], w=[rt_])
        for e in range(NE):
            self.TS("dve", sidxf[:, e, :], IO, base[:, e:e + 1], None, ALU.add, r=[rt_, meta], w=[meta])
        self.CP("dve", sidx_i, sidxf, r=[meta], w=[meta])
        S.release(mT, wo, rw, hx2f, lg, junk, self.zt, tsub, maskb16, *tt)
        oh4 = S.alloc("oh4", [16, 4, NE], F32)
        pr4 = S.alloc("pr4", [16, 4, NE], F32)
        pf4 = S.alloc("pf4", [16, 4], F32)
        shp = [128, 16, 4, NE]
        self.TT("dve", rank_all.ap[:, :, :], rank_all.ap[:, :, :], base.unsqueeze(1).to_broadcast([128, 16, NE]), ALU.add,
                r=[rank_all, rt_], w=[rank_all])
        self.TT("dve", oh4.ap[:, :, :, :], lgall.ap[:, :, 0:NE].unsqueeze(2).to_broadcast(shp),
                lgall.ap[:, :, 32:36].unsqueeze(3).to_broadcast(shp), ALU.is_equal, r=[lgall], w=[oh4])
        self.TT("dve", pr4.ap[:, :, :, :], oh4.ap[:, :, :, :], rank_all.ap[:, :, :].unsqueeze(2).to_broadcast(shp), ALU.mult,
                r=[oh4, rank_all], w=[pr4])
        self.S.op("dve", lambda e, o=pf4.ap[:, :, :], a=pr4.ap[:, :, :, :]: e.reduce_sum(o, a, axis=AX.X), [pr4], [pf4])
        self.CP("dve", posi_v[:, :, :], pf4.ap[:, :, :], r=[pf4], w=[posi])
        self.TT("pool", pr4.ap[:, :, :, :], oh4.ap[:, :, :, :], comb.ap[:, :, :].unsqueeze(2).to_broadcast(shp), ALU.mult,
                r=[oh4, comb, pf4], w=[pr4])
        self.S.op("dve", lambda e, o=wk.ap[:, :, :], a=pr4.ap[:, :, :, :]: e.reduce_sum(o, a, axis=AX.X), [pr4], [wk])
        for xi in range(16):
            for k in range(4):
                self.S.dma_fn("pool", lambda e, o=self.xs_d[:, :], off=posi_v[:, xi, k:k + 1], i_=xn2tm.ap[:, xi, :]:
                              e.indirect_dma_start(out=o, out_offset=bass.IndirectOffsetOnAxis(ap=off, axis=0), in_=i_, in_offset=None,
                                                   bounds_check=self.S.bc_reg, oob_is_err=False),
                              reads=[xsz, xn2tm, posi], writes=[xsd], sem_buf=xn2tm)
        S.release(oh4, pr4, pf4, rank_all, lgall)
        self.x1_bufs = xt
        self.rt_ = rt_
        self.xn2tm = xn2tm

    def phase_moe(self):
        S, P = self.S, self.P
        hx2T, comb, stat, cst = self.hx2T, self.comb, self.stat, self.cst
        identf, Gb = self.identf, self.Gb
        xt = self.x1_bufs
        combT = S.alloc("combT", [T], F32)
        for xi in range(16):
            pb = P[xi % 2]
            self.TR(pb.ap[0:NE, 0:128], comb.ap[:, xi, :], identf, r=[comb, cst], w=[pb])
            self.CP("act", combT.ap[0:NE, xi * 128:(xi + 1) * 128], pb.ap[0:NE, 0:128], r=[pb], w=[combT])
        S.release(comb)
        bias = S.alloc("ebias", [512 + D], F32)
        self.DMA("sp", bias.ap[:, 0:256], self.bgT, w=[bias])
        self.DMA("sp", bias.ap[:, 256:512], self.buT, w=[bias])
        self.DMA("sp", bias.ap[0:NE, 512:512 + D], self.b_down, w=[bias])
        self.TS("dve", bias.ap[:, 256:512], bias.ap[:, 256:512], 1.0, None, ALU.add, r=[bias], w=[bias])
        wg = S.alloc("wg", [KC, D], BF16)
        wu = S.alloc("wu", [KC, D], BF16)
        wd = S.alloc("wd", [KC, D], BF16)
        act = S.alloc("act", [KC, 512], BF16)
        oacc = S.alloc("oacc", [KC, 1024], F32)
        sel = [S.alloc("sel", [128], F32) for _ in range(2)]
        cwb = S.alloc("cw", [512], F32)
        gb_ = [S.alloc("g", [512], F32) for _ in range(2)]
        sb_ = [S.alloc("s", [512], F32) for _ in range(1)]
        ub_ = [S.alloc("u", [512], F32) for _ in range(2)]
        junk = S.alloc("junk4", [512], BF16)
        selv = self.c_sel
        out_bufs = []
        for half in range(2):
            t0 = half * 1024
            for dc in range(KC):
                for tb in range(2):
                    pb = P[4 + (dc * 2 + tb) % 2]
                    self.MM(pb.ap[:, :], bias.ap[0:NE, 512 + dc * 128:512 + (dc + 1) * 128], combT.ap[0:NE, t0 + tb * 512:t0 + (tb + 1) * 512],
                            r=[bias, combT], w=[pb])
                    self.CP("act", oacc.ap[:, dc, tb * 512:(tb + 1) * 512], pb.ap[:, :], r=[pb], w=[oacc])
            for e in range(NE):
                self.load_w(wg.ap[:, :, :], wg, self.w_gate[e], D, D)
                self.load_w(wu.ap[:, :, :], wu, self.w_up[e], D, D)
                self.load_w(wd.ap[:, :, :], wd, self.w_down[e], D, D)
                se = sel[e % 2]
                self.DMA("sp", se.ap[0:NE, :], selv[:, e * 128:(e + 1) * 128], w=[se])
                for tb in range(2):
                    tk = t0 + tb * 512
                    self.MM(P[6].ap[:, :], se.ap[0:NE, :], combT.ap[0:NE, tk:tk + 512], r=[se, combT], w=[P[6]])
                    self.CP("act", cwb.ap[:, :], P[6].ap[:, :], r=[P[6]], w=[cwb])
                    for f in range(KC):
                        pg, pu = P[f % 2], P[2 + f % 2]
                        g_, s_, u_ = gb_[f % 2], sb_[0], ub_[f % 2]
                        for kc in range(KC):
                            self.MM(pg.ap[:, :], wg.ap[:, kc, f * 128:(f + 1) * 128], hx2T.ap[:, kc, tk:tk + 512],
                                    start=(kc == 0), stop=(kc == KC - 1), r=[wg, hx2T], w=[pg])
                        for kc in range(KC):
                            self.MM(pu.ap[:, :], wu.ap[:, kc, f * 128:(f + 1) * 128], hx2T.ap[:, kc, tk:tk + 512],
                                    start=(kc == 0), stop=(kc == KC - 1), r=[wu, hx2T], w=[pu])
                        bcol = e * 8 + f
                        self.TS("dve", g_.ap[:, :], pg.ap[:, :], bias.ap[:, bcol:bcol + 1], 7.0, ALU.add, ALU.min, r=[pg, bias], w=[g_])
                        self.ACT(s_.ap[:, :], g_.ap[:, :], AF.Sigmoid, r=[g_], w=[s_], scale=1.702)
                        self.TS("dve", u_.ap[:, :], pu.ap[:, :], bias.ap[:, 256 + bcol:256 + bcol + 1], -6.0, ALU.add, ALU.max,
                                r=[pu, bias], w=[u_])
                        self.STT(u_.ap[:, :], u_.ap[:, :], 8.0, cwb.ap[:, :], ALU.min, ALU.mult, r=[u_, cwb], w=[u_])
                        self.TT("pool", g_.ap[:, :], g_.ap[:, :], s_.ap[:, :], ALU.mult, r=[g_, s_], w=[g_])
                        self.TT("pool", act.ap[:, f, :], g_.ap[:, :], u_.ap[:, :], ALU.mult, r=[g_, u_], w=[act])
                    for dc in range(KC):
                        pd = P[4 + dc % 2]
                        for f in range(KC):
                            self.MM(pd.ap[:, :], wd.ap[:, f, dc * 128:(dc + 1) * 128], act.ap[:, f, :],
                                    start=(f == 0), stop=(f == KC - 1), r=[wd, act], w=[pd])
                        self.TT("dve", oacc.ap[:, dc, tb * 512:(tb + 1) * 512], oacc.ap[:, dc, tb * 512:(tb + 1) * 512], pd.ap[:, :],
                                ALU.add, r=[oacc, pd], w=[oacc])
            for j in range(8):
                xi = half * 8 + j
                x_ = xt[xi % 2]
                self.DMA("sp", x_.ap[:, :], self.x1d[xi * 128:(xi + 1) * 128, :], w=[x_])
                pa, pb = P[(j % 2) * 2], P[(j % 2) * 2 + 1]
                for dc in range(KC):
                    pp = pa if dc < 4 else pb
                    self.TR(pp.ap[:, (dc % 4) * 128:(dc % 4 + 1) * 128], oacc.ap[:, dc, j * 128:(j + 1) * 128], identf,
                            r=[oacc, cst], w=[pp])
                sc = 40 + (j % 2) * 4
                for hf, pp in enumerate((pa, pb)):
                    self.ACT(junk.ap[:, :], pp.ap[:, :], AF.Square, r=[pp], w=[junk, stat], accum_out=stat.ap[:, sc + hf:sc + hf + 1])
                self.TT("dve", stat.ap[:, sc:sc + 1], stat.ap[:, sc:sc + 1], stat.ap[:, sc + 1:sc + 2], ALU.add, r=[stat], w=[stat])
                self.rstd_from_ss(stat.ap[:, sc:sc + 1], D, stat, stat.ap[:, sc + 2:sc + 3])
                for hf, pp in enumerate((pa, pb)):
                    self.STT(cwb.ap[:, :], pp.ap[:, :], stat.ap[:, sc:sc + 1], Gb.ap[:, 1, hf * 512:(hf + 1) * 512], ALU.mult, ALU.mult,
                             r=[pp, stat, Gb], w=[cwb])
                    self.TT("pool", x_.ap[:, hf * 512:(hf + 1) * 512], x_.ap[:, hf * 512:(hf + 1) * 512], cwb.ap[:, :], ALU.add,
                            r=[x_, cwb], w=[x_])
                self.DMA("pool", self.out[xi * 128:(xi + 1) * 128, :], x_.ap[:, :], r=[x_], sem=x_)
        self.final_bufs = list(xt)

    def phase_moe_sparse(self):
        S, P = self.S, self.P
        stat, cst, cstb, Gb = self.stat, self.cst, self.cstb, self.Gb
        identb, onesf, A2, modb = self.identb, self.onesf, self.A2, self.modb
        xsd, ysd, wk, posi_v, posi = self.xsd, self.ysd, self.wk, self.posi_v, self.posi
        meta, nt_i, sidx_i = self.meta, self.nt_i, self.sidx_i
        S.release(self.comb, self.xn2tm)
        bias = S.alloc("ebias", [512], F32)
        self.DMA("sp", bias.ap[:, 0:256], self.bgT, w=[bias])
        self.DMA("sp", bias.ap[:, 256:512], self.buT, w=[bias])
        self.TS("dve", bias.ap[:, 256:512], bias.ap[:, 256:512], 1.0, None, ALU.add, r=[bias], w=[bias])
        W = [[S.alloc("w%d" % k, [KC, D], BF16) for k in range(3)] for _ in range(2)]
        xg0 = [S.alloc("xg0", [2, D], BF16) for _ in range(2)]
        xgn = [S.alloc("xgn", [2, D], BF16) for _ in range(2)]
        xg = xg0 + xgn
        XT = [S.alloc("XT", [KC, SG], BF16) for _ in range(2)]
        acts = [S.alloc("act", [SG], BF16) for _ in range(KC)]
        gb_ = [S.alloc("g", [SG], F32) for _ in range(2)]
        sb_ = [S.alloc("s", [SG], F32) for _ in range(2)]
        ub_ = [S.alloc("u", [SG], F32) for _ in range(2)]
        ys = [S.alloc("ys", [D], F32) for _ in range(2)]
        bdr = [S.alloc("bdr", [D], F32) for _ in range(1)]
        bdrb = [S.alloc("bdrb", [D], BF16) for _ in range(2)]
        srcs = (self.w_gate, self.w_up, self.w_down)

        def prep(x_, xt_):
            for bk in range(2):
                pt = P[4 + bk]
                ptb = pt.ap.bitcast(BF16)
                for k4 in range(4):
                    kc = bk * 4 + k4
                    for h in range(2):
                        self.TR(ptb[:, k4 * SG + h * 128:k4 * SG + (h + 1) * 128], x_.ap[:, h, kc * 128:(kc + 1) * 128], identb,
                                r=[x_, cstb], w=[pt])
                for k4 in range(4):
                    kc = bk * 4 + k4
                    if k4 % 2 == 0:
                        self.TS("dve", xt_.ap[:, kc, :], ptb[:, k4 * SG:(k4 + 1) * SG], A2[:, kc:kc + 1], modb.ap[:, 24 + kc, 0:1],
                                ALU.mult, ALU.add, r=[pt, self.vec, modb], w=[xt_])
                    else:
                        self.ACT(xt_.ap[:, kc, :], ptb[:, k4 * SG:(k4 + 1) * SG], AF.Identity, r=[pt, self.vec, modb], w=[xt_],
                                 scale=A2[:, kc:kc + 1], bias=modb.ap[:, 24 + kc, 0:1])

        def loadw(e):
            for k in range(3):
                self.load_w(W[e % 2][k].ap[:, :, :], W[e % 2][k], srcs[k][e], D, D)
        def gather(buf, e, j):
            for h in range(2):
                self.S.dma_fn("pool", lambda en, o=buf.ap[:, h, :], off=sidx_i[:, e, 2 * j + h:2 * j + h + 1], i_=self.xs_d[:, :]:
                              en.indirect_dma_start(out=o, out_offset=None, in_=i_, in_offset=bass.IndirectOffsetOnAxis(ap=off, axis=0),
                                                    bounds_check=self.S.bc_reg, oob_is_err=False),
                              reads=[xsd, meta], writes=[buf], sem_buf=buf)
        gather(xg0[0], 0, 0)
        loadw(0)
        nst = 0
        ngs = 0
        for e in range(NE):
            gather(xgn[1], e, 1)
            if e + 1 < NE:
                gather(xg0[(e + 1) % 2], e + 1, 0)
                loadw(e + 1)
            wg, wu, wd = W[e % 2]
            br = bdr[0]
            self.DMA("sp", br.ap[0:1, :], self.b_down[e:e + 1, :], w=[br])
            brb = bdrb[e % 2]
            self.CP("act", brb.ap[0:1, :], br.ap[0:1, :], r=[br], w=[brb])
            prep(xg0[e % 2], XT[ngs % 2])
            S.regload(meta, nt_i[0:1, e:e + 1])
            for j in range(NSTEP):
                S.begin_group(j)
                xt_ = XT[ngs % 2]
                ngs += 1
                if j + 2 < NSTEP:
                    gather(xgn[j % 2], e, j + 2)
                for f in range(KC):
                    pg, pu = P[f % 2], P[2 + f % 2]
                    g_, s_, u_ = gb_[f % 2], sb_[f % 2], ub_[f % 2]
                    for kc in range(KC):
                        self.MM(pg.ap[:, 0:SG], wg.ap[:, kc, f * 128:(f + 1) * 128], xt_.ap[:, kc, :],
                                start=(kc == 0), stop=(kc == KC - 1), r=[wg, xt_], w=[pg])
                    for kc in range(KC):
                        self.MM(pu.ap[:, 0:SG], wu.ap[:, kc, f * 128:(f + 1) * 128], xt_.ap[:, kc, :],
                                start=(kc == 0), stop=(kc == KC - 1), r=[wu, xt_], w=[pu])
                    bcol = e * 8 + f
                    self.TS("dve", g_.ap[:, :], pg.ap[:, 0:SG], bias.ap[:, bcol:bcol + 1], 7.0, ALU.add, ALU.min, r=[pg, bias], w=[g_])
                    self.ACT(s_.ap[:, :], g_.ap[:, :], AF.Sigmoid, r=[g_], w=[s_], scale=1.702)
                    self.TS("dve", u_.ap[:, :], pu.ap[:, 0:SG], bias.ap[:, 256 + bcol:256 + bcol + 1], -6.0, ALU.add, ALU.max,
                            r=[pu, bias], w=[u_])
                    self.TT("pool", g_.ap[:, :], g_.ap[:, :], s_.ap[:, :], ALU.mult, r=[g_, s_], w=[g_])
                    self.STT(acts[f].ap[:, :], u_.ap[:, :], 8.0, g_.ap[:, :], ALU.min, ALU.mult, r=[u_, g_], w=[acts[f]])
                if j + 1 < NSTEP:
                    prep(xgn[(j + 1) % 2], XT[ngs % 2])
                for h in range(2):
                    y_ = ys[nst % 2]
                    nst += 1
                    for hf in range(2):
                        pd = P[6 + hf]
                        for f in range(KC):
                            self.MM(pd.ap[:, :], acts[f].ap[:, h * 128:(h + 1) * 128], wd.ap[:, f, hf * 512:(hf + 1) * 512],
                                    start=(f == 0), stop=False, r=[acts[f], wd], w=[pd])
                        self.MM(pd.ap[:, :], self.onesb[0:1, :], brb.ap[0:1, hf * 512:(hf + 1) * 512], start=False, stop=True,
                                r=[cstb, brb], w=[pd])
                        self.CP("act" if hf == 0 else "dve", y_.ap[:, hf * 512:(hf + 1) * 512], pd.ap[:, :], r=[pd], w=[y_])
                    self.S.dma_fn("pool", lambda en, o=self.ys_d[:, :], off=sidx_i[:, e, 2 * j + h:2 * j + h + 1], i_=y_.ap[:, :]:
                                  en.indirect_dma_start(out=o, out_offset=bass.IndirectOffsetOnAxis(ap=off, axis=0), in_=i_, in_offset=None,
                                                        bounds_check=self.S.bc_reg, oob_is_err=False),
                                  reads=[y_, meta], writes=[ysd], sem_buf=y_)
                S.end_group()
        S.release(*W[0], *W[1], *xg, *XT, *acts, *gb_, *sb_, *ub_, *bdr, *bdrb, bias)
        yk = [S.alloc("yk", [D], F32) for _ in range(8)]
        acc = [S.alloc("acc", [D], F32) for _ in range(2)]
        junk = S.alloc("junk5", [D], BF16)
        xt = self.x1_bufs
        outb = []
        for xi in range(16):
            x_ = xt[xi % 2]
            a_ = acc[xi % 2]
            self.DMA("sp", x_.ap[:, :], self.x1d[xi * 128:(xi + 1) * 128, :], w=[x_])
            for k in range(4):
                y_ = yk[(xi % 2) * 4 + k]
                self.S.dma_fn("pool", lambda e, o=y_.ap[:, :], off=posi_v[:, xi, k:k + 1], i_=self.ys_d[:, :]:
                              e.indirect_dma_start(out=o, out_offset=None, in_=i_, in_offset=bass.IndirectOffsetOnAxis(ap=off, axis=0),
                                                   bounds_check=self.S.bc_reg, oob_is_err=False),
                              reads=[ysd, posi], writes=[y_], sem_buf=y_)
                if k == 0:
                    self.TS("dve", a_.ap[:, :], y_.ap[:, :], wk.ap[:, xi, 0:1], None, ALU.mult, r=[y_, wk], w=[a_])
                else:
                    self.STT(a_.ap[:, :], y_.ap[:, :], wk.ap[:, xi, k:k + 1], a_.ap[:, :], ALU.mult, ALU.add, r=[y_, wk, a_], w=[a_])
            sc = 40 + (xi % 2) * 4
            self.ACT(junk.ap[:, :], a_.ap[:, :], AF.Square, r=[a_], w=[junk, stat], accum_out=stat.ap[:, sc:sc + 1])
            self.rstd_from_ss(stat.ap[:, sc:sc + 1], D, stat, stat.ap[:, sc + 2:sc + 3])
            if self.dbg is not None and xi in (0, 9):
                self.dump((0 if xi == 0 else 1) * 1024, a_, a_.ap[:, :], 1024)
                self.dump(2048 + (0 if xi == 0 else 1) * 8, wk, wk.ap[:, xi, :], 4)
                self.dump(2048 + 16 + (0 if xi == 0 else 1) * 8, posi, posi.ap[:, xi, :], 4)
            self.STT(a_.ap[:, :], a_.ap[:, :], stat.ap[:, sc:sc + 1], Gb.ap[:, 1, :], ALU.mult, ALU.mult, r=[a_, stat, Gb], w=[a_])
            self.TT("pool", x_.ap[:, :], x_.ap[:, :], a_.ap[:, :], ALU.add, r=[x_, a_], w=[x_])
            self.DMA("sp", self.out[xi * 128:(xi + 1) * 128, :], x_.ap[:, :], r=[x_], sem=x_)
        self.final_bufs = list(xt)

    def finish(self):
        S = self.S
        if self.stage < 90:
            z = S.alloc("zout", [128], F32)
            self.MS("pool", z.ap[:, :], 0.0, w=[z])
            self.DMA("pool", self.out[0:128, 0:128], z.ap[:, :], r=[z], sem=z)
            self.dbg_bufs.append(z)
        S.emit(final_bufs=self.dbg_bufs + getattr(self, "final_bufs", []))


def _host_inputs(inputs, b):
    f = lambda a: np.ascontiguousarray(np.asarray(a, dtype=np.float32))
    cst = _consts()
    m = {}
    m["x"] = f(inputs["x"][b])
    m["ctx"] = f(inputs["ctx"][b])
    cc = np.stack([_kc_layout(f(inputs["c"][b])), _kc_layout(f(inputs["c_ctx"]))], axis=-1)
    m["cc"] = f(cc.reshape(128, 16))
    m["w_mod"] = f(inputs["w_mod"][0])
    m["bmodT"] = _kc_layout(f(inputs["b_mod"][0]))
    m["bmod_row"] = f(inputs["b_mod"][0]).reshape(1, -1)
    gT = np.stack([_kc_layout(f(inputs[k][0])) for k in ("g_pre_mix", "g_post_mix", "g_pre_ffn", "g_post_ffn")], axis=1)
    m["gT"] = f(gT.reshape(128, 32))
    m["gpost_row"] = f(np.concatenate([f(inputs["g_post_mix"][0]), f(inputs["g_post_ffn"][0])]).reshape(1, -1))
    m["w_in"] = f(inputs["w_in"][0])
    wa2 = np.zeros((32, 512), np.float32)
    wa2[0:16, 0:256] = inputs["gla_w_a2_f"][0]
    wa2[16:32, 256:512] = inputs["gla_w_a2_b"][0]
    m["wa2"] = wa2
    m["ba_row"] = f(np.concatenate([f(inputs["gla_b_a_f"][0]), f(inputs["gla_b_a_b"][0])]).reshape(1, 512))
    m["gnb"] = f(np.tile(f(inputs["gla_g_norm"][0])[None, :], (128, 4)))
    m["gq"] = _kc_layout(f(inputs["mla_g_q"][0]))
    m["gkv"] = _kc_layout(f(inputs["mla_g_kv"][0]))
    m["w_uq"] = f(inputs["mla_w_uq"][0])
    m["w_uk"] = f(inputs["mla_w_uk"][0])
    m["w_uv"] = f(inputs["mla_w_uv"][0])
    m["w_br_gla"] = f(inputs["w_br_gla"][0])
    m["w_br_mla"] = f(inputs["w_br_mla"][0])
    m["w_out"] = f(inputs["w_out"][0])
    m["router_w"] = f(inputs["router_w"][0])
    m["router_b"] = f(inputs["router_b"][0]).reshape(1, NE)
    m["w_gate"] = f(inputs["w_gate"][0])
    m["w_up"] = f(inputs["w_up"][0])
    m["w_down"] = f(inputs["w_down"][0])
    bg = f(inputs["b_gate"][0]).reshape(NE, 8, 128).transpose(2, 0, 1)
    bu = f(inputs["b_up"][0]).reshape(NE, 8, 128).transpose(2, 0, 1)
    m["bgT"] = f(bg.reshape(128, NE * 8))
    m["buT"] = f(bu.reshape(128, NE * 8))
    m["b_down"] = f(inputs["b_down"][0])
    m["c_tri"] = f(cst["tri"].reshape(128, 512))
    m["c_mask"] = f(cst["mask"].reshape(128, 1024))
    m["c_ident"] = cst["ident"]
    m["c_ropet"] = f(cst["ropet"].reshape(128, 2 * NTOK))
    m["c_e96"] = cst["e96"]
    m["c_sel"] = cst["sel"]
    m["c_rt"] = cst["rt"]
    return m


_SHARED = ("w_mod", "bmodT", "bmod_row", "gT", "gpost_row", "w_in", "wa2", "ba_row", "gnb", "gq", "gkv", "w_uq", "w_uk",
           "w_uv", "w_br_gla", "w_br_mla", "w_out", "router_w", "router_b", "w_gate", "w_up", "w_down", "bgT", "buT",
           "b_down", "c_tri", "c_mask", "c_ident", "c_ropet", "c_e96", "c_sel", "c_rt")


def kernel(**inputs):
    k = K(stage=99)
    m0 = _host_inputs(inputs, 0)
    in_maps = [m0]
    for b in range(1, 8):
        mb = dict(m0)
        mb["x"] = np.ascontiguousarray(np.asarray(inputs["x"][b], dtype=np.float32))
        mb["ctx"] = np.ascontiguousarray(np.asarray(inputs["ctx"][b], dtype=np.float32))
        cc = np.stack([_kc_layout(np.asarray(inputs["c"][b], np.float32)),
                       _kc_layout(np.asarray(inputs["c_ctx"], np.float32))], axis=-1)
        mb["cc"] = np.ascontiguousarray(cc.reshape(128, 16))
        in_maps.append(mb)
    res = run_bass_kernel_spmd(k.nc, in_maps, core_ids=list(range(8)))
    return np.stack([np.asarray(r["out"], dtype=np.float32) for r in res.results], axis=0)
```

```python
import numpy as np
from contextlib import ExitStack
import concourse.bass as bass
import concourse.mybir as mybir
from concourse.bass_utils import run_bass_kernel_spmd

F32 = mybir.dt.float32
BF16 = mybir.dt.bfloat16
I32 = mybir.dt.int32
AF = mybir.ActivationFunctionType
ALU = mybir.AluOpType
AX = mybir.AxisListType
ENGS = ("pe", "act", "dve", "pool", "sp")

T = 2048
TC = 256
NT = 18
NTOK = 2304
D = 1024
KC = 8
D_IN = 4032
EPS = 1e-6
NE = 32
MLA_SCALE = 96 ** -0.5
SG = 256
NSTEP = 8
NSLOT = T * 4 + NE * SG
MOE_MODE = "sparse"


class Buf:
    __slots__ = ("name", "writers", "readers", "dma_sem", "dma_cnt", "ap", "inherit", "off", "words")

    def __init__(self, name, ap=None):
        self.name = name
        self.ap = ap
        self.writers = []
        self.readers = []
        self.inherit = []
        self.off = None
        self.dma_sem = None
        self.dma_cnt = 0


class Op:
    __slots__ = ("eng", "fn", "idx", "deps", "marked", "dma_buf", "dma_val", "mark_no", "grp")

    def __init__(self, eng, fn):
        self.eng = eng
        self.fn = fn
        self.grp = None
        self.deps = []
        self.marked = False
        self.dma_buf = None
        self.dma_val = 0
        self.mark_no = 0


class Sched:
    def __init__(self, nc, arena_words):
        self.nc = nc
        self.ops = {e: [] for e in ENGS}
        self.bufs = []
        self.arena = nc.alloc_sbuf_tensor("arena", [128, arena_words], F32)
        self.free = [(0, arena_words)]
        self.dead = []
        self.nbuf = 0
        self.cur_grp = None
        self.ngrp = 0

    def alloc(self, name, free_shape, dt=F32):
        n = 1
        for s in free_shape:
            n *= s
        words = (n * (2 if dt == BF16 else 4) + 3) // 4
        words = (words + 7) // 8 * 8
        off = None
        for i, (o, sz) in enumerate(self.free):
            if sz >= words:
                off = o
                if sz == words:
                    self.free.pop(i)
                else:
                    self.free[i] = (o + words, sz - words)
                break
        if off is None:
            raise RuntimeError("arena full allocating %s (%d words); free=%s" % (name, words, self.free))
        v = self.arena[:, off:off + words]
        if dt == BF16:
            v = v.bitcast(BF16)
        v = v[:, 0:n]
        if len(free_shape) == 2:
            v = v.rearrange("p (a b) -> p a b", b=free_shape[1])
        elif len(free_shape) == 3:
            v = v.rearrange("p (a b c) -> p a b c", b=free_shape[1], c=free_shape[2])
        self.nbuf += 1
        b = Buf("%s_%d" % (name, self.nbuf), v)
        b.off, b.words = off, words
        for (o, w, toks) in self.dead:
            if o < off + words and off < o + w:
                b.inherit.extend(toks)
        self.bufs.append(b)
        return b

    def release(self, *bufs):
        for b in bufs:
            toks = [("op", t[1], "raw") if t[0] == "op" else t for t in (b.writers + b.readers + b.inherit)]
            self.dead.append((b.off, b.words, toks))
            self.free.append((b.off, b.words))
        self.free.sort()
        m = []
        for o, w in self.free:
            if m and m[-1][0] + m[-1][1] == o:
                m[-1] = (m[-1][0], m[-1][1] + w)
            else:
                m.append((o, w))
        self.free = m

    def psum(self, name):
        t = self.nc.alloc_psum_tensor(name, [128, 512], F32)
        b = Buf(name, t[:, :])
        self.bufs.append(b)
        return b

    def virt(self, name):
        b = Buf(name, None)
        self.bufs.append(b)
        return b

    def _add(self, eng, fn, reads, writes, dma_buf=None):
        op = Op(eng, fn)
        op.idx = len(self.ops[eng])
        op.grp = self.cur_grp
        is_dma = dma_buf is not None
        deps = []
        for b in reads:
            deps.extend(b.writers)
            deps.extend(b.inherit)
            if b.off is None:
                deps.extend([("op", t[1], "raw") for t in b.readers if t[0] == "op" and t[1].eng != eng])
        for b in writes:
            deps.extend(b.inherit)
            deps.extend(b.readers)
            if is_dma and not b.readers and b.writers and all(w[0] == "dma" for w in b.writers):
                pass
            else:
                deps.extend(b.writers)
        fl = []
        for d in deps:
            if d[0] == "op":
                o = d[1]
                if o.eng == eng and not is_dma:
                    if eng == "pe" or eng == "sp":
                        continue
                    if d[2] != "raw":
                        continue
            fl.append(d)
        op.deps = fl
        if is_dma:
            dma_buf.dma_cnt += 16
            op.dma_buf = dma_buf
            op.dma_val = dma_buf.dma_cnt
            tok_w = ("dma", dma_buf, op.dma_val)
            tok_r = tok_w
        else:
            tok_w = ("op", op, "raw")
            tok_r = ("op", op, "war")
        for b in reads:
            b.readers.append(tok_r)
            if len(b.readers) > 64:
                last = {}
                keep = []
                for t in b.readers:
                    if t[0] == "op":
                        last[t[1].eng] = t
                    else:
                        keep.append(t)
                b.readers = keep[-32:] + list(last.values())
        for b in writes:
            if is_dma and not b.readers and b.writers and all(w[0] == "dma" for w in b.writers):
                b.writers.append(tok_w)
            else:
                b.writers = [tok_w]
                b.readers = []
        self.ops[eng].append(op)
        return op

    def op(self, eng, fn, reads=(), writes=()):
        return self._add(eng, fn, list(reads), list(writes))

    def dma(self, eng, out_ap, in_ap, reads=(), writes=(), sem_buf=None, **kw):
        if sem_buf is None:
            sem_buf = (list(writes) + list(reads))[0]

        def fn(e, out_ap=out_ap, in_ap=in_ap, kw=kw):
            return e.dma_start(out=out_ap, in_=in_ap, **kw)
        return self._add(eng, fn, list(reads), list(writes), dma_buf=sem_buf)

    def begin_group(self, thr):
        self.ngrp += 1
        self.cur_grp = (self.ngrp, thr)

    def end_group(self):
        self.cur_grp = None

    def regload(self, src_buf, ap):
        for e in ENGS:
            self._add(e, ("regload", ap), [src_buf], [])

    def dma_fn(self, eng, fn, reads=(), writes=(), sem_buf=None):
        return self._add(eng, fn, list(reads), list(writes), dma_buf=sem_buf)

    def emit(self, final_bufs=()):
        nc = self.nc
        for e in ENGS:
            for op in self.ops[e]:
                for d in op.deps:
                    if d[0] == "op":
                        d[1].marked = True
        for e in ENGS:
            n = 0
            for op in self.ops[e]:
                if op.marked:
                    n += 1
                    op.mark_no = n
        with ExitStack() as st:
            esem = {e: st.enter_context(nc.semaphore("s_" + e)) for e in ENGS}
            for b in self.bufs:
                if b.dma_cnt > 0:
                    b.dma_sem = st.enter_context(nc.semaphore("d_" + b.name))
            self.bc_reg = st.enter_context(nc.gpsimd.register("rbc"))
            engobj = {"pe": nc.tensor, "act": nc.scalar, "dve": nc.vector, "pool": nc.gpsimd, "sp": nc.sync}
            self.cregs = {e: st.enter_context(engobj[e].register("creg_" + e)) for e in ENGS}
            print("semaphores used:", 5 + sum(1 for b in self.bufs if b.dma_cnt > 0))
            block = st.enter_context(nc.Block())
            handles = {"pe": block.tensor, "act": block.scalar, "dve": block.vector,
                       "pool": block.gpsimd, "sp": block.sync}
            stats = {}
            for e in ENGS:
                ops = self.ops[e]

                def body(eng, ops=ops, e=e):
                    waited = {}
                    nw = [0]
                    if e == "pool":
                        eng.reg_mov(self.bc_reg, NSLOT - 1)
                    creg = self.cregs[e]

                    def emit_op(op, wt):
                        need = {}
                        for d in op.deps:
                            if d[0] == "op":
                                key = ("e", d[1].eng)
                                val = d[1].mark_no
                            else:
                                key = ("d", d[1].name)
                                val = d[2]
                            if val > need.get(key, (0, None))[0]:
                                need[key] = (val, d)
                        for key, (val, d) in need.items():
                            if wt.get(key, 0) >= val:
                                continue
                            wt[key] = val
                            sem = esem[d[1].eng] if d[0] == "op" else d[1].dma_sem
                            eng.wait_ge(sem, val)
                            nw[0] += 1
                        if isinstance(op.fn, tuple):
                            eng.reg_load(creg, op.fn[1])
                            return
                        ins = op.fn(eng)
                        if op.dma_buf is not None:
                            ins.then_inc(op.dma_buf.dma_sem, 16)
                        elif op.marked:
                            ins.then_inc(esem[e], 1)
                    def comp_of(gops):
                        comp = []
                        marked = [o for o in gops if o.marked and o.dma_buf is None]
                        if marked:
                            comp.append((esem[e], marked[0].mark_no - 1, len(marked)))
                        dm = {}
                        for o in gops:
                            if o.dma_buf is not None:
                                if o.dma_buf.name not in dm:
                                    dm[o.dma_buf.name] = [o.dma_buf.dma_sem, o.dma_val - 16, 0]
                                dm[o.dma_buf.name][2] += 16
                        comp.extend(tuple(v) for v in dm.values())
                        return comp

                    def chain(runs, k, wt):
                        rest = [o for r_ in runs[k:] for o in r_]
                        thr = runs[k][0].grp[1]
                        with eng.If_lt(creg, thr + 1):
                            for sem, before, delta in comp_of(rest):
                                if before > 0:
                                    eng.wait_ge(sem, before)
                                eng.sem_inc(sem, delta)
                        with eng.Else():
                            for o in runs[k]:
                                emit_op(o, wt)
                            if k + 1 < len(runs):
                                chain(runs, k + 1, wt)
                    i = 0
                    n = len(ops)
                    while i < n:
                        op = ops[i]
                        if op.grp is None:
                            emit_op(op, waited)
                            i += 1
                            continue
                        runs = []
                        j = i
                        while j < n and ops[j].grp is not None and (not runs or ops[j].grp[1] > runs[-1][0].grp[1] or ops[j].grp is runs[-1][0].grp):
                            if runs and ops[j].grp is runs[-1][0].grp:
                                runs[-1].append(ops[j])
                            else:
                                runs.append([ops[j]])
                            j += 1
                        chain(runs, 0, dict(waited))
                        i = j
                    if e == "sp":
                        for b in final_bufs:
                            if b.dma_cnt:
                                eng.wait_ge(b.dma_sem, b.dma_cnt)
                    stats[e] = (len(ops), nw[0])
                handles[e](body)
            self.stats = stats


def _consts():
    c = {}
    idx = np.arange(128)
    same = (idx[:, None] // 64) == (idx[None, :] // 64)
    s = idx[:, None]
    t = idx[None, :]
    tri = np.zeros((128, 4, 128), np.float32)
    tri[:, 0] = same & (s <= t)
    tri[:, 1] = same & (s >= t)
    tri[:, 2] = same & (s > t)
    tri[:, 3] = same & (s < t)
    c["tri"] = tri
    mask = np.zeros((128, 2, 4, 128), np.float32)
    mask[:, 0] = (same & (t >= s))[:, None, :]
    mask[:, 1] = (same & (t <= s))[:, None, :]
    c["mask"] = mask.reshape(128, 2, 512)
    c["ident"] = np.eye(128, dtype=np.float32)
    half = 16
    inv_freq = (10000.0 ** (-np.arange(0, half, 2, dtype=np.float32) / half)).astype(np.float32)
    tok = np.arange(T)
    ang_row = (tok // 64).astype(np.float32)[:, None] * inv_freq
    ang_col = (tok % 64).astype(np.float32)[:, None] * inv_freq
    cosr, sinr = np.cos(ang_row), np.sin(ang_row)
    cosc, sinc = np.cos(ang_col), np.sin(ang_col)
    cos32 = np.concatenate([cosr, cosr, cosc, cosc], axis=1)
    sin32 = np.concatenate([-sinr, sinr, -sinc, sinc], axis=1)
    ropet = np.zeros((128, 2, NTOK), np.float32)
    ropet[64:96, 0, :TC] = 1.0
    ropet[64:96, 0, TC:] = cos32.T
    ropet[64:96, 1, TC:] = sin32.T
    c["ropet"] = ropet
    e96 = np.zeros((128, 128), np.float32)
    e96[:96, 96] = 1.0
    c["e96"] = e96
    sel = np.zeros((NE, NE, 128), np.float32)
    for e in range(NE):
        sel[e, e, :] = 1.0
    c["sel"] = sel.reshape(NE, NE * 128)
    rt = np.zeros((128, 176), np.float32)
    rt[:, 0:128] = (s < t)
    ke = np.arange(NE)
    rt[0:NE, 128:160] = (ke[:, None] < ke[None, :])
    rt[:, 160:176] = np.arange(16, dtype=np.float32)[None, :] * 128 + idx[:, None]
    c["rt"] = rt
    return c


def _kc_layout(v):
    return np.ascontiguousarray(v.reshape(-1, 128).T)


class K:
    def __init__(self, stage=99, dbg=False):
        self.stage = stage
        nc = bass.Bass("TRN2", target_bir_lowering=False)
        self.nc = nc
        self.S = Sched(nc, 51200 + 1000)
        S = self.S
        dt = nc.dram_tensor
        self.din = {}

        def inp(name, shape):
            self.din[name] = dt(name, list(shape), F32, kind="ExternalInput").ap()
            return self.din[name]
        self.x = inp("x", [T, D])
        self.ctx = inp("ctx", [TC, D])
        self.cc = inp("cc", [128, 16])
        self.w_mod = inp("w_mod", [D, 6 * D])
        self.bmodT = inp("bmodT", [128, 48])
        self.bmod_row = inp("bmod_row", [1, 6 * D])
        self.gT = inp("gT", [128, 32])
        self.gpost_row = inp("gpost_row", [1, 2 * D])
        self.w_in = inp("w_in", [D, D_IN])
        self.wa2 = inp("wa2", [32, 512])
        self.ba_row = inp("ba_row", [1, 512])
        self.gnb = inp("gnb", [128, 512])
        self.gq = inp("gq", [128, 2])
        self.gkv = inp("gkv", [128, 1])
        self.w_uq = inp("w_uq", [256, 768])
        self.w_uk = inp("w_uk", [128, 512])
        self.w_uv = inp("w_uv", [128, 512])
        self.w_br_gla = inp("w_br_gla", [512, D])
        self.w_br_mla = inp("w_br_mla", [512, D])
        self.w_out = inp("w_out", [D, D])
        self.router_w = inp("router_w", [D, NE])
        self.router_b = inp("router_b", [1, NE])
        self.w_gate = inp("w_gate", [NE, D, D])
        self.w_up = inp("w_up", [NE, D, D])
        self.w_down = inp("w_down", [NE, D, D])
        self.bgT = inp("bgT", [128, NE * 8])
        self.buT = inp("buT", [128, NE * 8])
        self.b_down = inp("b_down", [NE, D])
        self.c_tri = inp("c_tri", [128, 512])
        self.c_mask = inp("c_mask", [128, 1024])
        self.c_ident = inp("c_ident", [128, 128])
        self.c_ropet = inp("c_ropet", [128, 2 * NTOK])
        self.c_e96 = inp("c_e96", [128, 128])
        self.c_sel = inp("c_sel", [NE, NE * 128])
        self.c_rt = inp("c_rt", [128, 176])
        self.xs_d = dt("xs_d", [NSLOT, D], BF16, kind="Internal").ap()
        self.ys_d = dt("ys_d", [NSLOT, D], F32, kind="Internal").ap()
        self.out = dt("out", [T, D], F32, kind="ExternalOutput").ap()
        self.x1d = dt("x1d", [T, D], F32, kind="Internal").ap()
        self.dbg = dt("dbg", [128, 8192], F32, kind="ExternalOutput").ap() if dbg else None
        self.P = [S.psum("ps%d" % i) for i in range(8)]
        self.wq_i = 0
        self.build()

    def MM(self, out, lhsT, rhs, start=True, stop=True, r=(), w=()):
        self.S.op("pe", lambda e: e.matmul(out, lhsT, rhs, start=start, stop=stop, skip_group_check=True), r, w)

    def TR(self, out, in_, ident, r=(), w=()):
        self.S.op("pe", lambda e: e.transpose(out, in_, ident), r, w)

    def ACT(self, out, in_, func, r=(), w=(), **kw):
        self.S.op("act", lambda e: e.activation(out, in_, func, **kw), r, w)

    def TT(self, eng, out, a, b, op, r=(), w=()):
        self.S.op(eng, lambda e: e.tensor_tensor(out, a, b, op=op), r, w)

    def TS(self, eng, out, a, s1, s2, op0, op1=None, r=(), w=()):
        if op1 is None:
            self.S.op(eng, lambda e: e.tensor_scalar(out, a, s1, None, op0=op0), r, w)
        else:
            self.S.op(eng, lambda e: e.tensor_scalar(out, a, s1, s2, op0=op0, op1=op1), r, w)

    def STT(self, out, a, s, b, op0, op1, r=(), w=()):
        self.S.op("dve", lambda e: e.scalar_tensor_tensor(out, a, s, b, op0=op0, op1=op1), r, w)

    def CP(self, eng, out, in_, r=(), w=()):
        if eng == "act":
            self.S.op("act", lambda e: e.copy(out, in_), r, w)
        else:
            self.S.op(eng, lambda e: e.tensor_copy(out, in_), r, w)

    def MS(self, eng, ap, val, w=()):
        self.S.op(eng, lambda e: e.memset(ap, val), (), w)

    def DMA(self, q, out, in_, r=(), w=(), sem=None, **kw):
        self.S.dma(q, out, in_, reads=r, writes=w, sem_buf=sem, **kw)

    def rstd_from_ss(self, ss_ap, n, buf, tmp_ap):
        self.ACT(tmp_ap, ss_ap, AF.Ln, r=[buf], w=[buf], scale=1.0 / n, bias=self.eps_ap)
        self.ACT(ss_ap, tmp_ap, AF.Exp, r=[buf], w=[buf], scale=-0.5)

    def load_w(self, dst_ap, dst_buf, src_ap, rows, cols, q="pool"):
        if rows > 128:
            self.DMA("pool", dst_ap, src_ap.rearrange("(k p) n -> p k n", p=128), w=[dst_buf])
        else:
            self.DMA("pool", dst_ap[0:rows, 0:cols], src_ap, w=[dst_buf])

    def dump(self, col, buf, ap, ncols, parts=128):
        if self.dbg is None:
            return
        S = self.S
        tmp = S.alloc("dbgtmp", [ncols], F32)
        self.CP("dve", tmp.ap[0:parts, :], ap, r=[buf], w=[tmp])
        self.DMA("pool", self.dbg[0:parts, col:col + ncols], tmp.ap[0:parts, :], r=[tmp], sem=tmp)
        self.dbg_bufs.append(tmp)

    def build(self):
        S = self.S
        P = self.P
        self.dbg_bufs = []
        cst = S.alloc("cst", [2048], F32)
        tri = cst.ap[:, 0:512].rearrange("p (a b) -> p a b", b=128)
        identf = cst.ap[:, 512:640]
        e96f = cst.ap[:, 640:768]
        self.eps_ap = cst.ap[:, 768:769]
        onesf = cst.ap[:, 896:1024]
        self.DMA("sp", cst.ap[:, 0:512], self.c_tri, w=[cst])
        self.DMA("sp", identf, self.c_ident, w=[cst])
        self.DMA("sp", e96f, self.c_e96, w=[cst])
        self.MS("pool", cst.ap[:, 768:769], EPS, w=[cst])
        self.MS("pool", onesf, 1.0, w=[cst])
        self.rowmask = cst.ap[:, 1024:1026]
        self.MS("pool", cst.ap[:, 1024:1026], 0.0, w=[cst])
        self.MS("pool", cst.ap[0:64, 1024:1025], 1.0, w=[cst])
        self.MS("pool", cst.ap[64:128, 1025:1026], 1.0, w=[cst])
        cstb = S.alloc("cstb", [2048], BF16)
        identb = cstb.ap[:, 0:128]
        onesb = cstb.ap[:, 128:256]
        e96b = cstb.ap[:, 256:384]
        maskb = cstb.ap[:, 1024:2048].rearrange("p (a b) -> p a b", b=512)
        self.CP("pool", identb, identf, r=[cst], w=[cstb])
        self.CP("pool", onesb, onesf, r=[cst], w=[cstb])
        self.CP("pool", e96b, e96f, r=[cst], w=[cstb])
        self.wstage = [S.alloc("wst", [8, 256], F32) for _ in range(3)]
        mstage = self.wstage[0]
        self.DMA("sp", mstage.ap[:, 0:4, :], self.c_mask.rearrange("p (a b) -> p a b", b=256), w=[mstage])
        self.CP("pool", cstb.ap[:, 1024:2048].rearrange("p (a b) -> p a b", b=256), mstage.ap[:, 0:4, :], r=[mstage], w=[cstb])
        stat = S.alloc("stat", [64], F32)

        modb = S.alloc("mod", [48, 2], F32)
        vec = S.alloc("vec", [16 + 48 + 32 + 32 + 16 + 16 + 8 + 8], F32)
        cc = vec.ap[:, 0:16].rearrange("p (a b) -> p a b", b=2)
        bmodT = vec.ap[:, 16:64]
        gT = vec.ap[:, 64:96].rearrange("p (a b) -> p a b", b=8)
        A1 = vec.ap[:, 96:112].rearrange("p (a b) -> p a b", b=2)
        B1v = None
        A2 = vec.ap[:, 112:120]
        gqv = vec.ap[:, 128:130]
        gkvv = vec.ap[:, 130:131]
        self.DMA("sp", vec.ap[:, 0:16], self.cc, w=[vec])
        self.DMA("sp", bmodT, self.bmodT, w=[vec])
        self.DMA("sp", vec.ap[:, 64:96], self.gT, w=[vec])
        self.DMA("sp", gqv, self.gq, w=[vec])
        self.DMA("sp", gkvv, self.gkv, w=[vec])
        self.ACT(cc, cc, AF.Silu, r=[vec], w=[vec])
        rows = S.alloc("rows", [4096], F32)
        self.DMA("sp", rows.ap[0:1, 2048:4096], self.gpost_row, w=[rows])
        browst = S.alloc("browst", [2048], F32)
        self.DMA("sp", browst.ap[0:1, 0:1024], self.bmod_row[:, 2048:3072], w=[browst])
        self.DMA("sp", browst.ap[0:1, 1024:2048], self.bmod_row[:, 5120:6144], w=[browst])
        wm_view = self.w_mod.rearrange("(k p) n -> p k n", p=128)
        ccb = S.alloc("ccb", [8, 2], BF16)
        self.CP("dve", ccb.ap[:, :, :], cc, r=[vec], w=[ccb])
        wmb = [S.alloc("wmb", [KC, 512], BF16) for _ in range(3)]
        for j in range(12):
            st = wmb[j % 3]
            self.DMA("pool", st.ap[:, :, :], wm_view[:, :, j * 512:(j + 1) * 512], w=[st])
            for m in range(4):
                col = (j * 4 + m) * 2
                for kc in range(KC):
                    self.MM(P[0].ap[:, col:col + 2], st.ap[:, kc, m * 128:(m + 1) * 128], ccb.ap[:, kc, :],
                            start=(kc == 0), stop=(kc == KC - 1), r=[st, ccb], w=[P[0]])
            if j in (4, 5, 10, 11):
                ro = {4: 0, 5: 512, 10: 1024, 11: 1536}[j]
                for kc in range(KC):
                    self.MM(P[1].ap[0:1, :], ccb.ap[:, kc, 0:1], st.ap[:, kc, :], start=(kc == 0), stop=(kc == KC - 1),
                            r=[st, ccb], w=[P[1]])
                self.TT("dve", rows.ap[0:1, ro:ro + 512], P[1].ap[0:1, :], browst.ap[0:1, ro:ro + 512], ALU.add,
                        r=[P[1], browst], w=[rows])
        S.release(ccb, *wmb)
        S.release(browst)
        self.TT("dve", modb.ap[:, :, :], P[0].ap[:, 0:96].rearrange("p (a b) -> p a b", b=2),
                bmodT.unsqueeze(2).to_broadcast([128, 48, 2]), ALU.add, r=[P[0], vec], w=[modb])
        self.STT(A1, modb.ap[:, 8:16, :], 1.0, gT[:, 0, :].unsqueeze(2).to_broadcast([128, 8, 2]), ALU.add, ALU.mult,
                 r=[modb, vec], w=[vec])
        self.STT(A2, modb.ap[:, 32:40, 0], 1.0, gT[:, 2, :], ALU.add, ALU.mult, r=[modb, vec], w=[vec])
        self.TT("dve", rows.ap[0:1, 0:2048], rows.ap[0:1, 0:2048], rows.ap[0:1, 2048:4096], ALU.mult, r=[rows], w=[rows])
        Gb = S.alloc("Gb", [2, 1024], F32)
        for g in range(2):
            for hh in range(2):
                pb = P[2 + hh]
                self.MM(pb.ap[:, :], onesf[0:1, :], rows.ap[0:1, g * 1024 + hh * 512: g * 1024 + hh * 512 + 512],
                        r=[cst, rows], w=[pb])
                self.CP("act", Gb.ap[:, g, hh * 512:(hh + 1) * 512], pb.ap[:, :], r=[pb], w=[Gb])
        S.release(rows, *self.wstage)
        if self.stage == 0:
            self.dump(0, modb, modb.ap[:, :, :].rearrange("p a b -> p (a b)"), 96)
            self.dump(96, vec, vec.ap[:, 96:120], 24)
            self.dump(128, Gb, Gb.ap[:, :, :].rearrange("p a b -> p (a b)"), 2048)
            return self.finish()

        hT = S.alloc("hT", [KC, NTOK], BF16)
        xts = [S.alloc("xt", [D], F32) for _ in range(3)]
        xns = [S.alloc("xn", [D], BF16) for _ in range(2)]
        junk = S.alloc("junk", [D], BF16)
        for i in range(NT):
            xt = xts[i % 3]
            xn = xns[i % 2]
            src = self.ctx[i * 128:(i + 1) * 128, :] if i < 2 else self.x[(i - 2) * 128:(i - 1) * 128, :]
            v = 1 if i < 2 else 0
            self.DMA("sp", xt.ap[:, :], src, w=[xt])
            ss = stat.ap[:, (i % 4) * 2:(i % 4) * 2 + 1]
            tmp = stat.ap[:, (i % 4) * 2 + 1:(i % 4) * 2 + 2]
            self.ACT(junk.ap[:, :], xt.ap[:, :], AF.Square, r=[xt], w=[junk, stat], accum_out=ss)
            self.rstd_from_ss(ss, D, stat, tmp)
            self.TS("dve", xn.ap[:, :], xt.ap[:, :], ss, None, ALU.mult, r=[xt, stat], w=[xn])
            pt = P[i % 2]
            ptb = pt.ap.bitcast(BF16)
            for kc in range(KC):
                self.TR(ptb[:, kc * 128:(kc + 1) * 128], xn.ap[:, kc * 128:(kc + 1) * 128], identb, r=[xn, cstb], w=[pt])
            for kc in range(KC):
                o = hT.ap[:, kc, i * 128:(i + 1) * 128]
                if kc % 2 == 0:
                    self.TS("dve", o, ptb[:, kc * 128:(kc + 1) * 128], A1[:, kc, v:v + 1], modb.ap[:, kc, v:v + 1],
                            ALU.mult, ALU.add, r=[pt, vec, modb], w=[hT])
                else:
                    self.ACT(o, ptb[:, kc * 128:(kc + 1) * 128], AF.Identity, r=[pt, vec, modb], w=[hT],
                             scale=A1[:, kc, v:v + 1], bias=modb.ap[:, kc, v:v + 1])
        S.release(*xts, *xns, junk)
        if self.stage == 1:
            for kc in range(2):
                self.dump(kc * 2304, hT, hT.ap[:, kc, :], 2304)
            return self.finish()
        self.cst, self.cstb, self.stat, self.vec, self.modb, self.Gb, self.hT, self.junk = cst, cstb, stat, vec, modb, Gb, hT, junk
        self.tri, self.identf, self.identb, self.onesf, self.onesb, self.e96b, self.maskb = tri, identf, identb, onesf, onesb, e96b, maskb
        self.A2, self.gqv, self.gkvv = A2, gqv, gkvv
        self.phase_gla()
        if self.stage == 25:
            return self.finish()
        if self.stage == 3:
            for hc in range(4):
                self.dump(hc * 2048, self.ogT, self.ogT.ap[:, hc, :], 2048)
            return self.finish()
        self.phase_mla()
        if self.stage == 4:
            for h in range(4):
                self.dump(h * 2048, self.omlaT, self.omlaT.ap[0:64, h, :], 2048, parts=64)
            return self.finish()
        self.phase_merge()
        if self.stage == 5:
            return self.finish()
        if MOE_MODE == "dense":
            self.phase_moe()
        else:
            self.phase_moe_sparse()
        self.finish()

    def phase_mla(self):
        S, P = self.S, self.P
        hT, cstb, cst, stat = self.hT, self.cstb, self.cst, self.stat
        identb, onesf, onesb, identf = self.identb, self.onesf, self.onesb, self.identf
        wB = S.alloc("wB", [KC, 416], BF16)
        self.load_w(wB.ap[:, :, :], wB, self.w_in[:, 1568:1984], 1024, 416)
        wkrs = S.alloc("wkrs", [KC, 32], BF16)
        for a, b_ in ((0, 8), (8, 0), (16, 24), (24, 16)):
            self.CP("pool", wkrs.ap[:, :, a:a + 8], wB.ap[:, :, 384 + b_:384 + b_ + 8], r=[wB], w=[wkrs])
        wuq = S.alloc("wuq", [2, 768], BF16)
        self.load_w(wuq.ap[:, :, :], wuq, self.w_uq, 256, 768)
        wuqs = S.alloc("wuqs", [2, 8, 32], BF16)
        for h in range(8):
            for a, b_ in ((0, 8), (8, 0), (16, 24), (24, 16)):
                self.CP("pool", wuqs.ap[:, :, h, a:a + 8], wuq.ap[:, :, h * 96 + 64 + b_:h * 96 + 64 + b_ + 8], r=[wuq], w=[wuqs])
        wukv = S.alloc("wukv", [1024], BF16)
        self.load_w(wukv.ap[:, 0:512], wukv, self.w_uk, 128, 512)
        self.load_w(wukv.ap[:, 512:1024], wukv, self.w_uv, 128, 512)
        zdqnT = S.alloc("zdqnT", [2, T], BF16)
        ckvT = S.alloc("ckvT", [NTOK], BF16)
        krT = S.alloc("krT", [NTOK], BF16)
        zn = [S.alloc("zn", [384], BF16) for _ in range(2)]
        junk = S.alloc("junk2", [384], F32)
        for i in range(NT):
            pa = P[i % 2]
            pt = P[2 + i % 2]
            ptb = pt.ap.bitcast(BF16)
            z = zn[i % 2]
            sc = (i % 4) * 4
            if i >= 2:
                for kc in range(KC):
                    self.MM(pa.ap[:, 0:256], hT.ap[:, kc, i * 128:(i + 1) * 128], wB.ap[:, kc, 0:256],
                            start=(kc == 0), stop=(kc == KC - 1), r=[wB, hT], w=[pa])
                self.ACT(junk.ap[:, 0:256], pa.ap[:, 0:256], AF.Square, r=[pa], w=[junk, stat], accum_out=stat.ap[:, sc:sc + 1])
                self.rstd_from_ss(stat.ap[:, sc:sc + 1], 256, stat, stat.ap[:, sc + 1:sc + 2])
                self.TS("dve", z.ap[:, 0:256], pa.ap[:, 0:256], stat.ap[:, sc:sc + 1], None, ALU.mult, r=[pa, stat], w=[z])
            for kc in range(KC):
                self.MM(pa.ap[:, 256:384], hT.ap[:, kc, i * 128:(i + 1) * 128], wB.ap[:, kc, 256:384],
                        start=(kc == 0), stop=(kc == KC - 1), r=[wB, hT], w=[pa])
            self.ACT(junk.ap[:, 256:384], pa.ap[:, 256:384], AF.Square, r=[pa], w=[junk, stat], accum_out=stat.ap[:, sc + 2:sc + 3])
            self.rstd_from_ss(stat.ap[:, sc + 2:sc + 3], 128, stat, stat.ap[:, sc + 3:sc + 4])
            self.TS("dve", z.ap[:, 256:384], pa.ap[:, 256:384], stat.ap[:, sc + 2:sc + 3], None, ALU.mult, r=[pa, stat], w=[z])
            if i >= 2:
                for c in range(2):
                    self.TR(ptb[:, c * 128:(c + 1) * 128], z.ap[:, c * 128:(c + 1) * 128], identb, r=[z, cstb], w=[pt])
                    self.TS("dve", zdqnT.ap[:, c, (i - 2) * 128:(i - 1) * 128], ptb[:, c * 128:(c + 1) * 128], self.gqv[:, c:c + 1], None,
                            ALU.mult, r=[pt, self.vec], w=[zdqnT])
            self.TR(ptb[:, 256:384], z.ap[:, 256:384], identb, r=[z, cstb], w=[pt])
            self.TS("dve", ckvT.ap[:, i * 128:(i + 1) * 128], ptb[:, 256:384], self.gkvv[:, 0:1], None, ALU.mult,
                    r=[pt, self.vec], w=[ckvT])
        TB_ALL = [(0, 256), (256, 512), (768, 512), (1280, 512), (1792, 512)]
        rp = [S.alloc("rp", [2, 512], F32) for _ in range(2)]
        rt = S.alloc("rt", [2, 512], F32)
        ropv = self.c_ropet.rearrange("p (a b) -> p a b", b=NTOK)
        for bi, (c0, nn) in enumerate(TB_ALL):
            r_ = rp[bi % 2]
            self.DMA("sp", r_.ap[64:96, :, 0:nn], ropv[64:96, :, c0:c0 + nn], w=[r_])
            for kc in range(KC):
                self.MM(P[4].ap[64:96, 0:nn], wB.ap[:, kc, 384:416], hT.ap[:, kc, c0:c0 + nn], start=(kc == 0), stop=(kc == KC - 1),
                        r=[wB, hT], w=[P[4]])
            for kc in range(KC):
                self.MM(P[5].ap[64:96, 0:nn], wkrs.ap[:, kc, :], hT.ap[:, kc, c0:c0 + nn], start=(kc == 0), stop=(kc == KC - 1),
                        r=[wkrs, hT], w=[P[5]])
            self.TT("dve", rt.ap[64:96, 0, 0:nn], P[4].ap[64:96, 0:nn], r_.ap[64:96, 0, 0:nn], ALU.mult, r=[P[4], r_], w=[rt])
            self.TT("dve", rt.ap[64:96, 1, 0:nn], P[5].ap[64:96, 0:nn], r_.ap[64:96, 1, 0:nn], ALU.mult, r=[P[5], r_], w=[rt])
            self.TT("pool", krT.ap[64:96, c0:c0 + nn], rt.ap[64:96, 0, 0:nn], rt.ap[64:96, 1, 0:nn], ALU.add, r=[rt], w=[krT])
        S.release(wB, wkrs, junk, *zn)
        v = S.alloc("v", [NT, 8, 128], BF16)
        self.MS("pool", v.ap[:, :, 0:4, 64:128], 0.0, w=[v])
        self.MS("pool", v.ap[:, :, 0:4, 64:65], 1.0, w=[v])
        self.MS("pool", v.ap[:, :, 4:8, 0:64], 0.0, w=[v])
        self.MS("pool", v.ap[:, :, 4:8, 0:1], 1.0, w=[v])
        for i in range(NT):
            pb = P[i % 2]
            self.MM(pb.ap[:, :], ckvT.ap[:, i * 128:(i + 1) * 128], wukv.ap[:, 512:1024], r=[ckvT, wukv], w=[pb])
            self.CP("act", v.ap[:, i, 0:4, 0:64], pb.ap[:, 0:256].rearrange("p (a b) -> p a b", b=64), r=[pb], w=[v])
            self.CP("dve", v.ap[:, i, 4:8, 64:128], pb.ap[:, 256:512].rearrange("p (a b) -> p a b", b=64), r=[pb], w=[v])
        omlaT = S.alloc("omlaT", [4, T], BF16)
        self.omlaT = omlaT
        kThs = [S.alloc("kTh", [NTOK], BF16) for _ in range(2)]
        qThs = [S.alloc("qTh", [T], BF16) for _ in range(2)]
        pT = [S.alloc("pT", [512], BF16) for _ in range(3)]
        rden = S.alloc("rden", [512], F32)
        bcs = S.alloc("bcs", [512], F32)
        sqb = S.alloc("sqb", [NTOK], BF16)
        mxs = [S.alloc("mx", [8], F32) for _ in range(2)]
        nb = [0]

        def build(h):
            kTh, qTh = kThs[h % 2], qThs[h % 2]
            for (c0, nn) in TB_ALL:
                pb = P[nb[0] % 2]
                nb[0] += 1
                self.MM(pb.ap[0:64, 0:nn], wukv.ap[:, h * 64:(h + 1) * 64], ckvT.ap[:, c0:c0 + nn], r=[wukv, ckvT], w=[pb])
                self.CP("dve", kTh.ap[0:64, c0:c0 + nn], pb.ap[0:64, 0:nn], r=[pb], w=[kTh])
            self.CP("pool", kTh.ap[64:96, :], krT.ap[64:96, :], r=[krT], w=[kTh])
            for qb in range(4):
                c0 = TC + qb * 512
                r_ = rp[qb % 2]
                self.DMA("sp", r_.ap[64:96, :, :], ropv[64:96, :, c0:c0 + 512], w=[r_])
                pq, pr = P[2], P[3]
                for c in range(2):
                    self.MM(pq.ap[0:96, :], wuq.ap[:, c, h * 96:(h + 1) * 96], zdqnT.ap[:, c, qb * 512:(qb + 1) * 512],
                            start=(c == 0), stop=(c == 1), r=[wuq, zdqnT], w=[pq])
                for c in range(2):
                    self.MM(pr.ap[64:96, :], wuqs.ap[:, c, h, :], zdqnT.ap[:, c, qb * 512:(qb + 1) * 512],
                            start=(c == 0), stop=(c == 1), r=[wuqs, zdqnT], w=[pr])
                self.CP("dve", qTh.ap[0:64, qb * 512:(qb + 1) * 512], pq.ap[0:64, :], r=[pq], w=[qTh])
                self.TT("dve", rt.ap[64:96, 0, :], pq.ap[64:96, :], r_.ap[64:96, 0, :], ALU.mult, r=[pq, r_], w=[rt])
                self.TT("dve", rt.ap[64:96, 1, :], pr.ap[64:96, :], r_.ap[64:96, 1, :], ALU.mult, r=[pr, r_], w=[rt])
                self.TT("pool", qTh.ap[64:96, qb * 512:(qb + 1) * 512], rt.ap[64:96, 0, :], rt.ap[64:96, 1, :], ALU.add, r=[rt], w=[qTh])

        def build_stab(h):
            kTh, qTh = kThs[h % 2], qThs[h % 2]
            sq = sqb
            mx = mxs[h % 2]
            pn = P[3]
            for which, src_, ntile in ((0, qTh, 16), (1, kTh, NT)):
                self.TT("pool", sq.ap[0:96, 0:ntile * 128], src_.ap[0:96, 0:ntile * 128], src_.ap[0:96, 0:ntile * 128], ALU.mult,
                        r=[src_], w=[sq])
                for t_ in range(ntile):
                    self.MM(pn.ap[:, which * 32 + t_:which * 32 + t_ + 1], sq.ap[0:96, t_ * 128:(t_ + 1) * 128], onesb[0:96, 0:1],
                            r=[sq, cstb], w=[pn])
                self.S.op("dve", lambda e, o=mx.ap[:, which:which + 1], a=pn.ap[:, which * 32:which * 32 + ntile]: e.reduce_max(o, a, axis=AX.X),
                          [pn], [mx])
            for which in range(2):
                self.TR(pn.ap[0:1, 64 + which * 128:64 + (which + 1) * 128], mx.ap[:, which:which + 1], identf, r=[mx, cst], w=[pn])
                self.S.op("dve", lambda e, o=mx.ap[0:1, 2 + which:3 + which], a=pn.ap[0:1, 64 + which * 128:64 + (which + 1) * 128]:
                          e.reduce_max(o, a, axis=AX.X), [pn], [mx])
            self.TT("dve", mx.ap[0:1, 4:5], mx.ap[0:1, 2:3], mx.ap[0:1, 3:4], ALU.mult, r=[mx], w=[mx])
            self.ACT(mx.ap[0:1, 5:6], mx.ap[0:1, 4:5], AF.Ln, r=[mx], w=[mx], bias=self.eps_ap[0:1, :])
            self.ACT(mx.ap[0:1, 6:7], mx.ap[0:1, 5:6], AF.Exp, r=[mx], w=[mx], scale=0.5)
            self.MM(pn.ap[:, 400:401], onesf[0:1, :], mx.ap[0:1, 6:7], r=[cst, mx], w=[pn])
            self.TS("dve", mx.ap[:, 7:8], pn.ap[:, 400:401], -1.02 * MLA_SCALE, None, ALU.mult, r=[pn], w=[mx])

        def tail(h, qb):
            po = P[6 + qb % 2]
            dr, lo = (64, 0) if h < 4 else (0, 64)
            self.CP("act", rden.ap[dr:dr + 1, :], po.ap[dr:dr + 1, :], r=[po], w=[rden])
            pbc = P[2]
            self.MM(pbc.ap[lo:lo + 64, :], onesf[dr:dr + 1, 0:64], rden.ap[dr:dr + 1, :], r=[cst, rden], w=[pbc])
            self.S.op("dve", lambda e, o=bcs.ap[lo:lo + 64, :], a=pbc.ap[lo:lo + 64, :]: e.reciprocal(o, a), [pbc], [bcs])
            self.TT("dve", omlaT.ap[lo:lo + 64, h % 4, qb * 512:(qb + 1) * 512], po.ap[lo:lo + 64, :], bcs.ap[lo:lo + 64, :], ALU.mult,
                    r=[po, bcs], w=[omlaT])
        build(0)
        build_stab(0)
        pend = None
        for h in range(8):
            kTh, qTh = kThs[h % 2], qThs[h % 2]
            if h + 1 < 8:
                build(h + 1)
            for qb in range(4):
                po = P[6 + qb % 2]

                def qk(kt, qb=qb, kTh=kTh, qTh=qTh):
                    self.MM(P[4 + kt % 2].ap[:, :], kTh.ap[0:96, kt * 128:(kt + 1) * 128], qTh.ap[0:96, qb * 512:(qb + 1) * 512],
                            r=[kTh, qTh], w=[P[4 + kt % 2]])
                qk(0)
                for kt in range(NT):
                    psb = P[4 + kt % 2]
                    p_ = pT[kt % 3]
                    if kt + 1 < NT:
                        qk(kt + 1)
                    self.ACT(p_.ap[:, :], psb.ap[:, :], AF.Exp, r=[psb, mxs[h % 2]], w=[p_], scale=MLA_SCALE, bias=mxs[h % 2].ap[:, 7:8])
                    self.MM(po.ap[:, :], v.ap[:, kt, h, :], p_.ap[:, :], start=(kt == 0), stop=(kt == NT - 1), r=[v, p_], w=[po])
                    if kt == 2 and pend is not None:
                        tail(*pend)
                        pend = None
                    if kt == 9 and qb == 2 and h + 1 < 8:
                        build_stab(h + 1)
                pend = (h, qb)
        tail(*pend)
        kTh, qTh = kThs[0], qThs[0]
        S.release(kThs[1], qThs[1], sqb, *mxs)
        S.release(wuq, wuqs, wukv, zdqnT, ckvT, krT, v, kTh, qTh, rden, bcs, rt, *pT, *rp)

    def phase_gla(self):
        S, P = self.S, self.P
        hT, cstb, cst, stat = self.hT, self.cstb, self.cst, self.stat
        TB_ALL = [(0, 256), (256, 512), (768, 512), (1280, 512), (1792, 512)]
        TB_X = TB_ALL[1:]
        wA = S.alloc("wA", [KC, 1056], BF16)
        self.load_w(wA.ap[:, :, 0:1024], wA, self.w_in[:, 0:1024], 1024, 1024)
        self.load_w(wA.ap[:, :, 1024:1056], wA, self.w_in[:, 1536:1568], 1024, 32)
        wzg = S.alloc("wzg", [KC, 512], BF16)
        self.load_w(wzg.ap[:, :, :], wzg, self.w_in[:, 1024:1536], 1024, 512)
        wa2b = S.alloc("wa2b", [1024], BF16)
        self.load_w(wa2b.ap[:, 0:512], wa2b, self.wa2, 32, 512)
        self.load_w(wa2b.ap[:, 512:1024], wa2b, self.ba_row, 1, 512)
        gnb = S.alloc("gnb", [512], F32)
        self.DMA("sp", gnb.ap[:, :], self.gnb, w=[gnb])
        self.xsz = S.virt("xsz")
        zt = S.alloc("zt", [D], BF16)
        self.zt = zt
        self.MS("pool", zt.ap[:, :], 0.0, w=[zt])
        for c in range(NSLOT // 128):
            self.DMA("sp", self.xs_d[c * 128:(c + 1) * 128, :], zt.ap[:, :], r=[zt], w=[self.xsz], sem=zt)
        gqT = S.alloc("gqT", [2, T], BF16)
        gkT = S.alloc("gkT", [2, NTOK], BF16)
        gk = S.alloc("gk", [NT, 256], BF16)
        gv = S.alloc("gv", [NT, 512], BF16)
        zaT = S.alloc("zaT", [NTOK], BF16)
        n = 0
        for (c0, nn) in TB_ALL:
            for m in range(4):
                if m < 2 and c0 < TC:
                    continue
                pb = P[n % 2]
                n += 1
                for kc in range(KC):
                    self.MM(pb.ap[:, 0:nn], wA.ap[:, kc, m * 128:(m + 1) * 128], hT.ap[:, kc, c0:c0 + nn],
                            start=(kc == 0), stop=(kc == KC - 1), r=[wA, hT], w=[pb])
                if m < 2:
                    self.CP("act", gqT.ap[:, m, c0 - TC:c0 - TC + nn], pb.ap[:, 0:nn], r=[pb], w=[gqT])
                else:
                    self.CP("dve", gkT.ap[:, m - 2, c0:c0 + nn], pb.ap[:, 0:nn], r=[pb], w=[gkT])
            pb = P[n % 2]
            n += 1
            for kc in range(KC):
                self.MM(pb.ap[0:32, 0:nn], wA.ap[:, kc, 1024:1056], hT.ap[:, kc, c0:c0 + nn],
                        start=(kc == 0), stop=(kc == KC - 1), r=[wA, hT], w=[pb])
            self.CP("act", zaT.ap[0:32, c0:c0 + nn], pb.ap[0:32, 0:nn], r=[pb], w=[zaT])
        for i in range(NT):
            pa, pv = P[2 + i % 2], P[4 + i % 2]
            for kc in range(KC):
                self.MM(pa.ap[:, 0:256], hT.ap[:, kc, i * 128:(i + 1) * 128], wA.ap[:, kc, 256:512],
                        start=(kc == 0), stop=(kc == KC - 1), r=[wA, hT], w=[pa])
            for kc in range(KC):
                self.MM(pv.ap[:, :], hT.ap[:, kc, i * 128:(i + 1) * 128], wA.ap[:, kc, 512:1024],
                        start=(kc == 0), stop=(kc == KC - 1), r=[wA, hT], w=[pv])
            self.CP("act", gk.ap[:, i, :], pa.ap[:, 0:256], r=[pa], w=[gk])
            self.CP("dve", gv.ap[:, i, :], pv.ap[:, :], r=[pv], w=[gv])
        S.release(wA)
        if self.stage == 2:
            for m in range(2):
                self.dump(m * 2048, gqT, gqT.ap[:, m, :], 2048)
            self.dump(4096, gv, gv.ap[:, 5, :], 512)
            self.dump(4608, gk, gk.ap[:, 5, :], 256)
            self.dump(4864, zaT, zaT.ap[:, 0:2304], 2304)
            self.ogT = gqT
            return

        ogT = S.alloc("ogT", [4, T], BF16)
        self.ogT = ogT
        hist = S.alloc("hist", [32, 256], BF16)
        Sst = [S.alloc("Sst", [256], F32) for _ in range(2)]
        Sbr = [S.alloc("Sbr", [256], BF16) for _ in range(2)]
        R = 2
        la_b = [S.alloc("la", [256], F32) for _ in range(R)]
        lt_b = [S.alloc("lt", [256], F32) for _ in range(R)]
        ET_b = [S.alloc("ET", [256], F32) for _ in range(R)]
        EI_b = [S.alloc("EI", [256], F32) for _ in range(R)]
        KS_b = [S.alloc("KS", [256], F32) for _ in range(R)]
        qd_b = [S.alloc("qd", [2, 2, 128], BF16) for _ in range(2)]
        qt_b = [S.alloc("qt", [2, 128], F32) for _ in range(2)]
        ki_b = [S.alloc("ki", [2, 128], BF16) for _ in range(R)]
        kte_b = [S.alloc("kte", [256], BF16) for _ in range(R)]
        att_b = [S.alloc("att", [512], BF16) for _ in range(R)]
        ot_b = S.alloc("ot", [512], F32)
        sq_b = S.alloc("sq", [512], F32)
        sz_b = S.alloc("sz", [512], F32)
        og_b = S.alloc("og", [512], BF16)
        self.cnt = 0
        tri, maskb, onesb, identb = self.tri, self.maskb, self.onesb, self.identb
        for d in range(2):
            self.MS("pool", Sst[d].ap[:, :], 0.0, w=[Sst[d]])
        self.MS("pool", Sbr[0].ap[:, :], 0.0, w=[Sbr[0]])

        def prep(i, d, want_q):
            c = self.cnt
            self.cnt += 1
            r = c % R
            pl, pc = P[0 + c % 2], P[2 + c % 2]
            la, lt, ET, EI, KS = la_b[r], lt_b[r], ET_b[r], EI_b[r], KS_b[r]
            self.MM(pl.ap[:, 0:256], zaT.ap[0:32, i * 128:(i + 1) * 128], wa2b.ap[0:32, d * 256:(d + 1) * 256],
                    start=True, stop=False, r=[zaT, wa2b], w=[pl])
            self.MM(pl.ap[:, 0:256], onesb[0:1, :], wa2b.ap[0:1, 512 + d * 256:512 + (d + 1) * 256],
                    start=False, stop=True, r=[cstb, wa2b], w=[pl])
            self.ACT(lt.ap[:, :], pl.ap[:, 0:256], AF.Abs, r=[pl], w=[lt])
            self.ACT(lt.ap[:, :], lt.ap[:, :], AF.Exp, r=[lt], w=[lt], scale=-1.0)
            self.ACT(lt.ap[:, :], lt.ap[:, :], AF.Ln, r=[lt], w=[lt], bias=self.onesf[:, 0:1])
            self.TS("dve", la.ap[:, :], pl.ap[:, 0:256], 0.0, 1.0 / 16, ALU.min, ALU.mult, r=[pl], w=[la])
            self.STT(la.ap[:, :], lt.ap[:, :], -1.0 / 16, la.ap[:, :], ALU.mult, ALU.add, r=[lt, la], w=[la])
            for j in range(2):
                self.MM(pc.ap[:, j * 128:(j + 1) * 128], la.ap[:, j * 128:(j + 1) * 128], tri[:, d, :], r=[la, cst], w=[pc])
            self.MM(pc.ap[:, 256:512], tri[:, 2 + d, :], la.ap[:, :], r=[la, cst], w=[pc])
            self.ACT(ET.ap[:, :], pc.ap[:, 0:256], AF.Exp, r=[pc], w=[ET])
            self.ACT(KS.ap[:, :], pc.ap[:, 256:512], AF.Exp, r=[pc], w=[KS])
            kte = kte_b[r]
            self.TT("pool", kte.ap[:, :], gk.ap[:, i, :], KS.ap[:, :], ALU.mult, r=[gk, KS], w=[kte])
            out = {"pl": pl, "pc": pc, "ET": ET, "kte": kte}
            if want_q:
                xc = (i - 2) * 128
                self.ACT(EI.ap[:, :], pc.ap[:, 0:256], AF.Exp, r=[pc], w=[EI], scale=-1.0)
                qd = qd_b[c % 2]
                ki = ki_b[r]
                qt = qt_b[c % 2]
                self.STT(qt.ap[:, :, :], gqT.ap[:, :, xc:xc + 128], 0.125, ET.ap[:, :].rearrange("p (a b) -> p a b", b=128),
                         ALU.mult, ALU.mult, r=[gqT, ET], w=[qt])
                for p in range(2):
                    self.TS("pool" if p else "dve", qd.ap[:, p, :, :], qt.ap[:, :, :], self.rowmask[:, p:p + 1], None, ALU.mult,
                            r=[qt, cst], w=[qd])
                self.TT("pool", ki.ap[:, :, :], gkT.ap[:, :, i * 128:(i + 1) * 128],
                        EI.ap[:, :].rearrange("p (a b) -> p a b", b=128), ALU.mult, r=[gkT, EI], w=[ki])
                out["qd"], out["ki"] = qd, ki
            return out

        def ds_update(i, d, b, cs):
            pl, ET, kte = b["pl"], b["ET"], b["kte"]
            for h in range(4):
                self.MM(pl.ap[(h % 2) * 64:(h % 2) * 64 + 64, 256 + (h // 2) * 128:256 + (h // 2) * 128 + 128],
                        kte.ap[cs:cs + 64, h * 64:(h + 1) * 64], gv.ap[cs:cs + 64, i, h * 128:(h + 1) * 128],
                        r=[kte, gv], w=[pl])
            tl = cs + 63 if d == 0 else cs
            for j in range(2):
                self.STT(Sst[d].ap[:, j * 128:(j + 1) * 128], Sst[d].ap[:, j * 128:(j + 1) * 128],
                         ET.ap[:, j * 128 + tl:j * 128 + tl + 1], pl.ap[:, 256 + j * 128:256 + (j + 1) * 128],
                         ALU.mult, ALU.add, r=[Sst[d], ET, pl], w=[Sst[d]])

        order_b = [1, 0] + list(range(17, 1, -1))
        for i in order_b:
            b = prep(i, 1, False)
            for cs in (64, 0):
                if i >= 2:
                    ch = (i - 2) * 2 + (1 if cs == 64 else 0)
                    self.CP("act", hist.ap[:, ch, :], Sst[1].ap[:, :], r=[Sst[1]], w=[hist])
                ds_update(i, 1, b, cs)

        if self.stage == 25:
            S.release(ot_b, sq_b, sz_b)
            self.dump(0, hist, hist.ap[:, 31, :], 256)
            self.dump(256, hist, hist.ap[:, 0, :], 256)
            return
        live = 0
        for i in range(NT):
            bf = prep(i, 0, i >= 2)
            if i < 2:
                for cs in (0, 64):
                    ds_update(i, 0, bf, cs)
                if i == 1:
                    self.CP("act", Sbr[0].ap[:, :], Sst[0].ap[:, :], r=[Sst[0]], w=[Sbr[0]])
                continue
            bb = prep(i, 1, True)
            xi = i - 2
            xc = xi * 128
            pos = (P[6], P[7])
            pss = (P[4], P[5])
            first = [True, True]
            acol = lambda h: ((h % 2) * 2 + h // 2) * 128
            for d, b in ((0, bf), (1, bb)):
                att = att_b[d]
                qd, ki = b["qd"], b["ki"]
                for h in range(4):
                    hp = (h % 2) * 64
                    self.MM(pss[h % 2].ap[:, (h // 2) * 128:(h // 2) * 128 + 128], ki.ap[:, h // 2, :],
                            qd.ap[:, h % 2, h // 2, :], r=[ki, qd], w=[pss[h % 2]])
                for p in range(2):
                    self.TT("dve", att.ap[:, p * 256:(p + 1) * 256], pss[p].ap[:, 0:256], maskb[:, d, 0:256], ALU.mult,
                            r=[pss[p], cstb], w=[att])
                for h in range(4):
                    self.MM(pos[h % 2].ap[:, (h // 2) * 128:(h // 2) * 128 + 128], att.ap[:, acol(h):acol(h) + 128],
                            gv.ap[:, i, h * 128:(h + 1) * 128], start=first[h % 2], stop=False, r=[att, gv], w=[pos[h % 2]])
                    first[h % 2] = False
            qd = bb["qd"]
            for cs in (64, 0):
                ch = xi * 2 + (1 if cs == 64 else 0)
                for h in range(4):
                    hp = (h % 2) * 64
                    self.MM(pos[h % 2].ap[cs:cs + 64, (h // 2) * 128:(h // 2) * 128 + 128], qd.ap[:, h % 2, h // 2, cs:cs + 64],
                            hist.ap[:, ch, (h // 2) * 128:(h // 2) * 128 + 128], start=False, stop=False,
                            r=[qd, hist], w=[pos[h % 2]])
            qd = bf["qd"]
            for cs in (0, 64):
                sb = Sbr[live % 2]
                for h in range(4):
                    hp = (h % 2) * 64
                    self.MM(pos[h % 2].ap[cs:cs + 64, (h // 2) * 128:(h // 2) * 128 + 128], qd.ap[:, h % 2, h // 2, cs:cs + 64],
                            sb.ap[:, (h // 2) * 128:(h // 2) * 128 + 128], start=False, stop=(cs == 64 and h >= 2),
                            r=[qd, sb], w=[pos[h % 2]])
                ds_update(i, 0, bf, cs)
                live += 1
                self.CP("act", Sbr[live % 2].ap[:, :], Sst[0].ap[:, :], r=[Sst[0]], w=[Sbr[live % 2]])
            pz = bf["pc"]
            for kc in range(KC):
                self.MM(pz.ap[:, :], hT.ap[:, kc, i * 128:(i + 1) * 128], wzg.ap[:, kc, :], start=(kc == 0), stop=(kc == KC - 1),
                        r=[hT, wzg], w=[pz])
            self.ACT(sz_b.ap[:, :], pz.ap[:, :], AF.Silu, r=[pz], w=[sz_b])
            for h in range(4):
                self.CP("act" if h % 2 == 0 else "dve", ot_b.ap[:, h * 128:(h + 1) * 128],
                        pos[h % 2].ap[:, (h // 2) * 128:(h // 2) * 128 + 128], r=[pos[h % 2]], w=[ot_b])
            self.TT("pool", sq_b.ap[:, :], ot_b.ap[:, :], ot_b.ap[:, :], ALU.mult, r=[ot_b], w=[sq_b])
            st4 = stat.ap[:, 16:20]
            st4b = stat.ap[:, 20:24]
            self.S.op("dve", lambda e, o=st4, a=sq_b.ap[:, :].rearrange("p (a b) -> p a b", b=128): e.reduce_sum(o, a, axis=AX.X),
                      [sq_b], [stat])
            self.ACT(st4b, st4, AF.Ln, r=[stat], w=[stat], scale=1.0 / 128, bias=self.eps_ap)
            self.ACT(st4, st4b, AF.Exp, r=[stat], w=[stat], scale=-0.5)
            self.TT("dve", sq_b.ap[:, :].rearrange("p (a b) -> p a b", b=128), ot_b.ap[:, :].rearrange("p (a b) -> p a b", b=128),
                    st4.unsqueeze(2).to_broadcast([128, 4, 128]), ALU.mult, r=[ot_b, stat], w=[sq_b])
            self.TT("pool", sq_b.ap[:, :], sq_b.ap[:, :], gnb.ap[:, :], ALU.mult, r=[sq_b, gnb], w=[sq_b])
            self.TT("dve", og_b.ap[:, :], sq_b.ap[:, :], sz_b.ap[:, :], ALU.mult, r=[sq_b, sz_b], w=[og_b])
            pt = P[4]
            ptb = pt.ap.bitcast(BF16)
            for h in range(4):
                self.TR(ptb[:, h * 128:(h + 1) * 128], og_b.ap[:, h * 128:(h + 1) * 128], identb, r=[og_b, cstb], w=[pt])
            self.CP("act", ogT.ap[:, :, xc:xc + 128], ptb[:, 0:512].rearrange("p (a b) -> p a b", b=128), r=[pt], w=[ogT])
        S.release(gqT, gkT, gk, gv, zaT, hist, wzg, wa2b, gnb, ot_b, sq_b, sz_b, og_b,
                  *Sst, *Sbr, *la_b, *lt_b, *ET_b, *EI_b, *KS_b, *qd_b, *qt_b, *ki_b, *kte_b, *att_b)

    def phase_merge(self):
        S, P = self.S, self.P
        hT, ogT, omlaT, stat, cst, cstb = self.hT, self.ogT, self.omlaT, self.stat, self.cst, self.cstb
        mT = S.alloc("mT", [KC, T], BF16)
        wbg = S.alloc("wbg", [4, D], BF16)
        self.load_w(wbg.ap[:, :, :], wbg, self.w_br_gla, 512, D)
        wbm = S.alloc("wbm", [4, D], BF16)
        src = self.w_br_mla.rearrange("(k p) n -> p k n", p=64)
        self.DMA("pool", wbm.ap[0:64, :, :], src[:, 0:4, :], w=[wbm])
        self.DMA("pool", wbm.ap[64:128, :, :], src[:, 4:8, :], w=[wbm])
        wz = [S.alloc("wz", [KC, 256], BF16) for _ in range(2)]
        sg = [S.alloc("sg", [512], F32) for _ in range(2)]
        sm = [S.alloc("sm", [512], F32) for _ in range(2)]
        n = 0
        for m in range(KC):
            w_ = wz[m % 2]
            self.load_w(w_.ap[:, :, 0:128], w_, self.w_in[:, 1984 + m * 128:1984 + (m + 1) * 128], 1024, 128)
            self.load_w(w_.ap[:, :, 128:256], w_, self.w_in[:, 3008 + m * 128:3008 + (m + 1) * 128], 1024, 128)
            for tb in range(4):
                c0, xc0 = TC + tb * 512, tb * 512
                o = (n % 2) * 4
                sg_, sm_ = sg[n % 2], sm[n % 2]
                n += 1
                for kc in range(KC):
                    self.MM(P[o].ap[:, :], w_.ap[:, kc, 0:128], hT.ap[:, kc, c0:c0 + 512], start=(kc == 0), stop=(kc == KC - 1),
                            r=[w_, hT], w=[P[o]])
                for kc in range(KC):
                    self.MM(P[o + 1].ap[:, :], w_.ap[:, kc, 128:256], hT.ap[:, kc, c0:c0 + 512], start=(kc == 0), stop=(kc == KC - 1),
                            r=[w_, hT], w=[P[o + 1]])
                for hc in range(4):
                    self.MM(P[o + 2].ap[:, :], wbg.ap[:, hc, m * 128:(m + 1) * 128], ogT.ap[:, hc, xc0:xc0 + 512],
                            start=(hc == 0), stop=(hc == 3), r=[wbg, ogT], w=[P[o + 2]])
                for j in range(4):
                    self.MM(P[o + 3].ap[:, :], wbm.ap[:, j, m * 128:(m + 1) * 128], omlaT.ap[:, j, xc0:xc0 + 512],
                            start=(j == 0), stop=(j == 3), r=[wbm, omlaT], w=[P[o + 3]])
                self.ACT(sg_.ap[:, :], P[o].ap[:, :], AF.Sigmoid, r=[P[o]], w=[sg_])
                self.ACT(sm_.ap[:, :], P[o + 1].ap[:, :], AF.Sigmoid, r=[P[o + 1]], w=[sm_])
                self.TT("dve", sg_.ap[:, :], sg_.ap[:, :], P[o + 2].ap[:, :], ALU.mult, r=[sg_, P[o + 2]], w=[sg_])
                self.TT("dve", sm_.ap[:, :], sm_.ap[:, :], P[o + 3].ap[:, :], ALU.mult, r=[sm_, P[o + 3]], w=[sm_])
                self.TT("pool", mT.ap[:, m, xc0:xc0 + 512], sg_.ap[:, :], sm_.ap[:, :], ALU.add, r=[sg_, sm_], w=[mT])
        S.release(hT, ogT, omlaT, wbg, wbm, *wz, *sg, *sm)
        wo = S.alloc("wo", [KC, D], BF16)
        self.load_w(wo.ap[:, :, :], wo, self.w_out, D, D)
        rw = S.alloc("rw", [KC * NE + NE], F32)
        rwv = rw.ap[:, 0:KC * NE].rearrange("p (a b) -> p a b", b=NE)
        self.DMA("sp", rwv, self.router_w.rearrange("(k p) n -> p k n", p=128), w=[rw])
        self.DMA("sp", rw.ap[0:1, KC * NE:KC * NE + NE], self.router_b, w=[rw])
        comb = S.alloc("comb", [16, NE], F32)
        self.comb = comb
        rt_ = S.alloc("rt", [176 + 144], F32)
        self.DMA("sp", rt_.ap[:, 0:176], self.c_rt, w=[rt_])
        tsub = S.alloc("tsub", [128], BF16)
        self.CP("pool", tsub.ap[:, :], rt_.ap[:, 0:128], r=[rt_], w=[tsub])
        Umat = rt_.ap[:, 128:160]
        IO = rt_.ap[:, 160:176]
        cb = rt_.ap[:, 176:208]
        posf4 = rt_.ap[:, 208:212]
        oh = rt_.ap[:, 224:256]
        oh2 = rt_.ap[:, 256:288]
        posf = rt_.ap[:, 288:320]
        self.MS("pool", rt_.ap[:, 176:320], 0.0, w=[rt_])
        wk = S.alloc("wk", [16, 4], F32)
        posi = S.alloc("posi", [16, 4], F32)
        posi_v = posi.ap.bitcast(I32)
        self.wk, self.posi, self.posi_v = wk, posi, posi_v
        maskb16 = S.alloc("maskb16", [NE], BF16)
        rank_all = S.alloc("rank", [16, NE], F32)
        lgall = S.alloc("lgall", [16, 40], F32)
        xn2tm = S.alloc("xn2tm", [16, D], BF16)
        xsz, xsd = self.xsz, S.virt("xsd")
        self.xsd, self.ysd = xsd, S.virt("ysd")
        xt = [S.alloc("xt2", [D], F32) for _ in range(2)]
        tt = [S.alloc("tt2", [D], F32) for _ in range(2)]
        hx2f = S.alloc("hx2f", [KC, 128], F32)
        lg = S.alloc("lg", [128], F32)
        junk = S.alloc("junk3", [D], BF16)
        Gb, A2, modb, identf, onesf = self.Gb, self.A2, self.modb, self.identf, self.onesf
        for xi in range(16):
            pa, pb = P[(xi % 2) * 2], P[(xi % 2) * 2 + 1]
            x_, t_ = xt[xi % 2], tt[xi % 2]
            self.DMA("sp", x_.ap[:, :], self.x[xi * 128:(xi + 1) * 128, :], w=[x_])
            for hf, pp in enumerate((pa, pb)):
                for m in range(KC):
                    self.MM(pp.ap[:, :], mT.ap[:, m, xi * 128:(xi + 1) * 128], wo.ap[:, m, hf * 512:(hf + 1) * 512],
                            start=(m == 0), stop=(m == KC - 1), r=[mT, wo], w=[pp])
            sc = 24 + (xi % 2) * 8
            for hf, pp in enumerate((pa, pb)):
                self.ACT(junk.ap[:, 0:512], pp.ap[:, :], AF.Square, r=[pp], w=[junk, stat], accum_out=stat.ap[:, sc + hf:sc + hf + 1])
            self.TT("dve", stat.ap[:, sc:sc + 1], stat.ap[:, sc:sc + 1], stat.ap[:, sc + 1:sc + 2], ALU.add, r=[stat], w=[stat])
            self.rstd_from_ss(stat.ap[:, sc:sc + 1], D, stat, stat.ap[:, sc + 2:sc + 3])
            for hf, pp in enumerate((pa, pb)):
                self.STT(t_.ap[:, hf * 512:(hf + 1) * 512], pp.ap[:, :], stat.ap[:, sc:sc + 1], Gb.ap[:, 0, hf * 512:(hf + 1) * 512],
                         ALU.mult, ALU.mult, r=[pp, stat, Gb], w=[t_])
            self.TT("pool", x_.ap[:, :], t_.ap[:, :], x_.ap[:, :], ALU.add, r=[t_, x_], w=[x_])
            self.DMA("pool", self.x1d[xi * 128:(xi + 1) * 128, :], x_.ap[:, :], r=[x_], sem=x_)
            self.ACT(junk.ap[:, :], x_.ap[:, :], AF.Square, r=[x_], w=[junk, stat], accum_out=stat.ap[:, sc + 3:sc + 4])
            self.rstd_from_ss(stat.ap[:, sc + 3:sc + 4], D, stat, stat.ap[:, sc + 4:sc + 5])
            self.TS("dve", t_.ap[:, :], x_.ap[:, :], stat.ap[:, sc + 3:sc + 4], None, ALU.mult, r=[x_, stat], w=[t_])
            for kc in range(KC):
                pt = P[4 + kc // 4]
                self.TR(pt.ap[:, (kc % 4) * 128:(kc % 4 + 1) * 128], t_.ap[:, kc * 128:(kc + 1) * 128], identf, r=[t_, cst], w=[pt])
            for kc in range(KC):
                pt = P[4 + kc // 4]
                src_ = pt.ap[:, (kc % 4) * 128:(kc % 4 + 1) * 128]
                if kc % 2 == 0:
                    self.TS("dve", hx2f.ap[:, kc, :], src_, A2[:, kc:kc + 1], modb.ap[:, 24 + kc, 0:1], ALU.mult, ALU.add,
                            r=[pt, self.vec, modb], w=[hx2f])
                else:
                    self.ACT(hx2f.ap[:, kc, :], src_, AF.Identity, r=[pt, self.vec, modb], w=[hx2f],
                             scale=A2[:, kc:kc + 1], bias=modb.ap[:, 24 + kc, 0:1])
            pr = P[6 + xi % 2]
            for kc in range(KC):
                self.MM(pr.ap[:, 0:NE], hx2f.ap[:, kc, :], rwv[:, kc, :], start=(kc == 0), stop=False, r=[hx2f, rw], w=[pr])
            self.MM(pr.ap[:, 0:NE], onesf[0:1, :], rw.ap[0:1, KC * NE:KC * NE + NE], start=False, stop=True, r=[cst, rw], w=[pr])
            self.CP("act", lg.ap[:, 0:NE], pr.ap[:, 0:NE], r=[pr], w=[lg])
            self.S.op("dve", lambda e, o=lg.ap[:, 32:40], a=lg.ap[:, 0:NE]: e.max(out=o, in_=a), [lg], [lg])
            self.TS("dve", lg.ap[:, 64:96], lg.ap[:, 0:NE], lg.ap[:, 35:36], None, ALU.is_ge, r=[lg], w=[lg])
            self.TS("dve", lg.ap[:, 40:41], lg.ap[:, 32:33], -1.0, None, ALU.mult, r=[lg], w=[lg])
            self.ACT(lg.ap[:, 96:128], lg.ap[:, 0:NE], AF.Exp, r=[lg], w=[lg], bias=lg.ap[:, 40:41])
            self.TT("dve", lg.ap[:, 96:128], lg.ap[:, 96:128], lg.ap[:, 64:96], ALU.mult, r=[lg], w=[lg])
            self.S.op("dve", lambda e, o=lg.ap[:, 41:42], a=lg.ap[:, 96:128]: e.reduce_sum(o, a, axis=AX.X), [lg], [lg])
            self.S.op("dve", lambda e, o=lg.ap[:, 42:43], a=lg.ap[:, 41:42]: e.reciprocal(o, a), [lg], [lg])
            self.TS("dve", comb.ap[:, xi, :], lg.ap[:, 96:128], lg.ap[:, 42:43], None, ALU.mult, r=[lg], w=[comb])
            self.CP("pool", maskb16.ap[:, :], lg.ap[:, 64:96], r=[lg], w=[maskb16])
            self.MM(pr.ap[:, 64:96], tsub.ap[:, :], maskb16.ap[:, :], r=[tsub, maskb16], w=[pr])
            self.MM(pr.ap[:, 128:160], self.onesb, maskb16.ap[:, :], r=[self.cstb, maskb16], w=[pr])
            self.TT("dve", rank_all.ap[:, xi, :], pr.ap[:, 64:96], cb, ALU.add, r=[pr, rt_], w=[rank_all])
            self.TT("dve", cb, cb, pr.ap[:, 128:160], ALU.add, r=[pr, rt_], w=[rt_])
            self.CP("dve", lgall.ap[:, xi, :], lg.ap[:, 0:40], r=[lg], w=[lgall])
            self.CP("pool", xn2tm.ap[:, xi, :], t_.ap[:, :], r=[t_], w=[xn2tm])
        meta = S.alloc("meta", [32 + 32 + 128 + 512 + 512], F32)
        self.meta = meta
        nt = meta.ap[:, 0:32]
        nt_i = meta.ap[:, 32:64].bitcast(I32)
        ntT = meta.ap[:, 64:192]
        sidxf = meta.ap[:, 192:704].rearrange("p (a b) -> p a b", b=16)
        sidx_i = meta.ap[:, 704:1216].bitcast(I32).rearrange("p (a b) -> p a b", b=16)
        self.nt_i, self.sidx_i = nt_i, sidx_i
        self.MS("pool", nt, 0.0, w=[meta])
        for m in range(NSTEP):
            self.STT(nt, cb, float(SG * m), nt, ALU.is_gt, ALU.add, r=[rt_, meta], w=[meta])
        self.CP("dve", nt_i, nt, r=[meta], w=[meta])
        pq = P[4]
        self.TR(pq.ap[0:NE, 0:128], nt, identf, r=[meta, cst], w=[pq])
        self.CP("act", ntT[0:NE, :], pq.ap[0:NE, 0:128], r=[pq], w=[meta])
        self.MM(pq.ap[:, 128:160], ntT[0:NE, :], Umat[0:NE, :], r=[meta, rt_], w=[pq])
        base = rt_.ap[:, 176:208]
        self.TS("dve", base, pq.ap[:, 128:160], float(SG), None, ALU.mult, r=[pq, rt_], w=[rt_])
        for e in range(NE):
            self.TS("dve", sidxf[:, e, :], IO, base[:, e:e + 1], None, ALU.add, r=[rt_, meta], w=[meta])
        self.CP("dve", sidx_i, sidxf, r=[meta], w=[meta])
        S.release(mT, wo, rw, hx2f, lg, junk, self.zt, tsub, maskb16, *tt)
        oh4 = S.alloc("oh4", [16, 4, NE], F32)
        pr4 = S.alloc("pr4", [16, 4, NE], F32)
        pf4 = S.alloc("pf4", [16, 4], F32)
        shp = [128, 16, 4, NE]
        self.TT("dve", rank_all.ap[:, :, :], rank_all.ap[:, :, :], base.unsqueeze(1).to_broadcast([128, 16, NE]), ALU.add,
                r=[rank_all, rt_], w=[rank_all])
        self.TT("dve", oh4.ap[:, :, :, :], lgall.ap[:, :, 0:NE].unsqueeze(2).to_broadcast(shp),
                lgall.ap[:, :, 32:36].unsqueeze(3).to_broadcast(shp), ALU.is_equal, r=[lgall], w=[oh4])
        self.TT("dve", pr4.ap[:, :, :, :], oh4.ap[:, :, :, :], rank_all.ap[:, :, :].unsqueeze(2).to_broadcast(shp), ALU.mult,
                r=[oh4, rank_all], w=[pr4])
        self.S.op("dve", lambda e, o=pf4.ap[:, :, :], a=pr4.ap[:, :, :, :]: e.reduce_sum(o, a, axis=AX.X), [pr4], [pf4])
        self.CP("dve", posi_v[:, :, :], pf4.ap[:, :, :], r=[pf4], w=[posi])
        self.TT("pool", pr4.ap[:, :, :, :], oh4.ap[:, :, :, :], comb.ap[:, :, :].unsqueeze(2).to_broadcast(shp), ALU.mult,
                r=[oh4, comb, pf4], w=[pr4])
        self.S.op("dve", lambda e, o=wk.ap[:, :, :], a=pr4.ap[:, :, :, :]: e.reduce_sum(o, a, axis=AX.X), [pr4], [wk])
        for xi in range(16):
            for k in range(4):
                self.S.dma_fn("pool", lambda e, o=self.xs_d[:, :], off=posi_v[:, xi, k:k + 1], i_=xn2tm.ap[:, xi, :]:
                              e.indirect_dma_start(out=o, out_offset=bass.IndirectOffsetOnAxis(ap=off, axis=0), in_=i_, in_offset=None,
                                                   bounds_check=self.S.bc_reg, oob_is_err=False),
                              reads=[xsz, xn2tm, posi], writes=[xsd], sem_buf=xn2tm)
        S.release(oh4, pr4, pf4, rank_all, lgall)
        self.x1_bufs = xt
        self.rt_ = rt_
        self.xn2tm = xn2tm

    def phase_moe(self):
        S, P = self.S, self.P
        hx2T, comb, stat, cst = self.hx2T, self.comb, self.stat, self.cst
        identf, Gb = self.identf, self.Gb
        xt = self.x1_bufs
        combT = S.alloc("combT", [T], F32)
        for xi in range(16):
            pb = P[xi % 2]
            self.TR(pb.ap[0:NE, 0:128], comb.ap[:, xi, :], identf, r=[comb, cst], w=[pb])
            self.CP("act", combT.ap[0:NE, xi * 128:(xi + 1) * 128], pb.ap[0:NE, 0:128], r=[pb], w=[combT])
        S.release(comb)
        bias = S.alloc("ebias", [512 + D], F32)
        self.DMA("sp", bias.ap[:, 0:256], self.bgT, w=[bias])
        self.DMA("sp", bias.ap[:, 256:512], self.buT, w=[bias])
        self.DMA("sp", bias.ap[0:NE, 512:512 + D], self.b_down, w=[bias])
        self.TS("dve", bias.ap[:, 256:512], bias.ap[:, 256:512], 1.0, None, ALU.add, r=[bias], w=[bias])
        wg = S.alloc("wg", [KC, D], BF16)
        wu = S.alloc("wu", [KC, D], BF16)
        wd = S.alloc("wd", [KC, D], BF16)
        act = S.alloc("act", [KC, 512], BF16)
        oacc = S.alloc("oacc", [KC, 1024], F32)
        sel = [S.alloc("sel", [128], F32) for _ in range(2)]
        cwb = S.alloc("cw", [512], F32)
        gb_ = [S.alloc("g", [512], F32) for _ in range(2)]
        sb_ = [S.alloc("s", [512], F32) for _ in range(1)]
        ub_ = [S.alloc("u", [512], F32) for _ in range(2)]
        junk = S.alloc("junk4", [512], BF16)
        selv = self.c_sel
        out_bufs = []
        for half in range(2):
            t0 = half * 1024
            for dc in range(KC):
                for tb in range(2):
                    pb = P[4 + (dc * 2 + tb) % 2]
                    self.MM(pb.ap[:, :], bias.ap[0:NE, 512 + dc * 128:512 + (dc + 1) * 128], combT.ap[0:NE, t0 + tb * 512:t0 + (tb + 1) * 512],
                            r=[bias, combT], w=[pb])
                    self.CP("act", oacc.ap[:, dc, tb * 512:(tb + 1) * 512], pb.ap[:, :], r=[pb], w=[oacc])
            for e in range(NE):
                self.load_w(wg.ap[:, :, :], wg, self.w_gate[e], D, D)
                self.load_w(wu.ap[:, :, :], wu, self.w_up[e], D, D)
                self.load_w(wd.ap[:, :, :], wd, self.w_down[e], D, D)
                se = sel[e % 2]
                self.DMA("sp", se.ap[0:NE, :], selv[:, e * 128:(e + 1) * 128], w=[se])
                for tb in range(2):
                    tk = t0 + tb * 512
                    self.MM(P[6].ap[:, :], se.ap[0:NE, :], combT.ap[0:NE, tk:tk + 512], r=[se, combT], w=[P[6]])
                    self.CP("act", cwb.ap[:, :], P[6].ap[:, :], r=[P[6]], w=[cwb])
                    for f in range(KC):
                        pg, pu = P[f % 2], P[2 + f % 2]
                        g_, s_, u_ = gb_[f % 2], sb_[0], ub_[f % 2]
                        for kc in range(KC):
                            self.MM(pg.ap[:, :], wg.ap[:, kc, f * 128:(f + 1) * 128], hx2T.ap[:, kc, tk:tk + 512],
                                    start=(kc == 0), stop=(kc == KC - 1), r=[wg, hx2T], w=[pg])
                        for kc in range(KC):
                            self.MM(pu.ap[:, :], wu.ap[:, kc, f * 128:(f + 1) * 128], hx2T.ap[:, kc, tk:tk + 512],
                                    start=(kc == 0), stop=(kc == KC - 1), r=[wu, hx2T], w=[pu])
                        bcol = e * 8 + f
                        self.TS("dve", g_.ap[:, :], pg.ap[:, :], bias.ap[:, bcol:bcol + 1], 7.0, ALU.add, ALU.min, r=[pg, bias], w=[g_])
                        self.ACT(s_.ap[:, :], g_.ap[:, :], AF.Sigmoid, r=[g_], w=[s_], scale=1.702)
                        self.TS("dve", u_.ap[:, :], pu.ap[:, :], bias.ap[:, 256 + bcol:256 + bcol + 1], -6.0, ALU.add, ALU.max,
                                r=[pu, bias], w=[u_])
                        self.STT(u_.ap[:, :], u_.ap[:, :], 8.0, cwb.ap[:, :], ALU.min, ALU.mult, r=[u_, cwb], w=[u_])
                        self.TT("pool", g_.ap[:, :], g_.ap[:, :], s_.ap[:, :], ALU.mult, r=[g_, s_], w=[g_])
                        self.TT("pool", act.ap[:, f, :], g_.ap[:, :], u_.ap[:, :], ALU.mult, r=[g_, u_], w=[act])
                    for dc in range(KC):
                        pd = P[4 + dc % 2]
                        for f in range(KC):
                            self.MM(pd.ap[:, :], wd.ap[:, f, dc * 128:(dc + 1) * 128], act.ap[:, f, :],
                                    start=(f == 0), stop=(f == KC - 1), r=[wd, act], w=[pd])
                        self.TT("dve", oacc.ap[:, dc, tb * 512:(tb + 1) * 512], oacc.ap[:, dc, tb * 512:(tb + 1) * 512], pd.ap[:, :],
                                ALU.add, r=[oacc, pd], w=[oacc])
            for j in range(8):
                xi = half * 8 + j
                x_ = xt[xi % 2]
                self.DMA("sp", x_.ap[:, :], self.x1d[xi * 128:(xi + 1) * 128, :], w=[x_])
                pa, pb = P[(j % 2) * 2], P[(j % 2) * 2 + 1]
                for dc in range(KC):
                    pp = pa if dc < 4 else pb
                    self.TR(pp.ap[:, (dc % 4) * 128:(dc % 4 + 1) * 128], oacc.ap[:, dc, j * 128:(j + 1) * 128], identf,
                            r=[oacc, cst], w=[pp])
                sc = 40 + (j % 2) * 4
                for hf, pp in enumerate((pa, pb)):
                    self.ACT(junk.ap[:, :], pp.ap[:, :], AF.Square, r=[pp], w=[junk, stat], accum_out=stat.ap[:, sc + hf:sc + hf + 1])
                self.TT("dve", stat.ap[:, sc:sc + 1], stat.ap[:, sc:sc + 1], stat.ap[:, sc + 1:sc + 2], ALU.add, r=[stat], w=[stat])
                self.rstd_from_ss(stat.ap[:, sc:sc + 1], D, stat, stat.ap[:, sc + 2:sc + 3])
                for hf, pp in enumerate((pa, pb)):
                    self.STT(cwb.ap[:, :], pp.ap[:, :], stat.ap[:, sc:sc + 1], Gb.ap[:, 1, hf * 512:(hf + 1) * 512], ALU.mult, ALU.mult,
                             r=[pp, stat, Gb], w=[cwb])
                    self.TT("pool", x_.ap[:, hf * 512:(hf + 1) * 512], x_.ap[:, hf * 512:(hf + 1) * 512], cwb.ap[:, :], ALU.add,
                            r=[x_, cwb], w=[x_])
                self.DMA("pool", self.out[xi * 128:(xi + 1) * 128, :], x_.ap[:, :], r=[x_], sem=x_)
        self.final_bufs = list(xt)

    def phase_moe_sparse(self):
        S, P = self.S, self.P
        stat, cst, cstb, Gb = self.stat, self.cst, self.cstb, self.Gb
        identb, onesf, A2, modb = self.identb, self.onesf, self.A2, self.modb
        xsd, ysd, wk, posi_v, posi = self.xsd, self.ysd, self.wk, self.posi_v, self.posi
        meta, nt_i, sidx_i = self.meta, self.nt_i, self.sidx_i
        S.release(self.comb, self.xn2tm)
        bias = S.alloc("ebias", [512], F32)
        self.DMA("sp", bias.ap[:, 0:256], self.bgT, w=[bias])
        self.DMA("sp", bias.ap[:, 256:512], self.buT, w=[bias])
        self.TS("dve", bias.ap[:, 256:512], bias.ap[:, 256:512], 1.0, None, ALU.add, r=[bias], w=[bias])
        W = [[S.alloc("w%d" % k, [KC, D], BF16) for k in range(3)] for _ in range(2)]
        xg0 = [S.alloc("xg0", [2, D], BF16) for _ in range(2)]
        xgn = [S.alloc("xgn", [2, D], BF16) for _ in range(2)]
        xg = xg0 + xgn
        XT = [S.alloc("XT", [KC, SG], BF16) for _ in range(2)]
        acts = [S.alloc("act", [SG], BF16) for _ in range(KC)]
        gb_ = [S.alloc("g", [SG], F32) for _ in range(2)]
        sb_ = [S.alloc("s", [SG], F32) for _ in range(2)]
        ub_ = [S.alloc("u", [SG], F32) for _ in range(2)]
        ys = [S.alloc("ys", [D], F32) for _ in range(2)]
        bdr = [S.alloc("bdr", [D], F32) for _ in range(1)]
        bdrb = [S.alloc("bdrb", [D], BF16) for _ in range(2)]
        srcs = (self.w_gate, self.w_up, self.w_down)

        def prep(x_, xt_):
            for bk in range(2):
                pt = P[4 + bk]
                ptb = pt.ap.bitcast(BF16)
                for k4 in range(4):
                    kc = bk * 4 + k4
                    for h in range(2):
                        self.TR(ptb[:, k4 * SG + h * 128:k4 * SG + (h + 1) * 128], x_.ap[:, h, kc * 128:(kc + 1) * 128], identb,
                                r=[x_, cstb], w=[pt])
                for k4 in range(4):
                    kc = bk * 4 + k4
                    if k4 % 2 == 0:
                        self.TS("dve", xt_.ap[:, kc, :], ptb[:, k4 * SG:(k4 + 1) * SG], A2[:, kc:kc + 1], modb.ap[:, 24 + kc, 0:1],
                                ALU.mult, ALU.add, r=[pt, self.vec, modb], w=[xt_])
                    else:
                        self.ACT(xt_.ap[:, kc, :], ptb[:, k4 * SG:(k4 + 1) * SG], AF.Identity, r=[pt, self.vec, modb], w=[xt_],
                                 scale=A2[:, kc:kc + 1], bias=modb.ap[:, 24 + kc, 0:1])

        def loadw(e):
            for k in range(3):
                self.load_w(W[e % 2][k].ap[:, :, :], W[e % 2][k], srcs[k][e], D, D)
        def gather(buf, e, j):
            for h in range(2):
                self.S.dma_fn("pool", lambda en, o=buf.ap[:, h, :], off=sidx_i[:, e, 2 * j + h:2 * j + h + 1], i_=self.xs_d[:, :]:
                              en.indirect_dma_start(out=o, out_offset=None, in_=i_, in_offset=bass.IndirectOffsetOnAxis(ap=off, axis=0),
                                                    bounds_check=self.S.bc_reg, oob_is_err=False),
                              reads=[xsd, meta], writes=[buf], sem_buf=buf)
        XT0 = [S.alloc("XT0", [KC, SG], BF16) for _ in range(2)]
        gather(xg0[0], 0, 0)
        gather(xg0[1], 1, 0)
        loadw(0)
        prep(xg0[0], XT0[0])
        nst = 0
        ngs = 0
        for e in range(NE):
            gather(xgn[1], e, 1)
            if e + 2 < NE:
                gather(xg0[e % 2], e + 2, 0)
            if e + 1 < NE:
                loadw(e + 1)
            wg, wu, wd = W[e % 2]
            br = bdr[0]
            self.DMA("sp", br.ap[0:1, :], self.b_down[e:e + 1, :], w=[br])
            brb = bdrb[e % 2]
            self.CP("act", brb.ap[0:1, :], br.ap[0:1, :], r=[br], w=[brb])
            if e + 1 < NE:
                prep(xg0[(e + 1) % 2], XT0[(e + 1) % 2])
            S.regload(meta, nt_i[0:1, e:e + 1])
            for j in range(NSTEP):
                S.begin_group(j)
                xt_ = XT0[e % 2] if j == 0 else XT[j % 2]
                ngs += 1
                if j + 2 < NSTEP:
                    gather(xgn[j % 2], e, j + 2)
                for f in range(KC):
                    pg, pu = P[f % 2], P[2 + f % 2]
                    g_, s_, u_ = gb_[f % 2], sb_[f % 2], ub_[f % 2]
                    for kc in range(KC):
                        self.MM(pg.ap[:, 0:SG], wg.ap[:, kc, f * 128:(f + 1) * 128], xt_.ap[:, kc, :],
                                start=(kc == 0), stop=(kc == KC - 1), r=[wg, xt_], w=[pg])
                    for kc in range(KC):
                        self.MM(pu.ap[:, 0:SG], wu.ap[:, kc, f * 128:(f + 1) * 128], xt_.ap[:, kc, :],
                                start=(kc == 0), stop=(kc == KC - 1), r=[wu, xt_], w=[pu])
                    bcol = e * 8 + f
                    self.TS("dve", g_.ap[:, :], pg.ap[:, 0:SG], bias.ap[:, bcol:bcol + 1], 7.0, ALU.add, ALU.min, r=[pg, bias], w=[g_])
                    self.ACT(s_.ap[:, :], g_.ap[:, :], AF.Sigmoid, r=[g_], w=[s_], scale=1.702)
                    self.TS("dve", u_.ap[:, :], pu.ap[:, 0:SG], bias.ap[:, 256 + bcol:256 + bcol + 1], -6.0, ALU.add, ALU.max,
                            r=[pu, bias], w=[u_])
                    self.TT("pool", g_.ap[:, :], g_.ap[:, :], s_.ap[:, :], ALU.mult, r=[g_, s_], w=[g_])
                    self.STT(acts[f].ap[:, :], u_.ap[:, :], 8.0, g_.ap[:, :], ALU.min, ALU.mult, r=[u_, g_], w=[acts[f]])
                if j + 1 < NSTEP:
                    prep(xgn[(j + 1) % 2], XT[(j + 1) % 2])
                for h in range(2):
                    y_ = ys[nst % 2]
                    nst += 1
                    for hf in range(2):
                        pd = P[6 + hf]
                        for f in range(KC):
                            self.MM(pd.ap[:, :], acts[f].ap[:, h * 128:(h + 1) * 128], wd.ap[:, f, hf * 512:(hf + 1) * 512],
                                    start=(f == 0), stop=False, r=[acts[f], wd], w=[pd])
                        self.MM(pd.ap[:, :], self.onesb[0:1, :], brb.ap[0:1, hf * 512:(hf + 1) * 512], start=False, stop=True,
                                r=[cstb, brb], w=[pd])
                        self.CP("act" if hf == 0 else "dve", y_.ap[:, hf * 512:(hf + 1) * 512], pd.ap[:, :], r=[pd], w=[y_])
                    self.S.dma_fn("pool", lambda en, o=self.ys_d[:, :], off=sidx_i[:, e, 2 * j + h:2 * j + h + 1], i_=y_.ap[:, :]:
                                  en.indirect_dma_start(out=o, out_offset=bass.IndirectOffsetOnAxis(ap=off, axis=0), in_=i_, in_offset=None,
                                                        bounds_check=self.S.bc_reg, oob_is_err=False),
                                  reads=[y_, meta], writes=[ysd], sem_buf=y_)
                S.end_group()
        S.release(*W[0], *W[1], *xg, *XT, *XT0, *acts, *gb_, *sb_, *ub_, *bdr, *bdrb, bias)
        yk = [S.alloc("yk", [D], F32) for _ in range(8)]
        acc = [S.alloc("acc", [D], F32) for _ in range(2)]
        junk = S.alloc("junk5", [D], BF16)
        xt = self.x1_bufs
        outb = []
        for xi in range(16):
            x_ = xt[xi % 2]
            a_ = acc[xi % 2]
            self.DMA("sp", x_.ap[:, :], self.x1d[xi * 128:(xi + 1) * 128, :], w=[x_])
            for k in range(4):
                y_ = yk[(xi % 2) * 4 + k]
                self.S.dma_fn("pool", lambda e, o=y_.ap[:, :], off=posi_v[:, xi, k:k + 1], i_=self.ys_d[:, :]:
                              e.indirect_dma_start(out=o, out_offset=None, in_=i_, in_offset=bass.IndirectOffsetOnAxis(ap=off, axis=0),
                                                   bounds_check=self.S.bc_reg, oob_is_err=False),
                              reads=[ysd, posi], writes=[y_], sem_buf=y_)
                if k == 0:
                    self.TS("dve", a_.ap[:, :], y_.ap[:, :], wk.ap[:, xi, 0:1], None, ALU.mult, r=[y_, wk], w=[a_])
                else:
                    self.STT(a_.ap[:, :], y_.ap[:, :], wk.ap[:, xi, k:k + 1], a_.ap[:, :], ALU.mult, ALU.add, r=[y_, wk, a_], w=[a_])
            sc = 40 + (xi % 2) * 4
            self.ACT(junk.ap[:, :], a_.ap[:, :], AF.Square, r=[a_], w=[junk, stat], accum_out=stat.ap[:, sc:sc + 1])
            self.rstd_from_ss(stat.ap[:, sc:sc + 1], D, stat, stat.ap[:, sc + 2:sc + 3])
            if self.dbg is not None and xi in (0, 9):
                self.dump((0 if xi == 0 else 1) * 1024, a_, a_.ap[:, :], 1024)
                self.dump(2048 + (0 if xi == 0 else 1) * 8, wk, wk.ap[:, xi, :], 4)
                self.dump(2048 + 16 + (0 if xi == 0 else 1) * 8, posi, posi.ap[:, xi, :], 4)
            self.STT(a_.ap[:, :], a_.ap[:, :], stat.ap[:, sc:sc + 1], Gb.ap[:, 1, :], ALU.mult, ALU.mult, r=[a_, stat, Gb], w=[a_])
            self.TT("pool", x_.ap[:, :], x_.ap[:, :], a_.ap[:, :], ALU.add, r=[x_, a_], w=[x_])
            self.DMA("sp", self.out[xi * 128:(xi + 1) * 128, :], x_.ap[:, :], r=[x_], sem=x_)
        self.final_bufs = list(xt)

    def finish(self):
        S = self.S
        if self.stage < 90:
            z = S.alloc("zout", [128], F32)
            self.MS("pool", z.ap[:, :], 0.0, w=[z])
            self.DMA("pool", self.out[0:128, 0:128], z.ap[:, :], r=[z], sem=z)
            self.dbg_bufs.append(z)
        S.emit(final_bufs=self.dbg_bufs + getattr(self, "final_bufs", []))


def _host_inputs(inputs, b):
    f = lambda a: np.ascontiguousarray(np.asarray(a, dtype=np.float32))
    cst = _consts()
    m = {}
    m["x"] = f(inputs["x"][b])
    m["ctx"] = f(inputs["ctx"][b])
    cc = np.stack([_kc_layout(f(inputs["c"][b])), _kc_layout(f(inputs["c_ctx"]))], axis=-1)
    m["cc"] = f(cc.reshape(128, 16))
    m["w_mod"] = f(inputs["w_mod"][0])
    m["bmodT"] = _kc_layout(f(inputs["b_mod"][0]))
    m["bmod_row"] = f(inputs["b_mod"][0]).reshape(1, -1)
    gT = np.stack([_kc_layout(f(inputs[k][0])) for k in ("g_pre_mix", "g_post_mix", "g_pre_ffn", "g_post_ffn")], axis=1)
    m["gT"] = f(gT.reshape(128, 32))
    m["gpost_row"] = f(np.concatenate([f(inputs["g_post_mix"][0]), f(inputs["g_post_ffn"][0])]).reshape(1, -1))
    m["w_in"] = f(inputs["w_in"][0])
    wa2 = np.zeros((32, 512), np.float32)
    wa2[0:16, 0:256] = inputs["gla_w_a2_f"][0]
    wa2[16:32, 256:512] = inputs["gla_w_a2_b"][0]
    m["wa2"] = wa2
    m["ba_row"] = f(np.concatenate([f(inputs["gla_b_a_f"][0]), f(inputs["gla_b_a_b"][0])]).reshape(1, 512))
    m["gnb"] = f(np.tile(f(inputs["gla_g_norm"][0])[None, :], (128, 4)))
    m["gq"] = _kc_layout(f(inputs["mla_g_q"][0]))
    m["gkv"] = _kc_layout(f(inputs["mla_g_kv"][0]))
    m["w_uq"] = f(inputs["mla_w_uq"][0])
    m["w_uk"] = f(inputs["mla_w_uk"][0])
    m["w_uv"] = f(inputs["mla_w_uv"][0])
    m["w_br_gla"] = f(inputs["w_br_gla"][0])
    m["w_br_mla"] = f(inputs["w_br_mla"][0])
    m["w_out"] = f(inputs["w_out"][0])
    m["router_w"] = f(inputs["router_w"][0])
    m["router_b"] = f(inputs["router_b"][0]).reshape(1, NE)
    m["w_gate"] = f(inputs["w_gate"][0])
    m["w_up"] = f(inputs["w_up"][0])
    m["w_down"] = f(inputs["w_down"][0])
    bg = f(inputs["b_gate"][0]).reshape(NE, 8, 128).transpose(2, 0, 1)
    bu = f(inputs["b_up"][0]).reshape(NE, 8, 128).transpose(2, 0, 1)
    m["bgT"] = f(bg.reshape(128, NE * 8))
    m["buT"] = f(bu.reshape(128, NE * 8))
    m["b_down"] = f(inputs["b_down"][0])
    m["c_tri"] = f(cst["tri"].reshape(128, 512))
    m["c_mask"] = f(cst["mask"].reshape(128, 1024))
    m["c_ident"] = cst["ident"]
    m["c_ropet"] = f(cst["ropet"].reshape(128, 2 * NTOK))
    m["c_e96"] = cst["e96"]
    m["c_sel"] = cst["sel"]
    m["c_rt"] = cst["rt"]
    return m


_SHARED = ("w_mod", "bmodT", "bmod_row", "gT", "gpost_row", "w_in", "wa2", "ba_row", "gnb", "gq", "gkv", "w_uq", "w_uk",
           "w_uv", "w_br_gla", "w_br_mla", "w_out", "router_w", "router_b", "w_gate", "w_up", "w_down", "bgT", "buT",
           "b_down", "c_tri", "c_mask", "c_ident", "c_ropet", "c_e96", "c_sel", "c_rt")


def kernel(**inputs):
    k = K(stage=99)
    m0 = _host_inputs(inputs, 0)
    in_maps = [m0]
    for b in range(1, 8):
        mb = dict(m0)
        mb["x"] = np.ascontiguousarray(np.asarray(inputs["x"][b], dtype=np.float32))
        mb["ctx"] = np.ascontiguousarray(np.asarray(inputs["ctx"][b], dtype=np.float32))
        cc = np.stack([_kc_layout(np.asarray(inputs["c"][b], np.float32)),
                       _kc_layout(np.asarray(inputs["c_ctx"], np.float32))], axis=-1)
        mb["cc"] = np.ascontiguousarray(cc.reshape(128, 16))
        in_maps.append(mb)
    res = run_bass_kernel_spmd(k.nc, in_maps, core_ids=list(range(8)))
    return np.stack([np.asarray(r["out"], dtype=np.float32) for r in res.results], axis=0)
```

```python
import numpy as np
from contextlib import ExitStack
import concourse.bass as bass
import concourse.mybir as mybir
from concourse.bass_utils import run_bass_kernel_spmd

F32 = mybir.dt.float32
BF16 = mybir.dt.bfloat16
I32 = mybir.dt.int32
AF = mybir.ActivationFunctionType
ALU = mybir.AluOpType
AX = mybir.AxisListType
ENGS = ("pe", "act", "dve", "pool", "sp")

T = 2048
TC = 256
NT = 18
NTOK = 2304
D = 1024
KC = 8
D_IN = 4032
EPS = 1e-6
NE = 32
MLA_SCALE = 96 ** -0.5
SG = 256
NSTEP = 8
NSLOT = T * 4 + NE * SG
MOE_MODE = "sparse"


class Buf:
    __slots__ = ("name", "writers", "readers", "dma_sem", "dma_cnt", "ap", "inherit", "off", "words")

    def __init__(self, name, ap=None):
        self.name = name
        self.ap = ap
        self.writers = []
        self.readers = []
        self.inherit = []
        self.off = None
        self.dma_sem = None
        self.dma_cnt = 0


class Op:
    __slots__ = ("eng", "fn", "idx", "deps", "marked", "dma_buf", "dma_val", "mark_no", "grp")

    def __init__(self, eng, fn):
        self.eng = eng
        self.fn = fn
        self.grp = None
        self.deps = []
        self.marked = False
        self.dma_buf = None
        self.dma_val = 0
        self.mark_no = 0


class Sched:
    def __init__(self, nc, arena_words):
        self.nc = nc
        self.ops = {e: [] for e in ENGS}
        self.bufs = []
        self.arena = nc.alloc_sbuf_tensor("arena", [128, arena_words], F32)
        self.free = [(0, arena_words)]
        self.dead = []
        self.nbuf = 0
        self.cur_grp = None
        self.ngrp = 0

    def alloc(self, name, free_shape, dt=F32):
        n = 1
        for s in free_shape:
            n *= s
        words = (n * (2 if dt == BF16 else 4) + 3) // 4
        words = (words + 7) // 8 * 8
        off = None
        for i, (o, sz) in enumerate(self.free):
            if sz >= words:
                off = o
                if sz == words:
                    self.free.pop(i)
                else:
                    self.free[i] = (o + words, sz - words)
                break
        if off is None:
            raise RuntimeError("arena full allocating %s (%d words); free=%s" % (name, words, self.free))
        v = self.arena[:, off:off + words]
        if dt == BF16:
            v = v.bitcast(BF16)
        v = v[:, 0:n]
        if len(free_shape) == 2:
            v = v.rearrange("p (a b) -> p a b", b=free_shape[1])
        elif len(free_shape) == 3:
            v = v.rearrange("p (a b c) -> p a b c", b=free_shape[1], c=free_shape[2])
        self.nbuf += 1
        b = Buf("%s_%d" % (name, self.nbuf), v)
        b.off, b.words = off, words
        for (o, w, toks) in self.dead:
            if o < off + words and off < o + w:
                b.inherit.extend(toks)
        self.bufs.append(b)
        return b

    def release(self, *bufs):
        for b in bufs:
            toks = [("op", t[1], "raw") if t[0] == "op" else t for t in (b.writers + b.readers + b.inherit)]
            self.dead.append((b.off, b.words, toks))
            self.free.append((b.off, b.words))
        self.free.sort()
        m = []
        for o, w in self.free:
            if m and m[-1][0] + m[-1][1] == o:
                m[-1] = (m[-1][0], m[-1][1] + w)
            else:
                m.append((o, w))
        self.free = m

    def psum(self, name):
        t = self.nc.alloc_psum_tensor(name, [128, 512], F32)
        b = Buf(name, t[:, :])
        self.bufs.append(b)
        return b

    def virt(self, name):
        b = Buf(name, None)
        self.bufs.append(b)
        return b

    def _add(self, eng, fn, reads, writes, dma_buf=None):
        op = Op(eng, fn)
        op.idx = len(self.ops[eng])
        op.grp = self.cur_grp
        is_dma = dma_buf is not None
        deps = []
        for b in reads:
            deps.extend(b.writers)
            deps.extend(b.inherit)
            if b.off is None:
                deps.extend([("op", t[1], "raw") for t in b.readers if t[0] == "op" and t[1].eng != eng])
        for b in writes:
            deps.extend(b.inherit)
            deps.extend(b.readers)
            if is_dma and not b.readers and b.writers and all(w[0] == "dma" for w in b.writers):
                pass
            else:
                deps.extend(b.writers)
        fl = []
        for d in deps:
            if d[0] == "op":
                o = d[1]
                if o.eng == eng and not is_dma:
                    if eng == "pe" or eng == "sp":
                        continue
                    if d[2] != "raw":
                        continue
            fl.append(d)
        op.deps = fl
        if is_dma:
            dma_buf.dma_cnt += 16
            op.dma_buf = dma_buf
            op.dma_val = dma_buf.dma_cnt
            tok_w = ("dma", dma_buf, op.dma_val)
            tok_r = tok_w
        else:
            tok_w = ("op", op, "raw")
            tok_r = ("op", op, "war")
        for b in reads:
            b.readers.append(tok_r)
            if len(b.readers) > 64:
                last = {}
                keep = []
                for t in b.readers:
                    if t[0] == "op":
                        last[t[1].eng] = t
                    else:
                        keep.append(t)
                b.readers = keep[-32:] + list(last.values())
        for b in writes:
            if is_dma and not b.readers and b.writers and all(w[0] == "dma" for w in b.writers):
                b.writers.append(tok_w)
            else:
                b.writers = [tok_w]
                b.readers = []
        self.ops[eng].append(op)
        return op

    def op(self, eng, fn, reads=(), writes=()):
        return self._add(eng, fn, list(reads), list(writes))

    def dma(self, eng, out_ap, in_ap, reads=(), writes=(), sem_buf=None, **kw):
        if sem_buf is None:
            sem_buf = (list(writes) + list(reads))[0]

        def fn(e, out_ap=out_ap, in_ap=in_ap, kw=kw):
            return e.dma_start(out=out_ap, in_=in_ap, **kw)
        return self._add(eng, fn, list(reads), list(writes), dma_buf=sem_buf)

    def begin_group(self, thr):
        self.ngrp += 1
        self.cur_grp = (self.ngrp, thr)

    def end_group(self):
        self.cur_grp = None

    def regload(self, src_buf, ap):
        for e in ENGS:
            self._add(e, ("regload", ap), [src_buf], [])

    def dma_fn(self, eng, fn, reads=(), writes=(), sem_buf=None):
        return self._add(eng, fn, list(reads), list(writes), dma_buf=sem_buf)

    def emit(self, final_bufs=()):
        nc = self.nc
        for e in ENGS:
            for op in self.ops[e]:
                for d in op.deps:
                    if d[0] == "op":
                        d[1].marked = True
        for e in ENGS:
            n = 0
            for op in self.ops[e]:
                if op.marked:
                    n += 1
                    op.mark_no = n
        with ExitStack() as st:
            esem = {e: st.enter_context(nc.semaphore("s_" + e)) for e in ENGS}
            for b in self.bufs:
                if b.dma_cnt > 0:
                    b.dma_sem = st.enter_context(nc.semaphore("d_" + b.name))
            self.bc_reg = st.enter_context(nc.gpsimd.register("rbc"))
            engobj = {"pe": nc.tensor, "act": nc.scalar, "dve": nc.vector, "pool": nc.gpsimd, "sp": nc.sync}
            self.cregs = {e: st.enter_context(engobj[e].register("creg_" + e)) for e in ENGS}
            print("semaphores used:", 5 + sum(1 for b in self.bufs if b.dma_cnt > 0))
            block = st.enter_context(nc.Block())
            handles = {"pe": block.tensor, "act": block.scalar, "dve": block.vector,
                       "pool": block.gpsimd, "sp": block.sync}
            stats = {}
            for e in ENGS:
                ops = self.ops[e]

                def body(eng, ops=ops, e=e):
                    waited = {}
                    nw = [0]
                    if e == "pool":
                        eng.reg_mov(self.bc_reg, NSLOT - 1)
                    creg = self.cregs[e]

                    def emit_op(op, wt):
                        need = {}
                        for d in op.deps:
                            if d[0] == "op":
                                key = ("e", d[1].eng)
                                val = d[1].mark_no
                            else:
                                key = ("d", d[1].name)
                                val = d[2]
                            if val > need.get(key, (0, None))[0]:
                                need[key] = (val, d)
                        for key, (val, d) in need.items():
                            if wt.get(key, 0) >= val:
                                continue
                            wt[key] = val
                            sem = esem[d[1].eng] if d[0] == "op" else d[1].dma_sem
                            eng.wait_ge(sem, val)
                            nw[0] += 1
                        if isinstance(op.fn, tuple):
                            eng.reg_load(creg, op.fn[1])
                            return
                        ins = op.fn(eng)
                        if op.dma_buf is not None:
                            ins.then_inc(op.dma_buf.dma_sem, 16)
                        elif op.marked:
                            ins.then_inc(esem[e], 1)
                    def comp_of(gops):
                        comp = []
                        marked = [o for o in gops if o.marked and o.dma_buf is None]
                        if marked:
                            comp.append((esem[e], marked[0].mark_no - 1, len(marked)))
                        dm = {}
                        for o in gops:
                            if o.dma_buf is not None:
                                if o.dma_buf.name not in dm:
                                    dm[o.dma_buf.name] = [o.dma_buf.dma_sem, o.dma_val - 16, 0]
                                dm[o.dma_buf.name][2] += 16
                        comp.extend(tuple(v) for v in dm.values())
                        return comp

                    def chain(runs, k, wt):
                        rest = [o for r_ in runs[k:] for o in r_]
                        thr = runs[k][0].grp[1]
                        with eng.If_lt(creg, thr + 1):
                            for sem, before, delta in comp_of(rest):
                                if before > 0:
                                    eng.wait_ge(sem, before)
                                eng.sem_inc(sem, delta)
                        with eng.Else():
                            for o in runs[k]:
                                emit_op(o, wt)
                            if k + 1 < len(runs):
                                chain(runs, k + 1, wt)
                    i = 0
                    n = len(ops)
                    while i < n:
                        op = ops[i]
                        if op.grp is None:
                            emit_op(op, waited)
                            i += 1
                            continue
                        runs = []
                        j = i
                        while j < n and ops[j].grp is not None and (not runs or ops[j].grp[1] > runs[-1][0].grp[1] or ops[j].grp is runs[-1][0].grp):
                            if runs and ops[j].grp is runs[-1][0].grp:
                                runs[-1].append(ops[j])
                            else:
                                runs.append([ops[j]])
                            j += 1
                        chain(runs, 0, dict(waited))
                        i = j
                    if e == "sp":
                        for b in final_bufs:
                            if b.dma_cnt:
                                eng.wait_ge(b.dma_sem, b.dma_cnt)
                    stats[e] = (len(ops), nw[0])
                handles[e](body)
            self.stats = stats


def _consts():
    c = {}
    idx = np.arange(128)
    same = (idx[:, None] // 64) == (idx[None, :] // 64)
    s = idx[:, None]
    t = idx[None, :]
    tri = np.zeros((128, 4, 128), np.float32)
    tri[:, 0] = same & (s <= t)
    tri[:, 1] = same & (s >= t)
    tri[:, 2] = same & (s > t)
    tri[:, 3] = same & (s < t)
    c["tri"] = tri
    mask = np.zeros((128, 2, 4, 128), np.float32)
    mask[:, 0] = (same & (t >= s))[:, None, :]
    mask[:, 1] = (same & (t <= s))[:, None, :]
    c["mask"] = mask.reshape(128, 2, 512)
    c["ident"] = np.eye(128, dtype=np.float32)
    half = 16
    inv_freq = (10000.0 ** (-np.arange(0, half, 2, dtype=np.float32) / half)).astype(np.float32)
    tok = np.arange(T)
    ang_row = (tok // 64).astype(np.float32)[:, None] * inv_freq
    ang_col = (tok % 64).astype(np.float32)[:, None] * inv_freq
    cosr, sinr = np.cos(ang_row), np.sin(ang_row)
    cosc, sinc = np.cos(ang_col), np.sin(ang_col)
    cos32 = np.concatenate([cosr, cosr, cosc, cosc], axis=1)
    sin32 = np.concatenate([-sinr, sinr, -sinc, sinc], axis=1)
    ropet = np.zeros((128, 2, NTOK), np.float32)
    ropet[64:96, 0, :TC] = 1.0
    ropet[64:96, 0, TC:] = cos32.T
    ropet[64:96, 1, TC:] = sin32.T
    c["ropet"] = ropet
    e96 = np.zeros((128, 128), np.float32)
    e96[:96, 96] = 1.0
    c["e96"] = e96
    sel = np.zeros((NE, NE, 128), np.float32)
    for e in range(NE):
        sel[e, e, :] = 1.0
    c["sel"] = sel.reshape(NE, NE * 128)
    rt = np.zeros((128, 176), np.float32)
    rt[:, 0:128] = (s < t)
    ke = np.arange(NE)
    rt[0:NE, 128:160] = (ke[:, None] < ke[None, :])
    rt[:, 160:176] = np.arange(16, dtype=np.float32)[None, :] * 128 + idx[:, None]
    c["rt"] = rt
    return c


def _kc_layout(v):
    return np.ascontiguousarray(v.reshape(-1, 128).T)


class K:
    def __init__(self, stage=99, dbg=False):
        self.stage = stage
        nc = bass.Bass("TRN2", target_bir_lowering=False)
        self.nc = nc
        self.S = Sched(nc, 51200 + 1000)
        S = self.S
        dt = nc.dram_tensor
        self.din = {}

        def inp(name, shape):
            self.din[name] = dt(name, list(shape), F32, kind="ExternalInput").ap()
            return self.din[name]
        self.x = inp("x", [T, D])
        self.ctx = inp("ctx", [TC, D])
        self.cc = inp("cc", [128, 16])
        self.w_mod = inp("w_mod", [D, 6 * D])
        self.bmodT = inp("bmodT", [128, 48])
        self.bmod_row = inp("bmod_row", [1, 6 * D])
        self.gT = inp("gT", [128, 32])
        self.gpost_row = inp("gpost_row", [1, 2 * D])
        self.w_in = inp("w_in", [D, D_IN])
        self.wa2 = inp("wa2", [32, 512])
        self.ba_row = inp("ba_row", [1, 512])
        self.gnb = inp("gnb", [128, 512])
        self.gq = inp("gq", [128, 2])
        self.gkv = inp("gkv", [128, 1])
        self.w_uq = inp("w_uq", [256, 768])
        self.w_uk = inp("w_uk", [128, 512])
        self.w_uv = inp("w_uv", [128, 512])
        self.w_br_gla = inp("w_br_gla", [512, D])
        self.w_br_mla = inp("w_br_mla", [512, D])
        self.w_out = inp("w_out", [D, D])
        self.router_w = inp("router_w", [D, NE])
        self.router_b = inp("router_b", [1, NE])
        self.w_gate = inp("w_gate", [NE, D, D])
        self.w_up = inp("w_up", [NE, D, D])
        self.w_down = inp("w_down", [NE, D, D])
        self.bgT = inp("bgT", [128, NE * 8])
        self.buT = inp("buT", [128, NE * 8])
        self.b_down = inp("b_down", [NE, D])
        self.c_tri = inp("c_tri", [128, 512])
        self.c_mask = inp("c_mask", [128, 1024])
        self.c_ident = inp("c_ident", [128, 128])
        self.c_ropet = inp("c_ropet", [128, 2 * NTOK])
        self.c_e96 = inp("c_e96", [128, 128])
        self.c_sel = inp("c_sel", [NE, NE * 128])
        self.c_rt = inp("c_rt", [128, 176])
        self.xs_d = dt("xs_d", [NSLOT, D], BF16, kind="Internal").ap()
        self.ys_d = dt("ys_d", [NSLOT, D], F32, kind="Internal").ap()
        self.out = dt("out", [T, D], F32, kind="ExternalOutput").ap()
        self.x1d = dt("x1d", [T, D], F32, kind="Internal").ap()
        self.dbg = dt("dbg", [128, 8192], F32, kind="ExternalOutput").ap() if dbg else None
        self.P = [S.psum("ps%d" % i) for i in range(8)]
        self.wq_i = 0
        self.build()

    def MM(self, out, lhsT, rhs, start=True, stop=True, r=(), w=()):
        self.S.op("pe", lambda e: e.matmul(out, lhsT, rhs, start=start, stop=stop, skip_group_check=True), r, w)

    def TR(self, out, in_, ident, r=(), w=()):
        self.S.op("pe", lambda e: e.transpose(out, in_, ident), r, w)

    def ACT(self, out, in_, func, r=(), w=(), **kw):
        self.S.op("act", lambda e: e.activation(out, in_, func, **kw), r, w)

    def TT(self, eng, out, a, b, op, r=(), w=()):
        self.S.op(eng, lambda e: e.tensor_tensor(out, a, b, op=op), r, w)

    def TS(self, eng, out, a, s1, s2, op0, op1=None, r=(), w=()):
        if op1 is None:
            self.S.op(eng, lambda e: e.tensor_scalar(out, a, s1, None, op0=op0), r, w)
        else:
            self.S.op(eng, lambda e: e.tensor_scalar(out, a, s1, s2, op0=op0, op1=op1), r, w)

    def STT(self, out, a, s, b, op0, op1, r=(), w=()):
        self.S.op("dve", lambda e: e.scalar_tensor_tensor(out, a, s, b, op0=op0, op1=op1), r, w)

    def CP(self, eng, out, in_, r=(), w=()):
        if eng == "act":
            self.S.op("act", lambda e: e.copy(out, in_), r, w)
        else:
            self.S.op(eng, lambda e: e.tensor_copy(out, in_), r, w)

    def MS(self, eng, ap, val, w=()):
        self.S.op(eng, lambda e: e.memset(ap, val), (), w)

    def DMA(self, q, out, in_, r=(), w=(), sem=None, **kw):
        self.S.dma(q, out, in_, reads=r, writes=w, sem_buf=sem, **kw)

    def rstd_from_ss(self, ss_ap, n, buf, tmp_ap):
        self.ACT(tmp_ap, ss_ap, AF.Ln, r=[buf], w=[buf], scale=1.0 / n, bias=self.eps_ap)
        self.ACT(ss_ap, tmp_ap, AF.Exp, r=[buf], w=[buf], scale=-0.5)

    def load_w(self, dst_ap, dst_buf, src_ap, rows, cols, q="pool"):
        if rows > 128:
            self.DMA("pool", dst_ap, src_ap.rearrange("(k p) n -> p k n", p=128), w=[dst_buf])
        else:
            self.DMA("pool", dst_ap[0:rows, 0:cols], src_ap, w=[dst_buf])

    def dump(self, col, buf, ap, ncols, parts=128):
        if self.dbg is None:
            return
        S = self.S
        tmp = S.alloc("dbgtmp", [ncols], F32)
        self.CP("dve", tmp.ap[0:parts, :], ap, r=[buf], w=[tmp])
        self.DMA("pool", self.dbg[0:parts, col:col + ncols], tmp.ap[0:parts, :], r=[tmp], sem=tmp)
        self.dbg_bufs.append(tmp)

    def build(self):
        S = self.S
        P = self.P
        self.dbg_bufs = []
        cst = S.alloc("cst", [2048], F32)
        tri = cst.ap[:, 0:512].rearrange("p (a b) -> p a b", b=128)
        identf = cst.ap[:, 512:640]
        e96f = cst.ap[:, 640:768]
        self.eps_ap = cst.ap[:, 768:769]
        onesf = cst.ap[:, 896:1024]
        self.DMA("sp", cst.ap[:, 0:512], self.c_tri, w=[cst])
        self.DMA("sp", identf, self.c_ident, w=[cst])
        self.DMA("sp", e96f, self.c_e96, w=[cst])
        self.MS("pool", cst.ap[:, 768:769], EPS, w=[cst])
        self.MS("pool", onesf, 1.0, w=[cst])
        self.rowmask = cst.ap[:, 1024:1026]
        self.MS("pool", cst.ap[:, 1024:1026], 0.0, w=[cst])
        self.MS("pool", cst.ap[0:64, 1024:1025], 1.0, w=[cst])
        self.MS("pool", cst.ap[64:128, 1025:1026], 1.0, w=[cst])
        cstb = S.alloc("cstb", [2048], BF16)
        identb = cstb.ap[:, 0:128]
        onesb = cstb.ap[:, 128:256]
        e96b = cstb.ap[:, 256:384]
        maskb = cstb.ap[:, 1024:2048].rearrange("p (a b) -> p a b", b=512)
        self.CP("pool", identb, identf, r=[cst], w=[cstb])
        self.CP("pool", onesb, onesf, r=[cst], w=[cstb])
        self.CP("pool", e96b, e96f, r=[cst], w=[cstb])
        self.wstage = [S.alloc("wst", [8, 256], F32) for _ in range(3)]
        mstage = self.wstage[0]
        self.DMA("sp", mstage.ap[:, 0:4, :], self.c_mask.rearrange("p (a b) -> p a b", b=256), w=[mstage])
        self.CP("pool", cstb.ap[:, 1024:2048].rearrange("p (a b) -> p a b", b=256), mstage.ap[:, 0:4, :], r=[mstage], w=[cstb])
        stat = S.alloc("stat", [64], F32)

        modb = S.alloc("mod", [48, 2], F32)
        vec = S.alloc("vec", [16 + 48 + 32 + 32 + 16 + 16 + 8 + 8], F32)
        cc = vec.ap[:, 0:16].rearrange("p (a b) -> p a b", b=2)
        bmodT = vec.ap[:, 16:64]
        gT = vec.ap[:, 64:96].rearrange("p (a b) -> p a b", b=8)
        A1 = vec.ap[:, 96:112].rearrange("p (a b) -> p a b", b=2)
        B1v = None
        A2 = vec.ap[:, 112:120]
        gqv = vec.ap[:, 128:130]
        gkvv = vec.ap[:, 130:131]
        self.DMA("sp", vec.ap[:, 0:16], self.cc, w=[vec])
        self.DMA("sp", bmodT, self.bmodT, w=[vec])
        self.DMA("sp", vec.ap[:, 64:96], self.gT, w=[vec])
        self.DMA("sp", gqv, self.gq, w=[vec])
        self.DMA("sp", gkvv, self.gkv, w=[vec])
        self.ACT(cc, cc, AF.Silu, r=[vec], w=[vec])
        rows = S.alloc("rows", [4096], F32)
        self.DMA("sp", rows.ap[0:1, 2048:4096], self.gpost_row, w=[rows])
        browst = S.alloc("browst", [2048], F32)
        self.DMA("sp", browst.ap[0:1, 0:1024], self.bmod_row[:, 2048:3072], w=[browst])
        self.DMA("sp", browst.ap[0:1, 1024:2048], self.bmod_row[:, 5120:6144], w=[browst])
        wm_view = self.w_mod.rearrange("(k p) n -> p k n", p=128)
        ccb = S.alloc("ccb", [8, 2], BF16)
        self.CP("dve", ccb.ap[:, :, :], cc, r=[vec], w=[ccb])
        wmb = [S.alloc("wmb", [KC, 512], BF16) for _ in range(3)]
        for j in range(12):
            st = wmb[j % 3]
            self.DMA("pool", st.ap[:, :, :], wm_view[:, :, j * 512:(j + 1) * 512], w=[st])
            for m in range(4):
                col = (j * 4 + m) * 2
                for kc in range(KC):
                    self.MM(P[0].ap[:, col:col + 2], st.ap[:, kc, m * 128:(m + 1) * 128], ccb.ap[:, kc, :],
                            start=(kc == 0), stop=(kc == KC - 1), r=[st, ccb], w=[P[0]])
            if j in (4, 5, 10, 11):
                ro = {4: 0, 5: 512, 10: 1024, 11: 1536}[j]
                for kc in range(KC):
                    self.MM(P[1].ap[0:1, :], ccb.ap[:, kc, 0:1], st.ap[:, kc, :], start=(kc == 0), stop=(kc == KC - 1),
                            r=[st, ccb], w=[P[1]])
                self.TT("dve", rows.ap[0:1, ro:ro + 512], P[1].ap[0:1, :], browst.ap[0:1, ro:ro + 512], ALU.add,
                        r=[P[1], browst], w=[rows])
        S.release(ccb, *wmb)
        S.release(browst)
        self.TT("dve", modb.ap[:, :, :], P[0].ap[:, 0:96].rearrange("p (a b) -> p a b", b=2),
                bmodT.unsqueeze(2).to_broadcast([128, 48, 2]), ALU.add, r=[P[0], vec], w=[modb])
        self.STT(A1, modb.ap[:, 8:16, :], 1.0, gT[:, 0, :].unsqueeze(2).to_broadcast([128, 8, 2]), ALU.add, ALU.mult,
                 r=[modb, vec], w=[vec])
        self.STT(A2, modb.ap[:, 32:40, 0], 1.0, gT[:, 2, :], ALU.add, ALU.mult, r=[modb, vec], w=[vec])
        self.TT("dve", rows.ap[0:1, 0:2048], rows.ap[0:1, 0:2048], rows.ap[0:1, 2048:4096], ALU.mult, r=[rows], w=[rows])
        Gb = S.alloc("Gb", [2, 1024], F32)
        for g in range(2):
            for hh in range(2):
                pb = P[2 + hh]
                self.MM(pb.ap[:, :], onesf[0:1, :], rows.ap[0:1, g * 1024 + hh * 512: g * 1024 + hh * 512 + 512],
                        r=[cst, rows], w=[pb])
                self.CP("act", Gb.ap[:, g, hh * 512:(hh + 1) * 512], pb.ap[:, :], r=[pb], w=[Gb])
        S.release(rows, *self.wstage)
        if self.stage == 0:
            self.dump(0, modb, modb.ap[:, :, :].rearrange("p a b -> p (a b)"), 96)
            self.dump(96, vec, vec.ap[:, 96:120], 24)
            self.dump(128, Gb, Gb.ap[:, :, :].rearrange("p a b -> p (a b)"), 2048)
            return self.finish()

        hT = S.alloc("hT", [KC, NTOK], BF16)
        xts = [S.alloc("xt", [D], F32) for _ in range(3)]
        xns = [S.alloc("xn", [D], BF16) for _ in range(2)]
        junk = S.alloc("junk", [D], BF16)
        for i in range(NT):
            xt = xts[i % 3]
            xn = xns[i % 2]
            src = self.ctx[i * 128:(i + 1) * 128, :] if i < 2 else self.x[(i - 2) * 128:(i - 1) * 128, :]
            v = 1 if i < 2 else 0
            self.DMA("sp", xt.ap[:, :], src, w=[xt])
            ss = stat.ap[:, (i % 4) * 2:(i % 4) * 2 + 1]
            tmp = stat.ap[:, (i % 4) * 2 + 1:(i % 4) * 2 + 2]
            self.ACT(junk.ap[:, :], xt.ap[:, :], AF.Square, r=[xt], w=[junk, stat], accum_out=ss)
            self.rstd_from_ss(ss, D, stat, tmp)
            self.TS("dve", xn.ap[:, :], xt.ap[:, :], ss, None, ALU.mult, r=[xt, stat], w=[xn])
            pt = P[i % 2]
            ptb = pt.ap.bitcast(BF16)
            for kc in range(KC):
                self.TR(ptb[:, kc * 128:(kc + 1) * 128], xn.ap[:, kc * 128:(kc + 1) * 128], identb, r=[xn, cstb], w=[pt])
            for kc in range(KC):
                o = hT.ap[:, kc, i * 128:(i + 1) * 128]
                if kc % 2 == 0:
                    self.TS("dve", o, ptb[:, kc * 128:(kc + 1) * 128], A1[:, kc, v:v + 1], modb.ap[:, kc, v:v + 1],
                            ALU.mult, ALU.add, r=[pt, vec, modb], w=[hT])
                else:
                    self.ACT(o, ptb[:, kc * 128:(kc + 1) * 128], AF.Identity, r=[pt, vec, modb], w=[hT],
                             scale=A1[:, kc, v:v + 1], bias=modb.ap[:, kc, v:v + 1])
        S.release(*xts, *xns, junk)
        if self.stage == 1:
            for kc in range(2):
                self.dump(kc * 2304, hT, hT.ap[:, kc, :], 2304)
            return self.finish()
        self.cst, self.cstb, self.stat, self.vec, self.modb, self.Gb, self.hT, self.junk = cst, cstb, stat, vec, modb, Gb, hT, junk
        self.tri, self.identf, self.identb, self.onesf, self.onesb, self.e96b, self.maskb = tri, identf, identb, onesf, onesb, e96b, maskb
        self.A2, self.gqv, self.gkvv = A2, gqv, gkvv
        self.phase_gla()
        if self.stage == 25:
            return self.finish()
        if self.stage == 3:
            for hc in range(4):
                self.dump(hc * 2048, self.ogT, self.ogT.ap[:, hc, :], 2048)
            return self.finish()
        self.phase_mla()
        if self.stage == 4:
            for h in range(4):
                self.dump(h * 2048, self.omlaT, self.omlaT.ap[0:64, h, :], 2048, parts=64)
            return self.finish()
        self.phase_merge()
        if self.stage == 5:
            return self.finish()
        if MOE_MODE == "dense":
            self.phase_moe()
        else:
            self.phase_moe_sparse()
        self.finish()

    def phase_mla(self):
        S, P = self.S, self.P
        hT, cstb, cst, stat = self.hT, self.cstb, self.cst, self.stat
        identb, onesf, onesb, identf = self.identb, self.onesf, self.onesb, self.identf
        wB = S.alloc("wB", [KC, 416], BF16)
        self.load_w(wB.ap[:, :, :], wB, self.w_in[:, 1568:1984], 1024, 416)
        wkrs = S.alloc("wkrs", [KC, 32], BF16)
        for a, b_ in ((0, 8), (8, 0), (16, 24), (24, 16)):
            self.CP("pool", wkrs.ap[:, :, a:a + 8], wB.ap[:, :, 384 + b_:384 + b_ + 8], r=[wB], w=[wkrs])
        wuq = S.alloc("wuq", [2, 768], BF16)
        self.load_w(wuq.ap[:, :, :], wuq, self.w_uq, 256, 768)
        wuqs = S.alloc("wuqs", [2, 8, 32], BF16)
        for h in range(8):
            for a, b_ in ((0, 8), (8, 0), (16, 24), (24, 16)):
                self.CP("pool", wuqs.ap[:, :, h, a:a + 8], wuq.ap[:, :, h * 96 + 64 + b_:h * 96 + 64 + b_ + 8], r=[wuq], w=[wuqs])
        wukv = S.alloc("wukv", [1024], BF16)
        self.load_w(wukv.ap[:, 0:512], wukv, self.w_uk, 128, 512)
        self.load_w(wukv.ap[:, 512:1024], wukv, self.w_uv, 128, 512)
        zdqnT = S.alloc("zdqnT", [2, T], BF16)
        ckvT = S.alloc("ckvT", [NTOK], BF16)
        krT = S.alloc("krT", [NTOK], BF16)
        zn = [S.alloc("zn", [384], BF16) for _ in range(2)]
        junk = S.alloc("junk2", [384], F32)
        for i in range(NT):
            pa = P[i % 2]
            pt = P[2 + i % 2]
            ptb = pt.ap.bitcast(BF16)
            z = zn[i % 2]
            sc = (i % 4) * 4
            if i >= 2:
                for kc in range(KC):
                    self.MM(pa.ap[:, 0:256], hT.ap[:, kc, i * 128:(i + 1) * 128], wB.ap[:, kc, 0:256],
                            start=(kc == 0), stop=(kc == KC - 1), r=[wB, hT], w=[pa])
                self.ACT(junk.ap[:, 0:256], pa.ap[:, 0:256], AF.Square, r=[pa], w=[junk, stat], accum_out=stat.ap[:, sc:sc + 1])
                self.rstd_from_ss(stat.ap[:, sc:sc + 1], 256, stat, stat.ap[:, sc + 1:sc + 2])
                self.TS("dve", z.ap[:, 0:256], pa.ap[:, 0:256], stat.ap[:, sc:sc + 1], None, ALU.mult, r=[pa, stat], w=[z])
            for kc in range(KC):
                self.MM(pa.ap[:, 256:384], hT.ap[:, kc, i * 128:(i + 1) * 128], wB.ap[:, kc, 256:384],
                        start=(kc == 0), stop=(kc == KC - 1), r=[wB, hT], w=[pa])
            self.ACT(junk.ap[:, 256:384], pa.ap[:, 256:384], AF.Square, r=[pa], w=[junk, stat], accum_out=stat.ap[:, sc + 2:sc + 3])
            self.rstd_from_ss(stat.ap[:, sc + 2:sc + 3], 128, stat, stat.ap[:, sc + 3:sc + 4])
            self.TS("dve", z.ap[:, 256:384], pa.ap[:, 256:384], stat.ap[:, sc + 2:sc + 3], None, ALU.mult, r=[pa, stat], w=[z])
            if i >= 2:
                for c in range(2):
                    self.TR(ptb[:, c * 128:(c + 1) * 128], z.ap[:, c * 128:(c + 1) * 128], identb, r=[z, cstb], w=[pt])
                    self.TS("dve", zdqnT.ap[:, c, (i - 2) * 128:(i - 1) * 128], ptb[:, c * 128:(c + 1) * 128], self.gqv[:, c:c + 1], None,
                            ALU.mult, r=[pt, self.vec], w=[zdqnT])
            self.TR(ptb[:, 256:384], z.ap[:, 256:384], identb, r=[z, cstb], w=[pt])
            self.TS("dve", ckvT.ap[:, i * 128:(i + 1) * 128], ptb[:, 256:384], self.gkvv[:, 0:1], None, ALU.mult,
                    r=[pt, self.vec], w=[ckvT])
        TB_ALL = [(0, 256), (256, 512), (768, 512), (1280, 512), (1792, 512)]
        rp = [S.alloc("rp", [2, 512], F32) for _ in range(2)]
        rt = S.alloc("rt", [2, 512], F32)
        ropv = self.c_ropet.rearrange("p (a b) -> p a b", b=NTOK)
        for bi, (c0, nn) in enumerate(TB_ALL):
            r_ = rp[bi % 2]
            self.DMA("sp", r_.ap[64:96, :, 0:nn], ropv[64:96, :, c0:c0 + nn], w=[r_])
            for kc in range(KC):
                self.MM(P[4].ap[64:96, 0:nn], wB.ap[:, kc, 384:416], hT.ap[:, kc, c0:c0 + nn], start=(kc == 0), stop=(kc == KC - 1),
                        r=[wB, hT], w=[P[4]])
            for kc in range(KC):
                self.MM(P[5].ap[64:96, 0:nn], wkrs.ap[:, kc, :], hT.ap[:, kc, c0:c0 + nn], start=(kc == 0), stop=(kc == KC - 1),
                        r=[wkrs, hT], w=[P[5]])
            self.TT("dve", rt.ap[64:96, 0, 0:nn], P[4].ap[64:96, 0:nn], r_.ap[64:96, 0, 0:nn], ALU.mult, r=[P[4], r_], w=[rt])
            self.TT("dve", rt.ap[64:96, 1, 0:nn], P[5].ap[64:96, 0:nn], r_.ap[64:96, 1, 0:nn], ALU.mult, r=[P[5], r_], w=[rt])
            self.TT("pool", krT.ap[64:96, c0:c0 + nn], rt.ap[64:96, 0, 0:nn], rt.ap[64:96, 1, 0:nn], ALU.add, r=[rt], w=[krT])
        S.release(wB, wkrs, junk, *zn)
        v = S.alloc("v", [NT, 8, 128], BF16)
        self.MS("pool", v.ap[:, :, 0:4, 64:128], 0.0, w=[v])
        self.MS("pool", v.ap[:, :, 0:4, 64:65], 1.0, w=[v])
        self.MS("pool", v.ap[:, :, 4:8, 0:64], 0.0, w=[v])
        self.MS("pool", v.ap[:, :, 4:8, 0:1], 1.0, w=[v])
        for i in range(NT):
            pb = P[i % 2]
            self.MM(pb.ap[:, :], ckvT.ap[:, i * 128:(i + 1) * 128], wukv.ap[:, 512:1024], r=[ckvT, wukv], w=[pb])
            self.CP("act", v.ap[:, i, 0:4, 0:64], pb.ap[:, 0:256].rearrange("p (a b) -> p a b", b=64), r=[pb], w=[v])
            self.CP("dve", v.ap[:, i, 4:8, 64:128], pb.ap[:, 256:512].rearrange("p (a b) -> p a b", b=64), r=[pb], w=[v])
        omlaT = S.alloc("omlaT", [4, T], BF16)
        self.omlaT = omlaT
        kThs = [S.alloc("kTh", [NTOK], BF16) for _ in range(2)]
        qThs = [S.alloc("qTh", [T], BF16) for _ in range(2)]
        pT = [S.alloc("pT", [512], BF16) for _ in range(3)]
        rden = S.alloc("rden", [512], F32)
        bcs = S.alloc("bcs", [512], F32)
        sqb = S.alloc("sqb", [NTOK], BF16)
        mxs = [S.alloc("mx", [8], F32) for _ in range(2)]
        nb = [0]

        def build(h):
            kTh, qTh = kThs[h % 2], qThs[h % 2]
            for (c0, nn) in TB_ALL:
                pb = P[nb[0] % 2]
                nb[0] += 1
                self.MM(pb.ap[0:64, 0:nn], wukv.ap[:, h * 64:(h + 1) * 64], ckvT.ap[:, c0:c0 + nn], r=[wukv, ckvT], w=[pb])
                self.CP("dve", kTh.ap[0:64, c0:c0 + nn], pb.ap[0:64, 0:nn], r=[pb], w=[kTh])
            self.CP("pool", kTh.ap[64:96, :], krT.ap[64:96, :], r=[krT], w=[kTh])
            for qb in range(4):
                c0 = TC + qb * 512
                r_ = rp[qb % 2]
                self.DMA("sp", r_.ap[64:96, :, :], ropv[64:96, :, c0:c0 + 512], w=[r_])
                pq, pr = P[2], P[3]
                for c in range(2):
                    self.MM(pq.ap[0:96, :], wuq.ap[:, c, h * 96:(h + 1) * 96], zdqnT.ap[:, c, qb * 512:(qb + 1) * 512],
                            start=(c == 0), stop=(c == 1), r=[wuq, zdqnT], w=[pq])
                for c in range(2):
                    self.MM(pr.ap[64:96, :], wuqs.ap[:, c, h, :], zdqnT.ap[:, c, qb * 512:(qb + 1) * 512],
                            start=(c == 0), stop=(c == 1), r=[wuqs, zdqnT], w=[pr])
                self.CP("dve", qTh.ap[0:64, qb * 512:(qb + 1) * 512], pq.ap[0:64, :], r=[pq], w=[qTh])
                self.TT("dve", rt.ap[64:96, 0, :], pq.ap[64:96, :], r_.ap[64:96, 0, :], ALU.mult, r=[pq, r_], w=[rt])
                self.TT("dve", rt.ap[64:96, 1, :], pr.ap[64:96, :], r_.ap[64:96, 1, :], ALU.mult, r=[pr, r_], w=[rt])
                self.TT("pool", qTh.ap[64:96, qb * 512:(qb + 1) * 512], rt.ap[64:96, 0, :], rt.ap[64:96, 1, :], ALU.add, r=[rt], w=[qTh])

        def build_stab(h):
            kTh, qTh = kThs[h % 2], qThs[h % 2]
            sq = sqb
            mx = mxs[h % 2]
            pn = P[3]
            for which, src_, ntile in ((0, qTh, 16), (1, kTh, NT)):
                self.TT("pool", sq.ap[0:96, 0:ntile * 128], src_.ap[0:96, 0:ntile * 128], src_.ap[0:96, 0:ntile * 128], ALU.mult,
                        r=[src_], w=[sq])
                for t_ in range(ntile):
                    self.MM(pn.ap[:, which * 32 + t_:which * 32 + t_ + 1], sq.ap[0:96, t_ * 128:(t_ + 1) * 128], onesb[0:96, 0:1],
                            r=[sq, cstb], w=[pn])
                self.S.op("dve", lambda e, o=mx.ap[:, which:which + 1], a=pn.ap[:, which * 32:which * 32 + ntile]: e.reduce_max(o, a, axis=AX.X),
                          [pn], [mx])
            for which in range(2):
                self.TR(pn.ap[0:1, 64 + which * 128:64 + (which + 1) * 128], mx.ap[:, which:which + 1], identf, r=[mx, cst], w=[pn])
                self.S.op("dve", lambda e, o=mx.ap[0:1, 2 + which:3 + which], a=pn.ap[0:1, 64 + which * 128:64 + (which + 1) * 128]:
                          e.reduce_max(o, a, axis=AX.X), [pn], [mx])
            self.TT("dve", mx.ap[0:1, 4:5], mx.ap[0:1, 2:3], mx.ap[0:1, 3:4], ALU.mult, r=[mx], w=[mx])
            self.ACT(mx.ap[0:1, 5:6], mx.ap[0:1, 4:5], AF.Ln, r=[mx], w=[mx], bias=self.eps_ap[0:1, :])
            self.ACT(mx.ap[0:1, 6:7], mx.ap[0:1, 5:6], AF.Exp, r=[mx], w=[mx], scale=0.5)
            self.MM(pn.ap[:, 400:401], onesf[0:1, :], mx.ap[0:1, 6:7], r=[cst, mx], w=[pn])
            self.TS("dve", mx.ap[:, 7:8], pn.ap[:, 400:401], -1.02 * MLA_SCALE, None, ALU.mult, r=[pn], w=[mx])

        def tail(h, qb):
            po = P[6 + qb % 2]
            dr, lo = (64, 0) if h < 4 else (0, 64)
            self.CP("act", rden.ap[dr:dr + 1, :], po.ap[dr:dr + 1, :], r=[po], w=[rden])
            pbc = P[2]
            self.MM(pbc.ap[lo:lo + 64, :], onesf[dr:dr + 1, 0:64], rden.ap[dr:dr + 1, :], r=[cst, rden], w=[pbc])
            self.S.op("dve", lambda e, o=bcs.ap[lo:lo + 64, :], a=pbc.ap[lo:lo + 64, :]: e.reciprocal(o, a), [pbc], [bcs])
            self.TT("dve", omlaT.ap[lo:lo + 64, h % 4, qb * 512:(qb + 1) * 512], po.ap[lo:lo + 64, :], bcs.ap[lo:lo + 64, :], ALU.mult,
                    r=[po, bcs], w=[omlaT])
        build(0)
        build_stab(0)
        pend = None
        for h in range(8):
            kTh, qTh = kThs[h % 2], qThs[h % 2]
            if h + 1 < 8:
                build(h + 1)
            for qb in range(4):
                po = P[6 + qb % 2]

                def qk(kt, qb=qb, kTh=kTh, qTh=qTh):
                    self.MM(P[4 + kt % 2].ap[:, :], kTh.ap[0:96, kt * 128:(kt + 1) * 128], qTh.ap[0:96, qb * 512:(qb + 1) * 512],
                            r=[kTh, qTh], w=[P[4 + kt % 2]])
                qk(0)
                for kt in range(NT):
                    psb = P[4 + kt % 2]
                    p_ = pT[kt % 3]
                    if kt + 1 < NT:
                        qk(kt + 1)
                    self.ACT(p_.ap[:, :], psb.ap[:, :], AF.Exp, r=[psb, mxs[h % 2]], w=[p_], scale=MLA_SCALE, bias=mxs[h % 2].ap[:, 7:8])
                    self.MM(po.ap[:, :], v.ap[:, kt, h, :], p_.ap[:, :], start=(kt == 0), stop=(kt == NT - 1), r=[v, p_], w=[po])
                    if kt == 2 and pend is not None:
                        tail(*pend)
                        pend = None
                    if kt == 9 and qb == 2 and h + 1 < 8:
                        build_stab(h + 1)
                pend = (h, qb)
        tail(*pend)
        kTh, qTh = kThs[0], qThs[0]
        S.release(kThs[1], qThs[1], sqb, *mxs)
        S.release(wuq, wuqs, wukv, zdqnT, ckvT, krT, v, kTh, qTh, rden, bcs, rt, *pT, *rp)

    def phase_gla(self):
        S, P = self.S, self.P
        hT, cstb, cst, stat = self.hT, self.cstb, self.cst, self.stat
        TB_ALL = [(0, 256), (256, 512), (768, 512), (1280, 512), (1792, 512)]
        TB_X = TB_ALL[1:]
        wA = S.alloc("wA", [KC, 1056], BF16)
        self.load_w(wA.ap[:, :, 0:1024], wA, self.w_in[:, 0:1024], 1024, 1024)
        self.load_w(wA.ap[:, :, 1024:1056], wA, self.w_in[:, 1536:1568], 1024, 32)
        wzg = S.alloc("wzg", [KC, 512], BF16)
        self.load_w(wzg.ap[:, :, :], wzg, self.w_in[:, 1024:1536], 1024, 512)
        wa2b = S.alloc("wa2b", [1024], BF16)
        self.load_w(wa2b.ap[:, 0:512], wa2b, self.wa2, 32, 512)
        self.load_w(wa2b.ap[:, 512:1024], wa2b, self.ba_row, 1, 512)
        gnb = S.alloc("gnb", [512], F32)
        self.DMA("sp", gnb.ap[:, :], self.gnb, w=[gnb])
        self.xsz = S.virt("xsz")
        zt = S.alloc("zt", [D], BF16)
        self.zt = zt
        self.MS("pool", zt.ap[:, :], 0.0, w=[zt])
        for c in range(NSLOT // 128):
            self.DMA("sp", self.xs_d[c * 128:(c + 1) * 128, :], zt.ap[:, :], r=[zt], w=[self.xsz], sem=zt)
        gqT = S.alloc("gqT", [2, T], BF16)
        gkT = S.alloc("gkT", [2, NTOK], BF16)
        gk = S.alloc("gk", [NT, 256], BF16)
        gv = S.alloc("gv", [NT, 512], BF16)
        zaT = S.alloc("zaT", [NTOK], BF16)
        n = 0
        for (c0, nn) in TB_ALL:
            for m in range(4):
                if m < 2 and c0 < TC:
                    continue
                pb = P[n % 2]
                n += 1
                for kc in range(KC):
                    self.MM(pb.ap[:, 0:nn], wA.ap[:, kc, m * 128:(m + 1) * 128], hT.ap[:, kc, c0:c0 + nn],
                            start=(kc == 0), stop=(kc == KC - 1), r=[wA, hT], w=[pb])
                if m < 2:
                    self.CP("act", gqT.ap[:, m, c0 - TC:c0 - TC + nn], pb.ap[:, 0:nn], r=[pb], w=[gqT])
                else:
                    self.CP("dve", gkT.ap[:, m - 2, c0:c0 + nn], pb.ap[:, 0:nn], r=[pb], w=[gkT])
            pb = P[n % 2]
            n += 1
            for kc in range(KC):
                self.MM(pb.ap[0:32, 0:nn], wA.ap[:, kc, 1024:1056], hT.ap[:, kc, c0:c0 + nn],
                        start=(kc == 0), stop=(kc == KC - 1), r=[wA, hT], w=[pb])
            self.CP("act", zaT.ap[0:32, c0:c0 + nn], pb.ap[0:32, 0:nn], r=[pb], w=[zaT])
        for i in range(NT):
            pa, pv = P[2 + i % 2], P[4 + i % 2]
            for kc in range(KC):
                self.MM(pa.ap[:, 0:256], hT.ap[:, kc, i * 128:(i + 1) * 128], wA.ap[:, kc, 256:512],
                        start=(kc == 0), stop=(kc == KC - 1), r=[wA, hT], w=[pa])
            for kc in range(KC):
                self.MM(pv.ap[:, :], hT.ap[:, kc, i * 128:(i + 1) * 128], wA.ap[:, kc, 512:1024],
                        start=(kc == 0), stop=(kc == KC - 1), r=[wA, hT], w=[pv])
            self.CP("act", gk.ap[:, i, :], pa.ap[:, 0:256], r=[pa], w=[gk])
            self.CP("dve", gv.ap[:, i, :], pv.ap[:, :], r=[pv], w=[gv])
        S.release(wA)
        if self.stage == 2:
            for m in range(2):
                self.dump(m * 2048, gqT, gqT.ap[:, m, :], 2048)
            self.dump(4096, gv, gv.ap[:, 5, :], 512)
            self.dump(4608, gk, gk.ap[:, 5, :], 256)
            self.dump(4864, zaT, zaT.ap[:, 0:2304], 2304)
            self.ogT = gqT
            return

        ogT = S.alloc("ogT", [4, T], BF16)
        self.ogT = ogT
        hist = S.alloc("hist", [32, 256], BF16)
        Sst = [S.alloc("Sst", [256], F32) for _ in range(2)]
        Sbr = [S.alloc("Sbr", [256], BF16) for _ in range(2)]
        R = 2
        la_b = [S.alloc("la", [256], F32) for _ in range(R)]
        lt_b = [S.alloc("lt", [256], F32) for _ in range(R)]
        ET_b = [S.alloc("ET", [256], F32) for _ in range(R)]
        EI_b = [S.alloc("EI", [256], F32) for _ in range(R)]
        KS_b = [S.alloc("KS", [256], F32) for _ in range(R)]
        qd_b = [S.alloc("qd", [2, 2, 128], BF16) for _ in range(2)]
        qt_b = [S.alloc("qt", [2, 128], F32) for _ in range(2)]
        ki_b = [S.alloc("ki", [2, 128], BF16) for _ in range(R)]
        kte_b = [S.alloc("kte", [256], BF16) for _ in range(R)]
        att_b = [S.alloc("att", [512], BF16) for _ in range(R)]
        ot_b = S.alloc("ot", [512], F32)
        sq_b = S.alloc("sq", [512], F32)
        sz_b = S.alloc("sz", [512], F32)
        og_b = S.alloc("og", [512], BF16)
        self.cnt = 0
        tri, maskb, onesb, identb = self.tri, self.maskb, self.onesb, self.identb
        for d in range(2):
            self.MS("pool", Sst[d].ap[:, :], 0.0, w=[Sst[d]])
        self.MS("pool", Sbr[0].ap[:, :], 0.0, w=[Sbr[0]])

        def prep_gen(i, d, want_q, c, out):
            r = c % R
            pl, pc = P[0 + c % 2], P[2 + c % 2]
            la, lt, ET, EI, KS = la_b[r], lt_b[r], ET_b[r], EI_b[r], KS_b[r]
            self.MM(pl.ap[:, 0:256], zaT.ap[0:32, i * 128:(i + 1) * 128], wa2b.ap[0:32, d * 256:(d + 1) * 256],
                    start=True, stop=False, r=[zaT, wa2b], w=[pl])
            self.MM(pl.ap[:, 0:256], onesb[0:1, :], wa2b.ap[0:1, 512 + d * 256:512 + (d + 1) * 256],
                    start=False, stop=True, r=[cstb, wa2b], w=[pl])
            yield
            self.ACT(lt.ap[:, :], pl.ap[:, 0:256], AF.Abs, r=[pl], w=[lt])
            self.TS("dve", la.ap[:, :], pl.ap[:, 0:256], 0.0, 1.0 / 16, ALU.min, ALU.mult, r=[pl], w=[la])
            yield
            self.ACT(lt.ap[:, :], lt.ap[:, :], AF.Exp, r=[lt], w=[lt], scale=-1.0)
            yield
            self.ACT(lt.ap[:, :], lt.ap[:, :], AF.Ln, r=[lt], w=[lt], bias=self.onesf[:, 0:1])
            yield
            self.STT(la.ap[:, :], lt.ap[:, :], -1.0 / 16, la.ap[:, :], ALU.mult, ALU.add, r=[lt, la], w=[la])
            yield
            for j in range(2):
                self.MM(pc.ap[:, j * 128:(j + 1) * 128], la.ap[:, j * 128:(j + 1) * 128], tri[:, d, :], r=[la, cst], w=[pc])
            self.MM(pc.ap[:, 256:512], tri[:, 2 + d, :], la.ap[:, :], r=[la, cst], w=[pc])
            yield
            self.ACT(ET.ap[:, :], pc.ap[:, 0:256], AF.Exp, r=[pc], w=[ET])
            self.ACT(KS.ap[:, :], pc.ap[:, 256:512], AF.Exp, r=[pc], w=[KS])
            kte = kte_b[r]
            out.update({"pl": pl, "pc": pc, "ET": ET, "kte": kte})
            if want_q:
                self.ACT(EI.ap[:, :], pc.ap[:, 0:256], AF.Exp, r=[pc], w=[EI], scale=-1.0)
            yield
            self.TT("pool", kte.ap[:, :], gk.ap[:, i, :], KS.ap[:, :], ALU.mult, r=[gk, KS], w=[kte])
            if want_q:
                xc = (i - 2) * 128
                qd = qd_b[c % 2]
                ki = ki_b[r]
                qt = qt_b[c % 2]
                self.STT(qt.ap[:, :, :], gqT.ap[:, :, xc:xc + 128], 0.125, ET.ap[:, :].rearrange("p (a b) -> p a b", b=128),
                         ALU.mult, ALU.mult, r=[gqT, ET], w=[qt])
                yield
                for p in range(2):
                    self.TS("pool" if p else "dve", qd.ap[:, p, :, :], qt.ap[:, :, :], self.rowmask[:, p:p + 1], None, ALU.mult,
                            r=[qt, cst], w=[qd])
                self.TT("pool", ki.ap[:, :, :], gkT.ap[:, :, i * 128:(i + 1) * 128],
                        EI.ap[:, :].rearrange("p (a b) -> p a b", b=128), ALU.mult, r=[gkT, EI], w=[ki])
                out["qd"], out["ki"] = qd, ki

        def prep_multi(specs):
            outs = [dict() for _ in specs]
            gens = []
            for (i, d, wq), o in zip(specs, outs):
                gens.append(prep_gen(i, d, wq, self.cnt, o))
                self.cnt += 1
            live_g = list(gens)
            while live_g:
                for g in list(live_g):
                    try:
                        next(g)
                    except StopIteration:
                        live_g.remove(g)
            return outs

        def prep(i, d, want_q):
            return prep_multi([(i, d, want_q)])[0]

        def ds_update(i, d, b, cs):
            pl, ET, kte = b["pl"], b["ET"], b["kte"]
            for h in range(4):
                self.MM(pl.ap[(h % 2) * 64:(h % 2) * 64 + 64, 256 + (h // 2) * 128:256 + (h // 2) * 128 + 128],
                        kte.ap[cs:cs + 64, h * 64:(h + 1) * 64], gv.ap[cs:cs + 64, i, h * 128:(h + 1) * 128],
                        r=[kte, gv], w=[pl])
            tl = cs + 63 if d == 0 else cs
            for j in range(2):
                self.STT(Sst[d].ap[:, j * 128:(j + 1) * 128], Sst[d].ap[:, j * 128:(j + 1) * 128],
                         ET.ap[:, j * 128 + tl:j * 128 + tl + 1], pl.ap[:, 256 + j * 128:256 + (j + 1) * 128],
                         ALU.mult, ALU.add, r=[Sst[d], ET, pl], w=[Sst[d]])

        order_b = [1, 0] + list(range(17, 1, -1))
        for k in range(0, len(order_b), 2):
            pair = order_b[k:k + 2]
            bs = prep_multi([(i, 1, False) for i in pair])
            for i, b in zip(pair, bs):
                for cs in (64, 0):
                    if i >= 2:
                        ch = (i - 2) * 2 + (1 if cs == 64 else 0)
                        self.CP("act", hist.ap[:, ch, :], Sst[1].ap[:, :], r=[Sst[1]], w=[hist])
                    ds_update(i, 1, b, cs)

        if self.stage == 25:
            S.release(ot_b, sq_b, sz_b)
            self.dump(0, hist, hist.ap[:, 31, :], 256)
            self.dump(256, hist, hist.ap[:, 0, :], 256)
            return
        live = 0
        ctxp = prep_multi([(0, 0, False), (1, 0, False)])
        for i in range(NT):
            if i < 2:
                bf = ctxp[i]
                for cs in (0, 64):
                    ds_update(i, 0, bf, cs)
                if i == 1:
                    self.CP("act", Sbr[0].ap[:, :], Sst[0].ap[:, :], r=[Sst[0]], w=[Sbr[0]])
                continue
            bf, bb = prep_multi([(i, 0, True), (i, 1, True)])
            xi = i - 2
            xc = xi * 128
            pos = (P[6], P[7])
            pss = (P[4], P[5])
            first = [True, True]
            acol = lambda h: ((h % 2) * 2 + h // 2) * 128
            for d, b in ((0, bf), (1, bb)):
                att = att_b[d]
                qd, ki = b["qd"], b["ki"]
                for h in range(4):
                    hp = (h % 2) * 64
                    self.MM(pss[h % 2].ap[:, (h // 2) * 128:(h // 2) * 128 + 128], ki.ap[:, h // 2, :],
                            qd.ap[:, h % 2, h // 2, :], r=[ki, qd], w=[pss[h % 2]])
                for p in range(2):
                    self.TT("dve", att.ap[:, p * 256:(p + 1) * 256], pss[p].ap[:, 0:256], maskb[:, d, 0:256], ALU.mult,
                            r=[pss[p], cstb], w=[att])
                for h in range(4):
                    self.MM(pos[h % 2].ap[:, (h // 2) * 128:(h // 2) * 128 + 128], att.ap[:, acol(h):acol(h) + 128],
                            gv.ap[:, i, h * 128:(h + 1) * 128], start=first[h % 2], stop=False, r=[att, gv], w=[pos[h % 2]])
                    first[h % 2] = False
            qd = bb["qd"]
            for cs in (64, 0):
                ch = xi * 2 + (1 if cs == 64 else 0)
                for h in range(4):
                    hp = (h % 2) * 64
                    self.MM(pos[h % 2].ap[cs:cs + 64, (h // 2) * 128:(h // 2) * 128 + 128], qd.ap[:, h % 2, h // 2, cs:cs + 64],
                            hist.ap[:, ch, (h // 2) * 128:(h // 2) * 128 + 128], start=False, stop=False,
                            r=[qd, hist], w=[pos[h % 2]])
            qd = bf["qd"]
            for cs in (0, 64):
                sb = Sbr[live % 2]
                for h in range(4):
                    hp = (h % 2) * 64
                    self.MM(pos[h % 2].ap[cs:cs + 64, (h // 2) * 128:(h // 2) * 128 + 128], qd.ap[:, h % 2, h // 2, cs:cs + 64],
                            sb.ap[:, (h // 2) * 128:(h // 2) * 128 + 128], start=False, stop=(cs == 64 and h >= 2),
                            r=[qd, sb], w=[pos[h % 2]])
                ds_update(i, 0, bf, cs)
                live += 1
                self.CP("act", Sbr[live % 2].ap[:, :], Sst[0].ap[:, :], r=[Sst[0]], w=[Sbr[live % 2]])
            pz = bf["pc"]
            for kc in range(KC):
                self.MM(pz.ap[:, :], hT.ap[:, kc, i * 128:(i + 1) * 128], wzg.ap[:, kc, :], start=(kc == 0), stop=(kc == KC - 1),
                        r=[hT, wzg], w=[pz])
            self.ACT(sz_b.ap[:, :], pz.ap[:, :], AF.Silu, r=[pz], w=[sz_b])
            for h in range(4):
                self.CP("act" if h % 2 == 0 else "dve", ot_b.ap[:, h * 128:(h + 1) * 128],
                        pos[h % 2].ap[:, (h // 2) * 128:(h // 2) * 128 + 128], r=[pos[h % 2]], w=[ot_b])
            self.TT("pool", sq_b.ap[:, :], ot_b.ap[:, :], ot_b.ap[:, :], ALU.mult, r=[ot_b], w=[sq_b])
            st4 = stat.ap[:, 16:20]
            st4b = stat.ap[:, 20:24]
            self.S.op("dve", lambda e, o=st4, a=sq_b.ap[:, :].rearrange("p (a b) -> p a b", b=128): e.reduce_sum(o, a, axis=AX.X),
                      [sq_b], [stat])
            self.ACT(st4b, st4, AF.Ln, r=[stat], w=[stat], scale=1.0 / 128, bias=self.eps_ap)
            self.ACT(st4, st4b, AF.Exp, r=[stat], w=[stat], scale=-0.5)
            self.TT("dve", sq_b.ap[:, :].rearrange("p (a b) -> p a b", b=128), ot_b.ap[:, :].rearrange("p (a b) -> p a b", b=128),
                    st4.unsqueeze(2).to_broadcast([128, 4, 128]), ALU.mult, r=[ot_b, stat], w=[sq_b])
            self.TT("pool", sq_b.ap[:, :], sq_b.ap[:, :], gnb.ap[:, :], ALU.mult, r=[sq_b, gnb], w=[sq_b])
            self.TT("dve", og_b.ap[:, :], sq_b.ap[:, :], sz_b.ap[:, :], ALU.mult, r=[sq_b, sz_b], w=[og_b])
            pt = P[4]
            ptb = pt.ap.bitcast(BF16)
            for h in range(4):
                self.TR(ptb[:, h * 128:(h + 1) * 128], og_b.ap[:, h * 128:(h + 1) * 128], identb, r=[og_b, cstb], w=[pt])
            self.CP("act", ogT.ap[:, :, xc:xc + 128], ptb[:, 0:512].rearrange("p (a b) -> p a b", b=128), r=[pt], w=[ogT])
        S.release(gqT, gkT, gk, gv, zaT, hist, wzg, wa2b, gnb, ot_b, sq_b, sz_b, og_b,
                  *Sst, *Sbr, *la_b, *lt_b, *ET_b, *EI_b, *KS_b, *qd_b, *qt_b, *ki_b, *kte_b, *att_b)

    def phase_merge(self):
        S, P = self.S, self.P
        hT, ogT, omlaT, stat, cst, cstb = self.hT, self.ogT, self.omlaT, self.stat, self.cst, self.cstb
        mT = S.alloc("mT", [KC, T], BF16)
        wbg = S.alloc("wbg", [4, D], BF16)
        self.load_w(wbg.ap[:, :, :], wbg, self.w_br_gla, 512, D)
        wbm = S.alloc("wbm", [4, D], BF16)
        src = self.w_br_mla.rearrange("(k p) n -> p k n", p=64)
        self.DMA("pool", wbm.ap[0:64, :, :], src[:, 0:4, :], w=[wbm])
        self.DMA("pool", wbm.ap[64:128, :, :], src[:, 4:8, :], w=[wbm])
        wz = [S.alloc("wz", [KC, 256], BF16) for _ in range(2)]
        sg = [S.alloc("sg", [512], F32) for _ in range(2)]
        sm = [S.alloc("sm", [512], F32) for _ in range(2)]
        n = 0
        for m in range(KC):
            w_ = wz[m % 2]
            self.load_w(w_.ap[:, :, 0:128], w_, self.w_in[:, 1984 + m * 128:1984 + (m + 1) * 128], 1024, 128)
            self.load_w(w_.ap[:, :, 128:256], w_, self.w_in[:, 3008 + m * 128:3008 + (m + 1) * 128], 1024, 128)
            for tb in range(4):
                c0, xc0 = TC + tb * 512, tb * 512
                o = (n % 2) * 4
                sg_, sm_ = sg[n % 2], sm[n % 2]
                n += 1
                for kc in range(KC):
                    self.MM(P[o].ap[:, :], w_.ap[:, kc, 0:128], hT.ap[:, kc, c0:c0 + 512], start=(kc == 0), stop=(kc == KC - 1),
                            r=[w_, hT], w=[P[o]])
                for kc in range(KC):
                    self.MM(P[o + 1].ap[:, :], w_.ap[:, kc, 128:256], hT.ap[:, kc, c0:c0 + 512], start=(kc == 0), stop=(kc == KC - 1),
                            r=[w_, hT], w=[P[o + 1]])
                for hc in range(4):
                    self.MM(P[o + 2].ap[:, :], wbg.ap[:, hc, m * 128:(m + 1) * 128], ogT.ap[:, hc, xc0:xc0 + 512],
                            start=(hc == 0), stop=(hc == 3), r=[wbg, ogT], w=[P[o + 2]])
                for j in range(4):
                    self.MM(P[o + 3].ap[:, :], wbm.ap[:, j, m * 128:(m + 1) * 128], omlaT.ap[:, j, xc0:xc0 + 512],
                            start=(j == 0), stop=(j == 3), r=[wbm, omlaT], w=[P[o + 3]])
                self.ACT(sg_.ap[:, :], P[o].ap[:, :], AF.Sigmoid, r=[P[o]], w=[sg_])
                self.ACT(sm_.ap[:, :], P[o + 1].ap[:, :], AF.Sigmoid, r=[P[o + 1]], w=[sm_])
                self.TT("dve", sg_.ap[:, :], sg_.ap[:, :], P[o + 2].ap[:, :], ALU.mult, r=[sg_, P[o + 2]], w=[sg_])
                self.TT("dve", sm_.ap[:, :], sm_.ap[:, :], P[o + 3].ap[:, :], ALU.mult, r=[sm_, P[o + 3]], w=[sm_])
                self.TT("pool", mT.ap[:, m, xc0:xc0 + 512], sg_.ap[:, :], sm_.ap[:, :], ALU.add, r=[sg_, sm_], w=[mT])
        S.release(hT, ogT, omlaT, wbg, wbm, *wz, *sg, *sm)
        wo = S.alloc("wo", [KC, D], BF16)
        self.load_w(wo.ap[:, :, :], wo, self.w_out, D, D)
        rw = S.alloc("rw", [KC * NE + NE], F32)
        rwv = rw.ap[:, 0:KC * NE].rearrange("p (a b) -> p a b", b=NE)
        self.DMA("sp", rwv, self.router_w.rearrange("(k p) n -> p k n", p=128), w=[rw])
        self.DMA("sp", rw.ap[0:1, KC * NE:KC * NE + NE], self.router_b, w=[rw])
        comb = S.alloc("comb", [16, NE], F32)
        self.comb = comb
        rt_ = S.alloc("rt", [176 + 144], F32)
        self.DMA("sp", rt_.ap[:, 0:176], self.c_rt, w=[rt_])
        tsub = S.alloc("tsub", [128], BF16)
        self.CP("pool", tsub.ap[:, :], rt_.ap[:, 0:128], r=[rt_], w=[tsub])
        Umat = rt_.ap[:, 128:160]
        IO = rt_.ap[:, 160:176]
        cb = rt_.ap[:, 176:208]
        posf4 = rt_.ap[:, 208:212]
        oh = rt_.ap[:, 224:256]
        oh2 = rt_.ap[:, 256:288]
        posf = rt_.ap[:, 288:320]
        self.MS("pool", rt_.ap[:, 176:320], 0.0, w=[rt_])
        wk = S.alloc("wk", [16, 4], F32)
        posi = S.alloc("posi", [16, 4], F32)
        posi_v = posi.ap.bitcast(I32)
        self.wk, self.posi, self.posi_v = wk, posi, posi_v
        maskb16 = S.alloc("maskb16", [NE], BF16)
        rank_all = S.alloc("rank", [16, NE], F32)
        lgall = S.alloc("lgall", [16, 40], F32)
        xn2tm = S.alloc("xn2tm", [16, D], BF16)
        xsz, xsd = self.xsz, S.virt("xsd")
        self.xsd, self.ysd = xsd, S.virt("ysd")
        xt = [S.alloc("xt2", [D], F32) for _ in range(2)]
        tt = [S.alloc("tt2", [D], F32) for _ in range(2)]
        hx2f = S.alloc("hx2f", [KC, 128], F32)
        lg = S.alloc("lg", [128], F32)
        junk = S.alloc("junk3", [D], BF16)
        Gb, A2, modb, identf, onesf = self.Gb, self.A2, self.modb, self.identf, self.onesf
        for xi in range(16):
            pa, pb = P[(xi % 2) * 2], P[(xi % 2) * 2 + 1]
            x_, t_ = xt[xi % 2], tt[xi % 2]
            self.DMA("sp", x_.ap[:, :], self.x[xi * 128:(xi + 1) * 128, :], w=[x_])
            for hf, pp in enumerate((pa, pb)):
                for m in range(KC):
                    self.MM(pp.ap[:, :], mT.ap[:, m, xi * 128:(xi + 1) * 128], wo.ap[:, m, hf * 512:(hf + 1) * 512],
                            start=(m == 0), stop=(m == KC - 1), r=[mT, wo], w=[pp])
            sc = 24 + (xi % 2) * 8
            for hf, pp in enumerate((pa, pb)):
                self.ACT(junk.ap[:, 0:512], pp.ap[:, :], AF.Square, r=[pp], w=[junk, stat], accum_out=stat.ap[:, sc + hf:sc + hf + 1])
            self.TT("dve", stat.ap[:, sc:sc + 1], stat.ap[:, sc:sc + 1], stat.ap[:, sc + 1:sc + 2], ALU.add, r=[stat], w=[stat])
            self.rstd_from_ss(stat.ap[:, sc:sc + 1], D, stat, stat.ap[:, sc + 2:sc + 3])
            for hf, pp in enumerate((pa, pb)):
                self.STT(t_.ap[:, hf * 512:(hf + 1) * 512], pp.ap[:, :], stat.ap[:, sc:sc + 1], Gb.ap[:, 0, hf * 512:(hf + 1) * 512],
                         ALU.mult, ALU.mult, r=[pp, stat, Gb], w=[t_])
            self.TT("pool", x_.ap[:, :], t_.ap[:, :], x_.ap[:, :], ALU.add, r=[t_, x_], w=[x_])
            self.DMA("pool", self.x1d[xi * 128:(xi + 1) * 128, :], x_.ap[:, :], r=[x_], sem=x_)
            self.ACT(junk.ap[:, :], x_.ap[:, :], AF.Square, r=[x_], w=[junk, stat], accum_out=stat.ap[:, sc + 3:sc + 4])
            self.rstd_from_ss(stat.ap[:, sc + 3:sc + 4], D, stat, stat.ap[:, sc + 4:sc + 5])
            self.TS("dve", t_.ap[:, :], x_.ap[:, :], stat.ap[:, sc + 3:sc + 4], None, ALU.mult, r=[x_, stat], w=[t_])
            for kc in range(KC):
                pt = P[4 + kc // 4]
                self.TR(pt.ap[:, (kc % 4) * 128:(kc % 4 + 1) * 128], t_.ap[:, kc * 128:(kc + 1) * 128], identf, r=[t_, cst], w=[pt])
            for kc in range(KC):
                pt = P[4 + kc // 4]
                src_ = pt.ap[:, (kc % 4) * 128:(kc % 4 + 1) * 128]
                if kc % 2 == 0:
                    self.TS("dve", hx2f.ap[:, kc, :], src_, A2[:, kc:kc + 1], modb.ap[:, 24 + kc, 0:1], ALU.mult, ALU.add,
                            r=[pt, self.vec, modb], w=[hx2f])
                else:
                    self.ACT(hx2f.ap[:, kc, :], src_, AF.Identity, r=[pt, self.vec, modb], w=[hx2f],
                             scale=A2[:, kc:kc + 1], bias=modb.ap[:, 24 + kc, 0:1])
            pr = P[6 + xi % 2]
            for kc in range(KC):
                self.MM(pr.ap[:, 0:NE], hx2f.ap[:, kc, :], rwv[:, kc, :], start=(kc == 0), stop=False, r=[hx2f, rw], w=[pr])
            self.MM(pr.ap[:, 0:NE], onesf[0:1, :], rw.ap[0:1, KC * NE:KC * NE + NE], start=False, stop=True, r=[cst, rw], w=[pr])
            self.CP("act", lg.ap[:, 0:NE], pr.ap[:, 0:NE], r=[pr], w=[lg])
            self.S.op("dve", lambda e, o=lg.ap[:, 32:40], a=lg.ap[:, 0:NE]: e.max(out=o, in_=a), [lg], [lg])
            self.TS("dve", lg.ap[:, 64:96], lg.ap[:, 0:NE], lg.ap[:, 35:36], None, ALU.is_ge, r=[lg], w=[lg])
            self.TS("dve", lg.ap[:, 40:41], lg.ap[:, 32:33], -1.0, None, ALU.mult, r=[lg], w=[lg])
            self.ACT(lg.ap[:, 96:128], lg.ap[:, 0:NE], AF.Exp, r=[lg], w=[lg], bias=lg.ap[:, 40:41])
            self.TT("dve", lg.ap[:, 96:128], lg.ap[:, 96:128], lg.ap[:, 64:96], ALU.mult, r=[lg], w=[lg])
            self.S.op("dve", lambda e, o=lg.ap[:, 41:42], a=lg.ap[:, 96:128]: e.reduce_sum(o, a, axis=AX.X), [lg], [lg])
            self.S.op("dve", lambda e, o=lg.ap[:, 42:43], a=lg.ap[:, 41:42]: e.reciprocal(o, a), [lg], [lg])
            self.TS("dve", comb.ap[:, xi, :], lg.ap[:, 96:128], lg.ap[:, 42:43], None, ALU.mult, r=[lg], w=[comb])
            self.CP("pool", maskb16.ap[:, :], lg.ap[:, 64:96], r=[lg], w=[maskb16])
            self.MM(pr.ap[:, 64:96], tsub.ap[:, :], maskb16.ap[:, :], r=[tsub, maskb16], w=[pr])
            self.MM(pr.ap[:, 128:160], self.onesb, maskb16.ap[:, :], r=[self.cstb, maskb16], w=[pr])
            self.TT("dve", rank_all.ap[:, xi, :], pr.ap[:, 64:96], cb, ALU.add, r=[pr, rt_], w=[rank_all])
            self.TT("dve", cb, cb, pr.ap[:, 128:160], ALU.add, r=[pr, rt_], w=[rt_])
            self.CP("dve", lgall.ap[:, xi, :], lg.ap[:, 0:40], r=[lg], w=[lgall])
            self.CP("pool", xn2tm.ap[:, xi, :], t_.ap[:, :], r=[t_], w=[xn2tm])
        meta = S.alloc("meta", [32 + 32 + 128 + 512 + 512], F32)
        self.meta = meta
        nt = meta.ap[:, 0:32]
        nt_i = meta.ap[:, 32:64].bitcast(I32)
        ntT = meta.ap[:, 64:192]
        sidxf = meta.ap[:, 192:704].rearrange("p (a b) -> p a b", b=16)
        sidx_i = meta.ap[:, 704:1216].bitcast(I32).rearrange("p (a b) -> p a b", b=16)
        self.nt_i, self.sidx_i = nt_i, sidx_i
        self.MS("pool", nt, 0.0, w=[meta])
        for m in range(NSTEP):
            self.STT(nt, cb, float(SG * m), nt, ALU.is_gt, ALU.add, r=[rt_, meta], w=[meta])
        self.CP("dve", nt_i, nt, r=[meta], w=[meta])
        pq = P[4]
        self.TR(pq.ap[0:NE, 0:128], nt, identf, r=[meta, cst], w=[pq])
        self.CP("act", ntT[0:NE, :], pq.ap[0:NE, 0:128], r=[pq], w=[meta])
        self.MM(pq.ap[:, 128:160], ntT[0:NE, :], Umat[0:NE, :], r=[meta, rt_], w=[pq])
        base = rt_.ap[:, 176:208]
        self.TS("dve", base, pq.ap[:, 128:160], float(SG), None, ALU.mult, r=[pq, rt_], w=[rt_])
        for e in range(NE):
            self.TS("dve", sidxf[:, e, :], IO, base[:, e:e + 1], None, ALU.add, r=[rt_, meta], w=[meta])
        self.CP("dve", sidx_i, sidxf, r=[meta], w=[meta])
        S.release(mT, wo, rw, hx2f, lg, junk, self.zt, tsub, maskb16, *tt)
        oh4 = S.alloc("oh4", [16, 4, NE], F32)
        pr4 = S.alloc("pr4", [16, 4, NE], F32)
        pf4 = S.alloc("pf4", [16, 4], F32)
        shp = [128, 16, 4, NE]
        self.TT("dve", rank_all.ap[:, :, :], rank_all.ap[:, :, :], base.unsqueeze(1).to_broadcast([128, 16, NE]), ALU.add,
                r=[rank_all, rt_], w=[rank_all])
        self.TT("dve", oh4.ap[:, :, :, :], lgall.ap[:, :, 0:NE].unsqueeze(2).to_broadcast(shp),
                lgall.ap[:, :, 32:36].unsqueeze(3).to_broadcast(shp), ALU.is_equal, r=[lgall], w=[oh4])
        self.TT("dve", pr4.ap[:, :, :, :], oh4.ap[:, :, :, :], rank_all.ap[:, :, :].unsqueeze(2).to_broadcast(shp), ALU.mult,
                r=[oh4, rank_all], w=[pr4])
        self.S.op("dve", lambda e, o=pf4.ap[:, :, :], a=pr4.ap[:, :, :, :]: e.reduce_sum(o, a, axis=AX.X), [pr4], [pf4])
        self.CP("dve", posi_v[:, :, :], pf4.ap[:, :, :], r=[pf4], w=[posi])
        self.TT("pool", pr4.ap[:, :, :, :], oh4.ap[:, :, :, :], comb.ap[:, :, :].unsqueeze(2).to_broadcast(shp), ALU.mult,
                r=[oh4, comb, pf4], w=[pr4])
        self.S.op("dve", lambda e, o=wk.ap[:, :, :], a=pr4.ap[:, :, :, :]: e.reduce_sum(o, a, axis=AX.X), [pr4], [wk])
        for xi in range(16):
            for k in range(4):
                self.S.dma_fn("pool", lambda e, o=self.xs_d[:, :], off=posi_v[:, xi, k:k + 1], i_=xn2tm.ap[:, xi, :]:
                              e.indirect_dma_start(out=o, out_offset=bass.IndirectOffsetOnAxis(ap=off, axis=0), in_=i_, in_offset=None,
                                                   bounds_check=self.S.bc_reg, oob_is_err=False),
                              reads=[xsz, xn2tm, posi], writes=[xsd], sem_buf=xn2tm)
        S.release(oh4, pr4, pf4, rank_all, lgall)
        self.x1_bufs = xt
        self.rt_ = rt_
        self.xn2tm = xn2tm

    def phase_moe(self):
        S, P = self.S, self.P
        hx2T, comb, stat, cst = self.hx2T, self.comb, self.stat, self.cst
        identf, Gb = self.identf, self.Gb
        xt = self.x1_bufs
        combT = S.alloc("combT", [T], F32)
        for xi in range(16):
            pb = P[xi % 2]
            self.TR(pb.ap[0:NE, 0:128], comb.ap[:, xi, :], identf, r=[comb, cst], w=[pb])
            self.CP("act", combT.ap[0:NE, xi * 128:(xi + 1) * 128], pb.ap[0:NE, 0:128], r=[pb], w=[combT])
        S.release(comb)
        bias = S.alloc("ebias", [512 + D], F32)
        self.DMA("sp", bias.ap[:, 0:256], self.bgT, w=[bias])
        self.DMA("sp", bias.ap[:, 256:512], self.buT, w=[bias])
        self.DMA("sp", bias.ap[0:NE, 512:512 + D], self.b_down, w=[bias])
        self.TS("dve", bias.ap[:, 256:512], bias.ap[:, 256:512], 1.0, None, ALU.add, r=[bias], w=[bias])
        wg = S.alloc("wg", [KC, D], BF16)
        wu = S.alloc("wu", [KC, D], BF16)
        wd = S.alloc("wd", [KC, D], BF16)
        act = S.alloc("act", [KC, 512], BF16)
        oacc = S.alloc("oacc", [KC, 1024], F32)
        sel = [S.alloc("sel", [128], F32) for _ in range(2)]
        cwb = S.alloc("cw", [512], F32)
        gb_ = [S.alloc("g", [512], F32) for _ in range(2)]
        sb_ = [S.alloc("s", [512], F32) for _ in range(1)]
        ub_ = [S.alloc("u", [512], F32) for _ in range(2)]
        junk = S.alloc("junk4", [512], BF16)
        selv = self.c_sel
        out_bufs = []
        for half in range(2):
            t0 = half * 1024
            for dc in range(KC):
                for tb in range(2):
                    pb = P[4 + (dc * 2 + tb) % 2]
                    self.MM(pb.ap[:, :], bias.ap[0:NE, 512 + dc * 128:512 + (dc + 1) * 128], combT.ap[0:NE, t0 + tb * 512:t0 + (tb + 1) * 512],
                            r=[bias, combT], w=[pb])
                    self.CP("act", oacc.ap[:, dc, tb * 512:(tb + 1) * 512], pb.ap[:, :], r=[pb], w=[oacc])
            for e in range(NE):
                self.load_w(wg.ap[:, :, :], wg, self.w_gate[e], D, D)
                self.load_w(wu.ap[:, :, :], wu, self.w_up[e], D, D)
                self.load_w(wd.ap[:, :, :], wd, self.w_down[e], D, D)
                se = sel[e % 2]
                self.DMA("sp", se.ap[0:NE, :], selv[:, e * 128:(e + 1) * 128], w=[se])
                for tb in range(2):
                    tk = t0 + tb * 512
                    self.MM(P[6].ap[:, :], se.ap[0:NE, :], combT.ap[0:NE, tk:tk + 512], r=[se, combT], w=[P[6]])
                    self.CP("act", cwb.ap[:, :], P[6].ap[:, :], r=[P[6]], w=[cwb])
                    for f in range(KC):
                        pg, pu = P[f % 2], P[2 + f % 2]
                        g_, s_, u_ = gb_[f % 2], sb_[0], ub_[f % 2]
                        for kc in range(KC):
                            self.MM(pg.ap[:, :], wg.ap[:, kc, f * 128:(f + 1) * 128], hx2T.ap[:, kc, tk:tk + 512],
                                    start=(kc == 0), stop=(kc == KC - 1), r=[wg, hx2T], w=[pg])
                        for kc in range(KC):
                            self.MM(pu.ap[:, :], wu.ap[:, kc, f * 128:(f + 1) * 128], hx2T.ap[:, kc, tk:tk + 512],
                                    start=(kc == 0), stop=(kc == KC - 1), r=[wu, hx2T], w=[pu])
                        bcol = e * 8 + f
                        self.TS("dve", g_.ap[:, :], pg.ap[:, :], bias.ap[:, bcol:bcol + 1], 7.0, ALU.add, ALU.min, r=[pg, bias], w=[g_])
                        self.ACT(s_.ap[:, :], g_.ap[:, :], AF.Sigmoid, r=[g_], w=[s_], scale=1.702)
                        self.TS("dve", u_.ap[:, :], pu.ap[:, :], bias.ap[:, 256 + bcol:256 + bcol + 1], -6.0, ALU.add, ALU.max,
                                r=[pu, bias], w=[u_])
                        self.STT(u_.ap[:, :], u_.ap[:, :], 8.0, cwb.ap[:, :], ALU.min, ALU.mult, r=[u_, cwb], w=[u_])
                        self.TT("pool", g_.ap[:, :], g_.ap[:, :], s_.ap[:, :], ALU.mult, r=[g_, s_], w=[g_])
                        self.TT("pool", act.ap[:, f, :], g_.ap[:, :], u_.ap[:, :], ALU.mult, r=[g_, u_], w=[act])
                    for dc in range(KC):
                        pd = P[4 + dc % 2]
                        for f in range(KC):
                            self.MM(pd.ap[:, :], wd.ap[:, f, dc * 128:(dc + 1) * 128], act.ap[:, f, :],
                                    start=(f == 0), stop=(f == KC - 1), r=[wd, act], w=[pd])
                        self.TT("dve", oacc.ap[:, dc, tb * 512:(tb + 1) * 512], oacc.ap[:, dc, tb * 512:(tb + 1) * 512], pd.ap[:, :],
                                ALU.add, r=[oacc, pd], w=[oacc])
            for j in range(8):
                xi = half * 8 + j
                x_ = xt[xi % 2]
                self.DMA("sp", x_.ap[:, :], self.x1d[xi * 128:(xi + 1) * 128, :], w=[x_])
                pa, pb = P[(j % 2) * 2], P[(j % 2) * 2 + 1]
                for dc in range(KC):
                    pp = pa if dc < 4 else pb
                    self.TR(pp.ap[:, (dc % 4) * 128:(dc % 4 + 1) * 128], oacc.ap[:, dc, j * 128:(j + 1) * 128], identf,
                            r=[oacc, cst], w=[pp])
                sc = 40 + (j % 2) * 4
                for hf, pp in enumerate((pa, pb)):
                    self.ACT(junk.ap[:, :], pp.ap[:, :], AF.Square, r=[pp], w=[junk, stat], accum_out=stat.ap[:, sc + hf:sc + hf + 1])
                self.TT("dve", stat.ap[:, sc:sc + 1], stat.ap[:, sc:sc + 1], stat.ap[:, sc + 1:sc + 2], ALU.add, r=[stat], w=[stat])
                self.rstd_from_ss(stat.ap[:, sc:sc + 1], D, stat, stat.ap[:, sc + 2:sc + 3])
                for hf, pp in enumerate((pa, pb)):
                    self.STT(cwb.ap[:, :], pp.ap[:, :], stat.ap[:, sc:sc + 1], Gb.ap[:, 1, hf * 512:(hf + 1) * 512], ALU.mult, ALU.mult,
                             r=[pp, stat, Gb], w=[cwb])
                    self.TT("pool", x_.ap[:, hf * 512:(hf + 1) * 512], x_.ap[:, hf * 512:(hf + 1) * 512], cwb.ap[:, :], ALU.add,
                            r=[x_, cwb], w=[x_])
                self.DMA("pool", self.out[xi * 128:(xi + 1) * 128, :], x_.ap[:, :], r=[x_], sem=x_)
        self.final_bufs = list(xt)

    def phase_moe_sparse(self):
        S, P = self.S, self.P
        stat, cst, cstb, Gb = self.stat, self.cst, self.cstb, self.Gb
        identb, onesf, A2, modb = self.identb, self.onesf, self.A2, self.modb
        xsd, ysd, wk, posi_v, posi = self.xsd, self.ysd, self.wk, self.posi_v, self.posi
        meta, nt_i, sidx_i = self.meta, self.nt_i, self.sidx_i
        S.release(self.comb, self.xn2tm)
        bias = S.alloc("ebias", [512], F32)
        self.DMA("sp", bias.ap[:, 0:256], self.bgT, w=[bias])
        self.DMA("sp", bias.ap[:, 256:512], self.buT, w=[bias])
        self.TS("dve", bias.ap[:, 256:512], bias.ap[:, 256:512], 1.0, None, ALU.add, r=[bias], w=[bias])
        W = [[S.alloc("w%d" % k, [KC, D], BF16) for k in range(3)] for _ in range(2)]
        xg0 = [S.alloc("xg0", [2, D], BF16) for _ in range(2)]
        xgn = [S.alloc("xgn", [2, D], BF16) for _ in range(2)]
        xg = xg0 + xgn
        XT = [S.alloc("XT", [KC, SG], BF16) for _ in range(2)]
        acts = [S.alloc("act", [SG], BF16) for _ in range(KC)]
        gb_ = [S.alloc("g", [SG], F32) for _ in range(2)]
        sb_ = [S.alloc("s", [SG], F32) for _ in range(2)]
        ub_ = [S.alloc("u", [SG], F32) for _ in range(2)]
        ys = [S.alloc("ys", [D], F32) for _ in range(2)]
        bdr = [S.alloc("bdr", [D], F32) for _ in range(1)]
        bdrb = [S.alloc("bdrb", [D], BF16) for _ in range(2)]
        srcs = (self.w_gate, self.w_up, self.w_down)

        def prep(x_, xt_):
            for bk in range(2):
                pt = P[4 + bk]
                ptb = pt.ap.bitcast(BF16)
                for k4 in range(4):
                    kc = bk * 4 + k4
                    for h in range(2):
                        self.TR(ptb[:, k4 * SG + h * 128:k4 * SG + (h + 1) * 128], x_.ap[:, h, kc * 128:(kc + 1) * 128], identb,
                                r=[x_, cstb], w=[pt])
                for k4 in range(4):
                    kc = bk * 4 + k4
                    if k4 % 2 == 0:
                        self.TS("dve", xt_.ap[:, kc, :], ptb[:, k4 * SG:(k4 + 1) * SG], A2[:, kc:kc + 1], modb.ap[:, 24 + kc, 0:1],
                                ALU.mult, ALU.add, r=[pt, self.vec, modb], w=[xt_])
                    else:
                        self.ACT(xt_.ap[:, kc, :], ptb[:, k4 * SG:(k4 + 1) * SG], AF.Identity, r=[pt, self.vec, modb], w=[xt_],
                                 scale=A2[:, kc:kc + 1], bias=modb.ap[:, 24 + kc, 0:1])

        def loadw(e):
            for k in range(3):
                self.load_w(W[e % 2][k].ap[:, :, :], W[e % 2][k], srcs[k][e], D, D)
        def gather(buf, e, j):
            for h in range(2):
                self.S.dma_fn("pool", lambda en, o=buf.ap[:, h, :], off=sidx_i[:, e, 2 * j + h:2 * j + h + 1], i_=self.xs_d[:, :]:
                              en.indirect_dma_start(out=o, out_offset=None, in_=i_, in_offset=bass.IndirectOffsetOnAxis(ap=off, axis=0),
                                                    bounds_check=self.S.bc_reg, oob_is_err=False),
                              reads=[xsd, meta], writes=[buf], sem_buf=buf)
        XT0 = [S.alloc("XT0", [KC, SG], BF16) for _ in range(2)]
        gather(xg0[0], 0, 0)
        gather(xg0[1], 1, 0)
        loadw(0)
        prep(xg0[0], XT0[0])
        nst = 0
        ngs = 0
        for e in range(NE):
            gather(xgn[1], e, 1)
            if e + 2 < NE:
                gather(xg0[e % 2], e + 2, 0)
            if e + 1 < NE:
                loadw(e + 1)
            wg, wu, wd = W[e % 2]
            br = bdr[0]
            self.DMA("sp", br.ap[0:1, :], self.b_down[e:e + 1, :], w=[br])
            brb = bdrb[e % 2]
            self.CP("act", brb.ap[0:1, :], br.ap[0:1, :], r=[br], w=[brb])
            if e + 1 < NE:
                prep(xg0[(e + 1) % 2], XT0[(e + 1) % 2])
            S.regload(meta, nt_i[0:1, e:e + 1])
            for j in range(NSTEP):
                S.begin_group(j)
                xt_ = XT0[e % 2] if j == 0 else XT[j % 2]
                ngs += 1
                if j + 2 < NSTEP:
                    gather(xgn[j % 2], e, j + 2)
                for f in range(KC):
                    pg, pu = P[f % 2], P[2 + f % 2]
                    g_, s_, u_ = gb_[f % 2], sb_[f % 2], ub_[f % 2]
                    for kc in range(KC):
                        self.MM(pg.ap[:, 0:SG], wg.ap[:, kc, f * 128:(f + 1) * 128], xt_.ap[:, kc, :],
                                start=(kc == 0), stop=(kc == KC - 1), r=[wg, xt_], w=[pg])
                    for kc in range(KC):
                        self.MM(pu.ap[:, 0:SG], wu.ap[:, kc, f * 128:(f + 1) * 128], xt_.ap[:, kc, :],
                                start=(kc == 0), stop=(kc == KC - 1), r=[wu, xt_], w=[pu])
                    bcol = e * 8 + f
                    self.TS("dve", g_.ap[:, :], pg.ap[:, 0:SG], bias.ap[:, bcol:bcol + 1], 7.0, ALU.add, ALU.min, r=[pg, bias], w=[g_])
                    self.ACT(s_.ap[:, :], g_.ap[:, :], AF.Sigmoid, r=[g_], w=[s_], scale=1.702)
                    self.TS("dve", u_.ap[:, :], pu.ap[:, 0:SG], bias.ap[:, 256 + bcol:256 + bcol + 1], -6.0, ALU.add, ALU.max,
                            r=[pu, bias], w=[u_])
                    self.TT("pool", g_.ap[:, :], g_.ap[:, :], s_.ap[:, :], ALU.mult, r=[g_, s_], w=[g_])
                    self.STT(acts[f].ap[:, :], u_.ap[:, :], 8.0, g_.ap[:, :], ALU.min, ALU.mult, r=[u_, g_], w=[acts[f]])
                if j + 1 < NSTEP:
                    prep(xgn[(j + 1) % 2], XT[(j + 1) % 2])
                for h in range(2):
                    y_ = ys[nst % 2]
                    nst += 1
                    for hf in range(2):
                        pd = P[6 + hf]
                        for f in range(KC):
                            self.MM(pd.ap[:, :], acts[f].ap[:, h * 128:(h + 1) * 128], wd.ap[:, f, hf * 512:(hf + 1) * 512],
                                    start=(f == 0), stop=False, r=[acts[f], wd], w=[pd])
                        self.MM(pd.ap[:, :], self.onesb[0:1, :], brb.ap[0:1, hf * 512:(hf + 1) * 512], start=False, stop=True,
                                r=[cstb, brb], w=[pd])
                        self.CP("act" if hf == 0 else "dve", y_.ap[:, hf * 512:(hf + 1) * 512], pd.ap[:, :], r=[pd], w=[y_])
                    self.S.dma_fn("pool", lambda en, o=self.ys_d[:, :], off=sidx_i[:, e, 2 * j + h:2 * j + h + 1], i_=y_.ap[:, :]:
                                  en.indirect_dma_start(out=o, out_offset=bass.IndirectOffsetOnAxis(ap=off, axis=0), in_=i_, in_offset=None,
                                                        bounds_check=self.S.bc_reg, oob_is_err=False),
                                  reads=[y_, meta], writes=[ysd], sem_buf=y_)
                S.end_group()
        S.release(*W[0], *W[1], *xg, *XT, *XT0, *acts, *gb_, *sb_, *ub_, *bdr, *bdrb, bias)
        yk = [S.alloc("yk", [D], F32) for _ in range(8)]
        acc = [S.alloc("acc", [D], F32) for _ in range(2)]
        junk = S.alloc("junk5", [D], BF16)
        xt = self.x1_bufs
        outb = []
        for xi in range(16):
            x_ = xt[xi % 2]
            a_ = acc[xi % 2]
            self.DMA("sp", x_.ap[:, :], self.x1d[xi * 128:(xi + 1) * 128, :], w=[x_])
            for k in range(4):
                y_ = yk[(xi % 2) * 4 + k]
                self.S.dma_fn("pool", lambda e, o=y_.ap[:, :], off=posi_v[:, xi, k:k + 1], i_=self.ys_d[:, :]:
                              e.indirect_dma_start(out=o, out_offset=None, in_=i_, in_offset=bass.IndirectOffsetOnAxis(ap=off, axis=0),
                                                   bounds_check=self.S.bc_reg, oob_is_err=False),
                              reads=[ysd, posi], writes=[y_], sem_buf=y_)
                if k == 0:
                    self.TS("dve", a_.ap[:, :], y_.ap[:, :], wk.ap[:, xi, 0:1], None, ALU.mult, r=[y_, wk], w=[a_])
                else:
                    self.STT(a_.ap[:, :], y_.ap[:, :], wk.ap[:, xi, k:k + 1], a_.ap[:, :], ALU.mult, ALU.add, r=[y_, wk, a_], w=[a_])
            sc = 40 + (xi % 2) * 4
            self.ACT(junk.ap[:, :], a_.ap[:, :], AF.Square, r=[a_], w=[junk, stat], accum_out=stat.ap[:, sc:sc + 1])
            self.rstd_from_ss(stat.ap[:, sc:sc + 1], D, stat, stat.ap[:, sc + 2:sc + 3])
            if self.dbg is not None and xi in (0, 9):
                self.dump((0 if xi == 0 else 1) * 1024, a_, a_.ap[:, :], 1024)
                self.dump(2048 + (0 if xi == 0 else 1) * 8, wk, wk.ap[:, xi, :], 4)
                self.dump(2048 + 16 + (0 if xi == 0 else 1) * 8, posi, posi.ap[:, xi, :], 4)
            self.STT(a_.ap[:, :], a_.ap[:, :], stat.ap[:, sc:sc + 1], Gb.ap[:, 1, :], ALU.mult, ALU.mult, r=[a_, stat, Gb], w=[a_])
            self.TT("pool", x_.ap[:, :], x_.ap[:, :], a_.ap[:, :], ALU.add, r=[x_, a_], w=[x_])
            self.DMA("sp", self.out[xi * 128:(xi + 1) * 128, :], x_.ap[:, :], r=[x_], sem=x_)
        self.final_bufs = list(xt)

    def finish(self):
        S = self.S
        if self.stage < 90:
            z = S.alloc("zout", [128], F32)
            self.MS("pool", z.ap[:, :], 0.0, w=[z])
            self.DMA("pool", self.out[0:128, 0:128], z.ap[:, :], r=[z], sem=z)
            self.dbg_bufs.append(z)
        S.emit(final_bufs=self.dbg_bufs + getattr(self, "final_bufs", []))


def _host_inputs(inputs, b):
    f = lambda a: np.ascontiguousarray(np.asarray(a, dtype=np.float32))
    cst = _consts()
    m = {}
    m["x"] = f(inputs["x"][b])
    m["ctx"] = f(inputs["ctx"][b])
    cc = np.stack([_kc_layout(f(inputs["c"][b])), _kc_layout(f(inputs["c_ctx"]))], axis=-1)
    m["cc"] = f(cc.reshape(128, 16))
    m["w_mod"] = f(inputs["w_mod"][0])
    m["bmodT"] = _kc_layout(f(inputs["b_mod"][0]))
    m["bmod_row"] = f(inputs["b_mod"][0]).reshape(1, -1)
    gT = np.stack([_kc_layout(f(inputs[k][0])) for k in ("g_pre_mix", "g_post_mix", "g_pre_ffn", "g_post_ffn")], axis=1)
    m["gT"] = f(gT.reshape(128, 32))
    m["gpost_row"] = f(np.concatenate([f(inputs["g_post_mix"][0]), f(inputs["g_post_ffn"][0])]).reshape(1, -1))
    m["w_in"] = f(inputs["w_in"][0])
    wa2 = np.zeros((32, 512), np.float32)
    wa2[0:16, 0:256] = inputs["gla_w_a2_f"][0]
    wa2[16:32, 256:512] = inputs["gla_w_a2_b"][0]
    m["wa2"] = wa2
    m["ba_row"] = f(np.concatenate([f(inputs["gla_b_a_f"][0]), f(inputs["gla_b_a_b"][0])]).reshape(1, 512))
    m["gnb"] = f(np.tile(f(inputs["gla_g_norm"][0])[None, :], (128, 4)))
    m["gq"] = _kc_layout(f(inputs["mla_g_q"][0]))
    m["gkv"] = _kc_layout(f(inputs["mla_g_kv"][0]))
    m["w_uq"] = f(inputs["mla_w_uq"][0])
    m["w_uk"] = f(inputs["mla_w_uk"][0])
    m["w_uv"] = f(inputs["mla_w_uv"][0])
    m["w_br_gla"] = f(inputs["w_br_gla"][0])
    m["w_br_mla"] = f(inputs["w_br_mla"][0])
    m["w_out"] = f(inputs["w_out"][0])
    m["router_w"] = f(inputs["router_w"][0])
    m["router_b"] = f(inputs["router_b"][0]).reshape(1, NE)
    m["w_gate"] = f(inputs["w_gate"][0])
    m["w_up"] = f(inputs["w_up"][0])
    m["w_down"] = f(inputs["w_down"][0])
    bg = f(inputs["b_gate"][0]).reshape(NE, 8, 128).transpose(2, 0, 1)
    bu = f(inputs["b_up"][0]).reshape(NE, 8, 128).transpose(2, 0, 1)
    m["bgT"] = f(bg.reshape(128, NE * 8))
    m["buT"] = f(bu.reshape(128, NE * 8))
    m["b_down"] = f(inputs["b_down"][0])
    m["c_tri"] = f(cst["tri"].reshape(128, 512))
    m["c_mask"] = f(cst["mask"].reshape(128, 1024))
    m["c_ident"] = cst["ident"]
    m["c_ropet"] = f(cst["ropet"].reshape(128, 2 * NTOK))
    m["c_e96"] = cst["e96"]
    m["c_sel"] = cst["sel"]
    m["c_rt"] = cst["rt"]
    return m


_SHARED = ("w_mod", "bmodT", "bmod_row", "gT", "gpost_row", "w_in", "wa2", "ba_row", "gnb", "gq", "gkv", "w_uq", "w_uk",
           "w_uv", "w_br_gla", "w_br_mla", "w_out", "router_w", "router_b", "w_gate", "w_up", "w_down", "bgT", "buT",
           "b_down", "c_tri", "c_mask", "c_ident", "c_ropet", "c_e96", "c_sel", "c_rt")


def kernel(**inputs):
    k = K(stage=99)
    m0 = _host_inputs(inputs, 0)
    in_maps = [m0]
    for b in range(1, 8):
        mb = dict(m0)
        mb["x"] = np.ascontiguousarray(np.asarray(inputs["x"][b], dtype=np.float32))
        mb["ctx"] = np.ascontiguousarray(np.asarray(inputs["ctx"][b], dtype=np.float32))
        cc = np.stack([_kc_layout(np.asarray(inputs["c"][b], np.float32)),
                       _kc_layout(np.asarray(inputs["c_ctx"], np.float32))], axis=-1)
        mb["cc"] = np.ascontiguousarray(cc.reshape(128, 16))
        in_maps.append(mb)
    res = run_bass_kernel_spmd(k.nc, in_maps, core_ids=list(range(8)))
    return np.stack([np.asarray(r["out"], dtype=np.float32) for r in res.results], axis=0)
```

```python
import numpy as np
from contextlib import ExitStack
import concourse.bass as bass
import concourse.mybir as mybir
from concourse.bass_utils import run_bass_kernel_spmd

F32 = mybir.dt.float32
BF16 = mybir.dt.bfloat16
I32 = mybir.dt.int32
AF = mybir.ActivationFunctionType
ALU = mybir.AluOpType
AX = mybir.AxisListType
ENGS = ("pe", "act", "dve", "pool", "sp")

T = 2048
TC = 256
NT = 18
NTOK = 2304
D = 1024
KC = 8
D_IN = 4032
EPS = 1e-6
NE = 32
MLA_SCALE = 96 ** -0.5
SG = 256
NSTEP = 8
NSLOT = T * 4 + NE * SG
MOE_MODE = "sparse"


class Buf:
    __slots__ = ("name", "writers", "readers", "dma_sem", "dma_cnt", "ap", "inherit", "off", "words")

    def __init__(self, name, ap=None):
        self.name = name
        self.ap = ap
        self.writers = []
        self.readers = []
        self.inherit = []
        self.off = None
        self.dma_sem = None
        self.dma_cnt = 0


class Op:
    __slots__ = ("eng", "fn", "idx", "deps", "marked", "dma_buf", "dma_val", "mark_no", "grp")

    def __init__(self, eng, fn):
        self.eng = eng
        self.fn = fn
        self.grp = None
        self.deps = []
        self.marked = False
        self.dma_buf = None
        self.dma_val = 0
        self.mark_no = 0


class Sched:
    def __init__(self, nc, arena_words):
        self.nc = nc
        self.ops = {e: [] for e in ENGS}
        self.bufs = []
        self.arena = nc.alloc_sbuf_tensor("arena", [128, arena_words], F32)
        self.free = [(0, arena_words)]
        self.dead = []
        self.nbuf = 0
        self.cur_grp = None
        self.ngrp = 0

    def alloc(self, name, free_shape, dt=F32):
        n = 1
        for s in free_shape:
            n *= s
        words = (n * (2 if dt == BF16 else 4) + 3) // 4
        words = (words + 7) // 8 * 8
        off = None
        for i, (o, sz) in enumerate(self.free):
            if sz >= words:
                off = o
                if sz == words:
                    self.free.pop(i)
                else:
                    self.free[i] = (o + words, sz - words)
                break
        if off is None:
            raise RuntimeError("arena full allocating %s (%d words); free=%s" % (name, words, self.free))
        v = self.arena[:, off:off + words]
        if dt == BF16:
            v = v.bitcast(BF16)
        v = v[:, 0:n]
        if len(free_shape) == 2:
            v = v.rearrange("p (a b) -> p a b", b=free_shape[1])
        elif len(free_shape) == 3:
            v = v.rearrange("p (a b c) -> p a b c", b=free_shape[1], c=free_shape[2])
        self.nbuf += 1
        b = Buf("%s_%d" % (name, self.nbuf), v)
        b.off, b.words = off, words
        for (o, w, toks) in self.dead:
            if o < off + words and off < o + w:
                b.inherit.extend(toks)
        self.bufs.append(b)
        return b

    def release(self, *bufs):
        for b in bufs:
            toks = [("op", t[1], "raw") if t[0] == "op" else t for t in (b.writers + b.readers + b.inherit)]
            self.dead.append((b.off, b.words, toks))
            self.free.append((b.off, b.words))
        self.free.sort()
        m = []
        for o, w in self.free:
            if m and m[-1][0] + m[-1][1] == o:
                m[-1] = (m[-1][0], m[-1][1] + w)
            else:
                m.append((o, w))
        self.free = m

    def psum(self, name):
        t = self.nc.alloc_psum_tensor(name, [128, 512], F32)
        b = Buf(name, t[:, :])
        self.bufs.append(b)
        return b

    def virt(self, name):
        b = Buf(name, None)
        self.bufs.append(b)
        return b

    def _add(self, eng, fn, reads, writes, dma_buf=None):
        op = Op(eng, fn)
        op.idx = len(self.ops[eng])
        op.grp = self.cur_grp
        is_dma = dma_buf is not None
        deps = []
        for b in reads:
            deps.extend(b.writers)
            deps.extend(b.inherit)
            if b.off is None:
                deps.extend([("op", t[1], "raw") for t in b.readers if t[0] == "op" and t[1].eng != eng])
        for b in writes:
            deps.extend(b.inherit)
            deps.extend(b.readers)
            if is_dma and not b.readers and b.writers and all(w[0] == "dma" for w in b.writers):
                pass
            else:
                deps.extend(b.writers)
        fl = []
        for d in deps:
            if d[0] == "op":
                o = d[1]
                if o.eng == eng and not is_dma:
                    if eng == "pe" or eng == "sp":
                        continue
                    if d[2] != "raw":
                        continue
            fl.append(d)
        op.deps = fl
        if is_dma:
            dma_buf.dma_cnt += 16
            op.dma_buf = dma_buf
            op.dma_val = dma_buf.dma_cnt
            tok_w = ("dma", dma_buf, op.dma_val)
            tok_r = tok_w
        else:
            tok_w = ("op", op, "raw")
            tok_r = ("op", op, "war")
        for b in reads:
            b.readers.append(tok_r)
            if len(b.readers) > 64:
                last = {}
                keep = []
                for t in b.readers:
                    if t[0] == "op":
                        last[t[1].eng] = t
                    else:
                        keep.append(t)
                b.readers = keep[-32:] + list(last.values())
        for b in writes:
            if is_dma and not b.readers and b.writers and all(w[0] == "dma" for w in b.writers):
                b.writers.append(tok_w)
            else:
                b.writers = [tok_w]
                b.readers = []
        self.ops[eng].append(op)
        return op

    def op(self, eng, fn, reads=(), writes=()):
        return self._add(eng, fn, list(reads), list(writes))

    def dma(self, eng, out_ap, in_ap, reads=(), writes=(), sem_buf=None, **kw):
        if sem_buf is None:
            sem_buf = (list(writes) + list(reads))[0]

        def fn(e, out_ap=out_ap, in_ap=in_ap, kw=kw):
            return e.dma_start(out=out_ap, in_=in_ap, **kw)
        return self._add(eng, fn, list(reads), list(writes), dma_buf=sem_buf)

    def begin_group(self, thr):
        self.ngrp += 1
        self.cur_grp = (self.ngrp, thr)

    def end_group(self):
        self.cur_grp = None

    def regload(self, src_buf, ap):
        for e in ENGS:
            self._add(e, ("regload", ap), [src_buf], [])

    def dma_fn(self, eng, fn, reads=(), writes=(), sem_buf=None):
        return self._add(eng, fn, list(reads), list(writes), dma_buf=sem_buf)

    def emit(self, final_bufs=()):
        nc = self.nc
        for e in ENGS:
            for op in self.ops[e]:
                for d in op.deps:
                    if d[0] == "op":
                        d[1].marked = True
        for e in ENGS:
            n = 0
            for op in self.ops[e]:
                if op.marked:
                    n += 1
                    op.mark_no = n
        with ExitStack() as st:
            esem = {e: st.enter_context(nc.semaphore("s_" + e)) for e in ENGS}
            for b in self.bufs:
                if b.dma_cnt > 0:
                    b.dma_sem = st.enter_context(nc.semaphore("d_" + b.name))
            self.bc_reg = st.enter_context(nc.gpsimd.register("rbc"))
            engobj = {"pe": nc.tensor, "act": nc.scalar, "dve": nc.vector, "pool": nc.gpsimd, "sp": nc.sync}
            self.cregs = {e: st.enter_context(engobj[e].register("creg_" + e)) for e in ENGS}
            print("semaphores used:", 5 + sum(1 for b in self.bufs if b.dma_cnt > 0))
            block = st.enter_context(nc.Block())
            handles = {"pe": block.tensor, "act": block.scalar, "dve": block.vector,
                       "pool": block.gpsimd, "sp": block.sync}
            stats = {}
            for e in ENGS:
                ops = self.ops[e]

                def body(eng, ops=ops, e=e):
                    waited = {}
                    nw = [0]
                    if e == "pool":
                        eng.reg_mov(self.bc_reg, NSLOT - 1)
                    creg = self.cregs[e]

                    def emit_op(op, wt):
                        need = {}
                        for d in op.deps:
                            if d[0] == "op":
                                key = ("e", d[1].eng)
                                val = d[1].mark_no
                            else:
                                key = ("d", d[1].name)
                                val = d[2]
                            if val > need.get(key, (0, None))[0]:
                                need[key] = (val, d)
                        for key, (val, d) in need.items():
                            if wt.get(key, 0) >= val:
                                continue
                            wt[key] = val
                            sem = esem[d[1].eng] if d[0] == "op" else d[1].dma_sem
                            eng.wait_ge(sem, val)
                            nw[0] += 1
                        if isinstance(op.fn, tuple):
                            eng.reg_load(creg, op.fn[1])
                            return
                        ins = op.fn(eng)
                        if op.dma_buf is not None:
                            ins.then_inc(op.dma_buf.dma_sem, 16)
                        elif op.marked:
                            ins.then_inc(esem[e], 1)
                    def comp_of(gops):
                        comp = []
                        marked = [o for o in gops if o.marked and o.dma_buf is None]
                        if marked:
                            comp.append((esem[e], marked[0].mark_no - 1, len(marked)))
                        dm = {}
                        for o in gops:
                            if o.dma_buf is not None:
                                if o.dma_buf.name not in dm:
                                    dm[o.dma_buf.name] = [o.dma_buf.dma_sem, o.dma_val - 16, 0]
                                dm[o.dma_buf.name][2] += 16
                        comp.extend(tuple(v) for v in dm.values())
                        return comp

                    def chain(runs, k, wt):
                        rest = [o for r_ in runs[k:] for o in r_]
                        thr = runs[k][0].grp[1]
                        with eng.If_lt(creg, thr + 1):
                            for sem, before, delta in comp_of(rest):
                                if before > 0:
                                    eng.wait_ge(sem, before)
                                eng.sem_inc(sem, delta)
                        with eng.Else():
                            for o in runs[k]:
                                emit_op(o, wt)
                            if k + 1 < len(runs):
                                chain(runs, k + 1, wt)
                    i = 0
                    n = len(ops)
                    while i < n:
                        op = ops[i]
                        if op.grp is None:
                            emit_op(op, waited)
                            i += 1
                            continue
                        runs = []
                        j = i
                        while j < n and ops[j].grp is not None and (not runs or ops[j].grp[1] > runs[-1][0].grp[1] or ops[j].grp is runs[-1][0].grp):
                            if runs and ops[j].grp is runs[-1][0].grp:
                                runs[-1].append(ops[j])
                            else:
                                runs.append([ops[j]])
                            j += 1
                        chain(runs, 0, dict(waited))
                        i = j
                    if e == "sp":
                        for b in final_bufs:
                            if b.dma_cnt:
                                eng.wait_ge(b.dma_sem, b.dma_cnt)
                    stats[e] = (len(ops), nw[0])
                handles[e](body)
            self.stats = stats


def _consts():
    c = {}
    idx = np.arange(128)
    same = (idx[:, None] // 64) == (idx[None, :] // 64)
    s = idx[:, None]
    t = idx[None, :]
    tri = np.zeros((128, 4, 128), np.float32)
    tri[:, 0] = same & (s <= t)
    tri[:, 1] = same & (s >= t)
    tri[:, 2] = same & (s > t)
    tri[:, 3] = same & (s < t)
    c["tri"] = tri
    mask = np.zeros((128, 2, 4, 128), np.float32)
    mask[:, 0] = (same & (t >= s))[:, None, :]
    mask[:, 1] = (same & (t <= s))[:, None, :]
    c["mask"] = mask.reshape(128, 2, 512)
    c["ident"] = np.eye(128, dtype=np.float32)
    half = 16
    inv_freq = (10000.0 ** (-np.arange(0, half, 2, dtype=np.float32) / half)).astype(np.float32)
    tok = np.arange(T)
    ang_row = (tok // 64).astype(np.float32)[:, None] * inv_freq
    ang_col = (tok % 64).astype(np.float32)[:, None] * inv_freq
    cosr, sinr = np.cos(ang_row), np.sin(ang_row)
    cosc, sinc = np.cos(ang_col), np.sin(ang_col)
    cos32 = np.concatenate([cosr, cosr, cosc, cosc], axis=1)
    sin32 = np.concatenate([-sinr, sinr, -sinc, sinc], axis=1)
    ropet = np.zeros((128, 2, NTOK), np.float32)
    ropet[64:96, 0, :TC] = 1.0
    ropet[64:96, 0, TC:] = cos32.T
    ropet[64:96, 1, TC:] = sin32.T
    c["ropet"] = ropet
    e96 = np.zeros((128, 128), np.float32)
    e96[:96, 96] = 1.0
    c["e96"] = e96
    sel = np.zeros((NE, NE, 128), np.float32)
    for e in range(NE):
        sel[e, e, :] = 1.0
    c["sel"] = sel.reshape(NE, NE * 128)
    rt = np.zeros((128, 176), np.float32)
    rt[:, 0:128] = (s < t)
    ke = np.arange(NE)
    rt[0:NE, 128:160] = (ke[:, None] < ke[None, :])
    rt[:, 160:176] = np.arange(16, dtype=np.float32)[None, :] * 128 + idx[:, None]
    c["rt"] = rt
    return c


def _kc_layout(v):
    return np.ascontiguousarray(v.reshape(-1, 128).T)


class K:
    def __init__(self, stage=99, dbg=False):
        self.stage = stage
        nc = bass.Bass("TRN2", target_bir_lowering=False)
        self.nc = nc
        self.S = Sched(nc, 51200 + 1000)
        S = self.S
        dt = nc.dram_tensor
        self.din = {}

        def inp(name, shape):
            self.din[name] = dt(name, list(shape), F32, kind="ExternalInput").ap()
            return self.din[name]
        self.x = inp("x", [T, D])
        self.ctx = inp("ctx", [TC, D])
        self.cc = inp("cc", [128, 16])
        self.w_mod = inp("w_mod", [D, 6 * D])
        self.bmodT = inp("bmodT", [128, 48])
        self.bmod_row = inp("bmod_row", [1, 6 * D])
        self.gT = inp("gT", [128, 32])
        self.gpost_row = inp("gpost_row", [1, 2 * D])
        self.w_in = inp("w_in", [D, D_IN])
        self.wa2 = inp("wa2", [32, 512])
        self.ba_row = inp("ba_row", [1, 512])
        self.gnb = inp("gnb", [128, 512])
        self.gq = inp("gq", [128, 2])
        self.gkv = inp("gkv", [128, 1])
        self.w_uq = inp("w_uq", [256, 768])
        self.w_uk = inp("w_uk", [128, 512])
        self.w_uv = inp("w_uv", [128, 512])
        self.w_br_gla = inp("w_br_gla", [512, D])
        self.w_br_mla = inp("w_br_mla", [512, D])
        self.w_out = inp("w_out", [D, D])
        self.router_w = inp("router_w", [D, NE])
        self.router_b = inp("router_b", [1, NE])
        self.w_gate = inp("w_gate", [NE, D, D])
        self.w_up = inp("w_up", [NE, D, D])
        self.w_down = inp("w_down", [NE, D, D])
        self.bgT = inp("bgT", [128, NE * 8])
        self.buT = inp("buT", [128, NE * 8])
        self.b_down = inp("b_down", [NE, D])
        self.c_tri = inp("c_tri", [128, 512])
        self.c_mask = inp("c_mask", [128, 1024])
        self.c_ident = inp("c_ident", [128, 128])
        self.c_ropet = inp("c_ropet", [128, 2 * NTOK])
        self.c_e96 = inp("c_e96", [128, 128])
        self.c_sel = inp("c_sel", [NE, NE * 128])
        self.c_rt = inp("c_rt", [128, 176])
        self.xs_d = dt("xs_d", [NSLOT, D], BF16, kind="Internal").ap()
        self.ys_d = dt("ys_d", [NSLOT, D], F32, kind="Internal").ap()
        self.out = dt("out", [T, D], F32, kind="ExternalOutput").ap()
        self.x1d = dt("x1d", [T, D], F32, kind="Internal").ap()
        self.dbg = dt("dbg", [128, 8192], F32, kind="ExternalOutput").ap() if dbg else None
        self.P = [S.psum("ps%d" % i) for i in range(8)]
        self.wq_i = 0
        self.build()

    def MM(self, out, lhsT, rhs, start=True, stop=True, r=(), w=()):
        self.S.op("pe", lambda e: e.matmul(out, lhsT, rhs, start=start, stop=stop, skip_group_check=True), r, w)

    def TR(self, out, in_, ident, r=(), w=()):
        self.S.op("pe", lambda e: e.transpose(out, in_, ident), r, w)

    def ACT(self, out, in_, func, r=(), w=(), **kw):
        self.S.op("act", lambda e: e.activation(out, in_, func, **kw), r, w)

    def TT(self, eng, out, a, b, op, r=(), w=()):
        self.S.op(eng, lambda e: e.tensor_tensor(out, a, b, op=op), r, w)

    def TS(self, eng, out, a, s1, s2, op0, op1=None, r=(), w=()):
        if op1 is None:
            self.S.op(eng, lambda e: e.tensor_scalar(out, a, s1, None, op0=op0), r, w)
        else:
            self.S.op(eng, lambda e: e.tensor_scalar(out, a, s1, s2, op0=op0, op1=op1), r, w)

    def STT(self, out, a, s, b, op0, op1, r=(), w=()):
        self.S.op("dve", lambda e: e.scalar_tensor_tensor(out, a, s, b, op0=op0, op1=op1), r, w)

    def CP(self, eng, out, in_, r=(), w=()):
        if eng == "act":
            self.S.op("act", lambda e: e.copy(out, in_), r, w)
        else:
            self.S.op(eng, lambda e: e.tensor_copy(out, in_), r, w)

    def MS(self, eng, ap, val, w=()):
        self.S.op(eng, lambda e: e.memset(ap, val), (), w)

    def DMA(self, q, out, in_, r=(), w=(), sem=None, **kw):
        self.S.dma(q, out, in_, reads=r, writes=w, sem_buf=sem, **kw)

    def rstd_from_ss(self, ss_ap, n, buf, tmp_ap):
        self.ACT(tmp_ap, ss_ap, AF.Ln, r=[buf], w=[buf], scale=1.0 / n, bias=self.eps_ap)
        self.ACT(ss_ap, tmp_ap, AF.Exp, r=[buf], w=[buf], scale=-0.5)

    def load_w(self, dst_ap, dst_buf, src_ap, rows, cols, q="pool"):
        if rows > 128:
            self.DMA("pool", dst_ap, src_ap.rearrange("(k p) n -> p k n", p=128), w=[dst_buf])
        else:
            self.DMA("pool", dst_ap[0:rows, 0:cols], src_ap, w=[dst_buf])

    def dump(self, col, buf, ap, ncols, parts=128):
        if self.dbg is None:
            return
        S = self.S
        tmp = S.alloc("dbgtmp", [ncols], F32)
        self.CP("dve", tmp.ap[0:parts, :], ap, r=[buf], w=[tmp])
        self.DMA("pool", self.dbg[0:parts, col:col + ncols], tmp.ap[0:parts, :], r=[tmp], sem=tmp)
        self.dbg_bufs.append(tmp)

    def build(self):
        S = self.S
        P = self.P
        self.dbg_bufs = []
        cst = S.alloc("cst", [2048], F32)
        tri = cst.ap[:, 0:512].rearrange("p (a b) -> p a b", b=128)
        identf = cst.ap[:, 512:640]
        e96f = cst.ap[:, 640:768]
        self.eps_ap = cst.ap[:, 768:769]
        onesf = cst.ap[:, 896:1024]
        self.DMA("sp", cst.ap[:, 0:512], self.c_tri, w=[cst])
        self.DMA("sp", identf, self.c_ident, w=[cst])
        self.DMA("sp", e96f, self.c_e96, w=[cst])
        self.MS("pool", cst.ap[:, 768:769], EPS, w=[cst])
        self.MS("pool", onesf, 1.0, w=[cst])
        self.rowmask = cst.ap[:, 1024:1026]
        self.MS("pool", cst.ap[:, 1024:1026], 0.0, w=[cst])
        self.MS("pool", cst.ap[0:64, 1024:1025], 1.0, w=[cst])
        self.MS("pool", cst.ap[64:128, 1025:1026], 1.0, w=[cst])
        cstb = S.alloc("cstb", [2048], BF16)
        identb = cstb.ap[:, 0:128]
        onesb = cstb.ap[:, 128:256]
        e96b = cstb.ap[:, 256:384]
        maskb = cstb.ap[:, 1024:2048].rearrange("p (a b) -> p a b", b=512)
        self.CP("pool", identb, identf, r=[cst], w=[cstb])
        self.CP("pool", onesb, onesf, r=[cst], w=[cstb])
        self.CP("pool", e96b, e96f, r=[cst], w=[cstb])
        self.wstage = [S.alloc("wst", [8, 256], F32) for _ in range(3)]
        mstage = self.wstage[0]
        self.DMA("sp", mstage.ap[:, 0:4, :], self.c_mask.rearrange("p (a b) -> p a b", b=256), w=[mstage])
        self.CP("pool", cstb.ap[:, 1024:2048].rearrange("p (a b) -> p a b", b=256), mstage.ap[:, 0:4, :], r=[mstage], w=[cstb])
        stat = S.alloc("stat", [64], F32)

        modb = S.alloc("mod", [48, 2], F32)
        vec = S.alloc("vec", [16 + 48 + 32 + 32 + 16 + 16 + 8 + 8], F32)
        cc = vec.ap[:, 0:16].rearrange("p (a b) -> p a b", b=2)
        bmodT = vec.ap[:, 16:64]
        gT = vec.ap[:, 64:96].rearrange("p (a b) -> p a b", b=8)
        A1 = vec.ap[:, 96:112].rearrange("p (a b) -> p a b", b=2)
        B1v = None
        A2 = vec.ap[:, 112:120]
        gqv = vec.ap[:, 128:130]
        gkvv = vec.ap[:, 130:131]
        self.DMA("sp", vec.ap[:, 0:16], self.cc, w=[vec])
        self.DMA("sp", bmodT, self.bmodT, w=[vec])
        self.DMA("sp", vec.ap[:, 64:96], self.gT, w=[vec])
        self.DMA("sp", gqv, self.gq, w=[vec])
        self.DMA("sp", gkvv, self.gkv, w=[vec])
        self.ACT(cc, cc, AF.Silu, r=[vec], w=[vec])
        rows = S.alloc("rows", [4096], F32)
        self.DMA("sp", rows.ap[0:1, 2048:4096], self.gpost_row, w=[rows])
        browst = S.alloc("browst", [2048], F32)
        self.DMA("sp", browst.ap[0:1, 0:1024], self.bmod_row[:, 2048:3072], w=[browst])
        self.DMA("sp", browst.ap[0:1, 1024:2048], self.bmod_row[:, 5120:6144], w=[browst])
        wm_view = self.w_mod.rearrange("(k p) n -> p k n", p=128)
        ccb = S.alloc("ccb", [8, 2], BF16)
        self.CP("dve", ccb.ap[:, :, :], cc, r=[vec], w=[ccb])
        wmb = [S.alloc("wmb", [KC, 512], BF16) for _ in range(3)]
        for j in range(12):
            st = wmb[j % 3]
            self.DMA("pool", st.ap[:, :, :], wm_view[:, :, j * 512:(j + 1) * 512], w=[st])
            for m in range(4):
                col = (j * 4 + m) * 2
                for kc in range(KC):
                    self.MM(P[0].ap[:, col:col + 2], st.ap[:, kc, m * 128:(m + 1) * 128], ccb.ap[:, kc, :],
                            start=(kc == 0), stop=(kc == KC - 1), r=[st, ccb], w=[P[0]])
            if j in (4, 5, 10, 11):
                ro = {4: 0, 5: 512, 10: 1024, 11: 1536}[j]
                for kc in range(KC):
                    self.MM(P[1].ap[0:1, :], ccb.ap[:, kc, 0:1], st.ap[:, kc, :], start=(kc == 0), stop=(kc == KC - 1),
                            r=[st, ccb], w=[P[1]])
                self.TT("dve", rows.ap[0:1, ro:ro + 512], P[1].ap[0:1, :], browst.ap[0:1, ro:ro + 512], ALU.add,
                        r=[P[1], browst], w=[rows])
        S.release(ccb, *wmb)
        S.release(browst)
        self.TT("dve", modb.ap[:, :, :], P[0].ap[:, 0:96].rearrange("p (a b) -> p a b", b=2),
                bmodT.unsqueeze(2).to_broadcast([128, 48, 2]), ALU.add, r=[P[0], vec], w=[modb])
        self.STT(A1, modb.ap[:, 8:16, :], 1.0, gT[:, 0, :].unsqueeze(2).to_broadcast([128, 8, 2]), ALU.add, ALU.mult,
                 r=[modb, vec], w=[vec])
        self.STT(A2, modb.ap[:, 32:40, 0], 1.0, gT[:, 2, :], ALU.add, ALU.mult, r=[modb, vec], w=[vec])
        self.TT("dve", rows.ap[0:1, 0:2048], rows.ap[0:1, 0:2048], rows.ap[0:1, 2048:4096], ALU.mult, r=[rows], w=[rows])
        Gb = S.alloc("Gb", [2, 1024], F32)
        for g in range(2):
            for hh in range(2):
                pb = P[2 + hh]
                self.MM(pb.ap[:, :], onesf[0:1, :], rows.ap[0:1, g * 1024 + hh * 512: g * 1024 + hh * 512 + 512],
                        r=[cst, rows], w=[pb])
                self.CP("act", Gb.ap[:, g, hh * 512:(hh + 1) * 512], pb.ap[:, :], r=[pb], w=[Gb])
        S.release(rows, *self.wstage)
        if self.stage == 0:
            self.dump(0, modb, modb.ap[:, :, :].rearrange("p a b -> p (a b)"), 96)
            self.dump(96, vec, vec.ap[:, 96:120], 24)
            self.dump(128, Gb, Gb.ap[:, :, :].rearrange("p a b -> p (a b)"), 2048)
            return self.finish()

        hT = S.alloc("hT", [KC, NTOK], BF16)
        xts = [S.alloc("xt", [D], F32) for _ in range(3)]
        xns = [S.alloc("xn", [D], BF16) for _ in range(2)]
        junk = S.alloc("junk", [D], BF16)
        for i in range(NT):
            xt = xts[i % 3]
            xn = xns[i % 2]
            src = self.ctx[i * 128:(i + 1) * 128, :] if i < 2 else self.x[(i - 2) * 128:(i - 1) * 128, :]
            v = 1 if i < 2 else 0
            self.DMA("sp", xt.ap[:, :], src, w=[xt])
            ss = stat.ap[:, (i % 4) * 2:(i % 4) * 2 + 1]
            tmp = stat.ap[:, (i % 4) * 2 + 1:(i % 4) * 2 + 2]
            self.ACT(junk.ap[:, :], xt.ap[:, :], AF.Square, r=[xt], w=[junk, stat], accum_out=ss)
            self.rstd_from_ss(ss, D, stat, tmp)
            self.TS("dve", xn.ap[:, :], xt.ap[:, :], ss, None, ALU.mult, r=[xt, stat], w=[xn])
            pt = P[i % 2]
            ptb = pt.ap.bitcast(BF16)
            for kc in range(KC):
                self.TR(ptb[:, kc * 128:(kc + 1) * 128], xn.ap[:, kc * 128:(kc + 1) * 128], identb, r=[xn, cstb], w=[pt])
            for kc in range(KC):
                o = hT.ap[:, kc, i * 128:(i + 1) * 128]
                if kc % 2 == 0:
                    self.TS("dve", o, ptb[:, kc * 128:(kc + 1) * 128], A1[:, kc, v:v + 1], modb.ap[:, kc, v:v + 1],
                            ALU.mult, ALU.add, r=[pt, vec, modb], w=[hT])
                else:
                    self.ACT(o, ptb[:, kc * 128:(kc + 1) * 128], AF.Identity, r=[pt, vec, modb], w=[hT],
                             scale=A1[:, kc, v:v + 1], bias=modb.ap[:, kc, v:v + 1])
        S.release(*xts, *xns, junk)
        if self.stage == 1:
            for kc in range(2):
                self.dump(kc * 2304, hT, hT.ap[:, kc, :], 2304)
            return self.finish()
        self.cst, self.cstb, self.stat, self.vec, self.modb, self.Gb, self.hT, self.junk = cst, cstb, stat, vec, modb, Gb, hT, junk
        self.tri, self.identf, self.identb, self.onesf, self.onesb, self.e96b, self.maskb = tri, identf, identb, onesf, onesb, e96b, maskb
        self.A2, self.gqv, self.gkvv = A2, gqv, gkvv
        self.phase_gla()
        if self.stage == 25:
            return self.finish()
        if self.stage == 3:
            for hc in range(4):
                self.dump(hc * 2048, self.ogT, self.ogT.ap[:, hc, :], 2048)
            return self.finish()
        self.phase_mla()
        if self.stage == 4:
            for h in range(4):
                self.dump(h * 2048, self.omlaT, self.omlaT.ap[0:64, h, :], 2048, parts=64)
            return self.finish()
        self.phase_merge()
        if self.stage == 5:
            return self.finish()
        if MOE_MODE == "dense":
            self.phase_moe()
        else:
            self.phase_moe_sparse()
        self.finish()

    def phase_mla(self):
        S, P = self.S, self.P
        hT, cstb, cst, stat = self.hT, self.cstb, self.cst, self.stat
        identb, onesf, onesb, identf = self.identb, self.onesf, self.onesb, self.identf
        wB = S.alloc("wB", [KC, 416], BF16)
        self.load_w(wB.ap[:, :, :], wB, self.w_in[:, 1568:1984], 1024, 416)
        wkrs = S.alloc("wkrs", [KC, 32], BF16)
        for a, b_ in ((0, 8), (8, 0), (16, 24), (24, 16)):
            self.CP("pool", wkrs.ap[:, :, a:a + 8], wB.ap[:, :, 384 + b_:384 + b_ + 8], r=[wB], w=[wkrs])
        wuq = S.alloc("wuq", [2, 768], BF16)
        self.load_w(wuq.ap[:, :, :], wuq, self.w_uq, 256, 768)
        wuqs = S.alloc("wuqs", [2, 8, 32], BF16)
        for h in range(8):
            for a, b_ in ((0, 8), (8, 0), (16, 24), (24, 16)):
                self.CP("pool", wuqs.ap[:, :, h, a:a + 8], wuq.ap[:, :, h * 96 + 64 + b_:h * 96 + 64 + b_ + 8], r=[wuq], w=[wuqs])
        wukv = S.alloc("wukv", [1024], BF16)
        self.load_w(wukv.ap[:, 0:512], wukv, self.w_uk, 128, 512)
        self.load_w(wukv.ap[:, 512:1024], wukv, self.w_uv, 128, 512)
        zdqnT = S.alloc("zdqnT", [2, T], BF16)
        ckvT = S.alloc("ckvT", [NTOK], BF16)
        krT = S.alloc("krT", [NTOK], BF16)
        zn = [S.alloc("zn", [384], BF16) for _ in range(2)]
        junk = S.alloc("junk2", [384], F32)
        for i in range(NT):
            pa = P[i % 2]
            pt = P[2 + i % 2]
            ptb = pt.ap.bitcast(BF16)
            z = zn[i % 2]
            sc = (i % 4) * 4
            if i >= 2:
                for kc in range(KC):
                    self.MM(pa.ap[:, 0:256], hT.ap[:, kc, i * 128:(i + 1) * 128], wB.ap[:, kc, 0:256],
                            start=(kc == 0), stop=(kc == KC - 1), r=[wB, hT], w=[pa])
                self.ACT(junk.ap[:, 0:256], pa.ap[:, 0:256], AF.Square, r=[pa], w=[junk, stat], accum_out=stat.ap[:, sc:sc + 1])
                self.rstd_from_ss(stat.ap[:, sc:sc + 1], 256, stat, stat.ap[:, sc + 1:sc + 2])
                self.TS("dve", z.ap[:, 0:256], pa.ap[:, 0:256], stat.ap[:, sc:sc + 1], None, ALU.mult, r=[pa, stat], w=[z])
            for kc in range(KC):
                self.MM(pa.ap[:, 256:384], hT.ap[:, kc, i * 128:(i + 1) * 128], wB.ap[:, kc, 256:384],
                        start=(kc == 0), stop=(kc == KC - 1), r=[wB, hT], w=[pa])
            self.ACT(junk.ap[:, 256:384], pa.ap[:, 256:384], AF.Square, r=[pa], w=[junk, stat], accum_out=stat.ap[:, sc + 2:sc + 3])
            self.rstd_from_ss(stat.ap[:, sc + 2:sc + 3], 128, stat, stat.ap[:, sc + 3:sc + 4])
            self.TS("dve", z.ap[:, 256:384], pa.ap[:, 256:384], stat.ap[:, sc + 2:sc + 3], None, ALU.mult, r=[pa, stat], w=[z])
            if i >= 2:
                for c in range(2):
                    self.TR(ptb[:, c * 128:(c + 1) * 128], z.ap[:, c * 128:(c + 1) * 128], identb, r=[z, cstb], w=[pt])
                    self.TS("dve", zdqnT.ap[:, c, (i - 2) * 128:(i - 1) * 128], ptb[:, c * 128:(c + 1) * 128], self.gqv[:, c:c + 1], None,
                            ALU.mult, r=[pt, self.vec], w=[zdqnT])
            self.TR(ptb[:, 256:384], z.ap[:, 256:384], identb, r=[z, cstb], w=[pt])
            self.TS("dve", ckvT.ap[:, i * 128:(i + 1) * 128], ptb[:, 256:384], self.gkvv[:, 0:1], None, ALU.mult,
                    r=[pt, self.vec], w=[ckvT])
        TB_ALL = [(0, 256), (256, 512), (768, 512), (1280, 512), (1792, 512)]
        rp = [S.alloc("rp", [2, 512], F32) for _ in range(2)]
        rt = S.alloc("rt", [2, 512], F32)
        ropv = self.c_ropet.rearrange("p (a b) -> p a b", b=NTOK)
        for bi, (c0, nn) in enumerate(TB_ALL):
            r_ = rp[bi % 2]
            self.DMA("sp", r_.ap[64:96, :, 0:nn], ropv[64:96, :, c0:c0 + nn], w=[r_])
            for kc in range(KC):
                self.MM(P[4].ap[64:96, 0:nn], wB.ap[:, kc, 384:416], hT.ap[:, kc, c0:c0 + nn], start=(kc == 0), stop=(kc == KC - 1),
                        r=[wB, hT], w=[P[4]])
            for kc in range(KC):
                self.MM(P[5].ap[64:96, 0:nn], wkrs.ap[:, kc, :], hT.ap[:, kc, c0:c0 + nn], start=(kc == 0), stop=(kc == KC - 1),
                        r=[wkrs, hT], w=[P[5]])
            self.TT("dve", rt.ap[64:96, 0, 0:nn], P[4].ap[64:96, 0:nn], r_.ap[64:96, 0, 0:nn], ALU.mult, r=[P[4], r_], w=[rt])
            self.TT("dve", rt.ap[64:96, 1, 0:nn], P[5].ap[64:96, 0:nn], r_.ap[64:96, 1, 0:nn], ALU.mult, r=[P[5], r_], w=[rt])
            self.TT("pool", krT.ap[64:96, c0:c0 + nn], rt.ap[64:96, 0, 0:nn], rt.ap[64:96, 1, 0:nn], ALU.add, r=[rt], w=[krT])
        S.release(wB, wkrs, junk, *zn)
        v = S.alloc("v", [NT, 8, 128], BF16)
        self.MS("pool", v.ap[:, :, 0:4, 64:128], 0.0, w=[v])
        self.MS("pool", v.ap[:, :, 0:4, 64:65], 1.0, w=[v])
        self.MS("pool", v.ap[:, :, 4:8, 0:64], 0.0, w=[v])
        self.MS("pool", v.ap[:, :, 4:8, 0:1], 1.0, w=[v])
        for i in range(NT):
            pb = P[i % 2]
            self.MM(pb.ap[:, :], ckvT.ap[:, i * 128:(i + 1) * 128], wukv.ap[:, 512:1024], r=[ckvT, wukv], w=[pb])
            self.CP("act", v.ap[:, i, 0:4, 0:64], pb.ap[:, 0:256].rearrange("p (a b) -> p a b", b=64), r=[pb], w=[v])
            self.CP("dve", v.ap[:, i, 4:8, 64:128], pb.ap[:, 256:512].rearrange("p (a b) -> p a b", b=64), r=[pb], w=[v])
        omlaT = S.alloc("omlaT", [4, T], BF16)
        self.omlaT = omlaT
        kThs = [S.alloc("kTh", [NTOK], BF16) for _ in range(2)]
        qThs = [S.alloc("qTh", [T], BF16) for _ in range(2)]
        pT = [S.alloc("pT", [512], BF16) for _ in range(3)]
        rden = S.alloc("rden", [512], F32)
        bcs = S.alloc("bcs", [512], F32)
        sqb = S.alloc("sqb", [NTOK], BF16)
        mxs = [S.alloc("mx", [8], F32) for _ in range(2)]
        nb = [0]

        def build(h):
            kTh, qTh = kThs[h % 2], qThs[h % 2]
            for (c0, nn) in TB_ALL:
                pb = P[nb[0] % 2]
                nb[0] += 1
                self.MM(pb.ap[0:64, 0:nn], wukv.ap[:, h * 64:(h + 1) * 64], ckvT.ap[:, c0:c0 + nn], r=[wukv, ckvT], w=[pb])
                self.CP("dve", kTh.ap[0:64, c0:c0 + nn], pb.ap[0:64, 0:nn], r=[pb], w=[kTh])
            self.CP("pool", kTh.ap[64:96, :], krT.ap[64:96, :], r=[krT], w=[kTh])
            for qb in range(4):
                c0 = TC + qb * 512
                r_ = rp[qb % 2]
                self.DMA("sp", r_.ap[64:96, :, :], ropv[64:96, :, c0:c0 + 512], w=[r_])
                pq, pr = P[2], P[3]
                for c in range(2):
                    self.MM(pq.ap[0:96, :], wuq.ap[:, c, h * 96:(h + 1) * 96], zdqnT.ap[:, c, qb * 512:(qb + 1) * 512],
                            start=(c == 0), stop=(c == 1), r=[wuq, zdqnT], w=[pq])
                for c in range(2):
                    self.MM(pr.ap[64:96, :], wuqs.ap[:, c, h, :], zdqnT.ap[:, c, qb * 512:(qb + 1) * 512],
                            start=(c == 0), stop=(c == 1), r=[wuqs, zdqnT], w=[pr])
                self.CP("dve", qTh.ap[0:64, qb * 512:(qb + 1) * 512], pq.ap[0:64, :], r=[pq], w=[qTh])
                self.TT("dve", rt.ap[64:96, 0, :], pq.ap[64:96, :], r_.ap[64:96, 0, :], ALU.mult, r=[pq, r_], w=[rt])
                self.TT("dve", rt.ap[64:96, 1, :], pr.ap[64:96, :], r_.ap[64:96, 1, :], ALU.mult, r=[pr, r_], w=[rt])
                self.TT("pool", qTh.ap[64:96, qb * 512:(qb + 1) * 512], rt.ap[64:96, 0, :], rt.ap[64:96, 1, :], ALU.add, r=[rt], w=[qTh])

        def build_stab(h):
            kTh, qTh = kThs[h % 2], qThs[h % 2]
            sq = sqb
            mx = mxs[h % 2]
            pn = P[3]
            for which, src_, ntile in ((0, qTh, 16), (1, kTh, NT)):
                self.TT("pool", sq.ap[0:96, 0:ntile * 128], src_.ap[0:96, 0:ntile * 128], src_.ap[0:96, 0:ntile * 128], ALU.mult,
                        r=[src_], w=[sq])
                for t_ in range(ntile):
                    self.MM(pn.ap[:, which * 32 + t_:which * 32 + t_ + 1], sq.ap[0:96, t_ * 128:(t_ + 1) * 128], onesb[0:96, 0:1],
                            r=[sq, cstb], w=[pn])
                self.S.op("dve", lambda e, o=mx.ap[:, which:which + 1], a=pn.ap[:, which * 32:which * 32 + ntile]: e.reduce_max(o, a, axis=AX.X),
                          [pn], [mx])
            for which in range(2):
                self.TR(pn.ap[0:1, 64 + which * 128:64 + (which + 1) * 128], mx.ap[:, which:which + 1], identf, r=[mx, cst], w=[pn])
                self.S.op("dve", lambda e, o=mx.ap[0:1, 2 + which:3 + which], a=pn.ap[0:1, 64 + which * 128:64 + (which + 1) * 128]:
                          e.reduce_max(o, a, axis=AX.X), [pn], [mx])
            self.TT("dve", mx.ap[0:1, 4:5], mx.ap[0:1, 2:3], mx.ap[0:1, 3:4], ALU.mult, r=[mx], w=[mx])
            self.ACT(mx.ap[0:1, 5:6], mx.ap[0:1, 4:5], AF.Ln, r=[mx], w=[mx], bias=self.eps_ap[0:1, :])
            self.ACT(mx.ap[0:1, 6:7], mx.ap[0:1, 5:6], AF.Exp, r=[mx], w=[mx], scale=0.5)
            self.MM(pn.ap[:, 400:401], onesf[0:1, :], mx.ap[0:1, 6:7], r=[cst, mx], w=[pn])
            self.TS("dve", mx.ap[:, 7:8], pn.ap[:, 400:401], -1.02 * MLA_SCALE, None, ALU.mult, r=[pn], w=[mx])

        def tail(h, qb):
            po = P[6 + qb % 2]
            dr, lo = (64, 0) if h < 4 else (0, 64)
            self.CP("act", rden.ap[dr:dr + 1, :], po.ap[dr:dr + 1, :], r=[po], w=[rden])
            pbc = P[2]
            self.MM(pbc.ap[lo:lo + 64, :], onesf[dr:dr + 1, 0:64], rden.ap[dr:dr + 1, :], r=[cst, rden], w=[pbc])
            self.S.op("dve", lambda e, o=bcs.ap[lo:lo + 64, :], a=pbc.ap[lo:lo + 64, :]: e.reciprocal(o, a), [pbc], [bcs])
            self.TT("dve", omlaT.ap[lo:lo + 64, h % 4, qb * 512:(qb + 1) * 512], po.ap[lo:lo + 64, :], bcs.ap[lo:lo + 64, :], ALU.mult,
                    r=[po, bcs], w=[omlaT])
        build(0)
        build_stab(0)
        pend = None
        for h in range(8):
            kTh, qTh = kThs[h % 2], qThs[h % 2]
            if h + 1 < 8:
                build(h + 1)
            for qb in range(4):
                po = P[6 + qb % 2]

                def qk(kt, qb=qb, kTh=kTh, qTh=qTh):
                    self.MM(P[4 + kt % 2].ap[:, :], kTh.ap[0:96, kt * 128:(kt + 1) * 128], qTh.ap[0:96, qb * 512:(qb + 1) * 512],
                            r=[kTh, qTh], w=[P[4 + kt % 2]])
                qk(0)
                for kt in range(NT):
                    psb = P[4 + kt % 2]
                    p_ = pT[kt % 3]
                    if kt + 1 < NT:
                        qk(kt + 1)
                    self.ACT(p_.ap[:, :], psb.ap[:, :], AF.Exp, r=[psb, mxs[h % 2]], w=[p_], scale=MLA_SCALE, bias=mxs[h % 2].ap[:, 7:8])
                    self.MM(po.ap[:, :], v.ap[:, kt, h, :], p_.ap[:, :], start=(kt == 0), stop=(kt == NT - 1), r=[v, p_], w=[po])
                    if kt == 2 and pend is not None:
                        tail(*pend)
                        pend = None
                    if kt == 9 and qb == 2 and h + 1 < 8:
                        build_stab(h + 1)
                pend = (h, qb)
        tail(*pend)
        kTh, qTh = kThs[0], qThs[0]
        S.release(kThs[1], qThs[1], sqb, *mxs)
        S.release(wuq, wuqs, wukv, zdqnT, ckvT, krT, v, kTh, qTh, rden, bcs, rt, *pT, *rp)

    def phase_gla(self):
        S, P = self.S, self.P
        hT, cstb, cst, stat = self.hT, self.cstb, self.cst, self.stat
        TB_ALL = [(0, 256), (256, 512), (768, 512), (1280, 512), (1792, 512)]
        TB_X = TB_ALL[1:]
        wA = S.alloc("wA", [KC, 1056], BF16)
        self.load_w(wA.ap[:, :, 0:1024], wA, self.w_in[:, 0:1024], 1024, 1024)
        self.load_w(wA.ap[:, :, 1024:1056], wA, self.w_in[:, 1536:1568], 1024, 32)
        wzg = S.alloc("wzg", [KC, 512], BF16)
        self.load_w(wzg.ap[:, :, :], wzg, self.w_in[:, 1024:1536], 1024, 512)
        wa2b = S.alloc("wa2b", [1024], BF16)
        self.load_w(wa2b.ap[:, 0:512], wa2b, self.wa2, 32, 512)
        self.load_w(wa2b.ap[:, 512:1024], wa2b, self.ba_row, 1, 512)
        gnb = S.alloc("gnb", [512], F32)
        self.DMA("sp", gnb.ap[:, :], self.gnb, w=[gnb])
        self.xsz = S.virt("xsz")
        zt = S.alloc("zt", [D], BF16)
        self.zt = zt
        self.MS("pool", zt.ap[:, :], 0.0, w=[zt])
        for c in range(NSLOT // 128):
            self.DMA("sp", self.xs_d[c * 128:(c + 1) * 128, :], zt.ap[:, :], r=[zt], w=[self.xsz], sem=zt)
        gqT = S.alloc("gqT", [2, T], BF16)
        gkT = S.alloc("gkT", [2, NTOK], BF16)
        gk = S.alloc("gk", [NT, 256], BF16)
        gv = S.alloc("gv", [NT, 512], BF16)
        zaT = S.alloc("zaT", [NTOK], BF16)
        n = 0
        for (c0, nn) in TB_ALL:
            for m in range(4):
                if m < 2 and c0 < TC:
                    continue
                pb = P[n % 2]
                n += 1
                for kc in range(KC):
                    self.MM(pb.ap[:, 0:nn], wA.ap[:, kc, m * 128:(m + 1) * 128], hT.ap[:, kc, c0:c0 + nn],
                            start=(kc == 0), stop=(kc == KC - 1), r=[wA, hT], w=[pb])
                if m < 2:
                    self.CP("act", gqT.ap[:, m, c0 - TC:c0 - TC + nn], pb.ap[:, 0:nn], r=[pb], w=[gqT])
                else:
                    self.CP("dve", gkT.ap[:, m - 2, c0:c0 + nn], pb.ap[:, 0:nn], r=[pb], w=[gkT])
            pb = P[n % 2]
            n += 1
            for kc in range(KC):
                self.MM(pb.ap[0:32, 0:nn], wA.ap[:, kc, 1024:1056], hT.ap[:, kc, c0:c0 + nn],
                        start=(kc == 0), stop=(kc == KC - 1), r=[wA, hT], w=[pb])
            self.CP("act", zaT.ap[0:32, c0:c0 + nn], pb.ap[0:32, 0:nn], r=[pb], w=[zaT])
        for i in range(NT):
            pa, pv = P[2 + i % 2], P[4 + i % 2]
            for kc in range(KC):
                self.MM(pa.ap[:, 0:256], hT.ap[:, kc, i * 128:(i + 1) * 128], wA.ap[:, kc, 256:512],
                        start=(kc == 0), stop=(kc == KC - 1), r=[wA, hT], w=[pa])
            for kc in range(KC):
                self.MM(pv.ap[:, :], hT.ap[:, kc, i * 128:(i + 1) * 128], wA.ap[:, kc, 512:1024],
                        start=(kc == 0), stop=(kc == KC - 1), r=[wA, hT], w=[pv])
            self.CP("act", gk.ap[:, i, :], pa.ap[:, 0:256], r=[pa], w=[gk])
            self.CP("dve", gv.ap[:, i, :], pv.ap[:, :], r=[pv], w=[gv])
        S.release(wA)
        if self.stage == 2:
            for m in range(2):
                self.dump(m * 2048, gqT, gqT.ap[:, m, :], 2048)
            self.dump(4096, gv, gv.ap[:, 5, :], 512)
            self.dump(4608, gk, gk.ap[:, 5, :], 256)
            self.dump(4864, zaT, zaT.ap[:, 0:2304], 2304)
            self.ogT = gqT
            return

        ogT = S.alloc("ogT", [4, T], BF16)
        self.ogT = ogT
        hist = S.alloc("hist", [32, 256], BF16)
        Sst = [S.alloc("Sst", [256], F32) for _ in range(2)]
        Sbr = [S.alloc("Sbr", [256], BF16) for _ in range(2)]
        R = 2
        la_b = [S.alloc("la", [256], F32) for _ in range(R)]
        lt_b = [S.alloc("lt", [256], F32) for _ in range(R)]
        ET_b = [S.alloc("ET", [256], F32) for _ in range(R)]
        EI_b = [S.alloc("EI", [256], F32) for _ in range(R)]
        KS_b = [S.alloc("KS", [256], F32) for _ in range(R)]
        qd_b = [S.alloc("qd", [2, 2, 128], BF16) for _ in range(2)]
        qt_b = [S.alloc("qt", [2, 128], F32) for _ in range(2)]
        ki_b = [S.alloc("ki", [2, 128], BF16) for _ in range(R)]
        kte_b = [S.alloc("kte", [256], BF16) for _ in range(R)]
        att_b = [S.alloc("att", [512], BF16) for _ in range(R)]
        ot_b = S.alloc("ot", [512], F32)
        sq_b = S.alloc("sq", [512], F32)
        sz_b = S.alloc("sz", [512], F32)
        og_b = S.alloc("og", [512], BF16)
        self.cnt = 0
        tri, maskb, onesb, identb = self.tri, self.maskb, self.onesb, self.identb
        for d in range(2):
            self.MS("pool", Sst[d].ap[:, :], 0.0, w=[Sst[d]])
        self.MS("pool", Sbr[0].ap[:, :], 0.0, w=[Sbr[0]])

        def prep_gen(i, d, want_q, c, out):
            r = c % R
            pl, pc = P[0 + c % 2], P[2 + c % 2]
            la, lt, ET, EI, KS = la_b[r], lt_b[r], ET_b[r], EI_b[r], KS_b[r]
            self.MM(pl.ap[:, 0:256], zaT.ap[0:32, i * 128:(i + 1) * 128], wa2b.ap[0:32, d * 256:(d + 1) * 256],
                    start=True, stop=False, r=[zaT, wa2b], w=[pl])
            self.MM(pl.ap[:, 0:256], onesb[0:1, :], wa2b.ap[0:1, 512 + d * 256:512 + (d + 1) * 256],
                    start=False, stop=True, r=[cstb, wa2b], w=[pl])
            yield
            self.ACT(lt.ap[:, :], pl.ap[:, 0:256], AF.Abs, r=[pl], w=[lt])
            self.TS("dve", la.ap[:, :], pl.ap[:, 0:256], 0.0, 1.0 / 16, ALU.min, ALU.mult, r=[pl], w=[la])
            yield
            self.ACT(lt.ap[:, :], lt.ap[:, :], AF.Exp, r=[lt], w=[lt], scale=-1.0)
            yield
            self.ACT(lt.ap[:, :], lt.ap[:, :], AF.Ln, r=[lt], w=[lt], bias=self.onesf[:, 0:1])
            yield
            self.STT(la.ap[:, :], lt.ap[:, :], -1.0 / 16, la.ap[:, :], ALU.mult, ALU.add, r=[lt, la], w=[la])
            yield
            for j in range(2):
                self.MM(pc.ap[:, j * 128:(j + 1) * 128], la.ap[:, j * 128:(j + 1) * 128], tri[:, d, :], r=[la, cst], w=[pc])
            self.MM(pc.ap[:, 256:512], tri[:, 2 + d, :], la.ap[:, :], r=[la, cst], w=[pc])
            yield
            self.ACT(ET.ap[:, :], pc.ap[:, 0:256], AF.Exp, r=[pc], w=[ET])
            self.ACT(KS.ap[:, :], pc.ap[:, 256:512], AF.Exp, r=[pc], w=[KS])
            kte = kte_b[r]
            out.update({"pl": pl, "pc": pc, "ET": ET, "kte": kte})
            if want_q:
                self.ACT(EI.ap[:, :], pc.ap[:, 0:256], AF.Exp, r=[pc], w=[EI], scale=-1.0)
            yield
            self.TT("dve", kte.ap[:, :], gk.ap[:, i, :], KS.ap[:, :], ALU.mult, r=[gk, KS], w=[kte])
            if want_q:
                xc = (i - 2) * 128
                qd = qd_b[c % 2]
                ki = ki_b[r]
                qt = qt_b[c % 2]
                self.STT(qt.ap[:, :, :], gqT.ap[:, :, xc:xc + 128], 0.125, ET.ap[:, :].rearrange("p (a b) -> p a b", b=128),
                         ALU.mult, ALU.mult, r=[gqT, ET], w=[qt])
                yield
                self.TS("dve", qd.ap[:, 0, :, :], qt.ap[:, :, :], self.rowmask[:, 0:1], None, ALU.mult, r=[qt, cst], w=[qd])
                self.ACT(qd.ap[:, 1, :, :], qt.ap[:, :, :], AF.Copy, r=[qt, cst], w=[qd], scale=self.rowmask[:, 1:2])
                self.TT("pool", ki.ap[:, :, :], gkT.ap[:, :, i * 128:(i + 1) * 128],
                        EI.ap[:, :].rearrange("p (a b) -> p a b", b=128), ALU.mult, r=[gkT, EI], w=[ki])
                out["qd"], out["ki"] = qd, ki

        def prep_multi(specs):
            outs = [dict() for _ in specs]
            gens = []
            for (i, d, wq), o in zip(specs, outs):
                gens.append(prep_gen(i, d, wq, self.cnt, o))
                self.cnt += 1
            live_g = list(gens)
            while live_g:
                for g in list(live_g):
                    try:
                        next(g)
                    except StopIteration:
                        live_g.remove(g)
            return outs

        def prep(i, d, want_q):
            return prep_multi([(i, d, want_q)])[0]

        def ds_update(i, d, b, cs):
            pl, ET, kte = b["pl"], b["ET"], b["kte"]
            for h in range(4):
                self.MM(pl.ap[(h % 2) * 64:(h % 2) * 64 + 64, 256 + (h // 2) * 128:256 + (h // 2) * 128 + 128],
                        kte.ap[cs:cs + 64, h * 64:(h + 1) * 64], gv.ap[cs:cs + 64, i, h * 128:(h + 1) * 128],
                        r=[kte, gv], w=[pl])
            tl = cs + 63 if d == 0 else cs
            for j in range(2):
                self.STT(Sst[d].ap[:, j * 128:(j + 1) * 128], Sst[d].ap[:, j * 128:(j + 1) * 128],
                         ET.ap[:, j * 128 + tl:j * 128 + tl + 1], pl.ap[:, 256 + j * 128:256 + (j + 1) * 128],
                         ALU.mult, ALU.add, r=[Sst[d], ET, pl], w=[Sst[d]])

        order_b = [1, 0] + list(range(17, 1, -1))
        for k in range(0, len(order_b), 2):
            pair = order_b[k:k + 2]
            bs = prep_multi([(i, 1, False) for i in pair])
            for i, b in zip(pair, bs):
                for cs in (64, 0):
                    if i >= 2:
                        ch = (i - 2) * 2 + (1 if cs == 64 else 0)
                        self.CP("act", hist.ap[:, ch, :], Sst[1].ap[:, :], r=[Sst[1]], w=[hist])
                    ds_update(i, 1, b, cs)

        if self.stage == 25:
            S.release(ot_b, sq_b, sz_b)
            self.dump(0, hist, hist.ap[:, 31, :], 256)
            self.dump(256, hist, hist.ap[:, 0, :], 256)
            return
        live = 0
        ctxp = prep_multi([(0, 0, False), (1, 0, False)])
        for i in range(NT):
            if i < 2:
                bf = ctxp[i]
                for cs in (0, 64):
                    ds_update(i, 0, bf, cs)
                if i == 1:
                    self.CP("act", Sbr[0].ap[:, :], Sst[0].ap[:, :], r=[Sst[0]], w=[Sbr[0]])
                continue
            bf, bb = prep_multi([(i, 0, True), (i, 1, True)])
            xi = i - 2
            xc = xi * 128
            pos = (P[6], P[7])
            pss = (P[4], P[5])
            first = [True, True]
            acol = lambda h: ((h % 2) * 2 + h // 2) * 128
            for d, b in ((0, bf), (1, bb)):
                att = att_b[d]
                qd, ki = b["qd"], b["ki"]
                for h in range(4):
                    hp = (h % 2) * 64
                    self.MM(pss[h % 2].ap[:, (h // 2) * 128:(h // 2) * 128 + 128], ki.ap[:, h // 2, :],
                            qd.ap[:, h % 2, h // 2, :], r=[ki, qd], w=[pss[h % 2]])
                for p in range(2):
                    self.TT("dve", att.ap[:, p * 256:(p + 1) * 256], pss[p].ap[:, 0:256], maskb[:, d, 0:256], ALU.mult,
                            r=[pss[p], cstb], w=[att])
                for h in range(4):
                    self.MM(pos[h % 2].ap[:, (h // 2) * 128:(h // 2) * 128 + 128], att.ap[:, acol(h):acol(h) + 128],
                            gv.ap[:, i, h * 128:(h + 1) * 128], start=first[h % 2], stop=False, r=[att, gv], w=[pos[h % 2]])
                    first[h % 2] = False
            qd = bb["qd"]
            for cs in (64, 0):
                ch = xi * 2 + (1 if cs == 64 else 0)
                for h in range(4):
                    hp = (h % 2) * 64
                    self.MM(pos[h % 2].ap[cs:cs + 64, (h // 2) * 128:(h // 2) * 128 + 128], qd.ap[:, h % 2, h // 2, cs:cs + 64],
                            hist.ap[:, ch, (h // 2) * 128:(h // 2) * 128 + 128], start=False, stop=False,
                            r=[qd, hist], w=[pos[h % 2]])
            qd = bf["qd"]
            for cs in (0, 64):
                sb = Sbr[live % 2]
                for h in range(4):
                    hp = (h % 2) * 64
                    self.MM(pos[h % 2].ap[cs:cs + 64, (h // 2) * 128:(h // 2) * 128 + 128], qd.ap[:, h % 2, h // 2, cs:cs + 64],
                            sb.ap[:, (h // 2) * 128:(h // 2) * 128 + 128], start=False, stop=(cs == 64 and h >= 2),
                            r=[qd, sb], w=[pos[h % 2]])
                ds_update(i, 0, bf, cs)
                live += 1
                self.CP("act", Sbr[live % 2].ap[:, :], Sst[0].ap[:, :], r=[Sst[0]], w=[Sbr[live % 2]])
            pz = bf["pc"]
            for kc in range(KC):
                self.MM(pz.ap[:, :], hT.ap[:, kc, i * 128:(i + 1) * 128], wzg.ap[:, kc, :], start=(kc == 0), stop=(kc == KC - 1),
                        r=[hT, wzg], w=[pz])
            self.ACT(sz_b.ap[:, :], pz.ap[:, :], AF.Silu, r=[pz], w=[sz_b])
            for h in range(4):
                self.CP("act" if h % 2 == 0 else "dve", ot_b.ap[:, h * 128:(h + 1) * 128],
                        pos[h % 2].ap[:, (h // 2) * 128:(h // 2) * 128 + 128], r=[pos[h % 2]], w=[ot_b])
            self.TT("dve", sq_b.ap[:, :], ot_b.ap[:, :], ot_b.ap[:, :], ALU.mult, r=[ot_b], w=[sq_b])
            st4 = stat.ap[:, 16:20]
            st4b = stat.ap[:, 20:24]
            self.S.op("dve", lambda e, o=st4, a=sq_b.ap[:, :].rearrange("p (a b) -> p a b", b=128): e.reduce_sum(o, a, axis=AX.X),
                      [sq_b], [stat])
            self.ACT(st4b, st4, AF.Ln, r=[stat], w=[stat], scale=1.0 / 128, bias=self.eps_ap)
            self.ACT(st4, st4b, AF.Exp, r=[stat], w=[stat], scale=-0.5)
            self.TT("dve", sq_b.ap[:, :].rearrange("p (a b) -> p a b", b=128), ot_b.ap[:, :].rearrange("p (a b) -> p a b", b=128),
                    st4.unsqueeze(2).to_broadcast([128, 4, 128]), ALU.mult, r=[ot_b, stat], w=[sq_b])
            self.TT("dve", sq_b.ap[:, :], sq_b.ap[:, :], gnb.ap[:, :], ALU.mult, r=[sq_b, gnb], w=[sq_b])
            self.TT("dve", og_b.ap[:, :], sq_b.ap[:, :], sz_b.ap[:, :], ALU.mult, r=[sq_b, sz_b], w=[og_b])
            pt = P[4]
            ptb = pt.ap.bitcast(BF16)
            for h in range(4):
                self.TR(ptb[:, h * 128:(h + 1) * 128], og_b.ap[:, h * 128:(h + 1) * 128], identb, r=[og_b, cstb], w=[pt])
            self.CP("act", ogT.ap[:, :, xc:xc + 128], ptb[:, 0:512].rearrange("p (a b) -> p a b", b=128), r=[pt], w=[ogT])
        S.release(gqT, gkT, gk, gv, zaT, hist, wzg, wa2b, gnb, ot_b, sq_b, sz_b, og_b,
                  *Sst, *Sbr, *la_b, *lt_b, *ET_b, *EI_b, *KS_b, *qd_b, *qt_b, *ki_b, *kte_b, *att_b)

    def phase_merge(self):
        S, P = self.S, self.P
        hT, ogT, omlaT, stat, cst, cstb = self.hT, self.ogT, self.omlaT, self.stat, self.cst, self.cstb
        mT = S.alloc("mT", [KC, T], BF16)
        wbg = S.alloc("wbg", [4, D], BF16)
        self.load_w(wbg.ap[:, :, :], wbg, self.w_br_gla, 512, D)
        wbm = S.alloc("wbm", [4, D], BF16)
        src = self.w_br_mla.rearrange("(k p) n -> p k n", p=64)
        self.DMA("pool", wbm.ap[0:64, :, :], src[:, 0:4, :], w=[wbm])
        self.DMA("pool", wbm.ap[64:128, :, :], src[:, 4:8, :], w=[wbm])
        wz = [S.alloc("wz", [KC, 256], BF16) for _ in range(2)]
        sg = [S.alloc("sg", [512], F32) for _ in range(2)]
        sm = [S.alloc("sm", [512], F32) for _ in range(2)]
        n = 0
        for m in range(KC):
            w_ = wz[m % 2]
            self.load_w(w_.ap[:, :, 0:128], w_, self.w_in[:, 1984 + m * 128:1984 + (m + 1) * 128], 1024, 128)
            self.load_w(w_.ap[:, :, 128:256], w_, self.w_in[:, 3008 + m * 128:3008 + (m + 1) * 128], 1024, 128)
            for tb in range(4):
                c0, xc0 = TC + tb * 512, tb * 512
                o = (n % 2) * 4
                sg_, sm_ = sg[n % 2], sm[n % 2]
                n += 1
                for kc in range(KC):
                    self.MM(P[o].ap[:, :], w_.ap[:, kc, 0:128], hT.ap[:, kc, c0:c0 + 512], start=(kc == 0), stop=(kc == KC - 1),
                            r=[w_, hT], w=[P[o]])
                for kc in range(KC):
                    self.MM(P[o + 1].ap[:, :], w_.ap[:, kc, 128:256], hT.ap[:, kc, c0:c0 + 512], start=(kc == 0), stop=(kc == KC - 1),
                            r=[w_, hT], w=[P[o + 1]])
                for hc in range(4):
                    self.MM(P[o + 2].ap[:, :], wbg.ap[:, hc, m * 128:(m + 1) * 128], ogT.ap[:, hc, xc0:xc0 + 512],
                            start=(hc == 0), stop=(hc == 3), r=[wbg, ogT], w=[P[o + 2]])
                for j in range(4):
                    self.MM(P[o + 3].ap[:, :], wbm.ap[:, j, m * 128:(m + 1) * 128], omlaT.ap[:, j, xc0:xc0 + 512],
                            start=(j == 0), stop=(j == 3), r=[wbm, omlaT], w=[P[o + 3]])
                self.ACT(sg_.ap[:, :], P[o].ap[:, :], AF.Sigmoid, r=[P[o]], w=[sg_])
                self.ACT(sm_.ap[:, :], P[o + 1].ap[:, :], AF.Sigmoid, r=[P[o + 1]], w=[sm_])
                self.TT("dve", sg_.ap[:, :], sg_.ap[:, :], P[o + 2].ap[:, :], ALU.mult, r=[sg_, P[o + 2]], w=[sg_])
                self.TT("dve", sm_.ap[:, :], sm_.ap[:, :], P[o + 3].ap[:, :], ALU.mult, r=[sm_, P[o + 3]], w=[sm_])
                self.TT("pool", mT.ap[:, m, xc0:xc0 + 512], sg_.ap[:, :], sm_.ap[:, :], ALU.add, r=[sg_, sm_], w=[mT])
        S.release(hT, ogT, omlaT, wbg, wbm, *wz, *sg, *sm)
        wo = S.alloc("wo", [KC, D], BF16)
        self.load_w(wo.ap[:, :, :], wo, self.w_out, D, D)
        rw = S.alloc("rw", [KC * NE + NE], F32)
        rwv = rw.ap[:, 0:KC * NE].rearrange("p (a b) -> p a b", b=NE)
        self.DMA("sp", rwv, self.router_w.rearrange("(k p) n -> p k n", p=128), w=[rw])
        self.DMA("sp", rw.ap[0:1, KC * NE:KC * NE + NE], self.router_b, w=[rw])
        comb = S.alloc("comb", [16, NE], F32)
        self.comb = comb
        rt_ = S.alloc("rt", [176 + 144], F32)
        self.DMA("sp", rt_.ap[:, 0:176], self.c_rt, w=[rt_])
        tsub = S.alloc("tsub", [128], BF16)
        self.CP("pool", tsub.ap[:, :], rt_.ap[:, 0:128], r=[rt_], w=[tsub])
        Umat = rt_.ap[:, 128:160]
        IO = rt_.ap[:, 160:176]
        cb = rt_.ap[:, 176:208]
        posf4 = rt_.ap[:, 208:212]
        oh = rt_.ap[:, 224:256]
        oh2 = rt_.ap[:, 256:288]
        posf = rt_.ap[:, 288:320]
        self.MS("pool", rt_.ap[:, 176:320], 0.0, w=[rt_])
        wk = S.alloc("wk", [16, 4], F32)
        posi = S.alloc("posi", [16, 4], F32)
        posi_v = posi.ap.bitcast(I32)
        self.wk, self.posi, self.posi_v = wk, posi, posi_v
        maskb16 = S.alloc("maskb16", [NE], BF16)
        rank_all = S.alloc("rank", [16, NE], F32)
        lgall = S.alloc("lgall", [16, 40], F32)
        xn2tm = S.alloc("xn2tm", [16, D], BF16)
        xsz, xsd = self.xsz, S.virt("xsd")
        self.xsd, self.ysd = xsd, S.virt("ysd")
        xt = [S.alloc("xt2", [D], F32) for _ in range(2)]
        tt = [S.alloc("tt2", [D], F32) for _ in range(2)]
        hx2fs = [S.alloc("hx2f", [KC, 128], F32) for _ in range(2)]
        lgs = [S.alloc("lg", [128], F32) for _ in range(2)]
        junk = S.alloc("junk3", [D], BF16)
        Gb, A2, modb, identf, onesf = self.Gb, self.A2, self.modb, self.identf, self.onesf
        for xi in range(16):
            pa, pb = P[(xi % 2) * 2], P[(xi % 2) * 2 + 1]
            x_, t_ = xt[xi % 2], tt[xi % 2]
            hx2f, lg = hx2fs[xi % 2], lgs[xi % 2]
            self.DMA("sp", x_.ap[:, :], self.x[xi * 128:(xi + 1) * 128, :], w=[x_])
            for hf, pp in enumerate((pa, pb)):
                for m in range(KC):
                    self.MM(pp.ap[:, :], mT.ap[:, m, xi * 128:(xi + 1) * 128], wo.ap[:, m, hf * 512:(hf + 1) * 512],
                            start=(m == 0), stop=(m == KC - 1), r=[mT, wo], w=[pp])
            sc = 24 + (xi % 2) * 8
            for hf, pp in enumerate((pa, pb)):
                self.ACT(junk.ap[:, 0:512], pp.ap[:, :], AF.Square, r=[pp], w=[junk, stat], accum_out=stat.ap[:, sc + hf:sc + hf + 1])
            self.TT("dve", stat.ap[:, sc:sc + 1], stat.ap[:, sc:sc + 1], stat.ap[:, sc + 1:sc + 2], ALU.add, r=[stat], w=[stat])
            self.rstd_from_ss(stat.ap[:, sc:sc + 1], D, stat, stat.ap[:, sc + 2:sc + 3])
            for hf, pp in enumerate((pa, pb)):
                self.STT(t_.ap[:, hf * 512:(hf + 1) * 512], pp.ap[:, :], stat.ap[:, sc:sc + 1], Gb.ap[:, 0, hf * 512:(hf + 1) * 512],
                         ALU.mult, ALU.mult, r=[pp, stat, Gb], w=[t_])
            self.TT("pool", x_.ap[:, :], t_.ap[:, :], x_.ap[:, :], ALU.add, r=[t_, x_], w=[x_])
            self.DMA("pool", self.x1d[xi * 128:(xi + 1) * 128, :], x_.ap[:, :], r=[x_], sem=x_)
            self.ACT(junk.ap[:, :], x_.ap[:, :], AF.Square, r=[x_], w=[junk, stat], accum_out=stat.ap[:, sc + 3:sc + 4])
            self.rstd_from_ss(stat.ap[:, sc + 3:sc + 4], D, stat, stat.ap[:, sc + 4:sc + 5])
            self.TS("dve", t_.ap[:, :], x_.ap[:, :], stat.ap[:, sc + 3:sc + 4], None, ALU.mult, r=[x_, stat], w=[t_])
            for kc in range(KC):
                pt = P[4 + kc // 4]
                self.TR(pt.ap[:, (kc % 4) * 128:(kc % 4 + 1) * 128], t_.ap[:, kc * 128:(kc + 1) * 128], identf, r=[t_, cst], w=[pt])
            for kc in range(KC):
                pt = P[4 + kc // 4]
                src_ = pt.ap[:, (kc % 4) * 128:(kc % 4 + 1) * 128]
                if kc % 2 == 0:
                    self.TS("dve", hx2f.ap[:, kc, :], src_, A2[:, kc:kc + 1], modb.ap[:, 24 + kc, 0:1], ALU.mult, ALU.add,
                            r=[pt, self.vec, modb], w=[hx2f])
                else:
                    self.ACT(hx2f.ap[:, kc, :], src_, AF.Identity, r=[pt, self.vec, modb], w=[hx2f],
                             scale=A2[:, kc:kc + 1], bias=modb.ap[:, 24 + kc, 0:1])
            pr = P[6 + xi % 2]
            for kc in range(KC):
                self.MM(pr.ap[:, 0:NE], hx2f.ap[:, kc, :], rwv[:, kc, :], start=(kc == 0), stop=False, r=[hx2f, rw], w=[pr])
            self.MM(pr.ap[:, 0:NE], onesf[0:1, :], rw.ap[0:1, KC * NE:KC * NE + NE], start=False, stop=True, r=[cst, rw], w=[pr])
            self.CP("act", lg.ap[:, 0:NE], pr.ap[:, 0:NE], r=[pr], w=[lg])
            self.S.op("dve", lambda e, o=lg.ap[:, 32:40], a=lg.ap[:, 0:NE]: e.max(out=o, in_=a), [lg], [lg])
            self.TS("dve", lg.ap[:, 64:96], lg.ap[:, 0:NE], lg.ap[:, 35:36], None, ALU.is_ge, r=[lg], w=[lg])
            self.TS("dve", lg.ap[:, 40:41], lg.ap[:, 32:33], -1.0, None, ALU.mult, r=[lg], w=[lg])
            self.ACT(lg.ap[:, 96:128], lg.ap[:, 0:NE], AF.Exp, r=[lg], w=[lg], bias=lg.ap[:, 40:41])
            self.TT("dve", lg.ap[:, 96:128], lg.ap[:, 96:128], lg.ap[:, 64:96], ALU.mult, r=[lg], w=[lg])
            self.S.op("dve", lambda e, o=lg.ap[:, 41:42], a=lg.ap[:, 96:128]: e.reduce_sum(o, a, axis=AX.X), [lg], [lg])
            self.S.op("dve", lambda e, o=lg.ap[:, 42:43], a=lg.ap[:, 41:42]: e.reciprocal(o, a), [lg], [lg])
            self.TS("dve", comb.ap[:, xi, :], lg.ap[:, 96:128], lg.ap[:, 42:43], None, ALU.mult, r=[lg], w=[comb])
            self.CP("pool", maskb16.ap[:, :], lg.ap[:, 64:96], r=[lg], w=[maskb16])
            self.MM(pr.ap[:, 64:96], tsub.ap[:, :], maskb16.ap[:, :], r=[tsub, maskb16], w=[pr])
            self.MM(pr.ap[:, 128:160], self.onesb, maskb16.ap[:, :], r=[self.cstb, maskb16], w=[pr])
            self.TT("dve", rank_all.ap[:, xi, :], pr.ap[:, 64:96], cb, ALU.add, r=[pr, rt_], w=[rank_all])
            self.TT("dve", cb, cb, pr.ap[:, 128:160], ALU.add, r=[pr, rt_], w=[rt_])
            self.CP("dve", lgall.ap[:, xi, :], lg.ap[:, 0:40], r=[lg], w=[lgall])
            self.CP("pool", xn2tm.ap[:, xi, :], t_.ap[:, :], r=[t_], w=[xn2tm])
        meta = S.alloc("meta", [32 + 32 + 128 + 512 + 512], F32)
        self.meta = meta
        nt = meta.ap[:, 0:32]
        nt_i = meta.ap[:, 32:64].bitcast(I32)
        ntT = meta.ap[:, 64:192]
        sidxf = meta.ap[:, 192:704].rearrange("p (a b) -> p a b", b=16)
        sidx_i = meta.ap[:, 704:1216].bitcast(I32).rearrange("p (a b) -> p a b", b=16)
        self.nt_i, self.sidx_i = nt_i, sidx_i
        self.MS("pool", nt, 0.0, w=[meta])
        for m in range(NSTEP):
            self.STT(nt, cb, float(SG * m), nt, ALU.is_gt, ALU.add, r=[rt_, meta], w=[meta])
        self.CP("dve", nt_i, nt, r=[meta], w=[meta])
        pq = P[4]
        self.TR(pq.ap[0:NE, 0:128], nt, identf, r=[meta, cst], w=[pq])
        self.CP("act", ntT[0:NE, :], pq.ap[0:NE, 0:128], r=[pq], w=[meta])
        self.MM(pq.ap[:, 128:160], ntT[0:NE, :], Umat[0:NE, :], r=[meta, rt_], w=[pq])
        base = rt_.ap[:, 176:208]
        self.TS("dve", base, pq.ap[:, 128:160], float(SG), None, ALU.mult, r=[pq, rt_], w=[rt_])
        for e in range(NE):
            self.TS("dve", sidxf[:, e, :], IO, base[:, e:e + 1], None, ALU.add, r=[rt_, meta], w=[meta])
        self.CP("dve", sidx_i, sidxf, r=[meta], w=[meta])
        S.release(mT, wo, rw, *hx2fs, *lgs, junk, self.zt, tsub, maskb16, *tt)
        oh4 = S.alloc("oh4", [16, 4, NE], F32)
        pr4 = S.alloc("pr4", [16, 4, NE], F32)
        pf4 = S.alloc("pf4", [16, 4], F32)
        shp = [128, 16, 4, NE]
        self.TT("dve", rank_all.ap[:, :, :], rank_all.ap[:, :, :], base.unsqueeze(1).to_broadcast([128, 16, NE]), ALU.add,
                r=[rank_all, rt_], w=[rank_all])
        self.TT("dve", oh4.ap[:, :, :, :], lgall.ap[:, :, 0:NE].unsqueeze(2).to_broadcast(shp),
                lgall.ap[:, :, 32:36].unsqueeze(3).to_broadcast(shp), ALU.is_equal, r=[lgall], w=[oh4])
        self.TT("dve", pr4.ap[:, :, :, :], oh4.ap[:, :, :, :], rank_all.ap[:, :, :].unsqueeze(2).to_broadcast(shp), ALU.mult,
                r=[oh4, rank_all], w=[pr4])
        self.S.op("dve", lambda e, o=pf4.ap[:, :, :], a=pr4.ap[:, :, :, :]: e.reduce_sum(o, a, axis=AX.X), [pr4], [pf4])
        self.CP("dve", posi_v[:, :, :], pf4.ap[:, :, :], r=[pf4], w=[posi])
        self.TT("pool", pr4.ap[:, :, :, :], oh4.ap[:, :, :, :], comb.ap[:, :, :].unsqueeze(2).to_broadcast(shp), ALU.mult,
                r=[oh4, comb, pf4], w=[pr4])
        self.S.op("dve", lambda e, o=wk.ap[:, :, :], a=pr4.ap[:, :, :, :]: e.reduce_sum(o, a, axis=AX.X), [pr4], [wk])
        for xi in range(16):
            for k in range(4):
                self.S.dma_fn("pool", lambda e, o=self.xs_d[:, :], off=posi_v[:, xi, k:k + 1], i_=xn2tm.ap[:, xi, :]:
                              e.indirect_dma_start(out=o, out_offset=bass.IndirectOffsetOnAxis(ap=off, axis=0), in_=i_, in_offset=None,
                                                   bounds_check=self.S.bc_reg, oob_is_err=False),
                              reads=[xsz, xn2tm, posi], writes=[xsd], sem_buf=xn2tm)
        S.release(oh4, pr4, pf4, rank_all, lgall)
        self.x1_bufs = xt
        self.rt_ = rt_
        self.xn2tm = xn2tm

    def phase_moe(self):
        S, P = self.S, self.P
        hx2T, comb, stat, cst = self.hx2T, self.comb, self.stat, self.cst
        identf, Gb = self.identf, self.Gb
        xt = self.x1_bufs
        combT = S.alloc("combT", [T], F32)
        for xi in range(16):
            pb = P[xi % 2]
            self.TR(pb.ap[0:NE, 0:128], comb.ap[:, xi, :], identf, r=[comb, cst], w=[pb])
            self.CP("act", combT.ap[0:NE, xi * 128:(xi + 1) * 128], pb.ap[0:NE, 0:128], r=[pb], w=[combT])
        S.release(comb)
        bias = S.alloc("ebias", [512 + D], F32)
        self.DMA("sp", bias.ap[:, 0:256], self.bgT, w=[bias])
        self.DMA("sp", bias.ap[:, 256:512], self.buT, w=[bias])
        self.DMA("sp", bias.ap[0:NE, 512:512 + D], self.b_down, w=[bias])
        self.TS("dve", bias.ap[:, 256:512], bias.ap[:, 256:512], 1.0, None, ALU.add, r=[bias], w=[bias])
        wg = S.alloc("wg", [KC, D], BF16)
        wu = S.alloc("wu", [KC, D], BF16)
        wd = S.alloc("wd", [KC, D], BF16)
        act = S.alloc("act", [KC, 512], BF16)
        oacc = S.alloc("oacc", [KC, 1024], F32)
        sel = [S.alloc("sel", [128], F32) for _ in range(2)]
        cwb = S.alloc("cw", [512], F32)
        gb_ = [S.alloc("g", [512], F32) for _ in range(2)]
        sb_ = [S.alloc("s", [512], F32) for _ in range(1)]
        ub_ = [S.alloc("u", [512], F32) for _ in range(2)]
        junk = S.alloc("junk4", [512], BF16)
        selv = self.c_sel
        out_bufs = []
        for half in range(2):
            t0 = half * 1024
            for dc in range(KC):
                for tb in range(2):
                    pb = P[4 + (dc * 2 + tb) % 2]
                    self.MM(pb.ap[:, :], bias.ap[0:NE, 512 + dc * 128:512 + (dc + 1) * 128], combT.ap[0:NE, t0 + tb * 512:t0 + (tb + 1) * 512],
                            r=[bias, combT], w=[pb])
                    self.CP("act", oacc.ap[:, dc, tb * 512:(tb + 1) * 512], pb.ap[:, :], r=[pb], w=[oacc])
            for e in range(NE):
                self.load_w(wg.ap[:, :, :], wg, self.w_gate[e], D, D)
                self.load_w(wu.ap[:, :, :], wu, self.w_up[e], D, D)
                self.load_w(wd.ap[:, :, :], wd, self.w_down[e], D, D)
                se = sel[e % 2]
                self.DMA("sp", se.ap[0:NE, :], selv[:, e * 128:(e + 1) * 128], w=[se])
                for tb in range(2):
                    tk = t0 + tb * 512
                    self.MM(P[6].ap[:, :], se.ap[0:NE, :], combT.ap[0:NE, tk:tk + 512], r=[se, combT], w=[P[6]])
                    self.CP("act", cwb.ap[:, :], P[6].ap[:, :], r=[P[6]], w=[cwb])
                    for f in range(KC):
                        pg, pu = P[f % 2], P[2 + f % 2]
                        g_, s_, u_ = gb_[f % 2], sb_[0], ub_[f % 2]
                        for kc in range(KC):
                            self.MM(pg.ap[:, :], wg.ap[:, kc, f * 128:(f + 1) * 128], hx2T.ap[:, kc, tk:tk + 512],
                                    start=(kc == 0), stop=(kc == KC - 1), r=[wg, hx2T], w=[pg])
                        for kc in range(KC):
                            self.MM(pu.ap[:, :], wu.ap[:, kc, f * 128:(f + 1) * 128], hx2T.ap[:, kc, tk:tk + 512],
                                    start=(kc == 0), stop=(kc == KC - 1), r=[wu, hx2T], w=[pu])
                        bcol = e * 8 + f
                        self.TS("dve", g_.ap[:, :], pg.ap[:, :], bias.ap[:, bcol:bcol + 1], 7.0, ALU.add, ALU.min, r=[pg, bias], w=[g_])
                        self.ACT(s_.ap[:, :], g_.ap[:, :], AF.Sigmoid, r=[g_], w=[s_], scale=1.702)
                        self.TS("dve", u_.ap[:, :], pu.ap[:, :], bias.ap[:, 256 + bcol:256 + bcol + 1], -6.0, ALU.add, ALU.max,
                                r=[pu, bias], w=[u_])
                        self.STT(u_.ap[:, :], u_.ap[:, :], 8.0, cwb.ap[:, :], ALU.min, ALU.mult, r=[u_, cwb], w=[u_])
                        self.TT("pool", g_.ap[:, :], g_.ap[:, :], s_.ap[:, :], ALU.mult, r=[g_, s_], w=[g_])
                        self.TT("pool", act.ap[:, f, :], g_.ap[:, :], u_.ap[:, :], ALU.mult, r=[g_, u_], w=[act])
                    for dc in range(KC):
                        pd = P[4 + dc % 2]
                        for f in range(KC):
                            self.MM(pd.ap[:, :], wd.ap[:, f, dc * 128:(dc + 1) * 128], act.ap[:, f, :],
                                    start=(f == 0), stop=(f == KC - 1), r=[wd, act], w=[pd])
                        self.TT("dve", oacc.ap[:, dc, tb * 512:(tb + 1) * 512], oacc.ap[:, dc, tb * 512:(tb + 1) * 512], pd.ap[:, :],
                                ALU.add, r=[oacc, pd], w=[oacc])
            for j in range(8):
                xi = half * 8 + j
                x_ = xt[xi % 2]
                self.DMA("sp", x_.ap[:, :], self.x1d[xi * 128:(xi + 1) * 128, :], w=[x_])
                pa, pb = P[(j % 2) * 2], P[(j % 2) * 2 + 1]
                for dc in range(KC):
                    pp = pa if dc < 4 else pb
                    self.TR(pp.ap[:, (dc % 4) * 128:(dc % 4 + 1) * 128], oacc.ap[:, dc, j * 128:(j + 1) * 128], identf,
                            r=[oacc, cst], w=[pp])
                sc = 40 + (j % 2) * 4
                for hf, pp in enumerate((pa, pb)):
                    self.ACT(junk.ap[:, :], pp.ap[:, :], AF.Square, r=[pp], w=[junk, stat], accum_out=stat.ap[:, sc + hf:sc + hf + 1])
                self.TT("dve", stat.ap[:, sc:sc + 1], stat.ap[:, sc:sc + 1], stat.ap[:, sc + 1:sc + 2], ALU.add, r=[stat], w=[stat])
                self.rstd_from_ss(stat.ap[:, sc:sc + 1], D, stat, stat.ap[:, sc + 2:sc + 3])
                for hf, pp in enumerate((pa, pb)):
                    self.STT(cwb.ap[:, :], pp.ap[:, :], stat.ap[:, sc:sc + 1], Gb.ap[:, 1, hf * 512:(hf + 1) * 512], ALU.mult, ALU.mult,
                             r=[pp, stat, Gb], w=[cwb])
                    self.TT("pool", x_.ap[:, hf * 512:(hf + 1) * 512], x_.ap[:, hf * 512:(hf + 1) * 512], cwb.ap[:, :], ALU.add,
                            r=[x_, cwb], w=[x_])
                self.DMA("pool", self.out[xi * 128:(xi + 1) * 128, :], x_.ap[:, :], r=[x_], sem=x_)
        self.final_bufs = list(xt)

    def phase_moe_sparse(self):
        S, P = self.S, self.P
        stat, cst, cstb, Gb = self.stat, self.cst, self.cstb, self.Gb
        identb, onesf, A2, modb = self.identb, self.onesf, self.A2, self.modb
        xsd, ysd, wk, posi_v, posi = self.xsd, self.ysd, self.wk, self.posi_v, self.posi
        meta, nt_i, sidx_i = self.meta, self.nt_i, self.sidx_i
        S.release(self.comb, self.xn2tm)
        bias = S.alloc("ebias", [512], F32)
        self.DMA("sp", bias.ap[:, 0:256], self.bgT, w=[bias])
        self.DMA("sp", bias.ap[:, 256:512], self.buT, w=[bias])
        self.TS("dve", bias.ap[:, 256:512], bias.ap[:, 256:512], 1.0, None, ALU.add, r=[bias], w=[bias])
        W = [[S.alloc("w%d" % k, [KC, D], BF16) for k in range(3)] for _ in range(2)]
        xg0 = [S.alloc("xg0", [2, D], BF16) for _ in range(2)]
        xgn = [S.alloc("xgn", [2, D], BF16) for _ in range(2)]
        xg = xg0 + xgn
        XT = [S.alloc("XT", [KC, SG], BF16) for _ in range(2)]
        acts = [S.alloc("act", [SG], BF16) for _ in range(KC)]
        gb_ = [S.alloc("g", [SG], F32) for _ in range(2)]
        sb_ = [S.alloc("s", [SG], F32) for _ in range(2)]
        ub_ = [S.alloc("u", [SG], F32) for _ in range(2)]
        ys = [S.alloc("ys", [D], F32) for _ in range(2)]
        bdr = [S.alloc("bdr", [D], F32) for _ in range(1)]
        bdrb = [S.alloc("bdrb", [D], BF16) for _ in range(2)]
        srcs = (self.w_gate, self.w_up, self.w_down)

        def prep(x_, xt_):
            for bk in range(2):
                pt = P[4 + bk]
                ptb = pt.ap.bitcast(BF16)
                for k4 in range(4):
                    kc = bk * 4 + k4
                    for h in range(2):
                        self.TR(ptb[:, k4 * SG + h * 128:k4 * SG + (h + 1) * 128], x_.ap[:, h, kc * 128:(kc + 1) * 128], identb,
                                r=[x_, cstb], w=[pt])
                for k4 in range(4):
                    kc = bk * 4 + k4
                    if k4 % 2 == 0:
                        self.TS("dve", xt_.ap[:, kc, :], ptb[:, k4 * SG:(k4 + 1) * SG], A2[:, kc:kc + 1], modb.ap[:, 24 + kc, 0:1],
                                ALU.mult, ALU.add, r=[pt, self.vec, modb], w=[xt_])
                    else:
                        self.ACT(xt_.ap[:, kc, :], ptb[:, k4 * SG:(k4 + 1) * SG], AF.Identity, r=[pt, self.vec, modb], w=[xt_],
                                 scale=A2[:, kc:kc + 1], bias=modb.ap[:, 24 + kc, 0:1])

        def loadw(e):
            for k in range(3):
                self.load_w(W[e % 2][k].ap[:, :, :], W[e % 2][k], srcs[k][e], D, D)
        def gather(buf, e, j):
            for h in range(2):
                self.S.dma_fn("pool", lambda en, o=buf.ap[:, h, :], off=sidx_i[:, e, 2 * j + h:2 * j + h + 1], i_=self.xs_d[:, :]:
                              en.indirect_dma_start(out=o, out_offset=None, in_=i_, in_offset=bass.IndirectOffsetOnAxis(ap=off, axis=0),
                                                    bounds_check=self.S.bc_reg, oob_is_err=False),
                              reads=[xsd, meta], writes=[buf], sem_buf=buf)
        XT0 = [S.alloc("XT0", [KC, SG], BF16) for _ in range(2)]
        gather(xg0[0], 0, 0)
        gather(xg0[1], 1, 0)
        loadw(0)
        prep(xg0[0], XT0[0])
        nst = 0
        ngs = 0
        for e in range(NE):
            gather(xgn[1], e, 1)
            if e + 2 < NE:
                gather(xg0[e % 2], e + 2, 0)
            if e + 1 < NE:
                loadw(e + 1)
            wg, wu, wd = W[e % 2]
            br = bdr[0]
            self.DMA("sp", br.ap[0:1, :], self.b_down[e:e + 1, :], w=[br])
            brb = bdrb[e % 2]
            self.CP("act", brb.ap[0:1, :], br.ap[0:1, :], r=[br], w=[brb])
            if e + 1 < NE:
                prep(xg0[(e + 1) % 2], XT0[(e + 1) % 2])
            S.regload(meta, nt_i[0:1, e:e + 1])
            for j in range(NSTEP):
                S.begin_group(j)
                xt_ = XT0[e % 2] if j == 0 else XT[j % 2]
                ngs += 1
                if j + 2 < NSTEP:
                    gather(xgn[j % 2], e, j + 2)
                for f in range(KC):
                    pg, pu = P[f % 2], P[2 + f % 2]
                    g_, s_, u_ = gb_[f % 2], sb_[f % 2], ub_[f % 2]
                    for kc in range(KC):
                        self.MM(pg.ap[:, 0:SG], wg.ap[:, kc, f * 128:(f + 1) * 128], xt_.ap[:, kc, :],
                                start=(kc == 0), stop=(kc == KC - 1), r=[wg, xt_], w=[pg])
                    for kc in range(KC):
                        self.MM(pu.ap[:, 0:SG], wu.ap[:, kc, f * 128:(f + 1) * 128], xt_.ap[:, kc, :],
                                start=(kc == 0), stop=(kc == KC - 1), r=[wu, xt_], w=[pu])
                    bcol = e * 8 + f
                    self.TS("dve", g_.ap[:, :], pg.ap[:, 0:SG], bias.ap[:, bcol:bcol + 1], 7.0, ALU.add, ALU.min, r=[pg, bias], w=[g_])
                    self.ACT(s_.ap[:, :], g_.ap[:, :], AF.Sigmoid, r=[g_], w=[s_], scale=1.702)
                    self.TS("dve", u_.ap[:, :], pu.ap[:, 0:SG], bias.ap[:, 256 + bcol:256 + bcol + 1], -6.0, ALU.add, ALU.max,
                            r=[pu, bias], w=[u_])
                    self.TT("pool", g_.ap[:, :], g_.ap[:, :], s_.ap[:, :], ALU.mult, r=[g_, s_], w=[g_])
                    self.STT(acts[f].ap[:, :], u_.ap[:, :], 8.0, g_.ap[:, :], ALU.min, ALU.mult, r=[u_, g_], w=[acts[f]])
                if j + 1 < NSTEP:
                    prep(xgn[(j + 1) % 2], XT[(j + 1) % 2])
                for h in range(2):
                    y_ = ys[nst % 2]
                    nst += 1
                    for hf in range(2):
                        pd = P[6 + hf]
                        for f in range(KC):
                            self.MM(pd.ap[:, :], acts[f].ap[:, h * 128:(h + 1) * 128], wd.ap[:, f, hf * 512:(hf + 1) * 512],
                                    start=(f == 0), stop=False, r=[acts[f], wd], w=[pd])
                        self.MM(pd.ap[:, :], self.onesb[0:1, :], brb.ap[0:1, hf * 512:(hf + 1) * 512], start=False, stop=True,
                                r=[cstb, brb], w=[pd])
                        self.CP("act" if hf == 0 else "dve", y_.ap[:, hf * 512:(hf + 1) * 512], pd.ap[:, :], r=[pd], w=[y_])
                    self.S.dma_fn("pool", lambda en, o=self.ys_d[:, :], off=sidx_i[:, e, 2 * j + h:2 * j + h + 1], i_=y_.ap[:, :]:
                                  en.indirect_dma_start(out=o, out_offset=bass.IndirectOffsetOnAxis(ap=off, axis=0), in_=i_, in_offset=None,
                                                        bounds_check=self.S.bc_reg, oob_is_err=False),
                                  reads=[y_, meta], writes=[ysd], sem_buf=y_)
                S.end_group()
        S.release(*W[0], *W[1], *xg, *XT, *XT0, *acts, *gb_, *sb_, *ub_, *bdr, *bdrb, bias)
        yk = [S.alloc("yk", [D], F32) for _ in range(8)]
        acc = [S.alloc("acc", [D], F32) for _ in range(2)]
        junk = S.alloc("junk5", [D], BF16)
        xt = self.x1_bufs
        outb = []
        for xi in range(16):
            x_ = xt[xi % 2]
            a_ = acc[xi % 2]
            self.DMA("sp", x_.ap[:, :], self.x1d[xi * 128:(xi + 1) * 128, :], w=[x_])
            for k in range(4):
                y_ = yk[(xi % 2) * 4 + k]
                self.S.dma_fn("pool", lambda e, o=y_.ap[:, :], off=posi_v[:, xi, k:k + 1], i_=self.ys_d[:, :]:
                              e.indirect_dma_start(out=o, out_offset=None, in_=i_, in_offset=bass.IndirectOffsetOnAxis(ap=off, axis=0),
                                                   bounds_check=self.S.bc_reg, oob_is_err=False),
                              reads=[ysd, posi], writes=[y_], sem_buf=y_)
                if k == 0:
                    self.TS("dve", a_.ap[:, :], y_.ap[:, :], wk.ap[:, xi, 0:1], None, ALU.mult, r=[y_, wk], w=[a_])
                else:
                    self.STT(a_.ap[:, :], y_.ap[:, :], wk.ap[:, xi, k:k + 1], a_.ap[:, :], ALU.mult, ALU.add, r=[y_, wk, a_], w=[a_])
            sc = 40 + (xi % 2) * 4
            self.ACT(junk.ap[:, :], a_.ap[:, :], AF.Square, r=[a_], w=[junk, stat], accum_out=stat.ap[:, sc:sc + 1])
            self.rstd_from_ss(stat.ap[:, sc:sc + 1], D, stat, stat.ap[:, sc + 2:sc + 3])
            if self.dbg is not None and xi in (0, 9):
                self.dump((0 if xi == 0 else 1) * 1024, a_, a_.ap[:, :], 1024)
                self.dump(2048 + (0 if xi == 0 else 1) * 8, wk, wk.ap[:, xi, :], 4)
                self.dump(2048 + 16 + (0 if xi == 0 else 1) * 8, posi, posi.ap[:, xi, :], 4)
            self.STT(a_.ap[:, :], a_.ap[:, :], stat.ap[:, sc:sc + 1], Gb.ap[:, 1, :], ALU.mult, ALU.mult, r=[a_, stat, Gb], w=[a_])
            self.TT("pool", x_.ap[:, :], x_.ap[:, :], a_.ap[:, :], ALU.add, r=[x_, a_], w=[x_])
            self.DMA("sp", self.out[xi * 128:(xi + 1) * 128, :], x_.ap[:, :], r=[x_], sem=x_)
        self.final_bufs = list(xt)

    def finish(self):
        S = self.S
        if self.stage < 90:
            z = S.alloc("zout", [128], F32)
            self.MS("pool", z.ap[:, :], 0.0, w=[z])
            self.DMA("pool", self.out[0:128, 0:128], z.ap[:, :], r=[z], sem=z)
            self.dbg_bufs.append(z)
        S.emit(final_bufs=self.dbg_bufs + getattr(self, "final_bufs", []))


def _host_inputs(inputs, b):
    f = lambda a: np.ascontiguousarray(np.asarray(a, dtype=np.float32))
    cst = _consts()
    m = {}
    m["x"] = f(inputs["x"][b])
    m["ctx"] = f(inputs["ctx"][b])
    cc = np.stack([_kc_layout(f(inputs["c"][b])), _kc_layout(f(inputs["c_ctx"]))], axis=-1)
    m["cc"] = f(cc.reshape(128, 16))
    m["w_mod"] = f(inputs["w_mod"][0])
    m["bmodT"] = _kc_layout(f(inputs["b_mod"][0]))
    m["bmod_row"] = f(inputs["b_mod"][0]).reshape(1, -1)
    gT = np.stack([_kc_layout(f(inputs[k][0])) for k in ("g_pre_mix", "g_post_mix", "g_pre_ffn", "g_post_ffn")], axis=1)
    m["gT"] = f(gT.reshape(128, 32))
    m["gpost_row"] = f(np.concatenate([f(inputs["g_post_mix"][0]), f(inputs["g_post_ffn"][0])]).reshape(1, -1))
    m["w_in"] = f(inputs["w_in"][0])
    wa2 = np.zeros((32, 512), np.float32)
    wa2[0:16, 0:256] = inputs["gla_w_a2_f"][0]
    wa2[16:32, 256:512] = inputs["gla_w_a2_b"][0]
    m["wa2"] = wa2
    m["ba_row"] = f(np.concatenate([f(inputs["gla_b_a_f"][0]), f(inputs["gla_b_a_b"][0])]).reshape(1, 512))
    m["gnb"] = f(np.tile(f(inputs["gla_g_norm"][0])[None, :], (128, 4)))
    m["gq"] = _kc_layout(f(inputs["mla_g_q"][0]))
    m["gkv"] = _kc_layout(f(inputs["mla_g_kv"][0]))
    m["w_uq"] = f(inputs["mla_w_uq"][0])
    m["w_uk"] = f(inputs["mla_w_uk"][0])
    m["w_uv"] = f(inputs["mla_w_uv"][0])
    m["w_br_gla"] = f(inputs["w_br_gla"][0])
    m["w_br_mla"] = f(inputs["w_br_mla"][0])
    m["w_out"] = f(inputs["w_out"][0])
    m["router_w"] = f(inputs["router_w"][0])
    m["router_b"] = f(inputs["router_b"][0]).reshape(1, NE)
    m["w_gate"] = f(inputs["w_gate"][0])
    m["w_up"] = f(inputs["w_up"][0])
    m["w_down"] = f(inputs["w_down"][0])
    bg = f(inputs["b_gate"][0]).reshape(NE, 8, 128).transpose(2, 0, 1)
    bu = f(inputs["b_up"][0]).reshape(NE, 8, 128).transpose(2, 0, 1)
    m["bgT"] = f(bg.reshape(128, NE * 8))
    m["buT"] = f(bu.reshape(128, NE * 8))
    m["b_down"] = f(inputs["b_down"][0])
    m["c_tri"] = f(cst["tri"].reshape(128, 512))
    m["c_mask"] = f(cst["mask"].reshape(128, 1024))
    m["c_ident"] = cst["ident"]
    m["c_ropet"] = f(cst["ropet"].reshape(128, 2 * NTOK))
    m["c_e96"] = cst["e96"]
    m["c_sel"] = cst["sel"]
    m["c_rt"] = cst["rt"]
    return m


_SHARED = ("w_mod", "bmodT", "bmod_row", "gT", "gpost_row", "w_in", "wa2", "ba_row", "gnb", "gq", "gkv", "w_uq", "w_uk",
           "w_uv", "w_br_gla", "w_br_mla", "w_out", "router_w", "router_b", "w_gate", "w_up", "w_down", "bgT", "buT",
           "b_down", "c_tri", "c_mask", "c_ident", "c_ropet", "c_e96", "c_sel", "c_rt")


def kernel(**inputs):
    k = K(stage=99)
    m0 = _host_inputs(inputs, 0)
    in_maps = [m0]
    for b in range(1, 8):
        mb = dict(m0)
        mb["x"] = np.ascontiguousarray(np.asarray(inputs["x"][b], dtype=np.float32))
        mb["ctx"] = np.ascontiguousarray(np.asarray(inputs["ctx"][b], dtype=np.float32))
        cc = np.stack([_kc_layout(np.asarray(inputs["c"][b], np.float32)),
                       _kc_layout(np.asarray(inputs["c_ctx"], np.float32))], axis=-1)
        mb["cc"] = np.ascontiguousarray(cc.reshape(128, 16))
        in_maps.append(mb)
    res = run_bass_kernel_spmd(k.nc, in_maps, core_ids=list(range(8)))
    return np.stack([np.asarray(r["out"], dtype=np.float32) for r in res.results], axis=0)
```

```python
import numpy as np
from contextlib import ExitStack
import concourse.bass as bass
import concourse.mybir as mybir
from concourse.bass_utils import run_bass_kernel_spmd

F32 = mybir.dt.float32
BF16 = mybir.dt.bfloat16
I32 = mybir.dt.int32
AF = mybir.ActivationFunctionType
ALU = mybir.AluOpType
AX = mybir.AxisListType
ENGS = ("pe", "act", "dve", "pool", "sp")

T = 2048
TC = 256
NT = 18
NTOK = 2304
D = 1024
KC = 8
D_IN = 4032
EPS = 1e-6
NE = 32
MLA_SCALE = 96 ** -0.5
SG = 256
NSTEP = 8
NSLOT = T * 4 + NE * SG
MOE_MODE = "sparse"


class Buf:
    __slots__ = ("name", "writers", "readers", "dma_sem", "dma_cnt", "ap", "inherit", "off", "words")

    def __init__(self, name, ap=None):
        self.name = name
        self.ap = ap
        self.writers = []
        self.readers = []
        self.inherit = []
        self.off = None
        self.dma_sem = None
        self.dma_cnt = 0


class Op:
    __slots__ = ("eng", "fn", "idx", "deps", "marked", "dma_buf", "dma_val", "mark_no", "grp")

    def __init__(self, eng, fn):
        self.eng = eng
        self.fn = fn
        self.grp = None
        self.deps = []
        self.marked = False
        self.dma_buf = None
        self.dma_val = 0
        self.mark_no = 0


class Sched:
    def __init__(self, nc, arena_words):
        self.nc = nc
        self.ops = {e: [] for e in ENGS}
        self.bufs = []
        self.arena = nc.alloc_sbuf_tensor("arena", [128, arena_words], F32)
        self.free = [(0, arena_words)]
        self.dead = []
        self.nbuf = 0
        self.cur_grp = None
        self.ngrp = 0

    def alloc(self, name, free_shape, dt=F32):
        n = 1
        for s in free_shape:
            n *= s
        words = (n * (2 if dt == BF16 else 4) + 3) // 4
        words = (words + 7) // 8 * 8
        off = None
        for i, (o, sz) in enumerate(self.free):
            if sz >= words:
                off = o
                if sz == words:
                    self.free.pop(i)
                else:
                    self.free[i] = (o + words, sz - words)
                break
        if off is None:
            raise RuntimeError("arena full allocating %s (%d words); free=%s" % (name, words, self.free))
        v = self.arena[:, off:off + words]
        if dt == BF16:
            v = v.bitcast(BF16)
        v = v[:, 0:n]
        if len(free_shape) == 2:
            v = v.rearrange("p (a b) -> p a b", b=free_shape[1])
        elif len(free_shape) == 3:
            v = v.rearrange("p (a b c) -> p a b c", b=free_shape[1], c=free_shape[2])
        self.nbuf += 1
        b = Buf("%s_%d" % (name, self.nbuf), v)
        b.off, b.words = off, words
        for (o, w, toks) in self.dead:
            if o < off + words and off < o + w:
                b.inherit.extend(toks)
        self.bufs.append(b)
        return b

    def release(self, *bufs):
        for b in bufs:
            toks = [("op", t[1], "raw") if t[0] == "op" else t for t in (b.writers + b.readers + b.inherit)]
            self.dead.append((b.off, b.words, toks))
            self.free.append((b.off, b.words))
        self.free.sort()
        m = []
        for o, w in self.free:
            if m and m[-1][0] + m[-1][1] == o:
                m[-1] = (m[-1][0], m[-1][1] + w)
            else:
                m.append((o, w))
        self.free = m

    def psum(self, name):
        t = self.nc.alloc_psum_tensor(name, [128, 512], F32)
        b = Buf(name, t[:, :])
        self.bufs.append(b)
        return b

    def virt(self, name):
        b = Buf(name, None)
        self.bufs.append(b)
        return b

    def _add(self, eng, fn, reads, writes, dma_buf=None):
        op = Op(eng, fn)
        op.idx = len(self.ops[eng])
        op.grp = self.cur_grp
        is_dma = dma_buf is not None
        deps = []
        for b in reads:
            deps.extend(b.writers)
            deps.extend(b.inherit)
            if b.off is None:
                deps.extend([("op", t[1], "raw") for t in b.readers if t[0] == "op" and t[1].eng != eng])
        for b in writes:
            deps.extend(b.inherit)
            deps.extend(b.readers)
            if is_dma and not b.readers and b.writers and all(w[0] == "dma" for w in b.writers):
                pass
            else:
                deps.extend(b.writers)
        fl = []
        for d in deps:
            if d[0] == "op":
                o = d[1]
                if o.eng == eng and not is_dma:
                    if eng == "pe" or eng == "sp":
                        continue
                    if d[2] != "raw":
                        continue
            fl.append(d)
        op.deps = fl
        if is_dma:
            dma_buf.dma_cnt += 16
            op.dma_buf = dma_buf
            op.dma_val = dma_buf.dma_cnt
            tok_w = ("dma", dma_buf, op.dma_val)
            tok_r = tok_w
        else:
            tok_w = ("op", op, "raw")
            tok_r = ("op", op, "war")
        for b in reads:
            b.readers.append(tok_r)
            if len(b.readers) > 64:
                last = {}
                keep = []
                for t in b.readers:
                    if t[0] == "op":
                        last[t[1].eng] = t
                    else:
                        keep.append(t)
                b.readers = keep[-32:] + list(last.values())
        for b in writes:
            if is_dma and not b.readers and b.writers and all(w[0] == "dma" for w in b.writers):
                b.writers.append(tok_w)
            else:
                b.writers = [tok_w]
                b.readers = []
        self.ops[eng].append(op)
        return op

    def op(self, eng, fn, reads=(), writes=()):
        return self._add(eng, fn, list(reads), list(writes))

    def dma(self, eng, out_ap, in_ap, reads=(), writes=(), sem_buf=None, **kw):
        if sem_buf is None:
            sem_buf = (list(writes) + list(reads))[0]

        def fn(e, out_ap=out_ap, in_ap=in_ap, kw=kw):
            return e.dma_start(out=out_ap, in_=in_ap, **kw)
        return self._add(eng, fn, list(reads), list(writes), dma_buf=sem_buf)

    def begin_group(self, thr):
        self.ngrp += 1
        self.cur_grp = (self.ngrp, thr)

    def end_group(self):
        self.cur_grp = None

    def regload(self, src_buf, ap):
        for e in ENGS:
            self._add(e, ("regload", ap), [src_buf], [])

    def dma_fn(self, eng, fn, reads=(), writes=(), sem_buf=None):
        return self._add(eng, fn, list(reads), list(writes), dma_buf=sem_buf)

    def emit(self, final_bufs=()):
        nc = self.nc
        for e in ENGS:
            for op in self.ops[e]:
                for d in op.deps:
                    if d[0] == "op":
                        d[1].marked = True
        for e in ENGS:
            n = 0
            for op in self.ops[e]:
                if op.marked:
                    n += 1
                    op.mark_no = n
        with ExitStack() as st:
            esem = {e: st.enter_context(nc.semaphore("s_" + e)) for e in ENGS}
            for b in self.bufs:
                if b.dma_cnt > 0:
                    b.dma_sem = st.enter_context(nc.semaphore("d_" + b.name))
            self.bc_reg = st.enter_context(nc.gpsimd.register("rbc"))
            engobj = {"pe": nc.tensor, "act": nc.scalar, "dve": nc.vector, "pool": nc.gpsimd, "sp": nc.sync}
            self.cregs = {e: st.enter_context(engobj[e].register("creg_" + e)) for e in ENGS}
            print("semaphores used:", 5 + sum(1 for b in self.bufs if b.dma_cnt > 0))
            block = st.enter_context(nc.Block())
            handles = {"pe": block.tensor, "act": block.scalar, "dve": block.vector,
                       "pool": block.gpsimd, "sp": block.sync}
            stats = {}
            for e in ENGS:
                ops = self.ops[e]

                def body(eng, ops=ops, e=e):
                    waited = {}
                    nw = [0]
                    if e == "pool":
                        eng.reg_mov(self.bc_reg, NSLOT - 1)
                    creg = self.cregs[e]

                    def emit_op(op, wt):
                        need = {}
                        for d in op.deps:
                            if d[0] == "op":
                                key = ("e", d[1].eng)
                                val = d[1].mark_no
                            else:
                                key = ("d", d[1].name)
                                val = d[2]
                            if val > need.get(key, (0, None))[0]:
                                need[key] = (val, d)
                        for key, (val, d) in need.items():
                            if wt.get(key, 0) >= val:
                                continue
                            wt[key] = val
                            sem = esem[d[1].eng] if d[0] == "op" else d[1].dma_sem
                            eng.wait_ge(sem, val)
                            nw[0] += 1
                        if isinstance(op.fn, tuple):
                            eng.reg_load(creg, op.fn[1])
                            return
                        ins = op.fn(eng)
                        if op.dma_buf is not None:
                            ins.then_inc(op.dma_buf.dma_sem, 16)
                        elif op.marked:
                            ins.then_inc(esem[e], 1)
                    def comp_of(gops):
                        comp = []
                        marked = [o for o in gops if o.marked and o.dma_buf is None]
                        if marked:
                            comp.append((esem[e], marked[0].mark_no - 1, len(marked)))
                        dm = {}
                        for o in gops:
                            if o.dma_buf is not None:
                                if o.dma_buf.name not in dm:
                                    dm[o.dma_buf.name] = [o.dma_buf.dma_sem, o.dma_val - 16, 0]
                                dm[o.dma_buf.name][2] += 16
                        comp.extend(tuple(v) for v in dm.values())
                        return comp

                    def chain(runs, k, wt):
                        rest = [o for r_ in runs[k:] for o in r_]
                        thr = runs[k][0].grp[1]
                        with eng.If_lt(creg, thr + 1):
                            for sem, before, delta in comp_of(rest):
                                if before > 0:
                                    eng.wait_ge(sem, before)
                                eng.sem_inc(sem, delta)
                        with eng.Else():
                            for o in runs[k]:
                                emit_op(o, wt)
                            if k + 1 < len(runs):
                                chain(runs, k + 1, wt)
                    i = 0
                    n = len(ops)
                    while i < n:
                        op = ops[i]
                        if op.grp is None:
                            emit_op(op, waited)
                            i += 1
                            continue
                        runs = []
                        j = i
                        while j < n and ops[j].grp is not None and (not runs or ops[j].grp[1] > runs[-1][0].grp[1] or ops[j].grp is runs[-1][0].grp):
                            if runs and ops[j].grp is runs[-1][0].grp:
                                runs[-1].append(ops[j])
                            else:
                                runs.append([ops[j]])
                            j += 1
                        chain(runs, 0, dict(waited))
                        i = j
                    if e == "sp":
                        for b in final_bufs:
                            if b.dma_cnt:
                                eng.wait_ge(b.dma_sem, b.dma_cnt)
                    stats[e] = (len(ops), nw[0])
                handles[e](body)
            self.stats = stats


def _consts():
    c = {}
    idx = np.arange(128)
    same = (idx[:, None] // 64) == (idx[None, :] // 64)
    s = idx[:, None]
    t = idx[None, :]
    tri = np.zeros((128, 4, 128), np.float32)
    tri[:, 0] = same & (s <= t)
    tri[:, 1] = same & (s >= t)
    tri[:, 2] = same & (s > t)
    tri[:, 3] = same & (s < t)
    c["tri"] = tri
    mask = np.zeros((128, 2, 4, 128), np.float32)
    mask[:, 0] = (same & (t >= s))[:, None, :]
    mask[:, 1] = (same & (t <= s))[:, None, :]
    c["mask"] = mask.reshape(128, 2, 512)
    c["ident"] = np.eye(128, dtype=np.float32)
    half = 16
    inv_freq = (10000.0 ** (-np.arange(0, half, 2, dtype=np.float32) / half)).astype(np.float32)
    tok = np.arange(T)
    ang_row = (tok // 64).astype(np.float32)[:, None] * inv_freq
    ang_col = (tok % 64).astype(np.float32)[:, None] * inv_freq
    cosr, sinr = np.cos(ang_row), np.sin(ang_row)
    cosc, sinc = np.cos(ang_col), np.sin(ang_col)
    cos32 = np.concatenate([cosr, cosr, cosc, cosc], axis=1)
    sin32 = np.concatenate([-sinr, sinr, -sinc, sinc], axis=1)
    ropet = np.zeros((128, 2, NTOK), np.float32)
    ropet[64:96, 0, :TC] = 1.0
    ropet[64:96, 0, TC:] = cos32.T
    ropet[64:96, 1, TC:] = sin32.T
    c["ropet"] = ropet
    e96 = np.zeros((128, 128), np.float32)
    e96[:96, 96] = 1.0
    c["e96"] = e96
    sel = np.zeros((NE, NE, 128), np.float32)
    for e in range(NE):
        sel[e, e, :] = 1.0
    c["sel"] = sel.reshape(NE, NE * 128)
    rt = np.zeros((128, 176), np.float32)
    rt[:, 0:128] = (s < t)
    ke = np.arange(NE)
    rt[0:NE, 128:160] = (ke[:, None] < ke[None, :])
    rt[:, 160:176] = np.arange(16, dtype=np.float32)[None, :] * 128 + idx[:, None]
    c["rt"] = rt
    return c


def _kc_layout(v):
    return np.ascontiguousarray(v.reshape(-1, 128).T)


class K:
    def __init__(self, stage=99, dbg=False):
        self.stage = stage
        nc = bass.Bass("TRN2", target_bir_lowering=False)
        self.nc = nc
        self.S = Sched(nc, 51200 + 1000)
        S = self.S
        dt = nc.dram_tensor
        self.din = {}

        def inp(name, shape):
            self.din[name] = dt(name, list(shape), F32, kind="ExternalInput").ap()
            return self.din[name]
        self.x = inp("x", [T, D])
        self.ctx = inp("ctx", [TC, D])
        self.cc = inp("cc", [128, 16])
        self.w_mod = inp("w_mod", [D, 6 * D])
        self.bmodT = inp("bmodT", [128, 48])
        self.bmod_row = inp("bmod_row", [1, 6 * D])
        self.gT = inp("gT", [128, 32])
        self.gpost_row = inp("gpost_row", [1, 2 * D])
        self.w_in = inp("w_in", [D, D_IN])
        self.wa2 = inp("wa2", [32, 512])
        self.ba_row = inp("ba_row", [1, 512])
        self.gnb = inp("gnb", [128, 512])
        self.gq = inp("gq", [128, 2])
        self.gkv = inp("gkv", [128, 1])
        self.w_uq = inp("w_uq", [256, 768])
        self.w_uk = inp("w_uk", [128, 512])
        self.w_uv = inp("w_uv", [128, 512])
        self.w_br_gla = inp("w_br_gla", [512, D])
        self.w_br_mla = inp("w_br_mla", [512, D])
        self.w_out = inp("w_out", [D, D])
        self.router_w = inp("router_w", [D, NE])
        self.router_b = inp("router_b", [1, NE])
        self.w_gate = inp("w_gate", [NE, D, D])
        self.w_up = inp("w_up", [NE, D, D])
        self.w_down = inp("w_down", [NE, D, D])
        self.bgT = inp("bgT", [128, NE * 8])
        self.buT = inp("buT", [128, NE * 8])
        self.b_down = inp("b_down", [NE, D])
        self.c_tri = inp("c_tri", [128, 512])
        self.c_mask = inp("c_mask", [128, 1024])
        self.c_ident = inp("c_ident", [128, 128])
        self.c_ropet = inp("c_ropet", [128, 2 * NTOK])
        self.c_e96 = inp("c_e96", [128, 128])
        self.c_sel = inp("c_sel", [NE, NE * 128])
        self.c_rt = inp("c_rt", [128, 176])
        self.xs_d = dt("xs_d", [NSLOT, D], BF16, kind="Internal").ap()
        self.ys_d = dt("ys_d", [NSLOT, D], F32, kind="Internal").ap()
        self.out = dt("out", [T, D], F32, kind="ExternalOutput").ap()
        self.x1d = dt("x1d", [T, D], F32, kind="Internal").ap()
        self.dbg = dt("dbg", [128, 8192], F32, kind="ExternalOutput").ap() if dbg else None
        self.P = [S.psum("ps%d" % i) for i in range(8)]
        self.wq_i = 0
        self.build()

    def MM(self, out, lhsT, rhs, start=True, stop=True, r=(), w=()):
        self.S.op("pe", lambda e: e.matmul(out, lhsT, rhs, start=start, stop=stop, skip_group_check=True), r, w)

    def TR(self, out, in_, ident, r=(), w=()):
        self.S.op("pe", lambda e: e.transpose(out, in_, ident), r, w)

    def ACT(self, out, in_, func, r=(), w=(), **kw):
        self.S.op("act", lambda e: e.activation(out, in_, func, **kw), r, w)

    def TT(self, eng, out, a, b, op, r=(), w=()):
        self.S.op(eng, lambda e: e.tensor_tensor(out, a, b, op=op), r, w)

    def TS(self, eng, out, a, s1, s2, op0, op1=None, r=(), w=()):
        if op1 is None:
            self.S.op(eng, lambda e: e.tensor_scalar(out, a, s1, None, op0=op0), r, w)
        else:
            self.S.op(eng, lambda e: e.tensor_scalar(out, a, s1, s2, op0=op0, op1=op1), r, w)

    def STT(self, out, a, s, b, op0, op1, r=(), w=()):
        self.S.op("dve", lambda e: e.scalar_tensor_tensor(out, a, s, b, op0=op0, op1=op1), r, w)

    def CP(self, eng, out, in_, r=(), w=()):
        if eng == "act":
            self.S.op("act", lambda e: e.copy(out, in_), r, w)
        else:
            self.S.op(eng, lambda e: e.tensor_copy(out, in_), r, w)

    def MS(self, eng, ap, val, w=()):
        self.S.op(eng, lambda e: e.memset(ap, val), (), w)

    def DMA(self, q, out, in_, r=(), w=(), sem=None, **kw):
        self.S.dma(q, out, in_, reads=r, writes=w, sem_buf=sem, **kw)

    def rstd_from_ss(self, ss_ap, n, buf, tmp_ap):
        self.ACT(tmp_ap, ss_ap, AF.Ln, r=[buf], w=[buf], scale=1.0 / n, bias=self.eps_ap)
        self.ACT(ss_ap, tmp_ap, AF.Exp, r=[buf], w=[buf], scale=-0.5)

    def load_w(self, dst_ap, dst_buf, src_ap, rows, cols, q="pool"):
        if rows > 128:
            self.DMA("pool", dst_ap, src_ap.rearrange("(k p) n -> p k n", p=128), w=[dst_buf])
        else:
            self.DMA("pool", dst_ap[0:rows, 0:cols], src_ap, w=[dst_buf])

    def dump(self, col, buf, ap, ncols, parts=128):
        if self.dbg is None:
            return
        S = self.S
        tmp = S.alloc("dbgtmp", [ncols], F32)
        self.CP("dve", tmp.ap[0:parts, :], ap, r=[buf], w=[tmp])
        self.DMA("pool", self.dbg[0:parts, col:col + ncols], tmp.ap[0:parts, :], r=[tmp], sem=tmp)
        self.dbg_bufs.append(tmp)

    def build(self):
        S = self.S
        P = self.P
        self.dbg_bufs = []
        cst = S.alloc("cst", [2048], F32)
        tri = cst.ap[:, 0:512].rearrange("p (a b) -> p a b", b=128)
        identf = cst.ap[:, 512:640]
        e96f = cst.ap[:, 640:768]
        self.eps_ap = cst.ap[:, 768:769]
        onesf = cst.ap[:, 896:1024]
        self.DMA("sp", cst.ap[:, 0:512], self.c_tri, w=[cst])
        self.DMA("sp", identf, self.c_ident, w=[cst])
        self.DMA("sp", e96f, self.c_e96, w=[cst])
        self.MS("pool", cst.ap[:, 768:769], EPS, w=[cst])
        self.MS("pool", onesf, 1.0, w=[cst])
        self.rowmask = cst.ap[:, 1024:1026]
        self.MS("pool", cst.ap[:, 1024:1026], 0.0, w=[cst])
        self.MS("pool", cst.ap[0:64, 1024:1025], 1.0, w=[cst])
        self.MS("pool", cst.ap[64:128, 1025:1026], 1.0, w=[cst])
        cstb = S.alloc("cstb", [2048], BF16)
        identb = cstb.ap[:, 0:128]
        onesb = cstb.ap[:, 128:256]
        e96b = cstb.ap[:, 256:384]
        maskb = cstb.ap[:, 1024:2048].rearrange("p (a b) -> p a b", b=512)
        self.CP("pool", identb, identf, r=[cst], w=[cstb])
        self.CP("pool", onesb, onesf, r=[cst], w=[cstb])
        self.CP("pool", e96b, e96f, r=[cst], w=[cstb])
        self.wstage = [S.alloc("wst", [8, 256], F32) for _ in range(3)]
        mstage = self.wstage[0]
        self.DMA("sp", mstage.ap[:, 0:4, :], self.c_mask.rearrange("p (a b) -> p a b", b=256), w=[mstage])
        self.CP("pool", cstb.ap[:, 1024:2048].rearrange("p (a b) -> p a b", b=256), mstage.ap[:, 0:4, :], r=[mstage], w=[cstb])
        stat = S.alloc("stat", [64], F32)

        modb = S.alloc("mod", [48, 2], F32)
        vec = S.alloc("vec", [16 + 48 + 32 + 32 + 16 + 16 + 8 + 8], F32)
        cc = vec.ap[:, 0:16].rearrange("p (a b) -> p a b", b=2)
        bmodT = vec.ap[:, 16:64]
        gT = vec.ap[:, 64:96].rearrange("p (a b) -> p a b", b=8)
        A1 = vec.ap[:, 96:112].rearrange("p (a b) -> p a b", b=2)
        B1v = None
        A2 = vec.ap[:, 112:120]
        gqv = vec.ap[:, 128:130]
        gkvv = vec.ap[:, 130:131]
        self.DMA("sp", vec.ap[:, 0:16], self.cc, w=[vec])
        self.DMA("sp", bmodT, self.bmodT, w=[vec])
        self.DMA("sp", vec.ap[:, 64:96], self.gT, w=[vec])
        self.DMA("sp", gqv, self.gq, w=[vec])
        self.DMA("sp", gkvv, self.gkv, w=[vec])
        self.ACT(cc, cc, AF.Silu, r=[vec], w=[vec])
        rows = S.alloc("rows", [4096], F32)
        self.DMA("sp", rows.ap[0:1, 2048:4096], self.gpost_row, w=[rows])
        browst = S.alloc("browst", [2048], F32)
        self.DMA("sp", browst.ap[0:1, 0:1024], self.bmod_row[:, 2048:3072], w=[browst])
        self.DMA("sp", browst.ap[0:1, 1024:2048], self.bmod_row[:, 5120:6144], w=[browst])
        wm_view = self.w_mod.rearrange("(k p) n -> p k n", p=128)
        ccb = S.alloc("ccb", [8, 2], BF16)
        self.CP("dve", ccb.ap[:, :, :], cc, r=[vec], w=[ccb])
        wmb = [S.alloc("wmb", [KC, 512], BF16) for _ in range(3)]
        for j in range(12):
            st = wmb[j % 3]
            self.DMA("pool", st.ap[:, :, :], wm_view[:, :, j * 512:(j + 1) * 512], w=[st])
            for m in range(4):
                col = (j * 4 + m) * 2
                for kc in range(KC):
                    self.MM(P[0].ap[:, col:col + 2], st.ap[:, kc, m * 128:(m + 1) * 128], ccb.ap[:, kc, :],
                            start=(kc == 0), stop=(kc == KC - 1), r=[st, ccb], w=[P[0]])
            if j in (4, 5, 10, 11):
                ro = {4: 0, 5: 512, 10: 1024, 11: 1536}[j]
                for kc in range(KC):
                    self.MM(P[1].ap[0:1, :], ccb.ap[:, kc, 0:1], st.ap[:, kc, :], start=(kc == 0), stop=(kc == KC - 1),
                            r=[st, ccb], w=[P[1]])
                self.TT("dve", rows.ap[0:1, ro:ro + 512], P[1].ap[0:1, :], browst.ap[0:1, ro:ro + 512], ALU.add,
                        r=[P[1], browst], w=[rows])
        S.release(ccb, *wmb)
        S.release(browst)
        self.TT("dve", modb.ap[:, :, :], P[0].ap[:, 0:96].rearrange("p (a b) -> p a b", b=2),
                bmodT.unsqueeze(2).to_broadcast([128, 48, 2]), ALU.add, r=[P[0], vec], w=[modb])
        self.STT(A1, modb.ap[:, 8:16, :], 1.0, gT[:, 0, :].unsqueeze(2).to_broadcast([128, 8, 2]), ALU.add, ALU.mult,
                 r=[modb, vec], w=[vec])
        self.STT(A2, modb.ap[:, 32:40, 0], 1.0, gT[:, 2, :], ALU.add, ALU.mult, r=[modb, vec], w=[vec])
        self.TT("dve", rows.ap[0:1, 0:2048], rows.ap[0:1, 0:2048], rows.ap[0:1, 2048:4096], ALU.mult, r=[rows], w=[rows])
        Gb = S.alloc("Gb", [2, 1024], F32)
        for g in range(2):
            for hh in range(2):
                pb = P[2 + hh]
                self.MM(pb.ap[:, :], onesf[0:1, :], rows.ap[0:1, g * 1024 + hh * 512: g * 1024 + hh * 512 + 512],
                        r=[cst, rows], w=[pb])
                self.CP("act", Gb.ap[:, g, hh * 512:(hh + 1) * 512], pb.ap[:, :], r=[pb], w=[Gb])
        S.release(rows, *self.wstage)
        if self.stage == 0:
            self.dump(0, modb, modb.ap[:, :, :].rearrange("p a b -> p (a b)"), 96)
            self.dump(96, vec, vec.ap[:, 96:120], 24)
            self.dump(128, Gb, Gb.ap[:, :, :].rearrange("p a b -> p (a b)"), 2048)
            return self.finish()

        hT = S.alloc("hT", [KC, NTOK], BF16)
        xts = [S.alloc("xt", [D], F32) for _ in range(3)]
        xns = [S.alloc("xn", [D], BF16) for _ in range(2)]
        junk = S.alloc("junk", [D], BF16)
        for i in range(NT):
            xt = xts[i % 3]
            xn = xns[i % 2]
            src = self.ctx[i * 128:(i + 1) * 128, :] if i < 2 else self.x[(i - 2) * 128:(i - 1) * 128, :]
            v = 1 if i < 2 else 0
            self.DMA("sp", xt.ap[:, :], src, w=[xt])
            ss = stat.ap[:, (i % 4) * 2:(i % 4) * 2 + 1]
            tmp = stat.ap[:, (i % 4) * 2 + 1:(i % 4) * 2 + 2]
            self.ACT(junk.ap[:, :], xt.ap[:, :], AF.Square, r=[xt], w=[junk, stat], accum_out=ss)
            self.rstd_from_ss(ss, D, stat, tmp)
            self.TS("dve", xn.ap[:, :], xt.ap[:, :], ss, None, ALU.mult, r=[xt, stat], w=[xn])
            pt = P[i % 2]
            ptb = pt.ap.bitcast(BF16)
            for kc in range(KC):
                self.TR(ptb[:, kc * 128:(kc + 1) * 128], xn.ap[:, kc * 128:(kc + 1) * 128], identb, r=[xn, cstb], w=[pt])
            for kc in range(KC):
                o = hT.ap[:, kc, i * 128:(i + 1) * 128]
                if kc % 2 == 0:
                    self.TS("dve", o, ptb[:, kc * 128:(kc + 1) * 128], A1[:, kc, v:v + 1], modb.ap[:, kc, v:v + 1],
                            ALU.mult, ALU.add, r=[pt, vec, modb], w=[hT])
                else:
                    self.ACT(o, ptb[:, kc * 128:(kc + 1) * 128], AF.Identity, r=[pt, vec, modb], w=[hT],
                             scale=A1[:, kc, v:v + 1], bias=modb.ap[:, kc, v:v + 1])
        S.release(*xts, *xns, junk)
        if self.stage == 1:
            for kc in range(2):
                self.dump(kc * 2304, hT, hT.ap[:, kc, :], 2304)
            return self.finish()
        self.cst, self.cstb, self.stat, self.vec, self.modb, self.Gb, self.hT, self.junk = cst, cstb, stat, vec, modb, Gb, hT, junk
        self.tri, self.identf, self.identb, self.onesf, self.onesb, self.e96b, self.maskb = tri, identf, identb, onesf, onesb, e96b, maskb
        self.A2, self.gqv, self.gkvv = A2, gqv, gkvv
        self.phase_gla()
        if self.stage == 25:
            return self.finish()
        if self.stage == 3:
            for hc in range(4):
                self.dump(hc * 2048, self.ogT, self.ogT.ap[:, hc, :], 2048)
            return self.finish()
        self.phase_mla()
        if self.stage == 4:
            for h in range(4):
                self.dump(h * 2048, self.omlaT, self.omlaT.ap[0:64, h, :], 2048, parts=64)
            return self.finish()
        self.phase_merge()
        if self.stage == 5:
            return self.finish()
        if MOE_MODE == "dense":
            self.phase_moe()
        else:
            self.phase_moe_sparse()
        self.finish()

    def phase_mla(self):
        S, P = self.S, self.P
        hT, cstb, cst, stat = self.hT, self.cstb, self.cst, self.stat
        identb, onesf, onesb, identf = self.identb, self.onesf, self.onesb, self.identf
        wB = S.alloc("wB", [KC, 416], BF16)
        self.load_w(wB.ap[:, :, :], wB, self.w_in[:, 1568:1984], 1024, 416)
        wkrs = S.alloc("wkrs", [KC, 32], BF16)
        for a, b_ in ((0, 8), (8, 0), (16, 24), (24, 16)):
            self.CP("pool", wkrs.ap[:, :, a:a + 8], wB.ap[:, :, 384 + b_:384 + b_ + 8], r=[wB], w=[wkrs])
        wuq = S.alloc("wuq", [2, 768], BF16)
        self.load_w(wuq.ap[:, :, :], wuq, self.w_uq, 256, 768)
        wuqs = S.alloc("wuqs", [2, 8, 32], BF16)
        for h in range(8):
            for a, b_ in ((0, 8), (8, 0), (16, 24), (24, 16)):
                self.CP("pool", wuqs.ap[:, :, h, a:a + 8], wuq.ap[:, :, h * 96 + 64 + b_:h * 96 + 64 + b_ + 8], r=[wuq], w=[wuqs])
        wukv = S.alloc("wukv", [1024], BF16)
        self.load_w(wukv.ap[:, 0:512], wukv, self.w_uk, 128, 512)
        self.load_w(wukv.ap[:, 512:1024], wukv, self.w_uv, 128, 512)
        zdqnT = S.alloc("zdqnT", [2, T], BF16)
        ckvT = S.alloc("ckvT", [NTOK], BF16)
        krT = S.alloc("krT", [NTOK], BF16)
        zn = [S.alloc("zn", [384], BF16) for _ in range(2)]
        junk = S.alloc("junk2", [384], F32)
        for i in range(NT):
            pa = P[i % 2]
            pt = P[2 + i % 2]
            ptb = pt.ap.bitcast(BF16)
            z = zn[i % 2]
            sc = (i % 4) * 4
            if i >= 2:
                for kc in range(KC):
                    self.MM(pa.ap[:, 0:256], hT.ap[:, kc, i * 128:(i + 1) * 128], wB.ap[:, kc, 0:256],
                            start=(kc == 0), stop=(kc == KC - 1), r=[wB, hT], w=[pa])
                self.ACT(junk.ap[:, 0:256], pa.ap[:, 0:256], AF.Square, r=[pa], w=[junk, stat], accum_out=stat.ap[:, sc:sc + 1])
                self.rstd_from_ss(stat.ap[:, sc:sc + 1], 256, stat, stat.ap[:, sc + 1:sc + 2])
                self.TS("dve", z.ap[:, 0:256], pa.ap[:, 0:256], stat.ap[:, sc:sc + 1], None, ALU.mult, r=[pa, stat], w=[z])
            for kc in range(KC):
                self.MM(pa.ap[:, 256:384], hT.ap[:, kc, i * 128:(i + 1) * 128], wB.ap[:, kc, 256:384],
                        start=(kc == 0), stop=(kc == KC - 1), r=[wB, hT], w=[pa])
            self.ACT(junk.ap[:, 256:384], pa.ap[:, 256:384], AF.Square, r=[pa], w=[junk, stat], accum_out=stat.ap[:, sc + 2:sc + 3])
            self.rstd_from_ss(stat.ap[:, sc + 2:sc + 3], 128, stat, stat.ap[:, sc + 3:sc + 4])
            self.TS("dve", z.ap[:, 256:384], pa.ap[:, 256:384], stat.ap[:, sc + 2:sc + 3], None, ALU.mult, r=[pa, stat], w=[z])
            if i >= 2:
                for c in range(2):
                    self.TR(ptb[:, c * 128:(c + 1) * 128], z.ap[:, c * 128:(c + 1) * 128], identb, r=[z, cstb], w=[pt])
                    self.TS("dve", zdqnT.ap[:, c, (i - 2) * 128:(i - 1) * 128], ptb[:, c * 128:(c + 1) * 128], self.gqv[:, c:c + 1], None,
                            ALU.mult, r=[pt, self.vec], w=[zdqnT])
            self.TR(ptb[:, 256:384], z.ap[:, 256:384], identb, r=[z, cstb], w=[pt])
            self.TS("dve", ckvT.ap[:, i * 128:(i + 1) * 128], ptb[:, 256:384], self.gkvv[:, 0:1], None, ALU.mult,
                    r=[pt, self.vec], w=[ckvT])
        TB_ALL = [(0, 256), (256, 512), (768, 512), (1280, 512), (1792, 512)]
        rp = [S.alloc("rp", [2, 512], F32) for _ in range(2)]
        rt = S.alloc("rt", [2, 512], F32)
        ropv = self.c_ropet.rearrange("p (a b) -> p a b", b=NTOK)
        for bi, (c0, nn) in enumerate(TB_ALL):
            r_ = rp[bi % 2]
            self.DMA("sp", r_.ap[64:96, :, 0:nn], ropv[64:96, :, c0:c0 + nn], w=[r_])
            for kc in range(KC):
                self.MM(P[4].ap[64:96, 0:nn], wB.ap[:, kc, 384:416], hT.ap[:, kc, c0:c0 + nn], start=(kc == 0), stop=(kc == KC - 1),
                        r=[wB, hT], w=[P[4]])
            for kc in range(KC):
                self.MM(P[5].ap[64:96, 0:nn], wkrs.ap[:, kc, :], hT.ap[:, kc, c0:c0 + nn], start=(kc == 0), stop=(kc == KC - 1),
                        r=[wkrs, hT], w=[P[5]])
            self.TT("dve", rt.ap[64:96, 0, 0:nn], P[4].ap[64:96, 0:nn], r_.ap[64:96, 0, 0:nn], ALU.mult, r=[P[4], r_], w=[rt])
            self.TT("dve", rt.ap[64:96, 1, 0:nn], P[5].ap[64:96, 0:nn], r_.ap[64:96, 1, 0:nn], ALU.mult, r=[P[5], r_], w=[rt])
            self.TT("pool", krT.ap[64:96, c0:c0 + nn], rt.ap[64:96, 0, 0:nn], rt.ap[64:96, 1, 0:nn], ALU.add, r=[rt], w=[krT])
        S.release(wB, wkrs, junk, *zn)
        v = S.alloc("v", [NT, 8, 128], BF16)
        self.MS("pool", v.ap[:, :, 0:4, 64:128], 0.0, w=[v])
        self.MS("pool", v.ap[:, :, 0:4, 64:65], 1.0, w=[v])
        self.MS("pool", v.ap[:, :, 4:8, 0:64], 0.0, w=[v])
        self.MS("pool", v.ap[:, :, 4:8, 0:1], 1.0, w=[v])
        for i in range(NT):
            pb = P[i % 2]
            self.MM(pb.ap[:, :], ckvT.ap[:, i * 128:(i + 1) * 128], wukv.ap[:, 512:1024], r=[ckvT, wukv], w=[pb])
            self.CP("act", v.ap[:, i, 0:4, 0:64], pb.ap[:, 0:256].rearrange("p (a b) -> p a b", b=64), r=[pb], w=[v])
            self.CP("dve", v.ap[:, i, 4:8, 64:128], pb.ap[:, 256:512].rearrange("p (a b) -> p a b", b=64), r=[pb], w=[v])
        omlaT = S.alloc("omlaT", [4, T], BF16)
        self.omlaT = omlaT
        kThs = [S.alloc("kTh", [NTOK], BF16) for _ in range(2)]
        qThs = [S.alloc("qTh", [T], BF16) for _ in range(2)]
        pT = [S.alloc("pT", [512], BF16) for _ in range(3)]
        rden = S.alloc("rden", [512], F32)
        bcs = S.alloc("bcs", [512], F32)
        sqb = S.alloc("sqb", [NTOK], BF16)
        mxs = [S.alloc("mx", [8], F32) for _ in range(2)]
        nb = [0]

        def build(h):
            kTh, qTh = kThs[h % 2], qThs[h % 2]
            for (c0, nn) in TB_ALL:
                pb = P[nb[0] % 2]
                nb[0] += 1
                self.MM(pb.ap[0:64, 0:nn], wukv.ap[:, h * 64:(h + 1) * 64], ckvT.ap[:, c0:c0 + nn], r=[wukv, ckvT], w=[pb])
                self.CP("dve", kTh.ap[0:64, c0:c0 + nn], pb.ap[0:64, 0:nn], r=[pb], w=[kTh])
            self.CP("pool", kTh.ap[64:96, :], krT.ap[64:96, :], r=[krT], w=[kTh])
            for qb in range(4):
                c0 = TC + qb * 512
                r_ = rp[qb % 2]
                self.DMA("sp", r_.ap[64:96, :, :], ropv[64:96, :, c0:c0 + 512], w=[r_])
                pq, pr = P[2], P[3]
                for c in range(2):
                    self.MM(pq.ap[0:96, :], wuq.ap[:, c, h * 96:(h + 1) * 96], zdqnT.ap[:, c, qb * 512:(qb + 1) * 512],
                            start=(c == 0), stop=(c == 1), r=[wuq, zdqnT], w=[pq])
                for c in range(2):
                    self.MM(pr.ap[64:96, :], wuqs.ap[:, c, h, :], zdqnT.ap[:, c, qb * 512:(qb + 1) * 512],
                            start=(c == 0), stop=(c == 1), r=[wuqs, zdqnT], w=[pr])
                self.CP("dve", qTh.ap[0:64, qb * 512:(qb + 1) * 512], pq.ap[0:64, :], r=[pq], w=[qTh])
                self.TT("dve", rt.ap[64:96, 0, :], pq.ap[64:96, :], r_.ap[64:96, 0, :], ALU.mult, r=[pq, r_], w=[rt])
                self.TT("dve", rt.ap[64:96, 1, :], pr.ap[64:96, :], r_.ap[64:96, 1, :], ALU.mult, r=[pr, r_], w=[rt])
                self.TT("pool", qTh.ap[64:96, qb * 512:(qb + 1) * 512], rt.ap[64:96, 0, :], rt.ap[64:96, 1, :], ALU.add, r=[rt], w=[qTh])

        def build_stab(h):
            kTh, qTh = kThs[h % 2], qThs[h % 2]
            sq = sqb
            mx = mxs[h % 2]
            pn = P[3]
            for which, src_, ntile in ((0, qTh, 16), (1, kTh, NT)):
                self.TT("pool", sq.ap[0:96, 0:ntile * 128], src_.ap[0:96, 0:ntile * 128], src_.ap[0:96, 0:ntile * 128], ALU.mult,
                        r=[src_], w=[sq])
                for t_ in range(ntile):
                    self.MM(pn.ap[:, which * 32 + t_:which * 32 + t_ + 1], sq.ap[0:96, t_ * 128:(t_ + 1) * 128], onesb[0:96, 0:1],
                            r=[sq, cstb], w=[pn])
                self.S.op("dve", lambda e, o=mx.ap[:, which:which + 1], a=pn.ap[:, which * 32:which * 32 + ntile]: e.reduce_max(o, a, axis=AX.X),
                          [pn], [mx])
            for which in range(2):
                self.TR(pn.ap[0:1, 64 + which * 128:64 + (which + 1) * 128], mx.ap[:, which:which + 1], identf, r=[mx, cst], w=[pn])
                self.S.op("dve", lambda e, o=mx.ap[0:1, 2 + which:3 + which], a=pn.ap[0:1, 64 + which * 128:64 + (which + 1) * 128]:
                          e.reduce_max(o, a, axis=AX.X), [pn], [mx])
            self.TT("dve", mx.ap[0:1, 4:5], mx.ap[0:1, 2:3], mx.ap[0:1, 3:4], ALU.mult, r=[mx], w=[mx])
            self.ACT(mx.ap[0:1, 5:6], mx.ap[0:1, 4:5], AF.Ln, r=[mx], w=[mx], bias=self.eps_ap[0:1, :])
            self.ACT(mx.ap[0:1, 6:7], mx.ap[0:1, 5:6], AF.Exp, r=[mx], w=[mx], scale=0.5)
            self.MM(pn.ap[:, 400:401], onesf[0:1, :], mx.ap[0:1, 6:7], r=[cst, mx], w=[pn])
            self.TS("dve", mx.ap[:, 7:8], pn.ap[:, 400:401], -1.02 * MLA_SCALE, None, ALU.mult, r=[pn], w=[mx])

        def tail(h, qb):
            po = P[6 + qb % 2]
            dr, lo = (64, 0) if h < 4 else (0, 64)
            self.CP("act", rden.ap[dr:dr + 1, :], po.ap[dr:dr + 1, :], r=[po], w=[rden])
            pbc = P[2]
            self.MM(pbc.ap[lo:lo + 64, :], onesf[dr:dr + 1, 0:64], rden.ap[dr:dr + 1, :], r=[cst, rden], w=[pbc])
            self.S.op("dve", lambda e, o=bcs.ap[lo:lo + 64, :], a=pbc.ap[lo:lo + 64, :]: e.reciprocal(o, a), [pbc], [bcs])
            self.TT("dve", omlaT.ap[lo:lo + 64, h % 4, qb * 512:(qb + 1) * 512], po.ap[lo:lo + 64, :], bcs.ap[lo:lo + 64, :], ALU.mult,
                    r=[po, bcs], w=[omlaT])
        build(0)
        build_stab(0)
        pend = None
        for h in range(8):
            kTh, qTh = kThs[h % 2], qThs[h % 2]
            if h + 1 < 8:
                build(h + 1)
            for qb in range(4):
                po = P[6 + qb % 2]

                def qk(kt, qb=qb, kTh=kTh, qTh=qTh):
                    self.MM(P[4 + kt % 2].ap[:, :], kTh.ap[0:96, kt * 128:(kt + 1) * 128], qTh.ap[0:96, qb * 512:(qb + 1) * 512],
                            r=[kTh, qTh], w=[P[4 + kt % 2]])
                qk(0)
                for kt in range(NT):
                    psb = P[4 + kt % 2]
                    p_ = pT[kt % 3]
                    if kt + 1 < NT:
                        qk(kt + 1)
                    self.ACT(p_.ap[:, :], psb.ap[:, :], AF.Exp, r=[psb, mxs[h % 2]], w=[p_], scale=MLA_SCALE, bias=mxs[h % 2].ap[:, 7:8])
                    self.MM(po.ap[:, :], v.ap[:, kt, h, :], p_.ap[:, :], start=(kt == 0), stop=(kt == NT - 1), r=[v, p_], w=[po])
                    if kt == 2 and pend is not None:
                        tail(*pend)
                        pend = None
                    if kt == 9 and qb == 2 and h + 1 < 8:
                        build_stab(h + 1)
                pend = (h, qb)
        tail(*pend)
        kTh, qTh = kThs[0], qThs[0]
        S.release(kThs[1], qThs[1], sqb, *mxs)
        S.release(wuq, wuqs, wukv, zdqnT, ckvT, krT, v, kTh, qTh, rden, bcs, rt, *pT, *rp)

    def phase_gla(self):
        S, P = self.S, self.P
        hT, cstb, cst, stat = self.hT, self.cstb, self.cst, self.stat
        TB_ALL = [(0, 256), (256, 512), (768, 512), (1280, 512), (1792, 512)]
        TB_X = TB_ALL[1:]
        wA = S.alloc("wA", [KC, 1056], BF16)
        self.load_w(wA.ap[:, :, 0:1024], wA, self.w_in[:, 0:1024], 1024, 1024)
        self.load_w(wA.ap[:, :, 1024:1056], wA, self.w_in[:, 1536:1568], 1024, 32)
        wzg = S.alloc("wzg", [KC, 512], BF16)
        self.load_w(wzg.ap[:, :, :], wzg, self.w_in[:, 1024:1536], 1024, 512)
        wa2b = S.alloc("wa2b", [1024], BF16)
        self.load_w(wa2b.ap[:, 0:512], wa2b, self.wa2, 32, 512)
        self.load_w(wa2b.ap[:, 512:1024], wa2b, self.ba_row, 1, 512)
        gnb = S.alloc("gnb", [512], F32)
        self.DMA("sp", gnb.ap[:, :], self.gnb, w=[gnb])
        self.xsz = S.virt("xsz")
        zt = S.alloc("zt", [D], BF16)
        self.zt = zt
        self.MS("pool", zt.ap[:, :], 0.0, w=[zt])
        for c in range(NSLOT // 128):
            self.DMA("sp", self.xs_d[c * 128:(c + 1) * 128, :], zt.ap[:, :], r=[zt], w=[self.xsz], sem=zt)
        gqT = S.alloc("gqT", [2, T], BF16)
        gkT = S.alloc("gkT", [2, NTOK], BF16)
        gk = S.alloc("gk", [NT, 256], BF16)
        gv = S.alloc("gv", [NT, 512], BF16)
        zaT = S.alloc("zaT", [NTOK], BF16)
        n = 0
        for (c0, nn) in TB_ALL:
            for m in range(4):
                if m < 2 and c0 < TC:
                    continue
                pb = P[n % 2]
                n += 1
                for kc in range(KC):
                    self.MM(pb.ap[:, 0:nn], wA.ap[:, kc, m * 128:(m + 1) * 128], hT.ap[:, kc, c0:c0 + nn],
                            start=(kc == 0), stop=(kc == KC - 1), r=[wA, hT], w=[pb])
                if m < 2:
                    self.CP("act", gqT.ap[:, m, c0 - TC:c0 - TC + nn], pb.ap[:, 0:nn], r=[pb], w=[gqT])
                else:
                    self.CP("dve", gkT.ap[:, m - 2, c0:c0 + nn], pb.ap[:, 0:nn], r=[pb], w=[gkT])
            pb = P[n % 2]
            n += 1
            for kc in range(KC):
                self.MM(pb.ap[0:32, 0:nn], wA.ap[:, kc, 1024:1056], hT.ap[:, kc, c0:c0 + nn],
                        start=(kc == 0), stop=(kc == KC - 1), r=[wA, hT], w=[pb])
            self.CP("act", zaT.ap[0:32, c0:c0 + nn], pb.ap[0:32, 0:nn], r=[pb], w=[zaT])
        for i in range(NT):
            pa, pv = P[2 + i % 2], P[4 + i % 2]
            for kc in range(KC):
                self.MM(pa.ap[:, 0:256], hT.ap[:, kc, i * 128:(i + 1) * 128], wA.ap[:, kc, 256:512],
                        start=(kc == 0), stop=(kc == KC - 1), r=[wA, hT], w=[pa])
            for kc in range(KC):
                self.MM(pv.ap[:, :], hT.ap[:, kc, i * 128:(i + 1) * 128], wA.ap[:, kc, 512:1024],
                        start=(kc == 0), stop=(kc == KC - 1), r=[wA, hT], w=[pv])
            self.CP("act", gk.ap[:, i, :], pa.ap[:, 0:256], r=[pa], w=[gk])
            self.CP("dve", gv.ap[:, i, :], pv.ap[:, :], r=[pv], w=[gv])
        S.release(wA)
        if self.stage == 2:
            for m in range(2):
                self.dump(m * 2048, gqT, gqT.ap[:, m, :], 2048)
            self.dump(4096, gv, gv.ap[:, 5, :], 512)
            self.dump(4608, gk, gk.ap[:, 5, :], 256)
            self.dump(4864, zaT, zaT.ap[:, 0:2304], 2304)
            self.ogT = gqT
            return

        ogT = S.alloc("ogT", [4, T], BF16)
        self.ogT = ogT
        hist = S.alloc("hist", [32, 256], BF16)
        Sst = [S.alloc("Sst", [256], F32) for _ in range(2)]
        Sbr = [S.alloc("Sbr", [256], BF16) for _ in range(2)]
        R = 2
        la_b = [S.alloc("la", [256], F32) for _ in range(R)]
        lt_b = [S.alloc("lt", [256], F32) for _ in range(R)]
        ET_b = [S.alloc("ET", [256], F32) for _ in range(R)]
        EI_b = [S.alloc("EI", [256], F32) for _ in range(R)]
        KS_b = [S.alloc("KS", [256], F32) for _ in range(R)]
        qd_b = [S.alloc("qd", [2, 2, 128], BF16) for _ in range(2)]
        qt_b = [S.alloc("qt", [2, 128], F32) for _ in range(2)]
        ki_b = [S.alloc("ki", [2, 128], BF16) for _ in range(R)]
        kte_b = [S.alloc("kte", [256], BF16) for _ in range(R)]
        att_b = [S.alloc("att", [512], BF16) for _ in range(R)]
        ot_b = S.alloc("ot", [512], F32)
        sq_b = S.alloc("sq", [512], F32)
        sz_b = S.alloc("sz", [512], F32)
        og_b = S.alloc("og", [512], BF16)
        self.cnt = 0
        tri, maskb, onesb, identb = self.tri, self.maskb, self.onesb, self.identb
        for d in range(2):
            self.MS("pool", Sst[d].ap[:, :], 0.0, w=[Sst[d]])
        self.MS("pool", Sbr[0].ap[:, :], 0.0, w=[Sbr[0]])

        def prep_gen(i, d, want_q, c, out):
            r = c % R
            pl, pc = P[0 + c % 2], P[2 + c % 2]
            la, lt, ET, EI, KS = la_b[r], lt_b[r], ET_b[r], EI_b[r], KS_b[r]
            self.MM(pl.ap[:, 0:256], zaT.ap[0:32, i * 128:(i + 1) * 128], wa2b.ap[0:32, d * 256:(d + 1) * 256],
                    start=True, stop=False, r=[zaT, wa2b], w=[pl])
            self.MM(pl.ap[:, 0:256], onesb[0:1, :], wa2b.ap[0:1, 512 + d * 256:512 + (d + 1) * 256],
                    start=False, stop=True, r=[cstb, wa2b], w=[pl])
            yield
            self.ACT(lt.ap[:, :], pl.ap[:, 0:256], AF.Abs, r=[pl], w=[lt])
            self.TS("dve", la.ap[:, :], pl.ap[:, 0:256], 0.0, 1.0 / 16, ALU.min, ALU.mult, r=[pl], w=[la])
            yield
            self.ACT(lt.ap[:, :], lt.ap[:, :], AF.Exp, r=[lt], w=[lt], scale=-1.0)
            yield
            self.ACT(lt.ap[:, :], lt.ap[:, :], AF.Ln, r=[lt], w=[lt], bias=self.onesf[:, 0:1])
            yield
            self.STT(la.ap[:, :], lt.ap[:, :], -1.0 / 16, la.ap[:, :], ALU.mult, ALU.add, r=[lt, la], w=[la])
            yield
            for j in range(2):
                self.MM(pc.ap[:, j * 128:(j + 1) * 128], la.ap[:, j * 128:(j + 1) * 128], tri[:, d, :], r=[la, cst], w=[pc])
            self.MM(pc.ap[:, 256:512], tri[:, 2 + d, :], la.ap[:, :], r=[la, cst], w=[pc])
            yield
            self.ACT(ET.ap[:, :], pc.ap[:, 0:256], AF.Exp, r=[pc], w=[ET])
            self.ACT(KS.ap[:, :], pc.ap[:, 256:512], AF.Exp, r=[pc], w=[KS])
            kte = kte_b[r]
            out.update({"pl": pl, "pc": pc, "ET": ET, "kte": kte})
            if want_q:
                self.ACT(EI.ap[:, :], pc.ap[:, 0:256], AF.Exp, r=[pc], w=[EI], scale=-1.0)
            yield
            self.TT("dve", kte.ap[:, :], gk.ap[:, i, :], KS.ap[:, :], ALU.mult, r=[gk, KS], w=[kte])
            if want_q:
                xc = (i - 2) * 128
                qd = qd_b[c % 2]
                ki = ki_b[r]
                qt = qt_b[c % 2]
                self.STT(qt.ap[:, :, :], gqT.ap[:, :, xc:xc + 128], 0.125, ET.ap[:, :].rearrange("p (a b) -> p a b", b=128),
                         ALU.mult, ALU.mult, r=[gqT, ET], w=[qt])
                yield
                self.TS("dve", qd.ap[:, 0, :, :], qt.ap[:, :, :], self.rowmask[:, 0:1], None, ALU.mult, r=[qt, cst], w=[qd])
                self.ACT(qd.ap[:, 1, :, :], qt.ap[:, :, :], AF.Copy, r=[qt, cst], w=[qd], scale=self.rowmask[:, 1:2])
                self.TT("pool", ki.ap[:, :, :], gkT.ap[:, :, i * 128:(i + 1) * 128],
                        EI.ap[:, :].rearrange("p (a b) -> p a b", b=128), ALU.mult, r=[gkT, EI], w=[ki])
                out["qd"], out["ki"] = qd, ki

        def prep_multi(specs):
            outs = [dict() for _ in specs]
            gens = []
            for (i, d, wq), o in zip(specs, outs):
                gens.append(prep_gen(i, d, wq, self.cnt, o))
                self.cnt += 1
            live_g = list(gens)
            while live_g:
                for g in list(live_g):
                    try:
                        next(g)
                    except StopIteration:
                        live_g.remove(g)
            return outs

        def prep(i, d, want_q):
            return prep_multi([(i, d, want_q)])[0]

        def ds_update(i, d, b, cs):
            pl, ET, kte = b["pl"], b["ET"], b["kte"]
            for h in range(4):
                self.MM(pl.ap[(h % 2) * 64:(h % 2) * 64 + 64, 256 + (h // 2) * 128:256 + (h // 2) * 128 + 128],
                        kte.ap[cs:cs + 64, h * 64:(h + 1) * 64], gv.ap[cs:cs + 64, i, h * 128:(h + 1) * 128],
                        r=[kte, gv], w=[pl])
            tl = cs + 63 if d == 0 else cs
            for j in range(2):
                self.STT(Sst[d].ap[:, j * 128:(j + 1) * 128], Sst[d].ap[:, j * 128:(j + 1) * 128],
                         ET.ap[:, j * 128 + tl:j * 128 + tl + 1], pl.ap[:, 256 + j * 128:256 + (j + 1) * 128],
                         ALU.mult, ALU.add, r=[Sst[d], ET, pl], w=[Sst[d]])

        order_b = [1, 0] + list(range(17, 1, -1))
        for k in range(0, len(order_b), 2):
            pair = order_b[k:k + 2]
            bs = prep_multi([(i, 1, False) for i in pair])
            for i, b in zip(pair, bs):
                for cs in (64, 0):
                    if i >= 2:
                        ch = (i - 2) * 2 + (1 if cs == 64 else 0)
                        self.CP("act", hist.ap[:, ch, :], Sst[1].ap[:, :], r=[Sst[1]], w=[hist])
                    ds_update(i, 1, b, cs)

        if self.stage == 25:
            S.release(ot_b, sq_b, sz_b)
            self.dump(0, hist, hist.ap[:, 31, :], 256)
            self.dump(256, hist, hist.ap[:, 0, :], 256)
            return
        live = 0
        ctxp = prep_multi([(0, 0, False), (1, 0, False)])
        for i in range(NT):
            if i < 2:
                bf = ctxp[i]
                for cs in (0, 64):
                    ds_update(i, 0, bf, cs)
                if i == 1:
                    self.CP("act", Sbr[0].ap[:, :], Sst[0].ap[:, :], r=[Sst[0]], w=[Sbr[0]])
                continue
            bf, bb = prep_multi([(i, 0, True), (i, 1, True)])
            xi = i - 2
            xc = xi * 128
            pos = (P[6], P[7])
            pss = (P[4], P[5])
            first = [True, True]
            acol = lambda h: ((h % 2) * 2 + h // 2) * 128
            for d, b in ((0, bf), (1, bb)):
                att = att_b[d]
                qd, ki = b["qd"], b["ki"]
                for h in range(4):
                    hp = (h % 2) * 64
                    self.MM(pss[h % 2].ap[:, (h // 2) * 128:(h // 2) * 128 + 128], ki.ap[:, h // 2, :],
                            qd.ap[:, h % 2, h // 2, :], r=[ki, qd], w=[pss[h % 2]])
                for p in range(2):
                    self.TT("dve", att.ap[:, p * 256:(p + 1) * 256], pss[p].ap[:, 0:256], maskb[:, d, 0:256], ALU.mult,
                            r=[pss[p], cstb], w=[att])
                for h in range(4):
                    self.MM(pos[h % 2].ap[:, (h // 2) * 128:(h // 2) * 128 + 128], att.ap[:, acol(h):acol(h) + 128],
                            gv.ap[:, i, h * 128:(h + 1) * 128], start=first[h % 2], stop=False, r=[att, gv], w=[pos[h % 2]])
                    first[h % 2] = False
            qd = bb["qd"]
            for cs in (64, 0):
                ch = xi * 2 + (1 if cs == 64 else 0)
                for h in range(4):
                    hp = (h % 2) * 64
                    self.MM(pos[h % 2].ap[cs:cs + 64, (h // 2) * 128:(h // 2) * 128 + 128], qd.ap[:, h % 2, h // 2, cs:cs + 64],
                            hist.ap[:, ch, (h // 2) * 128:(h // 2) * 128 + 128], start=False, stop=False,
                            r=[qd, hist], w=[pos[h % 2]])
            qd = bf["qd"]
            for cs in (0, 64):
                sb = Sbr[live % 2]
                for h in range(4):
                    hp = (h % 2) * 64
                    self.MM(pos[h % 2].ap[cs:cs + 64, (h // 2) * 128:(h // 2) * 128 + 128], qd.ap[:, h % 2, h // 2, cs:cs + 64],
                            sb.ap[:, (h // 2) * 128:(h // 2) * 128 + 128], start=False, stop=(cs == 64 and h >= 2),
                            r=[qd, sb], w=[pos[h % 2]])
                ds_update(i, 0, bf, cs)
                live += 1
                self.CP("act", Sbr[live % 2].ap[:, :], Sst[0].ap[:, :], r=[Sst[0]], w=[Sbr[live % 2]])
            pz = bf["pc"]
            for kc in range(KC):
                self.MM(pz.ap[:, :], hT.ap[:, kc, i * 128:(i + 1) * 128], wzg.ap[:, kc, :], start=(kc == 0), stop=(kc == KC - 1),
                        r=[hT, wzg], w=[pz])
            self.ACT(sz_b.ap[:, :], pz.ap[:, :], AF.Silu, r=[pz], w=[sz_b])
            for h in range(4):
                self.CP("act" if h % 2 == 0 else "dve", ot_b.ap[:, h * 128:(h + 1) * 128],
                        pos[h % 2].ap[:, (h // 2) * 128:(h // 2) * 128 + 128], r=[pos[h % 2]], w=[ot_b])
            self.TT("dve", sq_b.ap[:, :], ot_b.ap[:, :], ot_b.ap[:, :], ALU.mult, r=[ot_b], w=[sq_b])
            st4 = stat.ap[:, 16:20]
            st4b = stat.ap[:, 20:24]
            self.S.op("dve", lambda e, o=st4, a=sq_b.ap[:, :].rearrange("p (a b) -> p a b", b=128): e.reduce_sum(o, a, axis=AX.X),
                      [sq_b], [stat])
            self.ACT(st4b, st4, AF.Ln, r=[stat], w=[stat], scale=1.0 / 128, bias=self.eps_ap)
            self.ACT(st4, st4b, AF.Exp, r=[stat], w=[stat], scale=-0.5)
            self.TT("dve", sq_b.ap[:, :].rearrange("p (a b) -> p a b", b=128), ot_b.ap[:, :].rearrange("p (a b) -> p a b", b=128),
                    st4.unsqueeze(2).to_broadcast([128, 4, 128]), ALU.mult, r=[ot_b, stat], w=[sq_b])
            self.TT("dve", sq_b.ap[:, :], sq_b.ap[:, :], gnb.ap[:, :], ALU.mult, r=[sq_b, gnb], w=[sq_b])
            self.TT("dve", og_b.ap[:, :], sq_b.ap[:, :], sz_b.ap[:, :], ALU.mult, r=[sq_b, sz_b], w=[og_b])
            pt = P[4]
            ptb = pt.ap.bitcast(BF16)
            for h in range(4):
                self.TR(ptb[:, h * 128:(h + 1) * 128], og_b.ap[:, h * 128:(h + 1) * 128], identb, r=[og_b, cstb], w=[pt])
            self.CP("act", ogT.ap[:, :, xc:xc + 128], ptb[:, 0:512].rearrange("p (a b) -> p a b", b=128), r=[pt], w=[ogT])
        S.release(gqT, gkT, gk, gv, zaT, hist, wzg, wa2b, gnb, ot_b, sq_b, sz_b, og_b,
                  *Sst, *Sbr, *la_b, *lt_b, *ET_b, *EI_b, *KS_b, *qd_b, *qt_b, *ki_b, *kte_b, *att_b)

    def phase_merge(self):
        S, P = self.S, self.P
        hT, ogT, omlaT, stat, cst, cstb = self.hT, self.ogT, self.omlaT, self.stat, self.cst, self.cstb
        mT = S.alloc("mT", [KC, T], BF16)
        wbg = S.alloc("wbg", [4, D], BF16)
        self.load_w(wbg.ap[:, :, :], wbg, self.w_br_gla, 512, D)
        wbm = S.alloc("wbm", [4, D], BF16)
        src = self.w_br_mla.rearrange("(k p) n -> p k n", p=64)
        self.DMA("pool", wbm.ap[0:64, :, :], src[:, 0:4, :], w=[wbm])
        self.DMA("pool", wbm.ap[64:128, :, :], src[:, 4:8, :], w=[wbm])
        wz = [S.alloc("wz", [KC, 256], BF16) for _ in range(2)]
        sg = [S.alloc("sg", [512], F32) for _ in range(2)]
        sm = [S.alloc("sm", [512], F32) for _ in range(2)]
        n = 0
        for m in range(KC):
            w_ = wz[m % 2]
            self.load_w(w_.ap[:, :, 0:128], w_, self.w_in[:, 1984 + m * 128:1984 + (m + 1) * 128], 1024, 128)
            self.load_w(w_.ap[:, :, 128:256], w_, self.w_in[:, 3008 + m * 128:3008 + (m + 1) * 128], 1024, 128)
            for tb in range(4):
                c0, xc0 = TC + tb * 512, tb * 512
                o = (n % 2) * 4
                sg_, sm_ = sg[n % 2], sm[n % 2]
                n += 1
                for kc in range(KC):
                    self.MM(P[o].ap[:, :], w_.ap[:, kc, 0:128], hT.ap[:, kc, c0:c0 + 512], start=(kc == 0), stop=(kc == KC - 1),
                            r=[w_, hT], w=[P[o]])
                for kc in range(KC):
                    self.MM(P[o + 1].ap[:, :], w_.ap[:, kc, 128:256], hT.ap[:, kc, c0:c0 + 512], start=(kc == 0), stop=(kc == KC - 1),
                            r=[w_, hT], w=[P[o + 1]])
                for hc in range(4):
                    self.MM(P[o + 2].ap[:, :], wbg.ap[:, hc, m * 128:(m + 1) * 128], ogT.ap[:, hc, xc0:xc0 + 512],
                            start=(hc == 0), stop=(hc == 3), r=[wbg, ogT], w=[P[o + 2]])
                for j in range(4):
                    self.MM(P[o + 3].ap[:, :], wbm.ap[:, j, m * 128:(m + 1) * 128], omlaT.ap[:, j, xc0:xc0 + 512],
                            start=(j == 0), stop=(j == 3), r=[wbm, omlaT], w=[P[o + 3]])
                self.ACT(sg_.ap[:, :], P[o].ap[:, :], AF.Sigmoid, r=[P[o]], w=[sg_])
                self.ACT(sm_.ap[:, :], P[o + 1].ap[:, :], AF.Sigmoid, r=[P[o + 1]], w=[sm_])
                self.TT("dve", sg_.ap[:, :], sg_.ap[:, :], P[o + 2].ap[:, :], ALU.mult, r=[sg_, P[o + 2]], w=[sg_])
                self.TT("dve", sm_.ap[:, :], sm_.ap[:, :], P[o + 3].ap[:, :], ALU.mult, r=[sm_, P[o + 3]], w=[sm_])
                self.TT("pool", mT.ap[:, m, xc0:xc0 + 512], sg_.ap[:, :], sm_.ap[:, :], ALU.add, r=[sg_, sm_], w=[mT])
        S.release(hT, ogT, omlaT, wbg, wbm, *wz, *sg, *sm)
        wo = S.alloc("wo", [KC, D], BF16)
        self.load_w(wo.ap[:, :, :], wo, self.w_out, D, D)
        rw = S.alloc("rw", [KC * NE + NE], F32)
        rwv = rw.ap[:, 0:KC * NE].rearrange("p (a b) -> p a b", b=NE)
        self.DMA("sp", rwv, self.router_w.rearrange("(k p) n -> p k n", p=128), w=[rw])
        self.DMA("sp", rw.ap[0:1, KC * NE:KC * NE + NE], self.router_b, w=[rw])
        comb = S.alloc("comb", [16, NE], F32)
        self.comb = comb
        rt_ = S.alloc("rt", [176 + 144], F32)
        self.DMA("sp", rt_.ap[:, 0:176], self.c_rt, w=[rt_])
        tsub = S.alloc("tsub", [128], BF16)
        self.CP("pool", tsub.ap[:, :], rt_.ap[:, 0:128], r=[rt_], w=[tsub])
        Umat = rt_.ap[:, 128:160]
        IO = rt_.ap[:, 160:176]
        cb = rt_.ap[:, 176:208]
        posf4 = rt_.ap[:, 208:212]
        oh = rt_.ap[:, 224:256]
        oh2 = rt_.ap[:, 256:288]
        posf = rt_.ap[:, 288:320]
        self.MS("pool", rt_.ap[:, 176:320], 0.0, w=[rt_])
        wk = S.alloc("wk", [16, 4], F32)
        posi = S.alloc("posi", [16, 4], F32)
        posi_v = posi.ap.bitcast(I32)
        self.wk, self.posi, self.posi_v = wk, posi, posi_v
        maskb16 = S.alloc("maskb16", [NE], BF16)
        rank_all = S.alloc("rank", [16, NE], F32)
        lgall = S.alloc("lgall", [16, 40], F32)
        xn2tm = S.alloc("xn2tm", [16, D], BF16)
        xsz, xsd = self.xsz, S.virt("xsd")
        self.xsd, self.ysd = xsd, S.virt("ysd")
        xt = [S.alloc("xt2", [D], F32) for _ in range(2)]
        tt = [S.alloc("tt2", [D], F32) for _ in range(2)]
        hx2fs = [S.alloc("hx2f", [KC, 128], F32) for _ in range(2)]
        lgs = [S.alloc("lg", [128], F32) for _ in range(2)]
        junk = S.alloc("junk3", [D], BF16)
        Gb, A2, modb, identf, onesf = self.Gb, self.A2, self.modb, self.identf, self.onesf
        for xi in range(16):
            pa, pb = P[(xi % 2) * 2], P[(xi % 2) * 2 + 1]
            x_, t_ = xt[xi % 2], tt[xi % 2]
            hx2f, lg = hx2fs[xi % 2], lgs[xi % 2]
            self.DMA("sp", x_.ap[:, :], self.x[xi * 128:(xi + 1) * 128, :], w=[x_])
            for hf, pp in enumerate((pa, pb)):
                for m in range(KC):
                    self.MM(pp.ap[:, :], mT.ap[:, m, xi * 128:(xi + 1) * 128], wo.ap[:, m, hf * 512:(hf + 1) * 512],
                            start=(m == 0), stop=(m == KC - 1), r=[mT, wo], w=[pp])
            sc = 24 + (xi % 2) * 8
            for hf, pp in enumerate((pa, pb)):
                self.ACT(junk.ap[:, 0:512], pp.ap[:, :], AF.Square, r=[pp], w=[junk, stat], accum_out=stat.ap[:, sc + hf:sc + hf + 1])
            self.TT("dve", stat.ap[:, sc:sc + 1], stat.ap[:, sc:sc + 1], stat.ap[:, sc + 1:sc + 2], ALU.add, r=[stat], w=[stat])
            self.rstd_from_ss(stat.ap[:, sc:sc + 1], D, stat, stat.ap[:, sc + 2:sc + 3])
            for hf, pp in enumerate((pa, pb)):
                self.STT(t_.ap[:, hf * 512:(hf + 1) * 512], pp.ap[:, :], stat.ap[:, sc:sc + 1], Gb.ap[:, 0, hf * 512:(hf + 1) * 512],
                         ALU.mult, ALU.mult, r=[pp, stat, Gb], w=[t_])
            self.TT("pool", x_.ap[:, :], t_.ap[:, :], x_.ap[:, :], ALU.add, r=[t_, x_], w=[x_])
            self.DMA("pool", self.x1d[xi * 128:(xi + 1) * 128, :], x_.ap[:, :], r=[x_], sem=x_)
            self.ACT(junk.ap[:, :], x_.ap[:, :], AF.Square, r=[x_], w=[junk, stat], accum_out=stat.ap[:, sc + 3:sc + 4])
            self.rstd_from_ss(stat.ap[:, sc + 3:sc + 4], D, stat, stat.ap[:, sc + 4:sc + 5])
            self.TS("dve", t_.ap[:, :], x_.ap[:, :], stat.ap[:, sc + 3:sc + 4], None, ALU.mult, r=[x_, stat], w=[t_])
            for kc in range(KC):
                pt = P[4 + kc // 4]
                self.TR(pt.ap[:, (kc % 4) * 128:(kc % 4 + 1) * 128], t_.ap[:, kc * 128:(kc + 1) * 128], identf, r=[t_, cst], w=[pt])
            for kc in range(KC):
                pt = P[4 + kc // 4]
                src_ = pt.ap[:, (kc % 4) * 128:(kc % 4 + 1) * 128]
                if kc % 2 == 0:
                    self.TS("dve", hx2f.ap[:, kc, :], src_, A2[:, kc:kc + 1], modb.ap[:, 24 + kc, 0:1], ALU.mult, ALU.add,
                            r=[pt, self.vec, modb], w=[hx2f])
                else:
                    self.ACT(hx2f.ap[:, kc, :], src_, AF.Identity, r=[pt, self.vec, modb], w=[hx2f],
                             scale=A2[:, kc:kc + 1], bias=modb.ap[:, 24 + kc, 0:1])
            pr = P[6 + xi % 2]
            for kc in range(KC):
                self.MM(pr.ap[:, 0:NE], hx2f.ap[:, kc, :], rwv[:, kc, :], start=(kc == 0), stop=False, r=[hx2f, rw], w=[pr])
            self.MM(pr.ap[:, 0:NE], onesf[0:1, :], rw.ap[0:1, KC * NE:KC * NE + NE], start=False, stop=True, r=[cst, rw], w=[pr])
            self.CP("act", lg.ap[:, 0:NE], pr.ap[:, 0:NE], r=[pr], w=[lg])
            self.S.op("dve", lambda e, o=lg.ap[:, 32:40], a=lg.ap[:, 0:NE]: e.max(out=o, in_=a), [lg], [lg])
            self.TS("dve", lg.ap[:, 64:96], lg.ap[:, 0:NE], lg.ap[:, 35:36], None, ALU.is_ge, r=[lg], w=[lg])
            self.TS("dve", lg.ap[:, 40:41], lg.ap[:, 32:33], -1.0, None, ALU.mult, r=[lg], w=[lg])
            self.ACT(lg.ap[:, 96:128], lg.ap[:, 0:NE], AF.Exp, r=[lg], w=[lg], bias=lg.ap[:, 40:41])
            self.TT("dve", lg.ap[:, 96:128], lg.ap[:, 96:128], lg.ap[:, 64:96], ALU.mult, r=[lg], w=[lg])
            self.S.op("dve", lambda e, o=lg.ap[:, 41:42], a=lg.ap[:, 96:128]: e.reduce_sum(o, a, axis=AX.X), [lg], [lg])
            self.S.op("dve", lambda e, o=lg.ap[:, 42:43], a=lg.ap[:, 41:42]: e.reciprocal(o, a), [lg], [lg])
            self.TS("dve", comb.ap[:, xi, :], lg.ap[:, 96:128], lg.ap[:, 42:43], None, ALU.mult, r=[lg], w=[comb])
            self.CP("pool", maskb16.ap[:, :], lg.ap[:, 64:96], r=[lg], w=[maskb16])
            self.MM(pr.ap[:, 64:96], tsub.ap[:, :], maskb16.ap[:, :], r=[tsub, maskb16], w=[pr])
            self.MM(pr.ap[:, 128:160], self.onesb, maskb16.ap[:, :], r=[self.cstb, maskb16], w=[pr])
            self.TT("dve", rank_all.ap[:, xi, :], pr.ap[:, 64:96], cb, ALU.add, r=[pr, rt_], w=[rank_all])
            self.TT("dve", cb, cb, pr.ap[:, 128:160], ALU.add, r=[pr, rt_], w=[rt_])
            self.CP("dve", lgall.ap[:, xi, :], lg.ap[:, 0:40], r=[lg], w=[lgall])
            self.CP("pool", xn2tm.ap[:, xi, :], t_.ap[:, :], r=[t_], w=[xn2tm])
        meta = S.alloc("meta", [32 + 32 + 128 + 512 + 512], F32)
        self.meta = meta
        nt = meta.ap[:, 0:32]
        nt_i = meta.ap[:, 32:64].bitcast(I32)
        ntT = meta.ap[:, 64:192]
        sidxf = meta.ap[:, 192:704].rearrange("p (a b) -> p a b", b=16)
        sidx_i = meta.ap[:, 704:1216].bitcast(I32).rearrange("p (a b) -> p a b", b=16)
        self.nt_i, self.sidx_i = nt_i, sidx_i
        self.MS("pool", nt, 0.0, w=[meta])
        for m in range(NSTEP):
            self.STT(nt, cb, float(SG * m), nt, ALU.is_gt, ALU.add, r=[rt_, meta], w=[meta])
        self.CP("dve", nt_i, nt, r=[meta], w=[meta])
        pq = P[4]
        self.TR(pq.ap[0:NE, 0:128], nt, identf, r=[meta, cst], w=[pq])
        self.CP("act", ntT[0:NE, :], pq.ap[0:NE, 0:128], r=[pq], w=[meta])
        self.MM(pq.ap[:, 128:160], ntT[0:NE, :], Umat[0:NE, :], r=[meta, rt_], w=[pq])
        base = rt_.ap[:, 176:208]
        self.TS("dve", base, pq.ap[:, 128:160], float(SG), None, ALU.mult, r=[pq, rt_], w=[rt_])
        for e in range(NE):
            self.TS("dve", sidxf[:, e, :], IO, base[:, e:e + 1], None, ALU.add, r=[rt_, meta], w=[meta])
        self.CP("dve", sidx_i, sidxf, r=[meta], w=[meta])
        S.release(mT, wo, rw, *hx2fs, *lgs, junk, self.zt, tsub, maskb16, *tt)
        oh4 = S.alloc("oh4", [16, 4, NE], F32)
        pr4 = S.alloc("pr4", [16, 4, NE], F32)
        pf4 = S.alloc("pf4", [16, 4], F32)
        shp = [128, 16, 4, NE]
        self.TT("dve", rank_all.ap[:, :, :], rank_all.ap[:, :, :], base.unsqueeze(1).to_broadcast([128, 16, NE]), ALU.add,
                r=[rank_all, rt_], w=[rank_all])
        self.TT("dve", oh4.ap[:, :, :, :], lgall.ap[:, :, 0:NE].unsqueeze(2).to_broadcast(shp),
                lgall.ap[:, :, 32:36].unsqueeze(3).to_broadcast(shp), ALU.is_equal, r=[lgall], w=[oh4])
        self.TT("dve", pr4.ap[:, :, :, :], oh4.ap[:, :, :, :], rank_all.ap[:, :, :].unsqueeze(2).to_broadcast(shp), ALU.mult,
                r=[oh4, rank_all], w=[pr4])
        self.S.op("dve", lambda e, o=pf4.ap[:, :, :], a=pr4.ap[:, :, :, :]: e.reduce_sum(o, a, axis=AX.X), [pr4], [pf4])
        self.CP("dve", posi_v[:, :, :], pf4.ap[:, :, :], r=[pf4], w=[posi])
        self.TT("pool", pr4.ap[:, :, :, :], oh4.ap[:, :, :, :], comb.ap[:, :, :].unsqueeze(2).to_broadcast(shp), ALU.mult,
                r=[oh4, comb, pf4], w=[pr4])
        self.S.op("dve", lambda e, o=wk.ap[:, :, :], a=pr4.ap[:, :, :, :]: e.reduce_sum(o, a, axis=AX.X), [pr4], [wk])
        for xi in range(16):
            for k in range(4):
                self.S.dma_fn("pool", lambda e, o=self.xs_d[:, :], off=posi_v[:, xi, k:k + 1], i_=xn2tm.ap[:, xi, :]:
                              e.indirect_dma_start(out=o, out_offset=bass.IndirectOffsetOnAxis(ap=off, axis=0), in_=i_, in_offset=None,
                                                   bounds_check=self.S.bc_reg, oob_is_err=False),
                              reads=[xsz, xn2tm, posi], writes=[xsd], sem_buf=xn2tm)
        S.release(oh4, pr4, pf4, rank_all, lgall)
        self.x1_bufs = xt
        self.rt_ = rt_
        self.xn2tm = xn2tm

    def phase_moe(self):
        S, P = self.S, self.P
        hx2T, comb, stat, cst = self.hx2T, self.comb, self.stat, self.cst
        identf, Gb = self.identf, self.Gb
        xt = self.x1_bufs
        combT = S.alloc("combT", [T], F32)
        for xi in range(16):
            pb = P[xi % 2]
            self.TR(pb.ap[0:NE, 0:128], comb.ap[:, xi, :], identf, r=[comb, cst], w=[pb])
            self.CP("act", combT.ap[0:NE, xi * 128:(xi + 1) * 128], pb.ap[0:NE, 0:128], r=[pb], w=[combT])
        S.release(comb)
        bias = S.alloc("ebias", [512 + D], F32)
        self.DMA("sp", bias.ap[:, 0:256], self.bgT, w=[bias])
        self.DMA("sp", bias.ap[:, 256:512], self.buT, w=[bias])
        self.DMA("sp", bias.ap[0:NE, 512:512 + D], self.b_down, w=[bias])
        self.TS("dve", bias.ap[:, 256:512], bias.ap[:, 256:512], 1.0, None, ALU.add, r=[bias], w=[bias])
        wg = S.alloc("wg", [KC, D], BF16)
        wu = S.alloc("wu", [KC, D], BF16)
        wd = S.alloc("wd", [KC, D], BF16)
        act = S.alloc("act", [KC, 512], BF16)
        oacc = S.alloc("oacc", [KC, 1024], F32)
        sel = [S.alloc("sel", [128], F32) for _ in range(2)]
        cwb = S.alloc("cw", [512], F32)
        gb_ = [S.alloc("g", [512], F32) for _ in range(2)]
        sb_ = [S.alloc("s", [512], F32) for _ in range(1)]
        ub_ = [S.alloc("u", [512], F32) for _ in range(2)]
        junk = S.alloc("junk4", [512], BF16)
        selv = self.c_sel
        out_bufs = []
        for half in range(2):
            t0 = half * 1024
            for dc in range(KC):
                for tb in range(2):
                    pb = P[4 + (dc * 2 + tb) % 2]
                    self.MM(pb.ap[:, :], bias.ap[0:NE, 512 + dc * 128:512 + (dc + 1) * 128], combT.ap[0:NE, t0 + tb * 512:t0 + (tb + 1) * 512],
                            r=[bias, combT], w=[pb])
                    self.CP("act", oacc.ap[:, dc, tb * 512:(tb + 1) * 512], pb.ap[:, :], r=[pb], w=[oacc])
            for e in range(NE):
                self.load_w(wg.ap[:, :, :], wg, self.w_gate[e], D, D)
                self.load_w(wu.ap[:, :, :], wu, self.w_up[e], D, D)
                self.load_w(wd.ap[:, :, :], wd, self.w_down[e], D, D)
                se = sel[e % 2]
                self.DMA("sp", se.ap[0:NE, :], selv[:, e * 128:(e + 1) * 128], w=[se])
                for tb in range(2):
                    tk = t0 + tb * 512
                    self.MM(P[6].ap[:, :], se.ap[0:NE, :], combT.ap[0:NE, tk:tk + 512], r=[se, combT], w=[P[6]])
                    self.CP("act", cwb.ap[:, :], P[6].ap[:, :], r=[P[6]], w=[cwb])
                    for f in range(KC):
                        pg, pu = P[f % 2], P[2 + f % 2]
                        g_, s_, u_ = gb_[f % 2], sb_[0], ub_[f % 2]
                        for kc in range(KC):
                            self.MM(pg.ap[:, :], wg.ap[:, kc, f * 128:(f + 1) * 128], hx2T.ap[:, kc, tk:tk + 512],
                                    start=(kc == 0), stop=(kc == KC - 1), r=[wg, hx2T], w=[pg])
                        for kc in range(KC):
                            self.MM(pu.ap[:, :], wu.ap[:, kc, f * 128:(f + 1) * 128], hx2T.ap[:, kc, tk:tk + 512],
                                    start=(kc == 0), stop=(kc == KC - 1), r=[wu, hx2T], w=[pu])
                        bcol = e * 8 + f
                        self.TS("dve", g_.ap[:, :], pg.ap[:, :], bias.ap[:, bcol:bcol + 1], 7.0, ALU.add, ALU.min, r=[pg, bias], w=[g_])
                        self.ACT(s_.ap[:, :], g_.ap[:, :], AF.Sigmoid, r=[g_], w=[s_], scale=1.702)
                        self.TS("dve", u_.ap[:, :], pu.ap[:, :], bias.ap[:, 256 + bcol:256 + bcol + 1], -6.0, ALU.add, ALU.max,
                                r=[pu, bias], w=[u_])
                        self.STT(u_.ap[:, :], u_.ap[:, :], 8.0, cwb.ap[:, :], ALU.min, ALU.mult, r=[u_, cwb], w=[u_])
                        self.TT("pool", g_.ap[:, :], g_.ap[:, :], s_.ap[:, :], ALU.mult, r=[g_, s_], w=[g_])
                        self.TT("pool", act.ap[:, f, :], g_.ap[:, :], u_.ap[:, :], ALU.mult, r=[g_, u_], w=[act])
                    for dc in range(KC):
                        pd = P[4 + dc % 2]
                        for f in range(KC):
                            self.MM(pd.ap[:, :], wd.ap[:, f, dc * 128:(dc + 1) * 128], act.ap[:, f, :],
                                    start=(f == 0), stop=(f == KC - 1), r=[wd, act], w=[pd])
                        self.TT("dve", oacc.ap[:, dc, tb * 512:(tb + 1) * 512], oacc.ap[:, dc, tb * 512:(tb + 1) * 512], pd.ap[:, :],
                                ALU.add, r=[oacc, pd], w=[oacc])
            for j in range(8):
                xi = half * 8 + j
                x_ = xt[xi % 2]
                self.DMA("sp", x_.ap[:, :], self.x1d[xi * 128:(xi + 1) * 128, :], w=[x_])
                pa, pb = P[(j % 2) * 2], P[(j % 2) * 2 + 1]
                for dc in range(KC):
                    pp = pa if dc < 4 else pb
                    self.TR(pp.ap[:, (dc % 4) * 128:(dc % 4 + 1) * 128], oacc.ap[:, dc, j * 128:(j + 1) * 128], identf,
                            r=[oacc, cst], w=[pp])
                sc = 40 + (j % 2) * 4
                for hf, pp in enumerate((pa, pb)):
                    self.ACT(junk.ap[:, :], pp.ap[:, :], AF.Square, r=[pp], w=[junk, stat], accum_out=stat.ap[:, sc + hf:sc + hf + 1])
                self.TT("dve", stat.ap[:, sc:sc + 1], stat.ap[:, sc:sc + 1], stat.ap[:, sc + 1:sc + 2], ALU.add, r=[stat], w=[stat])
                self.rstd_from_ss(stat.ap[:, sc:sc + 1], D, stat, stat.ap[:, sc + 2:sc + 3])
                for hf, pp in enumerate((pa, pb)):
                    self.STT(cwb.ap[:, :], pp.ap[:, :], stat.ap[:, sc:sc + 1], Gb.ap[:, 1, hf * 512:(hf + 1) * 512], ALU.mult, ALU.mult,
                             r=[pp, stat, Gb], w=[cwb])
                    self.TT("pool", x_.ap[:, hf * 512:(hf + 1) * 512], x_.ap[:, hf * 512:(hf + 1) * 512], cwb.ap[:, :], ALU.add,
                            r=[x_, cwb], w=[x_])
                self.DMA("pool", self.out[xi * 128:(xi + 1) * 128, :], x_.ap[:, :], r=[x_], sem=x_)
        self.final_bufs = list(xt)

    def phase_moe_sparse(self):
        S, P = self.S, self.P
        stat, cst, cstb, Gb = self.stat, self.cst, self.cstb, self.Gb
        identb, onesf, A2, modb = self.identb, self.onesf, self.A2, self.modb
        xsd, ysd, wk, posi_v, posi = self.xsd, self.ysd, self.wk, self.posi_v, self.posi
        meta, nt_i, sidx_i = self.meta, self.nt_i, self.sidx_i
        S.release(self.comb, self.xn2tm)
        bias = S.alloc("ebias", [512], F32)
        self.DMA("sp", bias.ap[:, 0:256], self.bgT, w=[bias])
        self.DMA("sp", bias.ap[:, 256:512], self.buT, w=[bias])
        self.TS("dve", bias.ap[:, 256:512], bias.ap[:, 256:512], 1.0, None, ALU.add, r=[bias], w=[bias])
        W = [[S.alloc("w%d" % k, [KC, D], BF16) for k in range(3)] for _ in range(2)]
        xg0 = [S.alloc("xg0", [2, D], BF16) for _ in range(2)]
        xgn = [S.alloc("xgn", [2, D], BF16) for _ in range(2)]
        xg = xg0 + xgn
        XT = [S.alloc("XT", [KC, SG], BF16) for _ in range(2)]
        acts = [S.alloc("act", [SG], BF16) for _ in range(KC)]
        gb_ = [S.alloc("g", [SG], F32) for _ in range(2)]
        sb_ = [S.alloc("s", [SG], F32) for _ in range(2)]
        ub_ = [S.alloc("u", [SG], F32) for _ in range(2)]
        ys = [S.alloc("ys", [D], F32) for _ in range(2)]
        bdr = [S.alloc("bdr", [D], F32) for _ in range(1)]
        bdrb = [S.alloc("bdrb", [D], BF16) for _ in range(2)]
        srcs = (self.w_gate, self.w_up, self.w_down)

        def prep(x_, xt_):
            for bk in range(2):
                pt = P[4 + bk]
                ptb = pt.ap.bitcast(BF16)
                for k4 in range(4):
                    kc = bk * 4 + k4
                    for h in range(2):
                        self.TR(ptb[:, k4 * SG + h * 128:k4 * SG + (h + 1) * 128], x_.ap[:, h, kc * 128:(kc + 1) * 128], identb,
                                r=[x_, cstb], w=[pt])
                for k4 in range(4):
                    kc = bk * 4 + k4
                    if k4 % 2 == 0:
                        self.TS("dve", xt_.ap[:, kc, :], ptb[:, k4 * SG:(k4 + 1) * SG], A2[:, kc:kc + 1], modb.ap[:, 24 + kc, 0:1],
                                ALU.mult, ALU.add, r=[pt, self.vec, modb], w=[xt_])
                    else:
                        self.ACT(xt_.ap[:, kc, :], ptb[:, k4 * SG:(k4 + 1) * SG], AF.Identity, r=[pt, self.vec, modb], w=[xt_],
                                 scale=A2[:, kc:kc + 1], bias=modb.ap[:, 24 + kc, 0:1])

        def loadw(e):
            for k in range(3):
                self.load_w(W[e % 2][k].ap[:, :, :], W[e % 2][k], srcs[k][e], D, D)
        def gather(buf, e, j):
            for h in range(2):
                self.S.dma_fn("pool", lambda en, o=buf.ap[:, h, :], off=sidx_i[:, e, 2 * j + h:2 * j + h + 1], i_=self.xs_d[:, :]:
                              en.indirect_dma_start(out=o, out_offset=None, in_=i_, in_offset=bass.IndirectOffsetOnAxis(ap=off, axis=0),
                                                    bounds_check=self.S.bc_reg, oob_is_err=False),
                              reads=[xsd, meta], writes=[buf], sem_buf=buf)
        XT0 = [S.alloc("XT0", [KC, SG], BF16) for _ in range(2)]
        gather(xg0[0], 0, 0)
        gather(xg0[1], 1, 0)
        loadw(0)
        prep(xg0[0], XT0[0])
        nst = 0
        ngs = 0
        for e in range(NE):
            gather(xgn[1], e, 1)
            if e + 2 < NE:
                gather(xg0[e % 2], e + 2, 0)
            if e + 1 < NE:
                loadw(e + 1)
            wg, wu, wd = W[e % 2]
            br = bdr[0]
            self.DMA("sp", br.ap[0:1, :], self.b_down[e:e + 1, :], w=[br])
            brb = bdrb[e % 2]
            self.CP("act", brb.ap[0:1, :], br.ap[0:1, :], r=[br], w=[brb])
            if e + 1 < NE:
                prep(xg0[(e + 1) % 2], XT0[(e + 1) % 2])
            S.regload(meta, nt_i[0:1, e:e + 1])
            for j in range(NSTEP):
                S.begin_group(j)
                xt_ = XT0[e % 2] if j == 0 else XT[j % 2]
                ngs += 1
                if j + 2 < NSTEP:
                    gather(xgn[j % 2], e, j + 2)
                for f in range(KC):
                    pg, pu = P[f % 2], P[2 + f % 2]
                    g_, s_, u_ = gb_[f % 2], sb_[f % 2], ub_[f % 2]
                    for kc in range(KC):
                        self.MM(pg.ap[:, 0:SG], wg.ap[:, kc, f * 128:(f + 1) * 128], xt_.ap[:, kc, :],
                                start=(kc == 0), stop=(kc == KC - 1), r=[wg, xt_], w=[pg])
                    for kc in range(KC):
                        self.MM(pu.ap[:, 0:SG], wu.ap[:, kc, f * 128:(f + 1) * 128], xt_.ap[:, kc, :],
                                start=(kc == 0), stop=(kc == KC - 1), r=[wu, xt_], w=[pu])
                    bcol = e * 8 + f
                    self.TS("dve", g_.ap[:, :], pg.ap[:, 0:SG], bias.ap[:, bcol:bcol + 1], 7.0, ALU.add, ALU.min, r=[pg, bias], w=[g_])
                    self.ACT(s_.ap[:, :], g_.ap[:, :], AF.Sigmoid, r=[g_], w=[s_], scale=1.702)
                    self.TS("dve", u_.ap[:, :], pu.ap[:, 0:SG], bias.ap[:, 256 + bcol:256 + bcol + 1], -6.0, ALU.add, ALU.max,
                            r=[pu, bias], w=[u_])
                    self.TT("pool", g_.ap[:, :], g_.ap[:, :], s_.ap[:, :], ALU.mult, r=[g_, s_], w=[g_])
                    self.STT(acts[f].ap[:, :], u_.ap[:, :], 8.0, g_.ap[:, :], ALU.min, ALU.mult, r=[u_, g_], w=[acts[f]])
                if j + 1 < NSTEP:
                    prep(xgn[(j + 1) % 2], XT[(j + 1) % 2])
                for h in range(2):
                    y_ = ys[nst % 2]
                    nst += 1
                    for hf in range(2):
                        pd = P[6 + hf]
                        for f in range(KC):
                            self.MM(pd.ap[:, :], acts[f].ap[:, h * 128:(h + 1) * 128], wd.ap[:, f, hf * 512:(hf + 1) * 512],
                                    start=(f == 0), stop=False, r=[acts[f], wd], w=[pd])
                        self.MM(pd.ap[:, :], self.onesb[0:1, :], brb.ap[0:1, hf * 512:(hf + 1) * 512], start=False, stop=True,
                                r=[cstb, brb], w=[pd])
                        self.CP("act" if hf == 0 else "dve", y_.ap[:, hf * 512:(hf + 1) * 512], pd.ap[:, :], r=[pd], w=[y_])
                    self.S.dma_fn("pool", lambda en, o=self.ys_d[:, :], off=sidx_i[:, e, 2 * j + h:2 * j + h + 1], i_=y_.ap[:, :]:
                                  en.indirect_dma_start(out=o, out_offset=bass.IndirectOffsetOnAxis(ap=off, axis=0), in_=i_, in_offset=None,
                                                        bounds_check=self.S.bc_reg, oob_is_err=False),
                                  reads=[y_, meta], writes=[ysd], sem_buf=y_)
                S.end_group()
        S.release(*W[0], *W[1], *xg, *XT, *XT0, *acts, *gb_, *sb_, *ub_, *bdr, *bdrb, bias)
        NR = 4
        yk = [S.alloc("yk", [D], F32) for _ in range(4 * NR)]
        acc = [S.alloc("acc", [D], F32) for _ in range(2)]
        junk = S.alloc("junk5", [D], BF16)
        xt = self.x1_bufs
        outb = []
        for xi in range(16):
            x_ = xt[xi % 2]
            a_ = acc[xi % 2]
            self.DMA("sp", x_.ap[:, :], self.x1d[xi * 128:(xi + 1) * 128, :], w=[x_])
            for k in range(4):
                y_ = yk[(xi % NR) * 4 + k]
                self.S.dma_fn("pool", lambda e, o=y_.ap[:, :], off=posi_v[:, xi, k:k + 1], i_=self.ys_d[:, :]:
                              e.indirect_dma_start(out=o, out_offset=None, in_=i_, in_offset=bass.IndirectOffsetOnAxis(ap=off, axis=0),
                                                   bounds_check=self.S.bc_reg, oob_is_err=False),
                              reads=[ysd, posi], writes=[y_], sem_buf=y_)
                if k == 0:
                    self.TS("dve", a_.ap[:, :], y_.ap[:, :], wk.ap[:, xi, 0:1], None, ALU.mult, r=[y_, wk], w=[a_])
                else:
                    self.STT(a_.ap[:, :], y_.ap[:, :], wk.ap[:, xi, k:k + 1], a_.ap[:, :], ALU.mult, ALU.add, r=[y_, wk, a_], w=[a_])
            sc = 40 + (xi % 2) * 4
            self.ACT(junk.ap[:, :], a_.ap[:, :], AF.Square, r=[a_], w=[junk, stat], accum_out=stat.ap[:, sc:sc + 1])
            self.rstd_from_ss(stat.ap[:, sc:sc + 1], D, stat, stat.ap[:, sc + 2:sc + 3])
            if self.dbg is not None and xi in (0, 9):
                self.dump((0 if xi == 0 else 1) * 1024, a_, a_.ap[:, :], 1024)
                self.dump(2048 + (0 if xi == 0 else 1) * 8, wk, wk.ap[:, xi, :], 4)
                self.dump(2048 + 16 + (0 if xi == 0 else 1) * 8, posi, posi.ap[:, xi, :], 4)
            self.STT(a_.ap[:, :], a_.ap[:, :], stat.ap[:, sc:sc + 1], Gb.ap[:, 1, :], ALU.mult, ALU.mult, r=[a_, stat, Gb], w=[a_])
            self.TT("dve", x_.ap[:, :], x_.ap[:, :], a_.ap[:, :], ALU.add, r=[x_, a_], w=[x_])
            self.DMA("sp", self.out[xi * 128:(xi + 1) * 128, :], x_.ap[:, :], r=[x_], sem=x_)
        self.final_bufs = list(xt)

    def finish(self):
        S = self.S
        if self.stage < 90:
            z = S.alloc("zout", [128], F32)
            self.MS("pool", z.ap[:, :], 0.0, w=[z])
            self.DMA("pool", self.out[0:128, 0:128], z.ap[:, :], r=[z], sem=z)
            self.dbg_bufs.append(z)
        S.emit(final_bufs=self.dbg_bufs + getattr(self, "final_bufs", []))


def _host_inputs(inputs, b):
    f = lambda a: np.ascontiguousarray(np.asarray(a, dtype=np.float32))
    cst = _consts()
    m = {}
    m["x"] = f(inputs["x"][b])
    m["ctx"] = f(inputs["ctx"][b])
    cc = np.stack([_kc_layout(f(inputs["c"][b])), _kc_layout(f(inputs["c_ctx"]))], axis=-1)
    m["cc"] = f(cc.reshape(128, 16))
    m["w_mod"] = f(inputs["w_mod"][0])
    m["bmodT"] = _kc_layout(f(inputs["b_mod"][0]))
    m["bmod_row"] = f(inputs["b_mod"][0]).reshape(1, -1)
    gT = np.stack([_kc_layout(f(inputs[k][0])) for k in ("g_pre_mix", "g_post_mix", "g_pre_ffn", "g_post_ffn")], axis=1)
    m["gT"] = f(gT.reshape(128, 32))
    m["gpost_row"] = f(np.concatenate([f(inputs["g_post_mix"][0]), f(inputs["g_post_ffn"][0])]).reshape(1, -1))
    m["w_in"] = f(inputs["w_in"][0])
    wa2 = np.zeros((32, 512), np.float32)
    wa2[0:16, 0:256] = inputs["gla_w_a2_f"][0]
    wa2[16:32, 256:512] = inputs["gla_w_a2_b"][0]
    m["wa2"] = wa2
    m["ba_row"] = f(np.concatenate([f(inputs["gla_b_a_f"][0]), f(inputs["gla_b_a_b"][0])]).reshape(1, 512))
    m["gnb"] = f(np.tile(f(inputs["gla_g_norm"][0])[None, :], (128, 4)))
    m["gq"] = _kc_layout(f(inputs["mla_g_q"][0]))
    m["gkv"] = _kc_layout(f(inputs["mla_g_kv"][0]))
    m["w_uq"] = f(inputs["mla_w_uq"][0])
    m["w_uk"] = f(inputs["mla_w_uk"][0])
    m["w_uv"] = f(inputs["mla_w_uv"][0])
    m["w_br_gla"] = f(inputs["w_br_gla"][0])
    m["w_br_mla"] = f(inputs["w_br_mla"][0])
    m["w_out"] = f(inputs["w_out"][0])
    m["router_w"] = f(inputs["router_w"][0])
    m["router_b"] = f(inputs["router_b"][0]).reshape(1, NE)
    m["w_gate"] = f(inputs["w_gate"][0])
    m["w_up"] = f(inputs["w_up"][0])
    m["w_down"] = f(inputs["w_down"][0])
    bg = f(inputs["b_gate"][0]).reshape(NE, 8, 128).transpose(2, 0, 1)
    bu = f(inputs["b_up"][0]).reshape(NE, 8, 128).transpose(2, 0, 1)
    m["bgT"] = f(bg.reshape(128, NE * 8))
    m["buT"] = f(bu.reshape(128, NE * 8))
    m["b_down"] = f(inputs["b_down"][0])
    m["c_tri"] = f(cst["tri"].reshape(128, 512))
    m["c_mask"] = f(cst["mask"].reshape(128, 1024))
    m["c_ident"] = cst["ident"]
    m["c_ropet"] = f(cst["ropet"].reshape(128, 2 * NTOK))
    m["c_e96"] = cst["e96"]
    m["c_sel"] = cst["sel"]
    m["c_rt"] = cst["rt"]
    return m


_SHARED = ("w_mod", "bmodT", "bmod_row", "gT", "gpost_row", "w_in", "wa2", "ba_row", "gnb", "gq", "gkv", "w_uq", "w_uk",
           "w_uv", "w_br_gla", "w_br_mla", "w_out", "router_w", "router_b", "w_gate", "w_up", "w_down", "bgT", "buT",
           "b_down", "c_tri", "c_mask", "c_ident", "c_ropet", "c_e96", "c_sel", "c_rt")


def kernel(**inputs):
    k = K(stage=99)
    m0 = _host_inputs(inputs, 0)
    in_maps = [m0]
    for b in range(1, 8):
        mb = dict(m0)
        mb["x"] = np.ascontiguousarray(np.asarray(inputs["x"][b], dtype=np.float32))
        mb["ctx"] = np.ascontiguousarray(np.asarray(inputs["ctx"][b], dtype=np.float32))
        cc = np.stack([_kc_layout(np.asarray(inputs["c"][b], np.float32)),
                       _kc_layout(np.asarray(inputs["c_ctx"], np.float32))], axis=-1)
        mb["cc"] = np.ascontiguousarray(cc.reshape(128, 16))
        in_maps.append(mb)
    res = run_bass_kernel_spmd(k.nc, in_maps, core_ids=list(range(8)))
    return np.stack([np.asarray(r["out"], dtype=np.float32) for r in res.results], axis=0)
```
